# Optimizing a Trainium2 kernel written in Bass

```python
import math
import jax, jax.numpy as jnp
from jax import lax
import numpy as np

D_MODEL = 1024
BATCH = 16
SEQ = 2048
DEPTH = 2

GRID_W = 64
CTX_LEN = 256
HEAD_DIM = 64
ATTN_Q_HEADS = 8
ATTN_KV_HEADS = 2
ATTN_GROUP = ATTN_Q_HEADS // ATTN_KV_HEADS
WINDOW = 128
ATTN_BLOCK = 128
ROPE_BASE = 10000.0
ATTN_WIDTH = ATTN_Q_HEADS * HEAD_DIM
KV_WIDTH = ATTN_KV_HEADS * HEAD_DIM
POOL_WINDOWS = (2, 4, 8, 16)
POOL_GROUPS = len(POOL_WINDOWS)
POOL_CH = D_MODEL // 8
POOL_WIDTH = POOL_CH * POOL_GROUPS
EVEN_IN = ATTN_WIDTH + 2 * KV_WIDTH + POOL_WIDTH
SPLIT_EVEN = (ATTN_WIDTH, ATTN_WIDTH + KV_WIDTH, ATTN_WIDTH + 2 * KV_WIDTH)
MLSTM_HEADS = 4
MLSTM_WIDTH = D_MODEL
MLSTM_HEAD_DIM = MLSTM_WIDTH // MLSTM_HEADS
MLSTM_CHUNK = 64
SHORT_CONV = 3
ODD_IN = 3 * MLSTM_WIDTH + 4 * MLSTM_HEADS
SPLIT_ODD = (MLSTM_WIDTH, 2 * MLSTM_WIDTH, 3 * MLSTM_WIDTH)
D_FF = 2816
FFN_CONV = 3
EPS = 1e-6
N_EVEN = (DEPTH + 1) // 2
N_ODD = DEPTH // 2

kernel_name = "hybrid_swa_pool_mlstm_dit_block"

F32 = jnp.float32


def rmsnorm(x, g):
    x32 = x.astype(F32)
    y = x32 * lax.rsqrt(jnp.mean(x32 * x32, axis=-1, keepdims=True) + EPS)
    return (y * g.astype(F32)).astype(x.dtype)


def adaln(cvec, w, b):
    m = jax.nn.silu(cvec) @ w + b
    return jnp.split(m[:, None, :], 6, axis=-1)


def modulate(h, shift, scale):
    return h * (1 + scale) + shift


def dwconv1d(x, w, b):
    k = w.shape[0]
    y = lax.conv_general_dilated(x, w[:, None, :].astype(x.dtype), window_strides=(1,),
                                 padding=[(k // 2, k // 2)],
                                 dimension_numbers=('NWC', 'WIO', 'NWC'),
                                 feature_group_count=x.shape[-1])
    return y + b


def axial_rope_tables(t_len, dtype):
    rows = t_len // GRID_W
    row = jnp.repeat(jnp.arange(rows, dtype=F32), GRID_W)
    col = jnp.tile(jnp.arange(GRID_W, dtype=F32), rows)
    n_freq = HEAD_DIM // 4
    inv = ROPE_BASE ** (-jnp.arange(n_freq, dtype=F32) / n_freq)
    ang = jnp.stack([row[:, None] * inv, col[:, None] * inv], axis=1)
    return jnp.cos(ang).astype(dtype), jnp.sin(ang).astype(dtype)


def apply_axial_rope(x, cos, sin):
    b, t, h, d = x.shape
    xs = x.reshape(b, t, h, 2, 2, d // 4)
    x1, x2 = xs[..., 0, :], xs[..., 1, :]
    c, s = cos[None, :, None], sin[None, :, None]
    return jnp.stack([x1 * c - x2 * s, x1 * s + x2 * c], axis=-2).reshape(b, t, h, d)


def windowed_attention(q, k, v, k_ctx, v_ctx, sink):
    b, t = q.shape[:2]
    nb = t // ATTN_BLOCK
    scale = HEAD_DIM ** -0.5
    qb = q.reshape(b, nb, ATTN_BLOCK, ATTN_KV_HEADS, ATTN_GROUP, HEAD_DIM)

    def band(a):
        ab = a.reshape(b, nb, ATTN_BLOCK, ATTN_KV_HEADS, HEAD_DIM)
        ap = jnp.pad(ab, ((0, 0), (1, 1), (0, 0), (0, 0), (0, 0)))
        return jnp.concatenate([ap[:, :-2], ap[:, 1:-1], ap[:, 2:]], axis=2)

    kw, vw = band(k), band(v)
    blk = jnp.arange(nb)[:, None]
    qpos = blk * ATTN_BLOCK + jnp.arange(ATTN_BLOCK)[None, :]
    kpos = (blk - 1) * ATTN_BLOCK + jnp.arange(3 * ATTN_BLOCK)[None, :]
    valid = ((jnp.abs(qpos[:, :, None] - kpos[:, None, :]) <= WINDOW)
             & (kpos[:, None, :] >= 0) & (kpos[:, None, :] < t))
    s_loc = jnp.einsum('bnqhgd,bnkhd->bnhgqk', qb, kw).astype(F32) * scale
    s_loc = jnp.where(valid[None, :, None, None], s_loc, -jnp.inf)
    s_ctx = jnp.einsum('bnqhgd,bchd->bnhgqc', qb, k_ctx).astype(F32) * scale
    sink_l = sink.astype(F32).reshape(1, 1, ATTN_KV_HEADS, ATTN_GROUP, 1)
    m = jnp.maximum(jnp.maximum(s_loc.max(-1), s_ctx.max(-1)), sink_l)
    p_loc = jnp.exp(s_loc - m[..., None])
    p_ctx = jnp.exp(s_ctx - m[..., None])
    denom = p_loc.sum(-1) + p_ctx.sum(-1) + jnp.exp(sink_l - m)
    o = (jnp.einsum('bnhgqk,bnkhd->bnqhgd', p_loc.astype(v.dtype), vw)
         + jnp.einsum('bnhgqc,bchd->bnqhgd', p_ctx.astype(v.dtype), v_ctx))
    o = o / jnp.moveaxis(denom, -1, 2)[..., None]
    return o.reshape(b, t, ATTN_WIDTH).astype(q.dtype)


def context_attention(q, k, v, sink):
    b, c = q.shape[:2]
    qg = q.reshape(b, c, ATTN_KV_HEADS, ATTN_GROUP, HEAD_DIM)
    s = jnp.einsum('bqhgd,bkhd->bhgqk', qg, k).astype(F32) * HEAD_DIM ** -0.5
    sk = jnp.broadcast_to(sink.astype(F32).reshape(ATTN_KV_HEADS, ATTN_GROUP, 1, 1), s.shape[:-1] + (1,))
    p = jax.nn.softmax(jnp.concatenate([s, sk], axis=-1), axis=-1)[..., :-1]
    o = jnp.einsum('bhgqk,bkhd->bqhgd', p.astype(v.dtype), v)
    return o.reshape(b, c, ATTN_WIDTH)


def pool_mixer(u, pool_w, pool_scale):
    b, t, _ = u.shape
    u32 = u.astype(F32)
    pre = jnp.pad(jnp.cumsum(u32, axis=1), ((0, 0), (1, 0), (0, 0)))
    pos = jnp.arange(t)
    means = []
    for gi, w in enumerate(POOL_WINDOWS):
        lo = jnp.clip(pos - w // 2, 0, t)
        hi = jnp.clip(pos - w // 2 + w, 0, t)
        pg = pre[..., gi * POOL_CH:(gi + 1) * POOL_CH]
        means.append((pg[:, hi] - pg[:, lo]) / (hi - lo).astype(F32)[:, None])
    d = (jnp.concatenate(means, axis=-1) - u32).astype(u.dtype).reshape(b, t, POOL_GROUPS, POOL_CH)
    y = jnp.einsum('btgc,gce->btge', d, pool_w).reshape(b, t, POOL_WIDTH)
    return y * pool_scale


def even_mixer(h, hc, w_in, sink, pool_w, pool_scale, w_out, need_ctx):
    b, t, _ = h.shape
    cos, sin = axial_rope_tables(t, h.dtype)
    q, k, v, u = jnp.split(h @ w_in, SPLIT_EVEN, axis=-1)
    q = apply_axial_rope(q.reshape(b, t, ATTN_Q_HEADS, HEAD_DIM), cos, sin)
    k = apply_axial_rope(k.reshape(b, t, ATTN_KV_HEADS, HEAD_DIM), cos, sin)
    v = v.reshape(b, t, ATTN_KV_HEADS, HEAD_DIM)
    c = hc.shape[1]
    if need_ctx:
        qc, kc, vc, uc = jnp.split(hc @ w_in, SPLIT_EVEN, axis=-1)
    else:
        kc, vc = jnp.split(hc @ w_in[:, ATTN_WIDTH:ATTN_WIDTH + 2 * KV_WIDTH], 2, axis=-1)
    kc = kc.reshape(b, c, ATTN_KV_HEADS, HEAD_DIM)
    vc = vc.reshape(b, c, ATTN_KV_HEADS, HEAD_DIM)
    attn = windowed_attention(q, k, v, kc, vc, sink)
    out = jnp.concatenate([attn, pool_mixer(u, pool_w, pool_scale)], axis=-1) @ w_out
    out_c = None
    if need_ctx:
        attn_c = context_attention(qc.reshape(b, c, ATTN_Q_HEADS, HEAD_DIM), kc, vc, sink)
        out_c = jnp.concatenate([attn_c, pool_mixer(uc, pool_w, pool_scale)], axis=-1) @ w_out
    return out, out_c


def mlstm_inputs(h, w_in, gate_b, conv_w, conv_b, q_w, k_w, need_out):
    b, t, _ = h.shape
    if need_out:
        u, v, og, gates = jnp.split(h @ w_in, SPLIT_ODD, axis=-1)
    else:
        w_sel = jnp.concatenate([w_in[:, :2 * MLSTM_WIDTH], w_in[:, 3 * MLSTM_WIDTH:]], axis=1)
        u, v, gates = jnp.split(h @ w_sel, (MLSTM_WIDTH, 2 * MLSTM_WIDTH), axis=-1)
        og = None
    uc = jax.nn.silu(dwconv1d(u, conv_w, conv_b))
    uh = uc.reshape(b, t, MLSTM_HEADS, MLSTM_HEAD_DIM)
    k = jnp.einsum('bthd,hde->bthe', uh, k_w) * MLSTM_HEAD_DIM ** -0.5
    q = jnp.einsum('bthd,hde->bthe', uh, q_w) if need_out else None
    v = v.reshape(b, t, MLSTM_HEADS, MLSTM_HEAD_DIM)
    g = gates.astype(F32).reshape(b, t, 4, MLSTM_HEADS) + gate_b.astype(F32)
    g_fwd = (g[:, :, 0], jax.nn.log_sigmoid(g[:, :, 1]))
    g_bwd = (g[:, :, 2], jax.nn.log_sigmoid(g[:, :, 3]))
    return uc, og, q, k, v, g_fwd, g_bwd


def mlstm_zero_state(b):
    return (jnp.zeros((b, MLSTM_HEADS, MLSTM_HEAD_DIM, MLSTM_HEAD_DIM), F32),
            jnp.zeros((b, MLSTM_HEADS, MLSTM_HEAD_DIM), F32),
            jnp.zeros((b, MLSTM_HEADS), F32))


def mlstm_final_state(k, v, ig, lf):
    k, v = k.astype(F32), v.astype(F32)
    bsum = jnp.cumsum(lf, axis=1)
    b_end = bsum[:, -1]
    w = b_end[:, None] - bsum + ig
    m = jnp.maximum(b_end, w.max(axis=1))
    a = jnp.exp(w - m[:, None])
    cmat = jnp.einsum('bth,bthv,bthk->bhvk', a, v, k)
    nvec = jnp.einsum('bth,bthk->bhk', a, k)
    return (cmat, nvec, m)


def mlstm_chunkwise(q, k, v, ig, lf, state):
    b, t, h, d = q.shape
    nc, ln = t // MLSTM_CHUNK, MLSTM_CHUNK

    def to_chunks(a):
        a = a.astype(F32).reshape((b, nc, ln, h) + a.shape[3:])
        return jnp.moveaxis(a, (1, 3), (0, 2))

    causal = jnp.tril(jnp.ones((ln, ln), bool))

    def step(carry, xs):
        cmat, nvec, m = carry
        qc, kc, vc, ic, fc = xs
        bc = jnp.cumsum(fc, axis=-1)
        dlog = jnp.where(causal, bc[..., :, None] - bc[..., None, :] + ic[..., None, :], -jnp.inf)
        inter = bc + m[..., None]
        m_t = jnp.maximum(inter, dlog.max(-1))
        dw = jnp.exp(dlog - m_t[..., None])
        iw = jnp.exp(inter - m_t)
        s = jnp.einsum('bhtd,bhsd->bhts', qc, kc) * dw
        num = jnp.einsum('bhts,bhsd->bhtd', s, vc) + iw[..., None] * jnp.einsum('bhvk,bhtk->bhtv', cmat, qc)
        den = s.sum(-1) + iw * jnp.einsum('bhk,bhtk->bht', nvec, qc)
        hout = num / jnp.maximum(jnp.abs(den), jnp.exp(-m_t))[..., None]
        m_new = m_t[..., -1]
        wts = jnp.exp(bc[..., -1:] - bc + ic - m_new[..., None])
        decay = jnp.exp(bc[..., -1] + m - m_new)
        cmat = decay[..., None, None] * cmat + jnp.einsum('bhs,bhsv,bhsk->bhvk', wts, vc, kc)
        nvec = decay[..., None] * nvec + jnp.einsum('bhs,bhsk->bhk', wts, kc)
        return (cmat, nvec, m_new), hout

    _, hs = lax.scan(step, state, (to_chunks(q), to_chunks(k), to_chunks(v), to_chunks(ig), to_chunks(lf)))
    return jnp.moveaxis(hs, (0, 2), (1, 3)).reshape(b, t, h, d)


def mlstm_output(uc, og, q, k, v, g_fwd, g_bwd, state_f, state_b, norm_g, skip, w_out):
    b, t = uc.shape[:2]
    flip = lambda a: jnp.flip(a, axis=1)
    h_f = mlstm_chunkwise(q, k, v, g_fwd[0], g_fwd[1], state_f)
    h_b = flip(mlstm_chunkwise(flip(q), flip(k), flip(v), flip(g_bwd[0]), flip(g_bwd[1]), state_b))
    hs = h_f + h_b
    hs = hs * lax.rsqrt(jnp.mean(hs * hs, axis=-1, keepdims=True) + EPS)
    hs = (hs.reshape(b, t, MLSTM_WIDTH) * norm_g.astype(F32)).astype(uc.dtype)
    y = jax.nn.sigmoid(og) * (hs + skip * uc)
    return y @ w_out


def odd_mixer(h, hc, w_in, gate_b, conv_w, conv_b, q_w, k_w, norm_g, skip, w_out, need_ctx):
    flip = lambda a: jnp.flip(a, axis=1)
    uc, og, q, k, v, gf, gb = mlstm_inputs(h, w_in, gate_b, conv_w, conv_b, q_w, k_w, True)
    uc_c, og_c, q_c, k_c, v_c, gf_c, gb_c = mlstm_inputs(hc, w_in, gate_b, conv_w, conv_b, q_w, k_w, need_ctx)
    state_f = mlstm_final_state(k_c, v_c, gf_c[0], gf_c[1])
    state_b = mlstm_final_state(flip(k_c), flip(v_c), flip(gb_c[0]), flip(gb_c[1]))
    out = mlstm_output(uc, og, q, k, v, gf, gb, state_f, state_b, norm_g, skip, w_out)
    out_c = None
    if need_ctx:
        zero = mlstm_zero_state(hc.shape[0])
        out_c = mlstm_output(uc_c, og_c, q_c, k_c, v_c, gf_c, gb_c, zero, zero, norm_g, skip, w_out)
    return out, out_c


def ffn_sublayer(s, shift, scale, gate, g_pre, g_post, w_up, conv_w, conv_b, w_down):
    h = modulate(rmsnorm(s, g_pre), shift, scale)
    a = dwconv1d(h @ w_up, conv_w, conv_b)
    a_gate, a_val = jnp.split(a, 2, axis=-1)
    return s + gate * rmsnorm((jax.nn.silu(a_gate) * a_val) @ w_down, g_post)


def setup_inputs(seed: int = 0) -> dict:
    key = jax.random.key(seed)
    ks = jax.random.split(key, 24)
    nrm = lambda k, shape, s: jax.random.normal(k, shape, F32) * s
    d = D_MODEL
    fb = jnp.linspace(3.0, 6.0, MLSTM_HEADS)
    zh = jnp.zeros((MLSTM_HEADS,), F32)
    gate_base = jnp.stack([zh, fb, zh, fb])
    return {
        "x": nrm(ks[0], (BATCH, SEQ, d), 1.0),
        "c": nrm(ks[1], (BATCH, d), 1.0),
        "ctx": nrm(ks[2], (BATCH, CTX_LEN, d), 1.0),
        "c_ctx": nrm(ks[3], (d,), 1.0),
        "mod_w": nrm(ks[4], (DEPTH, d, 6 * d), d ** -0.5),
        "mod_b": nrm(ks[5], (DEPTH, 6 * d), 0.01),
        "norm_g": 1.0 + nrm(ks[6], (DEPTH, 4, d), 0.05),
        "attn_in_w": nrm(ks[7], (N_EVEN, d, EVEN_IN), d ** -0.5),
        "attn_sink": nrm(ks[8], (N_EVEN, ATTN_Q_HEADS), 0.5),
        "pool_w": nrm(ks[9], (N_EVEN, POOL_GROUPS, POOL_CH, POOL_CH), POOL_CH ** -0.5),
        "pool_scale": 1.0 + nrm(ks[10], (N_EVEN, POOL_WIDTH), 0.1),
        "attn_out_w": nrm(ks[11], (N_EVEN, ATTN_WIDTH + POOL_WIDTH, d), (ATTN_WIDTH + POOL_WIDTH) ** -0.5),
        "rec_in_w": nrm(ks[12], (N_ODD, d, ODD_IN), d ** -0.5),
        "rec_gate_b": gate_base[None] + nrm(ks[13], (N_ODD, 4, MLSTM_HEADS), 0.1),
        "rec_conv_w": nrm(ks[14], (N_ODD, SHORT_CONV, MLSTM_WIDTH), SHORT_CONV ** -0.5),
        "rec_conv_b": nrm(ks[15], (N_ODD, MLSTM_WIDTH), 0.01),
        "rec_q_w": nrm(ks[16], (N_ODD, MLSTM_HEADS, MLSTM_HEAD_DIM, MLSTM_HEAD_DIM), MLSTM_HEAD_DIM ** -0.5),
        "rec_k_w": nrm(ks[17], (N_ODD, MLSTM_HEADS, MLSTM_HEAD_DIM, MLSTM_HEAD_DIM), MLSTM_HEAD_DIM ** -0.5),
        "rec_norm_g": 1.0 + nrm(ks[18], (N_ODD, MLSTM_WIDTH), 0.05),
        "rec_skip": 1.0 + nrm(ks[19], (N_ODD, MLSTM_WIDTH), 0.1),
        "rec_out_w": nrm(ks[20], (N_ODD, MLSTM_WIDTH, d), MLSTM_WIDTH ** -0.5),
        "ffn_up_w": nrm(ks[21], (DEPTH, d, 2 * D_FF), d ** -0.5),
        "ffn_conv_w": nrm(ks[22], (DEPTH, FFN_CONV, 2 * D_FF), FFN_CONV ** -0.5),
        "ffn_conv_b": nrm(ks[23], (DEPTH, 2 * D_FF), 0.01),
        "ffn_down_w": nrm(jax.random.fold_in(ks[23], 1), (DEPTH, D_FF, d), D_FF ** -0.5),
    }


def reference(x, c, ctx, c_ctx, mod_w, mod_b, norm_g, attn_in_w, attn_sink, pool_w, pool_scale, attn_out_w,
              rec_in_w, rec_gate_b, rec_conv_w, rec_conv_b, rec_q_w, rec_k_w, rec_norm_g, rec_skip, rec_out_w,
              ffn_up_w, ffn_conv_w, ffn_conv_b, ffn_down_w):
    s_ctx = ctx
    for l in range(DEPTH):
        last = l == DEPTH - 1
        i = l // 2
        sh1, sc1, g1, sh2, sc2, g2 = adaln(c, mod_w[l], mod_b[l])
        csh1, csc1, cg1, csh2, csc2, cg2 = adaln(c_ctx[None, :], mod_w[l], mod_b[l])
        h = modulate(rmsnorm(x, norm_g[l, 0]), sh1, sc1)
        hc = modulate(rmsnorm(s_ctx, norm_g[l, 0]), csh1, csc1)
        if l % 2 == 0:
            mix, mix_c = even_mixer(h, hc, attn_in_w[i], attn_sink[i], pool_w[i], pool_scale[i],
                                    attn_out_w[i], not last)
        else:
            mix, mix_c = odd_mixer(h, hc, rec_in_w[i], rec_gate_b[i], rec_conv_w[i], rec_conv_b[i],
                                   rec_q_w[i], rec_k_w[i], rec_norm_g[i], rec_skip[i], rec_out_w[i], not last)
        x = x + g1 * rmsnorm(mix, norm_g[l, 1])
        x = ffn_sublayer(x, sh2, sc2, g2, norm_g[l, 2], norm_g[l, 3],
                         ffn_up_w[l], ffn_conv_w[l], ffn_conv_b[l], ffn_down_w[l])
        if not last:
            s_ctx = s_ctx + cg1 * rmsnorm(mix_c, norm_g[l, 1])
            s_ctx = ffn_sublayer(s_ctx, csh2, csc2, cg2, norm_g[l, 2], norm_g[l, 3],
                                 ffn_up_w[l], ffn_conv_w[l], ffn_conv_b[l], ffn_down_w[l])
    return x
```

```python
import math
from contextlib import ExitStack

import numpy as np
import concourse.bass as bass
import concourse.mybir as mybir
from concourse.bass_utils import run_bass_kernel_spmd

F32 = mybir.dt.float32
BF16 = mybir.dt.bfloat16
U8 = mybir.dt.uint8
AF = mybir.ActivationFunctionType
ALU = mybir.AluOpType

ENGS = ["pe", "act", "dve", "pool", "sp"]
import os as _os
N_DMA_SEMS = int(_os.environ.get("NDS", "24"))

D = 1024
T = 2048
CT = 256
NB = 2
TS = T + CT
DFF = 2816
EPS = 1e-6


class _Proxy:
    def __getattr__(self, name):
        def f(*a, **k):
            self.call = (name, a, k)
            return self
        return f


class Sched:
    def __init__(self, nc):
        self.nc = nc
        self.ops = {e: [] for e in ENGS}
        self.lastw = {}
        self.readers = {}
        self.dma_rr = {e: 0 for e in ENGS}
        self.dma_last = {}
        self.out_dmas = []
        self.pending_dmas = []

    def add(self, eng, fn, reads=(), writes=(), dma=False, is_output=False, extra_deps=()):
        ops = self.ops[eng]
        me = (eng, len(ops))
        deps = {}

        def dep(p, kind):
            if p is None or p == me:
                return
            if deps.get(p) == "raw":
                return
            deps[p] = kind

        for r in reads:
            dep(self.lastw.get(r), "raw")
        for r in writes:
            dep(self.lastw.get(r), "order")
            for rd in self.readers.get(r, ()):
                dep(rd, "order")
        for p in extra_deps:
            dep(p, "raw")
        prox = _Proxy()
        fn(prox)
        rec = dict(call=prox.call, dma=dma, signal=False, slot=None)
        if dma:
            slot = self.dma_rr[eng] % N_DMA_SEMS
            self.dma_rr[eng] += 1
            rec["slot"] = slot
            prev = self.dma_last.get((eng, slot))
            if prev is not None:
                dep(prev, "raw")
            self.dma_last[(eng, slot)] = me
            self.pending_dmas.append(me)
            if is_output:
                self.out_dmas.append(me)
        final = {}
        for p, kind in deps.items():
            pe, pi = p
            prod = self.ops[pe][pi]
            if pe == eng and not prod["dma"] and not dma:
                if kind == "order" or eng in ("pe", "sp"):
                    continue
            final[p] = kind
        rec["deps"] = final
        ops.append(rec)
        for r in reads:
            lst = self.readers.setdefault(r, [])
            if not dma:
                lst[:] = [q for q in lst if not (q[0] == eng and not self.ops[q[0]][q[1]]["dma"])]
            lst.append(me)
        for r in writes:
            self.lastw[r] = me
            self.readers[r] = []
        return me

    def barrier(self):
        lasts = []
        for e in ENGS:
            for i in range(len(self.ops[e]) - 1, -1, -1):
                if not self.ops[e][i]["dma"]:
                    if not self.ops[e][i].get("nop"):
                        lasts.append((e, i))
                    break
        pend = list(self.pending_dmas)
        self.pending_dmas = []
        for e in ENGS:
            me = self.add(e, lambda eng: eng.nop(), extra_deps=[p for p in lasts if p[0] != e] + pend)
            self.ops[me[0]][me[1]]["nop"] = True

    def emit(self, stack):
        nc = self.nc
        for e in ENGS:
            for rec in self.ops[e]:
                for (pe, pi) in rec["deps"]:
                    self.ops[pe][pi]["signal"] = True
        esem = {e: stack.enter_context(nc.semaphore("s_" + e)) for e in ENGS}
        dsem = {e: [None] * N_DMA_SEMS for e in ENGS}
        for e in ENGS:
            for s in range(min(N_DMA_SEMS, self.dma_rr[e])):
                dsem[e][s] = stack.enter_context(nc.semaphore("d_%s_%d" % (e, s)))
        cnt = {e: 0 for e in ENGS}
        dcnt = {}
        for e in ENGS:
            for rec in self.ops[e]:
                if rec["dma"]:
                    k = (e, rec["slot"])
                    dcnt[k] = dcnt.get(k, 0) + 16
                    rec["ev"] = (k, dcnt[k])
                elif rec["signal"]:
                    cnt[e] += 1
                    rec["ev"] = (e, cnt[e])
                else:
                    rec["ev"] = None
        self.stats = {e: [len(self.ops[e]), 0, cnt[e]] for e in ENGS}
        block = stack.enter_context(nc.Block())
        sched = self

        def semof(key):
            if isinstance(key, tuple):
                return dsem[key[0]][key[1]]
            return esem[key]

        def run(e, eng):
            known = {}
            for rec in sched.ops[e]:
                need = {}
                for (pe, pi) in rec["deps"]:
                    key, val = sched.ops[pe][pi]["ev"]
                    if known.get(key, 0) >= val:
                        continue
                    if need.get(key, 0) < val:
                        need[key] = val
                for key, val in need.items():
                    eng.wait_ge(semof(key), val)
                    known[key] = val
                    sched.stats[e][1] += 1
                nm, a_, k_ = rec["call"]
                ins = getattr(eng, nm)(*a_, **k_)
                if rec["dma"]:
                    ins.then_inc(semof(rec["ev"][0]), 16)
                elif rec["signal"]:
                    ins.then_inc(esem[e], 1)
            return known

        @block.tensor
        def _(eng):
            run("pe", eng)

        @block.scalar
        def _(eng):
            run("act", eng)

        @block.vector
        def _(eng):
            run("dve", eng)

        @block.gpsimd
        def _(eng):
            run("pool", eng)

        @block.sync
        def _(eng):
            known = run("sp", eng)
            for (pe, pi) in sched.out_dmas:
                key, val = sched.ops[pe][pi]["ev"]
                if known.get(key, 0) < val:
                    eng.wait_ge(semof(key), val)
                    known[key] = val


DT_SIZE = {F32: 4, BF16: 2, U8: 1}


class Arena:
    def __init__(self, tens, size):
        self.t = tens
        self.size = size
        self.off = 0

    def reset(self):
        self.off = 0

    def alloc(self, shape, dt):
        n = 1
        for s in shape[1:]:
            n *= s
        nbytes = (n * DT_SIZE[dt] + 63) // 64 * 64
        assert self.off + nbytes <= self.size, ("arena overflow", self.off, nbytes, self.size)
        ap = self.t[0:shape[0], self.off:self.off + n * DT_SIZE[dt]].bitcast(dt)
        self.off += nbytes
        if len(shape) == 3:
            ap = ap.rearrange("p (a b) -> p a b", b=shape[2])
        elif len(shape) == 4:
            ap = ap.rearrange("p (a b c) -> p a b c", b=shape[2], c=shape[3])
        return ap


def tiles_of(r0, nr):
    return list(range(r0 // 128, (r0 + nr - 1) // 128 + 1))


class _Stop(Exception):
    pass


def build_program(dbg=None, lim=None):
    dbg = dbg or set()
    nc = bass.Bass("TRN2", target_bir_lowering=False)

    def din(name, shape, dt=F32):
        return nc.dram_tensor(name, list(shape), dt, kind="ExternalInput").ap()

    def dscr(name, shape, dt=F32):
        kind = "ExternalOutput" if name in dbg else "Internal"
        return nc.dram_tensor(name, list(shape), dt, kind=kind).ap()

    x_in = din("x", [NB, T, D])
    ctx_in = din("ctx", [NB, CT, D])
    cvec = din("cvec", [3, D])
    mod_w = din("mod_w", [2, D, 6 * D])
    mod_b = din("mod_b", [2, 6 * D])
    norm_g = din("norm_g", [2, 4, D])
    w_in0 = din("w_in0", [D, 21 * 128])
    attn_sink = din("attn_sink", [8])
    pool_w = din("pool_w", [4, 128, 128])
    pool_scale = din("pool_scale", [512])
    attn_out_w = din("attn_out_w", [D, D])
    rec_in_w = din("rec_in_w", [D, 3088])
    rec_gate_b = din("rec_gate_b", [16])
    rec_conv_w = din("rec_conv_w", [3, D])
    rec_conv_b = din("rec_conv_b", [D])
    rec_q_w = din("rec_q_w", [4, 256, 256])
    rec_k_w = din("rec_k_w", [4, 256, 256])
    rec_norm_g = din("rec_norm_g", [D])
    rec_skip = din("rec_skip", [D])
    rec_out_w = din("rec_out_w", [D, D])
    ffn_up_w = din("ffn_up_w", [2, D, 2 * DFF])
    ffn_conv_w = din("ffn_conv_w", [2, 3, 2 * DFF])
    ffn_conv_b = din("ffn_conv_b", [2, 2 * DFF])
    ffn_down_w = din("ffn_down_w", [2, DFF, D])
    ident_in = din("ident", [128, 128])
    rope_c = din("rope_c", [128, T])
    rope_s = din("rope_s", [128, T])
    amask_in = din("amask", [2, 128, 512])
    pool_edge = din("pool_edge", [128, 64])
    lmask_in = din("lmask", [2, 128, 128])

    out = nc.dram_tensor("out", [NB, T, D], F32, kind="ExternalOutput").ap()

    GV = dscr("GV", [2, 2, 3, D])
    QT0 = dscr("QT0", [NB, 8 * 128, TS], BF16)
    V0 = dscr("V0", [NB, TS, 130], BF16)
    U0 = dscr("U0", [NB, 512, TS])
    CATP = dscr("CATP", [NB, 512, TS], BF16)
    XA = dscr("XA", [NB, T, D])
    CA = dscr("CA", [NB, CT, D])
    XB = dscr("XB", [NB, T, D])
    CB = dscr("CB", [NB, CT, D])
    GT = dscr("GT", [NB, DFF, TS], BF16)

    st = ExitStack()
    with st:
        st.enter_context(nc.allow_non_contiguous_dma(reason="small strided parameter loads"))
        S = Sched(nc)
        parts = {}

        def wpart(base):
            lst = parts.setdefault(base, [])
            name = (base, len(lst))
            lst.append(name)
            return [name]

        def rparts(base):
            return list(parts.get(base, []))

        def sb(name, shape, dt):
            return st.enter_context(nc.sbuf_tensor("sb_" + name, list(shape), dt))

        ARENA_BYTES = 192 * 1024
        arena = Arena(sb("arena", [128, ARENA_BYTES], U8), ARENA_BYTES)
        PS2 = [st.enter_context(nc.psum_tensor("ps%d" % i, [128, 1024], F32)) for i in range(4)]

        def bank(i):
            return PS2[i // 2][:, (i % 2) * 512:(i % 2) * 512 + 512]

        def bank_bf(i):
            return PS2[i // 2][:, (i % 2) * 512:(i % 2) * 512 + 512].bitcast(BF16)

        ident = sb("ident", [128, 128], F32)
        identb = sb("identb", [128, 128], BF16)
        modT = [sb("modT%d" % l, [128, 48, 3], F32) for l in range(2)]
        A1 = [sb("A1_%d" % l, [128, 3, 8], F32) for l in range(2)]
        A2 = [sb("A2_%d" % l, [128, 3, 8], F32) for l in range(2)]
        SH1 = [sb("SH1_%d" % l, [128, 3, 8], F32) for l in range(2)]
        SH2 = [sb("SH2_%d" % l, [128, 3, 8], F32) for l in range(2)]
        epsb = sb("epsb", [128, 1], F32)

        def V(fn, reads, writes):
            return S.add("dve", fn, reads, writes)

        def A(fn, reads, writes):
            return S.add("act", fn, reads, writes)

        def G(fn, reads, writes):
            return S.add("pool", fn, reads, writes)

        def PE(fn, reads, writes):
            return S.add("pe", fn, reads, writes)

        def DMA(q, out_ap, in_ap, reads, writes, is_output=False):
            return S.add(q, lambda e: e.dma_start(out=out_ap, in_=in_ap), reads, writes, dma=True,
                         is_output=is_output)

        def dma3(q, dst3, src3, nmid, reads, writes):
            for j in range(nmid):
                DMA(q, dst3(j), src3(j), reads, writes)

        def load_wb(dst3, src2, K, blocks, res):
            for (c0, cw, key) in blocks:
                for k in range(K):
                    DMA("pool", dst3[:, k, c0:c0 + cw], src2[k * 128:(k + 1) * 128, c0:c0 + cw], [], [(res, key)])

        def load_w(dst3, src2, K, N, res):
            for k in range(K):
                c0 = 0
                while c0 < N:
                    cw = min(2048, N - c0)
                    DMA("pool", dst3[:, k, c0:c0 + cw], src2[k * 128:(k + 1) * 128, c0:c0 + cw], [], [res])
                    c0 += cw

        def chk(k):
            if lim is not None and k > lim:
                raise _Stop()

        try:
            DMA("sp", ident[:], ident_in, [], ["ident"])
            V(lambda e: e.tensor_copy(out=identb[:], in_=ident[:]), ["ident"], ["identb"])
            V(lambda e: e.memset(epsb[:], EPS), [], ["epsb"])

            arena.reset()
            cT = arena.alloc([128, 8, 3], F32)
            sT = arena.alloc([128, 8, 3], BF16)
            mb = arena.alloc([128, 48], F32)
            ng = arena.alloc([128, 4, 8], F32)
            g1t = arena.alloc([128, 8, 3], F32)
            g2t = arena.alloc([128, 8, 3], F32)
            mwt = [arena.alloc([128, 8, 512], BF16) for _ in range(3)]
            for r in range(3):
                DMA("sp", cT[:, :, r], cvec[r].rearrange("(k p) -> p k", p=128), [], ["cT"])
            A(lambda e: e.activation(out=sT, in_=cT, func=AF.Silu), ["cT"], ["sT"])
            for l in range(2):
                DMA("sp", mb, mod_b[l].rearrange("(c p) -> p c", p=128), [], ["mb"])
                for j in range(4):
                    DMA("sp", ng[:, j, :], norm_g[l, j].rearrange("(k p) -> p k", p=128), [], ["ng"])
                psm = bank(0)
                for nch in range(12):
                    wt = mwt[nch % 3]
                    wres = "mwt%d" % (nch % 3)
                    load_w(wt, mod_w[l][:, nch * 512:(nch + 1) * 512], 8, 512, wres)
                    for fc in range(4):
                        col = (nch * 4 + fc) * 3
                        for k in range(8):
                            PE(lambda e, wt=wt, k=k, fc=fc, col=col: e.matmul(
                                psm[:, col:col + 3], lhsT=wt[:, k, fc * 128:(fc + 1) * 128], rhs=sT[:, k, :],
                                start=(k == 0), stop=(k == 7)), [wres, "sT"], ["ps0"])
                mT = modT[l]
                V(lambda e, mT=mT: e.tensor_tensor(
                    out=mT[:], in0=psm[:, 0:144].rearrange("p (c r) -> p c r", r=3),
                    in1=mb.unsqueeze(2).to_broadcast([128, 48, 3]), op=ALU.add), ["ps0", "mb"], ["modT%d" % l])

                def ngb(j):
                    return ng[:, j, :].unsqueeze(2).to_broadcast([128, 8, 3])

                mres = ["modT%d" % l, "ng"]
                V(lambda e, mT=mT, l=l: e.scalar_tensor_tensor(
                    out=A1[l][:].rearrange("p r k -> p k r"), in0=mT[:, 8:16, :], scalar=1.0, in1=ngb(0),
                    op0=ALU.add, op1=ALU.mult), mres, ["A1_%d" % l])
                V(lambda e, mT=mT, l=l: e.scalar_tensor_tensor(
                    out=A2[l][:].rearrange("p r k -> p k r"), in0=mT[:, 32:40, :], scalar=1.0, in1=ngb(2),
                    op0=ALU.add, op1=ALU.mult), mres, ["A2_%d" % l])
                V(lambda e, mT=mT, l=l: e.tensor_copy(
                    out=SH1[l][:].rearrange("p r k -> p k r"), in_=mT[:, 0:8, :]), mres, ["SH1_%d" % l])
                V(lambda e, mT=mT, l=l: e.tensor_copy(
                    out=SH2[l][:].rearrange("p r k -> p k r"), in_=mT[:, 24:32, :]), mres, ["SH2_%d" % l])
                V(lambda e, mT=mT: e.tensor_tensor(out=g1t, in0=mT[:, 16:24, :], in1=ngb(1), op=ALU.mult),
                  mres, ["g1t"])
                V(lambda e, mT=mT: e.tensor_tensor(out=g2t, in0=mT[:, 40:48, :], in1=ngb(3), op=ALU.mult),
                  mres, ["g2t"])
                for r in range(3):
                    DMA("sp", GV[l, 0, r].rearrange("(k p) -> p k", p=128), g1t[:, :, r], ["g1t"], wpart(("GV", l, 0)))
                    DMA("sp", GV[l, 1, r].rearrange("(k p) -> p k", p=128), g2t[:, :, r], ["g2t"], wpart(("GV", l, 1)))

            ctxb = {}
            rr = {"n": 0, "r": 0}

            def alloc_norm_bufs():
                ctxb["xt"] = [arena.alloc([128, D], F32) for _ in range(3)]
                ctxb["junk"] = arena.alloc([128, D], BF16)
                ctxb["xn"] = [arena.alloc([128, D], BF16) for _ in range(3)]
                ctxb["st"] = [arena.alloc([128, 4], F32) for _ in range(3)]
                ctxb["tmpT"] = [arena.alloc([128, 8, 128], F32) for _ in range(2)]

            def rstd_chain(src_ap, srcres, stt, stres, nr):
                junk = ctxb["junk"]
                A(lambda e: e.activation(out=junk[0:nr, :], in_=src_ap, func=AF.Square, accum_out=stt[0:nr, 0:1]),
                  srcres, ["junk", stres])
                A(lambda e: e.activation(out=stt[0:nr, 1:2], in_=stt[0:nr, 0:1], func=AF.Sqrt, scale=1.0 / D,
                                         bias=epsb[0:nr, :]), [stres, "epsb"], [stres])
                V(lambda e: e.reciprocal(out=stt[0:nr, 1:2], in_=stt[0:nr, 1:2]), [stres], [stres])

            def make_hT(src, srcres_fn, r0, nr, Aap, SHap, ares, hT, hres, c0):
                i = rr["n"]
                rr["n"] += 1
                s3, s2 = i % 3, i % 2
                xt, xn, stt, tmpT = ctxb["xt"][s3], ctxb["xn"][s3], ctxb["st"][s3], ctxb["tmpT"][s2]
                xres, nres, sres, tres = "xt%d" % s3, "xn%d" % s3, "st%d" % s3, "tmpT%d" % s2
                pb = 6 + s2
                pres = "ps%d" % pb
                DMA("sp", xt[0:nr, :], src[r0:r0 + nr, :], srcres_fn(r0, nr), [xres])
                rstd_chain(xt[0:nr, :], [xres], stt, sres, nr)
                V(lambda e: e.tensor_scalar(out=xn[0:nr, :], in0=xt[0:nr, :], scalar1=stt[0:nr, 1:2], scalar2=None,
                                            op0=ALU.mult), [xres, sres], [nres])
                pt = bank_bf(pb).rearrange("p (k t) -> p k t", t=128)
                for k in range(8):
                    PE(lambda e, k=k: e.transpose(out=pt[:, k, 0:nr], in_=xn[0:nr, k * 128:(k + 1) * 128],
                                                  identity=identb[0:nr, 0:nr]), [nres, "identb"], [pres])
                V(lambda e: e.tensor_tensor(out=tmpT[:, :, 0:nr], in0=pt[:, :, 0:nr],
                                            in1=Aap.unsqueeze(2).to_broadcast([128, 8, nr]), op=ALU.mult),
                  [pres, ares], [tres])
                V(lambda e: e.tensor_tensor(out=hT[:, :, c0:c0 + nr], in0=tmpT[:, :, 0:nr],
                                            in1=SHap.unsqueeze(2).to_broadcast([128, 8, nr]), op=ALU.add),
                  [tres, ares], [hres])

            def residual_update(mix_ap, mixres, src, srcres, dst, dstres, r0, nr, gb, gbres, is_output=False):
                i = rr["r"]
                rr["r"] += 1
                s3, s2 = i % 3, i % 2
                xt, stt = ctxb["xt"][s3], ctxb["st"][s3]
                tmp = ctxb["mtmp"][s2]
                xres, sres, tres = "xt%d" % s3, "st%d" % s3, "mtmp%d" % s2
                DMA("sp", xt[0:nr, :], src[r0:r0 + nr, :], srcres, [xres])
                rstd_chain(mix_ap, mixres, stt, sres, nr)
                V(lambda e: e.scalar_tensor_tensor(out=tmp[0:nr, :], in0=mix_ap, scalar=stt[0:nr, 1:2],
                                                   in1=gb[0:nr, :], op0=ALU.mult, op1=ALU.mult),
                  mixres + [sres, gbres], [tres])
                V(lambda e: e.tensor_tensor(out=xt[0:nr, :], in0=xt[0:nr, :], in1=tmp[0:nr, :], op=ALU.add),
                  [xres, tres], [xres])
                DMA("sp", dst[r0:r0 + nr, :], xt[0:nr, :], [xres], dstres, is_output=is_output)

            def load_gb(l, j, r, tile, res):
                DMA("sp", tile, GV[l, j, r].partition_broadcast(128), rparts(("GV", l, j)), [res])

            def xres_fn(name, b):
                return lambda r0, nr: [(name, b, t) for t in tiles_of(r0, nr)]

            chk(1)
            S.barrier()
            arena.reset()
            alloc_norm_bufs()
            win = arena.alloc([128, 8, 21 * 128], BF16)
            rc = arena.alloc([128, T], F32)
            rs = arena.alloc([128, T], F32)
            hTw = [arena.alloc([128, 8, 512], BF16) for _ in range(2)]
            rt1 = [arena.alloc([128, 512], F32) for _ in range(2)]
            rt2 = [arena.alloc([128, 512], F32) for _ in range(2)]
            qst = [arena.alloc([128, 512], BF16) for _ in range(4)]
            ust = [arena.alloc([128, 512], F32) for _ in range(2)]
            vst = [arena.alloc([128, 130], BF16) for _ in range(2)]
            load_wb(win, w_in0, 8, [(j * 128, 128, j) for j in
                                    [x for p_ in range(8) for x in (p_, p_ + 8)] + [17, 18, 19, 20, 16]], "win")
            DMA("sp", rc, rope_c, [], ["rc"])
            DMA("sp", rs, rope_s, [], ["rs"])
            for s in range(2):
                V(lambda e, s=s: e.memset(vst[s][:, 64:65], 1.0), [], ["vst%d" % s])
                V(lambda e, s=s: e.memset(vst[s][:, 129:130], 1.0), [], ["vst%d" % s])
            cnt = {"w": 0, "q": 0, "u": 0, "v": 0, "pp": 0}
            for b in range(NB):
                for w in range(5):
                    isx = w < 4
                    nt = 512 if isx else 256
                    tok0 = w * 512
                    row = b if isx else 2
                    src = x_in[b] if isx else ctx_in[b]
                    hs = cnt["w"] % 2
                    cnt["w"] += 1
                    hT, hres = hTw[hs], "hTw%d" % hs
                    for i in range(nt // 128):
                        make_hT(src, lambda r0, nr: [], (tok0 if isx else 0) + i * 128 if isx else i * 128, 128,
                                A1[0][:, row, :], SH1[0][:, row, :], "A1_0", hT, hres, i * 128)

                    def proj(j, pb):
                        for k in range(8):
                            PE(lambda e, k=k: e.matmul(bank(pb)[:, 0:nt], lhsT=win[:, k, j * 128:(j + 1) * 128],
                                                       rhs=hT[:, k, 0:nt], start=(k == 0), stop=(k == 7)),
                               [("win", j), hres], ["ps%d" % pb])

                    for j in range(8):
                        pa = (cnt["pp"] % 2) * 2
                        cnt["pp"] += 1
                        qs = cnt["q"] % 4
                        cnt["q"] += 1
                        qt_, qres = qst[qs], "qst%d" % qs
                        proj(j, pa)
                        if isx:
                            proj(j + 8, pa + 1)
                            ts_ = cnt["q"] % 2
                            t1, t2 = rt1[ts_], rt2[ts_]
                            V(lambda e, t1=t1, pa=pa: e.tensor_tensor(out=t1[:, 0:nt], in0=bank(pa)[:, 0:nt],
                                                                      in1=rc[:, tok0:tok0 + nt], op=ALU.mult),
                              ["ps%d" % pa, "rc"], ["rt1_%d" % ts_])
                            V(lambda e, t2=t2, pa=pa: e.tensor_tensor(out=t2[:, 0:nt], in0=bank(pa + 1)[:, 0:nt],
                                                                      in1=rs[:, tok0:tok0 + nt], op=ALU.mult),
                              ["ps%d" % (pa + 1), "rs"], ["rt2_%d" % ts_])
                            G(lambda e, t1=t1, t2=t2, qt_=qt_: e.tensor_tensor(out=qt_[:, 0:nt], in0=t1[:, 0:nt],
                                                                               in1=t2[:, 0:nt], op=ALU.add),
                              ["rt1_%d" % ts_, "rt2_%d" % ts_], [qres])
                        else:
                            A(lambda e, qt_=qt_, pa=pa: e.activation(out=qt_[:, 0:nt], in_=bank(pa)[:, 0:nt],
                                                                     func=AF.Copy), ["ps%d" % pa], [qres])
                        DMA("sp", QT0[b, j * 128:(j + 1) * 128, tok0:tok0 + nt], qt_[:, 0:nt], [qres],
                            wpart(("QT0", b)))
                    for j in range(17, 21):
                        pa = (cnt["pp"] % 2) * 2
                        cnt["pp"] += 1
                        us = cnt["u"] % 2
                        cnt["u"] += 1
                        proj(j, pa)
                        A(lambda e, us=us, pa=pa: e.activation(out=ust[us][:, 0:nt], in_=bank(pa)[:, 0:nt],
                                                               func=AF.Copy), ["ps%d" % pa], ["ust%d" % us])
                        DMA("sp", U0[b, (j - 17) * 128:(j - 16) * 128, tok0:tok0 + nt], ust[us][:, 0:nt],
                            ["ust%d" % us], wpart(("U0", b, j - 17)))
                    for i in range(nt // 128):
                        vs = cnt["v"] % 2
                        cnt["v"] += 1
                        for k in range(8):
                            PE(lambda e, k=k, i=i: e.matmul(bank(4)[:, 0:128], lhsT=hT[:, k, i * 128:(i + 1) * 128],
                                                           rhs=win[:, k, 16 * 128:17 * 128], start=(k == 0),
                                                           stop=(k == 7)), [("win", 16), hres], ["ps4"])
                        A(lambda e, vs=vs: e.activation(
                            out=vst[vs][:].rearrange("p (h d) -> p h d", d=65)[:, :, 0:64],
                            in_=bank(4)[:, 0:128].rearrange("p (h d) -> p h d", d=64), func=AF.Copy),
                          ["ps4"], ["vst%d" % vs])
                        DMA("sp", V0[b, tok0 + i * 128:tok0 + (i + 1) * 128, :], vst[vs][:], ["vst%d" % vs],
                            wpart(("V0", b)))

            chk(2)
            S.barrier()
            arena.reset()
            PADL = 16
            ub = [arena.alloc([128, TS + 48], F32) for _ in range(2)]
            pa_ = [arena.alloc([128, TS + 48], F32) for _ in range(2)]
            pb_ = [arena.alloc([128, TS + 48], F32) for _ in range(2)]
            dTt = [arena.alloc([128, TS], BF16) for _ in range(2)]
            cst = [arena.alloc([128, 512], BF16) for _ in range(2)]
            pwt = arena.alloc([128, 4, 128], BF16)
            psc = arena.alloc([128, 4], F32)
            pedge = arena.alloc([128, 64], F32)
            etmp = arena.alloc([128, 8], F32)
            for g_ in range(4):
                DMA("pool", pwt[:, g_, :], pool_w[g_], [], ["pwt"])
            DMA("sp", psc, pool_scale.rearrange("(g p) -> p g", p=128), [], ["psc"])
            DMA("sp", pedge, pool_edge, [], ["pedge"])
            segs = [(PADL, T, 0), (PADL + T + 16, CT, T)]
            pc = 0
            for b in range(NB):
                for g in range(4):
                    sl = pc % 2
                    pc += 1
                    u, p1, p2, dT = ub[sl], pa_[sl], pb_[sl], dTt[sl]
                    ur, p1r, p2r, dr = "ub%d" % sl, "pa%d" % sl, "pb%d" % sl, "dT%d" % sl
                    V(lambda e, u=u: e.memset(u[:, 0:PADL], 0.0), [], [ur])
                    V(lambda e, u=u: e.memset(u[:, PADL + T:PADL + T + 16], 0.0), [], [ur])
                    V(lambda e, u=u: e.memset(u[:, PADL + T + 16 + CT:TS + 48], 0.0), [], [ur])
                    DMA("sp", u[:, PADL:PADL + T], U0[b, g * 128:(g + 1) * 128, 0:T], rparts(("U0", b, g)), [ur])
                    DMA("sp", u[:, PADL + T + 16:PADL + T + 16 + CT], U0[b, g * 128:(g + 1) * 128, T:TS],
                        rparts(("U0", b, g)), [ur])
                    wlen = 2 ** (g + 1)
                    half = wlen // 2
                    NW = TS + 48
                    cur, curres, ln = u, ur, 1
                    bufs = [(p1, p1r), (p2, p2r)]
                    bi = 0
                    while ln < wlen:
                        nxt, nres = bufs[bi % 2]
                        bi += 1
                        n_el = NW - 2 * ln
                        V(lambda e, cur=cur, nxt=nxt, ln=ln, n_el=n_el: e.tensor_tensor(
                            out=nxt[:, 0:n_el], in0=cur[:, 0:n_el], in1=cur[:, ln:ln + n_el], op=ALU.add),
                          [curres], [nres])
                        cur, curres = nxt, nres
                        ln *= 2
                    for (off, L, toff) in segs:
                        V(lambda e, cur=cur, off=off, L=L, toff=toff: e.scalar_tensor_tensor(
                            out=dT[:, toff:toff + L], in0=cur[:, off - half:off - half + L], scalar=1.0 / wlen,
                            in1=u[:, off:off + L], op0=ALU.mult, op1=ALU.subtract), [curres, ur], [dr])
                        for side in range(2):
                            ne = half if side == 0 else half - 1
                            if ne == 0:
                                continue
                            t0 = 0 if side == 0 else L - half + 1
                            ec = (g * 2 + side) * 8
                            V(lambda e, cur=cur, off=off, t0=t0, ne=ne, ec=ec: e.tensor_tensor(
                                out=etmp[:, 0:ne], in0=cur[:, off - half + t0:off - half + t0 + ne],
                                in1=pedge[:, ec:ec + ne], op=ALU.mult), [curres, "pedge"], ["etmp"])
                            V(lambda e, off=off, t0=t0, ne=ne, toff=toff: e.tensor_tensor(
                                out=dT[:, toff + t0:toff + t0 + ne], in0=etmp[:, 0:ne],
                                in1=u[:, off + t0:off + t0 + ne], op=ALU.subtract), ["etmp", ur], [dr])
                    for w in range(5):
                        nt = 512 if w < 4 else 256
                        tok0 = w * 512
                        pbk = w % 2
                        cs = (pc + w) % 2
                        PE(lambda e, dT=dT, nt=nt, tok0=tok0, pbk=pbk, g=g: e.matmul(
                            bank(pbk)[:, 0:nt], lhsT=pwt[:, g, :], rhs=dT[:, tok0:tok0 + nt], start=True, stop=True),
                           ["pwt", dr], ["ps%d" % pbk])
                        A(lambda e, nt=nt, pbk=pbk, cs=cs, g=g: e.activation(
                            out=cst[cs][:, 0:nt], in_=bank(pbk)[:, 0:nt], func=AF.Copy, scale=psc[:, g:g + 1]),
                          ["ps%d" % pbk, "psc"], ["cst%d" % cs])
                        DMA("sp", CATP[b, g * 128:(g + 1) * 128, tok0:tok0 + nt], cst[cs][:, 0:nt], ["cst%d" % cs],
                            wpart(("CATP", b)))

            chk(2.05)
            S.barrier()
            arena.reset()
            alloc_norm_bufs()
            ctxb["mtmp"] = [arena.alloc([128, D], F32) for _ in range(2)]
            wout = arena.alloc([128, 8, D], BF16)
            qt = arena.alloc([128, 8, TS], BF16)
            vx = arena.alloc([128, 18, 130], BF16)
            am = arena.alloc([128, 2, 512], BF16)
            esb = arena.alloc([128, 8], F32)
            PT = [arena.alloc([128, 512], BF16) for _ in range(10)]
            atok = [arena.alloc([128, 512], BF16) for _ in range(2)]
            catT = [arena.alloc([128, 8, 128], BF16) for _ in range(2)]
            den = [arena.alloc([128, 8], F32) for _ in range(2)]
            gbx = arena.alloc([128, D], F32)
            gbc = arena.alloc([128, D], F32)
            load_w(wout, attn_out_w, 8, D, "wout")
            for m_ in range(2):
                DMA("pool", am[:, m_, :], amask_in[m_], [], ["am"])
            DMA("sp", esb, attn_sink.partition_broadcast(128), [], ["esb"])
            A(lambda e: e.activation(out=esb, in_=esb, func=AF.Exp), ["esb"], ["esb"])
            import os
            if os.environ.get("SWAP"):
                load_gb(0, 0, 0, gbx, "gbx")
            load_gb(0, 0, 2, gbc, "gbc")
            chk(2.1)
            ac = {"pt": 0, "blk": 0}
            for b in range(NB):
                import os
                SK = os.environ.get("SKIP", "")
                if "q" not in SK:
                    dma3("sp", lambda j: qt[:, j, :], lambda j: QT0[b, j * 128:(j + 1) * 128, :], 8, rparts(("QT0", b)), ["qt"])
                if "v" not in SK:
                    dma3("sp", lambda j: vx[:, j, :], lambda j: V0[b, j * 128:(j + 1) * 128, :], 18, rparts(("V0", b)), ["vx"])
                if "g" not in SK:
                    load_gb(0, 0, b, gbx, "gbx")
                chk(2.2)
                for n in range(18):
                    isx = n < 16
                    bs = ac["blk"] % 2
                    ac["blk"] += 1
                    at_, atres = atok[bs], "atok%d" % bs
                    cT_, cres = catT[bs], "catT%d" % bs
                    dn, dres = den[bs], "den%d" % bs
                    if isx:
                        chunks = []
                        if n > 0:
                            chunks.append((n - 1, 0))
                        chunks.append((n, None))
                        if n < 15:
                            chunks.append((n + 1, 1))
                        chunks += [(16, None), (17, None)]
                    else:
                        chunks = [(16, None), (17, None)]
                    if "c" in SK:
                        V(lambda e: e.memset(cT_[:, 4:8, :], 0.0), [], [cres])
                    else:
                        dma3("sp", lambda j: cT_[:, 4 + j, :], lambda j: CATP[b, j * 128:(j + 1) * 128, n * 128:(n + 1) * 128], 4,
                             rparts(("CATP", b)), [cres])
                    for h in range(2):
                        pts = []
                        for (kc, mk) in chunks:
                            pi = ac["pt"] % 10
                            ac["pt"] += 1
                            sbk = pi % 2
                            pts.append((pi, kc))
                            for g in range(4):
                                s_ = g % 2
                                c_ = 2 * h + g // 2
                                PE(lambda e, kc=kc, g=g, s_=s_, c_=c_, sbk=sbk: e.matmul(
                                    bank(sbk)[:, g * 128:(g + 1) * 128],
                                    lhsT=qt[:, 4 + 2 * h + s_, kc * 128:(kc + 1) * 128],
                                    rhs=qt[:, c_, n * 128:(n + 1) * 128],
                                    start=True, stop=True), ["qt"], ["ps%d" % sbk])
                            A(lambda e, pi=pi, sbk=sbk: e.activation(out=PT[pi][:], in_=bank(sbk), func=AF.Exp,
                                                                   scale=0.125), ["ps%d" % sbk], ["PT%d" % pi])
                            if mk is not None:
                                V(lambda e, pi=pi, mk=mk: e.tensor_tensor(out=PT[pi][:], in0=PT[pi][:],
                                                                           in1=am[:, mk, :], op=ALU.mult),
                                  ["PT%d" % pi, "am"], ["PT%d" % pi])
                        chk(2.3)
                        ob = 2 + h
                        ov = bank(ob)[:, 0:260].rearrange("p (g d) -> p g d", d=65)
                        for g in range(4):
                            for ci, (pi, kc) in enumerate(pts):
                                PE(lambda e, g=g, pi=pi, kc=kc, ci=ci, ob=ob: e.matmul(
                                    bank(ob)[:, g * 65:(g + 1) * 65], lhsT=PT[pi][:, g * 128:(g + 1) * 128],
                                    rhs=vx[:, kc, h * 65:(h + 1) * 65], start=(ci == 0), stop=(ci == len(pts) - 1)),
                                   ["PT%d" % pi, "vx"], ["ps%d" % ob])
                        chk(2.5)
                        V(lambda e, ov=ov, dn=dn, h=h: e.tensor_tensor(out=dn[:, h * 4:(h + 1) * 4], in0=ov[:, :, 64],
                                                                       in1=esb[:, h * 4:(h + 1) * 4], op=ALU.add),
                          ["ps%d" % ob, "esb"], [dres])
                        V(lambda e, dn=dn, h=h: e.reciprocal(out=dn[:, h * 4:(h + 1) * 4], in_=dn[:, h * 4:(h + 1) * 4]),
                          [dres], [dres])
                        V(lambda e, ov=ov, dn=dn, h=h, at_=at_: e.tensor_tensor(
                            out=at_[:, h * 256:(h + 1) * 256].rearrange("p (g d) -> p g d", d=64), in0=ov[:, :, 0:64],
                            in1=dn[:, h * 4:(h + 1) * 4].unsqueeze(2).to_broadcast([128, 4, 64]), op=ALU.mult),
                          ["ps%d" % ob, dres], [atres])
                    chk(2.6)
                    ptv = bank_bf(4).rearrange("p (k t) -> p k t", t=128)
                    for c_ in range(4):
                        PE(lambda e, c_=c_, at_=at_: e.transpose(out=ptv[:, c_, :], in_=at_[:, c_ * 128:(c_ + 1) * 128],
                                                                identity=identb[:]), [atres, "identb"], ["ps4"])
                    A(lambda e, cT_=cT_: e.activation(out=cT_[:, 0:4, :], in_=ptv[:, 0:4, :], func=AF.Copy),
                      ["ps4"], [cres])
                    chk(2.7)
                    mix = PS2[3]
                    for hf in range(2):
                        for k in range(8):
                            PE(lambda e, k=k, hf=hf, cT_=cT_: e.matmul(mix[:, hf * 512:(hf + 1) * 512], lhsT=cT_[:, k, :],
                                                                      rhs=wout[:, k, hf * 512:(hf + 1) * 512],
                                                                      start=(k == 0), stop=(k == 7)),
                               [cres, "wout"], ["ps%d" % (6 + hf)])
                    chk(2.8)
                    if isx:
                        residual_update(mix[:, :], ["ps6", "ps7"], x_in[b], [], XA[b], [("XA", b, n)], n * 128, 128,
                                        gbx, "gbx")
                    else:
                        residual_update(mix[:, :], ["ps6", "ps7"], ctx_in[b], [], CA[b], [("CA", b, n - 16)],
                                        (n - 16) * 128, 128, gbc, "gbc")

            def ffn_layer(l, segs_fn, final):
                chk(4 + 5 * l)
                S.barrier()
                arena.reset()
                alloc_norm_bufs()
                wup = arena.alloc([128, 8, 2 * DFF], BF16)
                cw = arena.alloc([128, 3, 44], F32)
                cb = arena.alloc([128, 44], F32)
                hTf = [arena.alloc([128, 8, 512], BF16) for _ in range(2)]
                ta = [arena.alloc([128, 512], F32) for _ in range(2)]
                tb = [arena.alloc([128, 512], F32) for _ in range(2)]
                sg = [arena.alloc([128, 512], F32) for _ in range(2)]
                tv = [arena.alloc([128, 512], F32) for _ in range(2)]
                tw = [arena.alloc([128, 512], F32) for _ in range(2)]
                gst = [arena.alloc([128, 512], BF16) for _ in range(3)]
                load_wb(wup, ffn_up_w[l], 8, [(j * 128, 128, j) for j in [x for c_ in range(22) for x in (c_, 22 + c_)]], "wup")
                for j in range(3):
                    DMA("sp", cw[:, j, :], ffn_conv_w[l, j].rearrange("(c p) -> p c", p=128), [], ["cw"])
                DMA("sp", cb, ffn_conv_b[l].rearrange("(c p) -> p c", p=128), [], ["cb"])
                fc = {"w": 0, "c": 0, "g": 0}
                for b in range(NB):
                    for (src, sname, L, toff, row) in segs_fn(b):
                        wins = []
                        o = 0
                        while o < L:
                            r0 = max(o - 1, 0)
                            c0 = 1 if o == 0 else 0
                            nr = min(512 - c0, L - r0)
                            rz = (r0 + nr == L)
                            ncols = c0 + nr + (1 if rz else 0)
                            if ncols > 512:
                                nr -= 1
                                rz = False
                                ncols = 512
                            nout = ncols - 2
                            wins.append((r0, nr, c0, rz, ncols, o, nout))
                            o += nout
                        for (r0, nr, c0, rz, ncols, o, nout) in wins:
                            hs = fc["w"] % 2
                            fc["w"] += 1
                            hT, hres = hTf[hs], "hTf%d" % hs
                            if c0 == 1:
                                V(lambda e, hT=hT: e.memset(hT[:, :, 0:1], 0.0), [], [hres])
                            if rz:
                                V(lambda e, hT=hT, cc=c0 + nr: e.memset(hT[:, :, cc:cc + 1], 0.0), [], [hres])
                            rr_ = r0
                            while rr_ < r0 + nr:
                                n_ = min(128, r0 + nr - rr_)
                                make_hT(src, xres_fn(sname, b), rr_, n_, A2[l][:, row, :], SH2[l][:, row, :],
                                        "A2_%d" % l, hT, hres, c0 + rr_ - r0)
                                rr_ += n_
                            for c in range(22):
                                s2 = fc["c"] % 2
                                fc["c"] += 1
                                pg, pv = (s2 * 2), (s2 * 2 + 1)
                                for (jc, pbk) in ((c, pg), (22 + c, pv)):
                                    for k in range(8):
                                        PE(lambda e, k=k, jc=jc, pbk=pbk, hT=hT, ncols=ncols: e.matmul(
                                            bank(pbk)[:, 0:ncols], lhsT=wup[:, k, jc * 128:(jc + 1) * 128],
                                            rhs=hT[:, k, 0:ncols], start=(k == 0), stop=(k == 7)),
                                           [("wup", jc), hres], ["ps%d" % pbk])
                                n = nout
                                A(lambda e, s2=s2, pg=pg, c=c, n=n: e.activation(
                                    out=ta[s2][:, 0:n], in_=bank(pg)[:, 1:1 + n], func=AF.Identity,
                                    scale=cw[:, 1, c:c + 1], bias=cb[:, c:c + 1]), ["ps%d" % pg, "cw", "cb"], ["ta%d" % s2])
                                V(lambda e, s2=s2, pg=pg, c=c, n=n: e.scalar_tensor_tensor(
                                    out=tb[s2][:, 0:n], in0=bank(pg)[:, 0:n], scalar=cw[:, 0, c:c + 1],
                                    in1=ta[s2][:, 0:n], op0=ALU.mult, op1=ALU.add), ["ps%d" % pg, "cw", "ta%d" % s2],
                                  ["tb%d" % s2])
                                V(lambda e, s2=s2, pg=pg, c=c, n=n: e.scalar_tensor_tensor(
                                    out=ta[s2][:, 0:n], in0=bank(pg)[:, 2:2 + n], scalar=cw[:, 2, c:c + 1],
                                    in1=tb[s2][:, 0:n], op0=ALU.mult, op1=ALU.add), ["ps%d" % pg, "cw", "tb%d" % s2],
                                  ["ta%d" % s2])
                                A(lambda e, s2=s2, n=n: e.activation(out=sg[s2][:, 0:n], in_=ta[s2][:, 0:n], func=AF.Silu),
                                  ["ta%d" % s2], ["sg%d" % s2])
                                cv = 22 + c
                                A(lambda e, s2=s2, pv=pv, cv=cv, n=n: e.activation(
                                    out=tv[s2][:, 0:n], in_=bank(pv)[:, 1:1 + n], func=AF.Identity,
                                    scale=cw[:, 1, cv:cv + 1], bias=cb[:, cv:cv + 1]), ["ps%d" % pv, "cw", "cb"],
                                  ["tv%d" % s2])
                                V(lambda e, s2=s2, pv=pv, cv=cv, n=n: e.scalar_tensor_tensor(
                                    out=tw[s2][:, 0:n], in0=bank(pv)[:, 0:n], scalar=cw[:, 0, cv:cv + 1],
                                    in1=tv[s2][:, 0:n], op0=ALU.mult, op1=ALU.add), ["ps%d" % pv, "cw", "tv%d" % s2],
                                  ["tw%d" % s2])
                                V(lambda e, s2=s2, pv=pv, cv=cv, n=n: e.scalar_tensor_tensor(
                                    out=tv[s2][:, 0:n], in0=bank(pv)[:, 2:2 + n], scalar=cw[:, 2, cv:cv + 1],
                                    in1=tw[s2][:, 0:n], op0=ALU.mult, op1=ALU.add), ["ps%d" % pv, "cw", "tw%d" % s2],
                                  ["tv%d" % s2])
                                gs = fc["g"] % 3
                                fc["g"] += 1
                                G(lambda e, s2=s2, gs=gs, n=n: e.tensor_tensor(out=gst[gs][:, 0:n], in0=sg[s2][:, 0:n],
                                                                               in1=tv[s2][:, 0:n], op=ALU.mult),
                                  ["sg%d" % s2, "tv%d" % s2], ["gst%d" % gs])
                                DMA("sp", GT[b, c * 128:(c + 1) * 128, toff + o:toff + o + n], gst[gs][:, 0:n],
                                    ["gst%d" % gs], wpart(("GT", b, toff)))
                chk(5 + 5 * l)
                S.barrier()
                arena.reset()
                alloc_norm_bufs()
                ctxb["mtmp"] = [arena.alloc([128, D], F32) for _ in range(2)]
                wdn = arena.alloc([128, 22, D], BF16)
                gTw = [arena.alloc([128, 22, 512], BF16) for _ in range(2)]
                gb = [arena.alloc([128, D], F32) for _ in range(2)]
                for c_ in range(22):
                    DMA("pool", wdn[:, c_, :], ffn_down_w[l][c_ * 128:(c_ + 1) * 128, :], [], [("wdn", c_)])
                dc = {"w": 0, "m": 0}
                load_gb(l, 1, 2, gb[1], "gbf1")
                for b in range(NB):
                    load_gb(l, 1, b, gb[0], "gbf0")
                    for (src, sname, L, toff, row, dst, dname, is_out) in final(b):
                        o = 0
                        while o < L:
                            nt = min(512, L - o)
                            ws = dc["w"] % 2
                            dc["w"] += 1
                            dma3("sp", lambda j: gTw[ws][:, j, 0:nt],
                                 lambda j: GT[b, j * 128:(j + 1) * 128, toff + o:toff + o + nt], 22,
                                 rparts(("GT", b, toff)), ["gTw%d" % ws])
                            for i in range(nt // 128):
                                ms = dc["m"] % 2
                                dc["m"] += 1
                                mix = PS2[2 + ms]
                                for hf in range(2):
                                    for c in range(22):
                                        PE(lambda e, c=c, hf=hf, ws=ws, i=i, mix=mix: e.matmul(
                                            mix[:, hf * 512:(hf + 1) * 512], lhsT=gTw[ws][:, c, i * 128:(i + 1) * 128],
                                            rhs=wdn[:, c, hf * 512:(hf + 1) * 512], start=(c == 0), stop=(c == 21)),
                                           ["gTw%d" % ws, ("wdn", c)], ["ps%d" % (4 + 2 * ms + hf)])
                                r0 = o + i * 128
                                g_ = gb[0] if row < 2 else gb[1]
                                residual_update(mix[:, :], ["ps%d" % (4 + 2 * ms), "ps%d" % (5 + 2 * ms)], src,
                                                [(sname, b, r0 // 128)], dst, [(dname, b, r0 // 128)], r0, 128, g_,
                                                "gbf0" if row < 2 else "gbf1", is_output=is_out)
                            o += nt

            ffn_layer(0,
                      lambda b: [(XA[b], "XA", T, 0, b), (CA[b], "CA", CT, T, 2)],
                      lambda b: [(XA[b], "XA", T, 0, b, XB[b], "XB", False), (CA[b], "CA", CT, T, 2, CB[b], "CB", False)])

            UC1 = dscr("UC1", [NB, D, T], BF16)
            OG1 = dscr("OG1", [NB, D, T], BF16)
            QT1 = dscr("QT1", [NB, D, T], BF16)
            KT1 = dscr("KT1", [NB, D, TS], BF16)
            KK1 = dscr("KK1", [NB, TS, D], BF16)
            VV1 = dscr("VV1", [NB, TS, D], BF16)
            GG1 = dscr("GG1", [NB, TS, 16])
            SC1 = dscr("SC1", [NB, TS, 24])
            DC1 = dscr("DC1", [NB, 2 * 4 * 18])

            def make_windows(L):
                wins = []
                o = 0
                while o < L:
                    r0 = max(o - 1, 0)
                    c0 = 1 if o == 0 else 0
                    nr = min(512 - c0, L - r0)
                    rz = (r0 + nr == L)
                    ncols = c0 + nr + (1 if rz else 0)
                    if ncols > 512:
                        nr -= 1
                        rz = False
                        ncols = 512
                    nout = ncols - 2
                    wins.append((r0, nr, c0, rz, ncols, o, nout))
                    o += nout
                return wins

            chk(6)
            S.barrier()
            arena.reset()
            alloc_norm_bufs()
            win1 = arena.alloc([128, 8, 3088], BF16)
            qw = arena.alloc([128, 4, 2, 256], BF16)
            kw = arena.alloc([128, 4, 2, 256], BF16)
            cwr = arena.alloc([128, 3, 8], F32)
            cbr = arena.alloc([128, 8], F32)
            gbias = arena.alloc([128, 16], F32)
            hT1 = [arena.alloc([128, 8, 512], BF16) for _ in range(2)]
            ucw = [arena.alloc([128, 8, 512], BF16) for _ in range(2)]
            ta1 = [arena.alloc([128, 512], F32) for _ in range(2)]
            tb1 = [arena.alloc([128, 512], F32) for _ in range(2)]
            fst = [arena.alloc([128, 512], BF16) for _ in range(4)]
            tst = [arena.alloc([128, D], BF16) for _ in range(2)]
            gst1 = [arena.alloc([128, 16], F32) for _ in range(2)]
            load_wb(win1, rec_in_w, 8, [(j * 128, 128, j) for j in range(8)] + [(1024, 512, "v0"), (1536, 512, "v1"),
                                         (3072, 16, "g")] + [(2048 + j * 128, 128, 16 + j) for j in range(8)], "win1")
            for h in range(4):
                for dc in range(2):
                    DMA("pool", qw[:, h, dc, :], rec_q_w[h, dc * 128:(dc + 1) * 128, :], [], ["qw"])
                    DMA("pool", kw[:, h, dc, :], rec_k_w[h, dc * 128:(dc + 1) * 128, :], [], ["kw"])
            for j in range(3):
                DMA("sp", cwr[:, j, :], rec_conv_w[j].rearrange("(c p) -> p c", p=128), [], ["cwr"])
            DMA("sp", cbr, rec_conv_b.rearrange("(c p) -> p c", p=128), [], ["cbr"])
            DMA("sp", gbias, rec_gate_b.partition_broadcast(128), [], ["gbias"])
            c1 = {"w": 0, "p": 0, "f": 0, "t": 0, "g": 0}

            def nbank():
                c1["p"] += 1
                return c1["p"] % 4

            for b in range(NB):
                for (src, sname, L, toff, row, isx) in ((CB[b], "CB", CT, 0, 2, False), (XB[b], "XB", T, CT, b, True)):
                    for (r0, nr, c0, rz, ncols, o, n) in make_windows(L):
                        ws = c1["w"] % 2
                        c1["w"] += 1
                        hT, hres = hT1[ws], "hT1_%d" % ws
                        uc_, ures = ucw[ws], "ucw%d" % ws
                        if c0 == 1:
                            V(lambda e: e.memset(hT[:, :, 0:1], 0.0), [], [hres])
                        if rz:
                            V(lambda e: e.memset(hT[:, :, c0 + nr:c0 + nr + 1], 0.0), [], [hres])
                        rr_ = r0
                        while rr_ < r0 + nr:
                            n_ = min(128, r0 + nr - rr_)
                            make_hT(src, xres_fn(sname, b), rr_, n_, A1[1][:, row, :], SH1[1][:, row, :], "A1_1",
                                    hT, hres, c0 + rr_ - r0)
                            rr_ += n_

                        def fm_proj(col0, pbk):
                            for k in range(8):
                                PE(lambda e: e.matmul(bank(pbk)[:, 0:ncols], lhsT=win1[:, k, col0:col0 + 128],
                                                      rhs=hT[:, k, 0:ncols], start=(k == 0), stop=(k == 7)),
                                   [("win1", col0 // 128), hres], ["ps%d" % pbk])

                        for j in range(8):
                            pbk = nbank()
                            s2 = j % 2
                            fm_proj(j * 128, pbk)
                            A(lambda e: e.activation(out=ta1[s2][:, 0:n], in_=bank(pbk)[:, 1:1 + n], func=AF.Identity,
                                                     scale=cwr[:, 1, j:j + 1], bias=cbr[:, j:j + 1]),
                              ["ps%d" % pbk, "cwr", "cbr"], ["ta1_%d" % s2])
                            V(lambda e: e.scalar_tensor_tensor(out=tb1[s2][:, 0:n], in0=bank(pbk)[:, 0:n],
                                                               scalar=cwr[:, 0, j:j + 1], in1=ta1[s2][:, 0:n],
                                                               op0=ALU.mult, op1=ALU.add),
                              ["ps%d" % pbk, "cwr", "ta1_%d" % s2], ["tb1_%d" % s2])
                            V(lambda e: e.scalar_tensor_tensor(out=ta1[s2][:, 0:n], in0=bank(pbk)[:, 2:2 + n],
                                                               scalar=cwr[:, 2, j:j + 1], in1=tb1[s2][:, 0:n],
                                                               op0=ALU.mult, op1=ALU.add),
                              ["ps%d" % pbk, "cwr", "tb1_%d" % s2], ["ta1_%d" % s2])
                            A(lambda e: e.activation(out=uc_[:, j, 0:n], in_=ta1[s2][:, 0:n], func=AF.Silu),
                              ["ta1_%d" % s2], [ures])
                            if isx:
                                DMA("sp", UC1[b, j * 128:(j + 1) * 128, o:o + n], uc_[:, j, 0:n], [ures],
                                    wpart(("UC1", b)))
                        if isx:
                            for j in range(8):
                                pbk = nbank()
                                fs = c1["f"] % 4
                                c1["f"] += 1
                                fm_proj(2048 + j * 128, pbk)
                                A(lambda e: e.activation(out=fst[fs][:, 0:n], in_=bank(pbk)[:, 1:1 + n],
                                                         func=AF.Sigmoid), ["ps%d" % pbk], ["fst%d" % fs])
                                DMA("sp", OG1[b, j * 128:(j + 1) * 128, o:o + n], fst[fs][:, 0:n], ["fst%d" % fs],
                                    wpart(("OG1", b)))
                        for h in range(4):
                            for ec in range(2):
                                for (wt_, wres_, dst, scl, need) in ((kw, "kw", KT1, 1.0 / 16, True),
                                                                     (qw, "qw", QT1, 1.0, isx)):
                                    if not need:
                                        continue
                                    pbk = nbank()
                                    fs = c1["f"] % 4
                                    c1["f"] += 1
                                    for dc in range(2):
                                        PE(lambda e: e.matmul(bank(pbk)[:, 0:n],
                                                              lhsT=wt_[:, h, dc, ec * 128:(ec + 1) * 128],
                                                              rhs=uc_[:, 2 * h + dc, 0:n], start=(dc == 0),
                                                              stop=(dc == 1)), [wres_, ures], ["ps%d" % pbk])
                                    A(lambda e: e.activation(out=fst[fs][:, 0:n], in_=bank(pbk)[:, 0:n], func=AF.Copy,
                                                             scale=scl), ["ps%d" % pbk], ["fst%d" % fs])
                                    tcol = (toff + o) if dst is KT1 else o
                                    DMA("sp", dst[b, h * 256 + ec * 128:h * 256 + (ec + 1) * 128, tcol:tcol + n],
                                        fst[fs][:, 0:n], ["fst%d" % fs],
                                        wpart(("KT1", b)) if dst is KT1 else wpart(("QT1", b)))
                        i0 = 0
                        while i0 < n:
                            m = min(128, n - i0)
                            trow = toff + o + i0
                            ts_ = c1["t"] % 2
                            c1["t"] += 1
                            mix = PS2[2]
                            for h in range(4):
                                for dc in range(2):
                                    PE(lambda e: e.matmul(mix[0:m, h * 256:(h + 1) * 256],
                                                          lhsT=uc_[:, 2 * h + dc, i0:i0 + m], rhs=kw[:, h, dc, :],
                                                          start=(dc == 0), stop=(dc == 1)),
                                       [ures, "kw"], ["ps4", "ps5"])
                            A(lambda e: e.activation(out=tst[ts_][0:m, :], in_=mix[0:m, :], func=AF.Copy,
                                                     scale=1.0 / 16), ["ps4", "ps5"], ["tst%d" % ts_])
                            DMA("sp", KK1[b, trow:trow + m, :], tst[ts_][0:m, :], ["tst%d" % ts_], wpart(("KK1", b)))
                            ts_ = c1["t"] % 2
                            c1["t"] += 1
                            mix = PS2[3]
                            for hf in range(2):
                                for k in range(8):
                                    PE(lambda e: e.matmul(mix[0:m, hf * 512:(hf + 1) * 512],
                                                          lhsT=hT[:, k, 1 + i0:1 + i0 + m],
                                                          rhs=win1[:, k, 1024 + hf * 512:1024 + (hf + 1) * 512],
                                                          start=(k == 0), stop=(k == 7)),
                                       [hres, ("win1", "v%d" % hf)], ["ps%d" % (6 + hf)])
                            A(lambda e: e.activation(out=tst[ts_][0:m, :], in_=mix[0:m, :], func=AF.Copy),
                              ["ps6", "ps7"], ["tst%d" % ts_])
                            DMA("sp", VV1[b, trow:trow + m, :], tst[ts_][0:m, :], ["tst%d" % ts_], wpart(("VV1", b)))
                            gs = c1["g"] % 2
                            c1["g"] += 1
                            pbk = nbank()
                            for k in range(8):
                                PE(lambda e: e.matmul(bank(pbk)[0:m, 0:16], lhsT=hT[:, k, 1 + i0:1 + i0 + m],
                                                      rhs=win1[:, k, 3072:3088], start=(k == 0), stop=(k == 7)),
                                   [hres, ("win1", "g")], ["ps%d" % pbk])
                            V(lambda e: e.tensor_tensor(out=gst1[gs][0:m, :], in0=bank(pbk)[0:m, 0:16],
                                                        in1=gbias[0:m, :], op=ALU.add),
                              ["ps%d" % pbk, "gbias"], ["gst1_%d" % gs])
                            DMA("sp", GG1[b, trow:trow + m, :], gst1[gs][0:m, :], ["gst1_%d" % gs], wpart(("GG1", b)))
                            i0 += m

            chk(7)
            S.barrier()
            arena.reset()
            gg = arena.alloc([128, 18, 16], F32)
            ones4 = arena.alloc([4, TS], F32)
            IG = [arena.alloc([4, TS], F32) for _ in range(2)]
            FG = [arena.alloc([4, TS], F32) for _ in range(2)]
            t_a = arena.alloc([4, TS], F32)
            t_b = arena.alloc([4, TS], F32)
            Bc = arena.alloc([4, TS], F32)
            Mc = arena.alloc([4, TS], F32)
            QY = [[arena.alloc([4, TS], F32) for _ in range(3)] for _ in range(2)]
            Mpv = arena.alloc([4, 18], F32)
            dcy = arena.alloc([4, 18], F32)
            scs = arena.alloc([128, 18, 24], F32)
            V(lambda e: e.memset(ones4, 1.0), [], ["ones4"])

            def lb_tile(q):
                return q + 2 if q < 16 else q - 16

            for b in range(NB):
                dma3("sp", lambda j: gg[:, j, :], lambda j: GG1[b, j * 128:(j + 1) * 128, :], 18, rparts(("GG1", b)), ["gg"])
                for dr in range(2):
                    for ti, dstt, dres in ((0, IG[dr], "IG%d" % dr), (1, FG[dr], "FG%d" % dr)):
                        ty = dr * 2 + ti
                        for q in range(18):
                            tl = q if dr == 0 else lb_tile(q)
                            pq = PS2[q // 8]
                            PE(lambda e: e.matmul(pq[0:4, (q % 8) * 128:(q % 8 + 1) * 128],
                                                  lhsT=gg[:, tl, ty * 4:(ty + 1) * 4], rhs=ident[:, :], start=True,
                                                  stop=True), ["gg", "ident"], ["ps%d" % (2 * (q // 8) + (q % 8) // 4)])
                        for pi in range(3):
                            ncol = 1024 if pi < 2 else 256
                            A(lambda e: e.activation(out=dstt[:, pi * 1024:pi * 1024 + ncol], in_=PS2[pi][0:4, 0:ncol],
                                                     func=AF.Copy), ["ps%d" % (2 * pi), "ps%d" % (2 * pi + 1)], [dres])
                for dr in range(2):
                    ig, fg = IG[dr], FG[dr]
                    igr, fgr = "IG%d" % dr, "FG%d" % dr

                    def dview(ap):
                        return ap if dr == 0 else ap[:, ::-1]

                    A(lambda e: e.activation(out=t_a, in_=fg, func=AF.Abs), [fgr], ["t_a"])
                    A(lambda e: e.activation(out=t_a, in_=t_a, func=AF.Exp, scale=-1.0), ["t_a"], ["t_a"])
                    A(lambda e: e.activation(out=t_a, in_=t_a, func=AF.Ln, bias=1.0), ["t_a"], ["t_a"])
                    V(lambda e: e.tensor_scalar(out=t_b, in0=fg, scalar1=0.0, scalar2=None, op0=ALU.min), [fgr], ["t_b"])
                    V(lambda e: e.tensor_tensor(out=t_b, in0=t_b, in1=t_a, op=ALU.subtract), ["t_a", "t_b"], ["t_b"])
                    V(lambda e: e.tensor_tensor_scan(out=dview(Bc), data0=dview(ones4), data1=dview(t_b), initial=0.0,
                                                     op0=ALU.mult, op1=ALU.add), ["ones4", "t_b"], ["Bc"])
                    V(lambda e: e.tensor_tensor(out=t_a, in0=ig, in1=Bc, op=ALU.subtract), [igr, "Bc"], ["t_a"])
                    V(lambda e: e.tensor_tensor_scan(out=dview(Mc), data0=dview(ones4), data1=dview(t_a), initial=0.0,
                                                     op0=ALU.mult, op1=ALU.max), ["ones4", "t_a"], ["Mc"])
                    M3 = Mc.rearrange("p (q t) -> p q t", t=128)
                    a3 = t_a.rearrange("p (q t) -> p q t", t=128)
                    B3 = Bc.rearrange("p (q t) -> p q t", t=128)
                    V(lambda e: e.memset(Mpv, 0.0), [], ["Mpv"])
                    if dr == 0:
                        V(lambda e: e.tensor_copy(out=Mpv[:, 1:18], in_=M3[:, 0:17, 127]), ["Mc"], ["Mpv"])
                        Mend = M3[:, :, 127]
                    else:
                        V(lambda e: e.tensor_copy(out=Mpv[:, 0:17], in_=M3[:, 1:18, 0]), ["Mc"], ["Mpv"])
                        Mend = M3[:, :, 0]
                    Mpb = Mpv.unsqueeze(2).to_broadcast([4, 18, 128])
                    q0, q1, q2 = QY[dr]
                    qr = ["QY%d_%d" % (dr, i) for i in range(3)]
                    V(lambda e: e.tensor_tensor(out=q0.rearrange("p (q t) -> p q t", t=128), in0=a3, in1=Mpb,
                                                op=ALU.subtract), ["t_a", "Mpv"], [qr[0]])
                    A(lambda e: e.activation(out=q0, in_=q0, func=AF.Exp), [qr[0]], [qr[0]])
                    V(lambda e: e.tensor_tensor(out=q1.rearrange("p (q t) -> p q t", t=128), in0=a3,
                                                in1=Mend.unsqueeze(2).to_broadcast([4, 18, 128]), op=ALU.subtract),
                      ["t_a", "Mc"], [qr[1]])
                    A(lambda e: e.activation(out=q1, in_=q1, func=AF.Exp), [qr[1]], [qr[1]])
                    V(lambda e: e.scalar_tensor_tensor(out=q2.rearrange("p (q t) -> p q t", t=128), in0=B3, scalar=-1.0,
                                                       in1=Mpb, op0=ALU.mult, op1=ALU.subtract), ["Bc", "Mpv"], [qr[2]])
                    A(lambda e: e.activation(out=q2, in_=q2, func=AF.Exp), [qr[2]], [qr[2]])
                    V(lambda e: e.tensor_tensor(out=dcy, in0=Mpv, in1=Mend, op=ALU.subtract), ["Mpv", "Mc"], ["dcy"])
                    A(lambda e: e.activation(out=dcy, in_=dcy, func=AF.Exp), ["dcy"], ["dcy"])
                    dcv = DC1[b].rearrange("(d h q) -> d h q", d=2, h=4)[dr]
                    if dr == 0:
                        DMA("sp", dcv, dcy, ["dcy"], wpart(("DC1", b)))
                    else:
                        DMA("sp", dcv[:, 2:18], dcy[:, 0:16], ["dcy"], wpart(("DC1", b)))
                        DMA("sp", dcv[:, 0:2], dcy[:, 16:18], ["dcy"], wpart(("DC1", b)))
                    pst = bank(6)
                    for q in range(18):
                        tl = q if dr == 0 else lb_tile(q)
                        for qi in range(3):
                            col = tl * 24 + (dr * 3 + qi) * 4
                            PE(lambda e: e.matmul(pst[:, col:col + 4], lhsT=QY[dr][qi][:, q * 128:(q + 1) * 128],
                                                  rhs=ident[0:4, 0:4], start=True, stop=True), [qr[qi], "ident"], ["ps6"])
                A(lambda e: e.activation(out=scs, in_=bank(6)[:, 0:432].rearrange("p (n c) -> p n c", c=24), func=AF.Copy),
                  ["ps6"], ["scs"])
                dma3("sp", lambda j: SC1[b, j * 128:(j + 1) * 128, :], lambda j: scs[:, j, :], 18, ["scs"], wpart(("SC1", b)))

            chk(8)
            S.barrier()
            arena.reset()
            alloc_norm_bufs()
            ctxb["mtmp"] = [arena.alloc([128, D], F32) for _ in range(2)]
            wo1 = arena.alloc([128, 8, D], BF16)
            QTh = arena.alloc([128, 2, T], BF16)
            KTh = arena.alloc([128, 2, TS], BF16)
            KKh = arena.alloc([128, 18, 256], BF16)
            VVh = arena.alloc([128, 18, 257], BF16)
            ogh = arena.alloc([128, 2, T], BF16)
            uch = arena.alloc([128, 2, T], BF16)
            hnT = arena.alloc([128, 2, T], BF16)
            tyy = arena.alloc([128, T], F32)
            scb = arena.alloc([128, 18, 24], F32)
            dcb = arena.alloc([128, 144], F32)
            lmk = arena.alloc([128, 2, 128], F32)
            Sst = [arena.alloc([128, 2, 257], F32) for _ in range(2)]
            Sbf = [arena.alloc([128, 2, 257], BF16) for _ in range(2)]
            hs = arena.alloc([128, 16, 256], F32)
            ATb = [arena.alloc([128, 128], BF16) for _ in range(2)]
            Ktl = [arena.alloc([128, 256], BF16) for _ in range(2)]
            dnn = [arena.alloc([128, 2], F32) for _ in range(2)]
            hnb = [arena.alloc([128, 256], BF16) for _ in range(2)]
            yT = arena.alloc([128, 8, T], BF16)
            rng = arena.alloc([128, 8], F32)
            rsk = arena.alloc([128, 8], F32)
            gb1 = arena.alloc([128, D], F32)
            load_w(wo1, rec_out_w, 8, D, "wo1")
            for m_ in range(2):
                DMA("sp", lmk[:, m_, :], lmask_in[m_], [], ["lmk"])
            DMA("sp", rng, rec_norm_g.rearrange("(c p) -> p c", p=128), [], ["rng"])
            DMA("sp", rsk, rec_skip.rearrange("(c p) -> p c", p=128), [], ["rsk"])
            V(lambda e: e.memset(VVh[:, :, 256:257], 1.0), [], ["VVh"])
            c3 = {"i": 0}
            for b in range(NB):
                dma3("sp", lambda j: scb[:, j, :], lambda j: SC1[b, j * 128:(j + 1) * 128, :], 18, rparts(("SC1", b)), ["scb"])
                DMA("sp", dcb, DC1[b].partition_broadcast(128), rparts(("DC1", b)), ["dcb"])
                load_gb(1, 0, b, gb1, "gb1")
                for h in range(4):
                    dma3("sp", lambda j: QTh[:, j, :], lambda j: QT1[b, h * 256 + j * 128:h * 256 + (j + 1) * 128, :], 2,
                         rparts(("QT1", b)), ["QTh"])
                    dma3("sp", lambda j: KTh[:, j, :], lambda j: KT1[b, h * 256 + j * 128:h * 256 + (j + 1) * 128, :], 2,
                         rparts(("KT1", b)), ["KTh"])
                    dma3("sp", lambda j: KKh[:, j, :], lambda j: KK1[b, j * 128:(j + 1) * 128, h * 256:(h + 1) * 256], 18,
                         rparts(("KK1", b)), ["KKh"])
                    dma3("sp", lambda j: VVh[:, j, 0:256], lambda j: VV1[b, j * 128:(j + 1) * 128, h * 256:(h + 1) * 256], 18,
                         rparts(("VV1", b)), ["VVh"])
                    dma3("sp", lambda j: ogh[:, j, :], lambda j: OG1[b, h * 256 + j * 128:h * 256 + (j + 1) * 128, :], 2,
                         rparts(("OG1", b)), ["ogh"])
                    dma3("sp", lambda j: uch[:, j, :], lambda j: UC1[b, h * 256 + j * 128:h * 256 + (j + 1) * 128, :], 2,
                         rparts(("UC1", b)), ["uch"])
                    for dr in range(2):
                        V(lambda e: e.memset(Sst[dr], 0.0), [], ["Sst%d" % dr])
                        V(lambda e: e.memset(Sbf[dr], 0.0), [], ["Sbf%d" % dr])
                    order = [list(range(18)), [1, 0] + list(range(17, 1, -1))]
                    first_dir_done = set()
                    for step in range(18):
                        for dr in range(2):
                            tl = order[dr][step]
                            isx = tl >= 2
                            xt_ = tl - 2
                            pb0 = dr * 4
                            sres, bres = "Sst%d" % dr, "Sbf%d" % dr
                            ci = c3["i"] % 2
                            c3["i"] += 1
                            colA = (dr * 3 + 0) * 4 + h
                            colB = (dr * 3 + 1) * 4 + h
                            colF = (dr * 3 + 2) * 4 + h
                            if isx:
                                pslot = pb0 + step % 2
                                pA = bank(pslot)[:, 0:128]
                                for dc in range(2):
                                    PE(lambda e: e.matmul(pA[:, 0:128], lhsT=KTh[:, dc, tl * 128:(tl + 1) * 128],
                                                          rhs=QTh[:, dc, xt_ * 128:(xt_ + 1) * 128], start=(dc == 0),
                                                          stop=(dc == 1)), ["KTh", "QTh"], ["ps%d" % pslot])
                                V(lambda e: e.scalar_tensor_tensor(out=ATb[ci], in0=pA[:, 0:128],
                                                                   scalar=scb[:, tl, colA:colA + 1], in1=lmk[:, dr, :],
                                                                   op0=ALU.mult, op1=ALU.mult),
                                  ["ps%d" % pslot, "scb", "lmk"], ["ATb%d" % ci])
                                pO = bank(pslot)[:, 128:512]
                                PE(lambda e: e.matmul(pO[:, 0:257], lhsT=ATb[ci], rhs=VVh[:, tl, :], start=True, stop=False),
                                   ["ATb%d" % ci, "VVh"], ["ps%d" % pslot])
                                for dc in range(2):
                                    PE(lambda e: e.matmul(pO[:, 0:257], lhsT=QTh[:, dc, xt_ * 128:(xt_ + 1) * 128],
                                                          rhs=Sbf[dr][:, dc, :], start=False, stop=(dc == 1)),
                                       ["QTh", bres], ["ps%d" % pslot])
                                dn = dnn[ci]
                                A(lambda e: e.activation(out=dn[:, 0:1], in_=pO[:, 256:257], func=AF.Abs),
                                  ["ps%d" % pslot], ["dnn%d" % ci])
                                V(lambda e: e.tensor_tensor(out=dn[:, 0:1], in0=dn[:, 0:1],
                                                            in1=scb[:, tl, colF:colF + 1], op=ALU.max),
                                  ["dnn%d" % ci, "scb"], ["dnn%d" % ci])
                                V(lambda e: e.reciprocal(out=dn[:, 1:2], in_=dn[:, 0:1]), ["dnn%d" % ci], ["dnn%d" % ci])
                                hres_ = ("hs", xt_)
                                if xt_ not in first_dir_done:
                                    first_dir_done.add(xt_)
                                    V(lambda e: e.tensor_scalar(out=hs[:, xt_, :], in0=pO[:, 0:256], scalar1=dn[:, 1:2],
                                                                scalar2=None, op0=ALU.mult),
                                      ["ps%d" % pslot, "dnn%d" % ci], [hres_])
                                else:
                                    V(lambda e: e.scalar_tensor_tensor(out=hs[:, xt_, :], in0=pO[:, 0:256],
                                                                       scalar=dn[:, 1:2], in1=hs[:, xt_, :],
                                                                       op0=ALU.mult, op1=ALU.add),
                                      ["ps%d" % pslot, "dnn%d" % ci, hres_], [hres_])
                            if step < 17:
                                A(lambda e: e.activation(out=Ktl[ci], in_=KKh[:, tl, :], func=AF.Copy,
                                                         scale=scb[:, tl, colB:colB + 1]), ["KKh", "scb"], ["Ktl%d" % ci])
                                dcol = (dr * 4 + h) * 18 + tl
                                for dc in range(2):
                                    pS = bank(pb0 + 2 + dc)
                                    PE(lambda e: e.matmul(pS[:, 0:257], lhsT=Ktl[ci][:, dc * 128:(dc + 1) * 128],
                                                          rhs=VVh[:, tl, :], start=True, stop=True),
                                       ["Ktl%d" % ci, "VVh"], ["ps%d" % (pb0 + 2 + dc)])
                                    V(lambda e: e.scalar_tensor_tensor(out=Sst[dr][:, dc, :], in0=Sst[dr][:, dc, :],
                                                                       scalar=dcb[:, dcol:dcol + 1], in1=pS[:, 0:257],
                                                                       op0=ALU.mult, op1=ALU.add),
                                      [sres, "dcb", "ps%d" % (pb0 + 2 + dc)], [sres])
                                A(lambda e: e.activation(out=Sbf[dr], in_=Sst[dr], func=AF.Copy), [sres], [bres])
                    for xt_ in range(16):
                        ci = xt_ % 2
                        stt = ctxb["st"][xt_ % 3]
                        sres_ = "st%d" % (xt_ % 3)
                        junk = ctxb["junk"]
                        A(lambda e: e.activation(out=junk[:, 0:256], in_=hs[:, xt_, :], func=AF.Square,
                                                 accum_out=stt[:, 0:1]), [("hs", xt_)], ["junk", sres_])
                        A(lambda e: e.activation(out=stt[:, 1:2], in_=stt[:, 0:1], func=AF.Sqrt, scale=1.0 / 256,
                                                 bias=epsb[:, :]), [sres_, "epsb"], [sres_])
                        V(lambda e: e.reciprocal(out=stt[:, 1:2], in_=stt[:, 1:2]), [sres_], [sres_])
                        V(lambda e: e.tensor_scalar(out=hnb[ci], in0=hs[:, xt_, :], scalar1=stt[:, 1:2], scalar2=None,
                                                    op0=ALU.mult), [("hs", xt_), sres_], ["hnb%d" % ci])
                        ptv = bank_bf(6 + ci).rearrange("p (k t) -> p k t", t=128)
                        for dc in range(2):
                            PE(lambda e: e.transpose(out=ptv[:, dc, :], in_=hnb[ci][:, dc * 128:(dc + 1) * 128],
                                                     identity=identb[:]), ["hnb%d" % ci, "identb"], ["ps%d" % (6 + ci)])
                        A(lambda e: e.activation(out=hnT[:, :, xt_ * 128:(xt_ + 1) * 128], in_=ptv[:, 0:2, :],
                                                 func=AF.Copy), ["ps%d" % (6 + ci)], ["hnT"])
                    for dc in range(2):
                        fcx = 2 * h + dc
                        V(lambda e: e.tensor_scalar(out=tyy, in0=hnT[:, dc, :], scalar1=rng[:, fcx:fcx + 1], scalar2=None,
                                                    op0=ALU.mult), ["hnT", "rng"], ["tyy"])
                        V(lambda e: e.scalar_tensor_tensor(out=tyy, in0=uch[:, dc, :], scalar=rsk[:, fcx:fcx + 1], in1=tyy,
                                                           op0=ALU.mult, op1=ALU.add), ["uch", "rsk", "tyy"], ["tyy"])
                        V(lambda e: e.tensor_tensor(out=yT[:, fcx, :], in0=tyy, in1=ogh[:, dc, :], op=ALU.mult),
                          ["tyy", "ogh"], [("yT", fcx)])
                for n in range(16):
                    mix = PS2[n % 2]
                    for hf in range(2):
                        for k in range(8):
                            PE(lambda e: e.matmul(mix[:, hf * 512:(hf + 1) * 512], lhsT=yT[:, k, n * 128:(n + 1) * 128],
                                                  rhs=wo1[:, k, hf * 512:(hf + 1) * 512], start=(k == 0), stop=(k == 7)),
                               [("yT", k), "wo1"], ["ps%d" % (2 * (n % 2) + hf)])
                    residual_update(mix[:, :], ["ps%d" % (2 * (n % 2)), "ps%d" % (2 * (n % 2) + 1)], XB[b],
                                    [("XB", b, n)], XA[b], [("XA", b, n)], n * 128, 128, gb1, "gb1")

            ffn_layer(1,
                      lambda b: [(XA[b], "XA", T, 0, b)],
                      lambda b: [(XA[b], "XA", T, 0, b, out[b], "out", True)])

        except _Stop:
            S.barrier()
        S.emit(st)
        build_program.stats = S.stats
    return nc


def _consts():
    ident = np.eye(128, dtype=np.float32)
    inv = (10000.0 ** (-np.arange(16, dtype=np.float32) / 16)).astype(np.float32)
    t = np.arange(T)
    row = (t // 64).astype(np.float32)
    col = (t % 64).astype(np.float32)
    rc = np.zeros((128, T), np.float32)
    rs = np.zeros((128, T), np.float32)
    for p in range(128):
        d = p % 64
        axis, half, f = d // 32, (d % 32) // 16, d % 16
        pos = row if axis == 0 else col
        ang = (pos * inv[f]).astype(np.float32)
        rc[p] = np.cos(ang)
        rs[p] = np.sin(ang) * (-1.0 if half == 0 else 1.0)
    j = np.arange(128)[:, None]
    i = np.arange(128)[None, :]
    prev = (j >= i).astype(np.float32)
    nxt = (j <= i).astype(np.float32)
    amask = np.stack([np.tile(prev, (1, 4)), np.tile(nxt, (1, 4))]).astype(np.float32)
    pe = np.zeros((4, 2, 8), np.float32)
    for g in range(4):
        w = 2 ** (g + 1)
        half = w // 2
        for k in range(half):
            pe[g, 0, k] = 1.0 / (k + half)
        for k in range(half - 1):
            pe[g, 1, k] = 1.0 / (2 * half - 1 - k)
    pool_edge = np.tile(pe.reshape(1, 64), (128, 1)).astype(np.float32)
    lmask = np.stack([(j <= i).astype(np.float32), (j >= i).astype(np.float32)])
    return dict(ident=ident, rope_c=rc, rope_s=rs, amask=amask, pool_edge=pool_edge, lmask=lmask)


def _perm_head():
    p = np.zeros(64, np.int64)
    for d in range(64):
        axis, half, f = d // 32, (d % 32) // 16, d % 16
        p[d] = axis * 32 + (1 - half) * 16 + f
    return p


def _prep_shared(inp):
    w = np.asarray(inp["attn_in_w"][0], np.float32)
    ph = _perm_head()
    wz = np.concatenate([w, np.zeros((w.shape[0], 1), np.float32)], axis=1)
    Z = np.full(64, w.shape[1], np.int64)
    q = [np.arange(c * 128, (c + 1) * 128) for c in range(4)]
    k0 = 512 + np.arange(64)
    k1 = 576 + np.arange(64)
    kz = [np.concatenate([k0, Z]), np.concatenate([Z, k0]), np.concatenate([k1, Z]), np.concatenate([Z, k1])]
    base = q + kz

    def partner(idx):
        o = idx.copy()
        for h0 in range(0, 128, 64):
            blk = idx[h0:h0 + 64]
            o[h0:h0 + 64] = blk[ph]
        return o

    cols = base + [partner(c) for c in base] + [np.arange(640, 768)] + [np.arange(768 + g * 128, 896 + g * 128) for g in range(4)]
    cols = np.concatenate(cols)
    w = wz
    sh = dict(
        mod_w=np.ascontiguousarray(inp["mod_w"], np.float32),
        mod_b=np.ascontiguousarray(inp["mod_b"], np.float32),
        norm_g=np.ascontiguousarray(inp["norm_g"], np.float32),
        w_in0=np.ascontiguousarray(w[:, cols]),
        attn_sink=np.ascontiguousarray(inp["attn_sink"][0], np.float32),
        pool_w=np.ascontiguousarray(inp["pool_w"][0], np.float32),
        pool_scale=np.ascontiguousarray(inp["pool_scale"][0], np.float32),
        attn_out_w=np.ascontiguousarray(inp["attn_out_w"][0], np.float32),
        rec_in_w=np.ascontiguousarray(inp["rec_in_w"][0], np.float32),
        rec_gate_b=np.ascontiguousarray(inp["rec_gate_b"][0].reshape(16), np.float32),
        rec_conv_w=np.ascontiguousarray(inp["rec_conv_w"][0], np.float32),
        rec_conv_b=np.ascontiguousarray(inp["rec_conv_b"][0], np.float32),
        rec_q_w=np.ascontiguousarray(inp["rec_q_w"][0], np.float32),
        rec_k_w=np.ascontiguousarray(inp["rec_k_w"][0], np.float32),
        rec_norm_g=np.ascontiguousarray(inp["rec_norm_g"][0], np.float32),
        rec_skip=np.ascontiguousarray(inp["rec_skip"][0], np.float32),
        rec_out_w=np.ascontiguousarray(inp["rec_out_w"][0], np.float32),
        ffn_up_w=np.ascontiguousarray(inp["ffn_up_w"], np.float32),
        ffn_conv_w=np.ascontiguousarray(inp["ffn_conv_w"], np.float32),
        ffn_conv_b=np.ascontiguousarray(inp["ffn_conv_b"], np.float32),
        ffn_down_w=np.ascontiguousarray(inp["ffn_down_w"], np.float32),
    )
    sh.update(_consts())
    return sh


def make_in_maps(inp, cores):
    sh = _prep_shared(inp)
    x = np.asarray(inp["x"], np.float32)
    c = np.asarray(inp["c"], np.float32)
    ctx = np.asarray(inp["ctx"], np.float32)
    c_ctx = np.asarray(inp["c_ctx"], np.float32)
    maps = []
    for i in cores:
        m = dict(sh)
        m["x"] = np.ascontiguousarray(x[NB * i:NB * (i + 1)])
        m["ctx"] = np.ascontiguousarray(ctx[NB * i:NB * (i + 1)])
        m["cvec"] = np.ascontiguousarray(np.concatenate([c[NB * i:NB * (i + 1)], c_ctx[None, :]], axis=0))
        maps.append(m)
    return maps


def kernel(**inputs):
    nc = build_program()
    maps = make_in_maps(inputs, list(range(8)))
    res = run_bass_kernel_spmd(nc, maps, core_ids=list(range(8)))
    return np.concatenate([np.asarray(r["out"], np.float32) for r in res.results], axis=0)
```

```python
import math
from contextlib import ExitStack

import numpy as np
import concourse.bass as bass
import concourse.mybir as mybir
from concourse.bass_utils import run_bass_kernel_spmd

F32 = mybir.dt.float32
BF16 = mybir.dt.bfloat16
U8 = mybir.dt.uint8
AF = mybir.ActivationFunctionType
ALU = mybir.AluOpType

ENGS = ["pe", "act", "dve", "pool", "sp"]
import os as _os
N_DMA_SEMS = int(_os.environ.get("NDS", "24"))

D = 1024
T = 2048
CT = 256
NB = 2
TS = T + CT
DFF = 2816
EPS = 1e-6


class _Proxy:
    def __getattr__(self, name):
        def f(*a, **k):
            self.call = (name, a, k)
            return self
        return f


class Sched:
    def __init__(self, nc):
        self.nc = nc
        self.ops = {e: [] for e in ENGS}
        self.lastw = {}
        self.readers = {}
        self.dma_rr = {e: 0 for e in ENGS}
        self.dma_last = {}
        self.out_dmas = []
        self.pending_dmas = []

    def add(self, eng, fn, reads=(), writes=(), dma=False, is_output=False, extra_deps=()):
        ops = self.ops[eng]
        me = (eng, len(ops))
        deps = {}

        def dep(p, kind):
            if p is None or p == me:
                return
            if deps.get(p) == "raw":
                return
            deps[p] = kind

        for r in reads:
            dep(self.lastw.get(r), "raw")
        for r in writes:
            dep(self.lastw.get(r), "order")
            for rd in self.readers.get(r, ()):
                dep(rd, "order")
        for p in extra_deps:
            dep(p, "raw")
        prox = _Proxy()
        fn(prox)
        rec = dict(call=prox.call, dma=dma, signal=False, slot=None)
        if dma:
            slot = self.dma_rr[eng] % N_DMA_SEMS
            self.dma_rr[eng] += 1
            rec["slot"] = slot
            prev = self.dma_last.get((eng, slot))
            if prev is not None:
                dep(prev, "raw")
            self.dma_last[(eng, slot)] = me
            self.pending_dmas.append(me)
            if is_output:
                self.out_dmas.append(me)
        final = {}
        for p, kind in deps.items():
            pe, pi = p
            prod = self.ops[pe][pi]
            if pe == eng and not prod["dma"] and not dma:
                if kind == "order" or eng in ("pe", "sp"):
                    continue
            final[p] = kind
        rec["deps"] = final
        ops.append(rec)
        for r in reads:
            lst = self.readers.setdefault(r, [])
            if not dma:
                lst[:] = [q for q in lst if not (q[0] == eng and not self.ops[q[0]][q[1]]["dma"])]
            lst.append(me)
        for r in writes:
            self.lastw[r] = me
            self.readers[r] = []
        return me

    def barrier(self):
        lasts = []
        for e in ENGS:
            for i in range(len(self.ops[e]) - 1, -1, -1):
                if not self.ops[e][i]["dma"]:
                    if not self.ops[e][i].get("nop"):
                        lasts.append((e, i))
                    break
        pend = list(self.pending_dmas)
        self.pending_dmas = []
        for e in ENGS:
            me = self.add(e, lambda eng: eng.nop(), extra_deps=[p for p in lasts if p[0] != e] + pend)
            self.ops[me[0]][me[1]]["nop"] = True

    def emit(self, stack):
        nc = self.nc
        for e in ENGS:
            for rec in self.ops[e]:
                for (pe, pi) in rec["deps"]:
                    self.ops[pe][pi]["signal"] = True
        esem = {e: stack.enter_context(nc.semaphore("s_" + e)) for e in ENGS}
        dsem = {e: [None] * N_DMA_SEMS for e in ENGS}
        for e in ENGS:
            for s in range(min(N_DMA_SEMS, self.dma_rr[e])):
                dsem[e][s] = stack.enter_context(nc.semaphore("d_%s_%d" % (e, s)))
        cnt = {e: 0 for e in ENGS}
        dcnt = {}
        for e in ENGS:
            for rec in self.ops[e]:
                if rec["dma"]:
                    k = (e, rec["slot"])
                    dcnt[k] = dcnt.get(k, 0) + 16
                    rec["ev"] = (k, dcnt[k])
                elif rec["signal"]:
                    cnt[e] += 1
                    rec["ev"] = (e, cnt[e])
                else:
                    rec["ev"] = None
        self.stats = {e: [len(self.ops[e]), 0, cnt[e]] for e in ENGS}
        block = stack.enter_context(nc.Block())
        sched = self

        def semof(key):
            if isinstance(key, tuple):
                return dsem[key[0]][key[1]]
            return esem[key]

        def run(e, eng):
            known = {}
            for rec in sched.ops[e]:
                need = {}
                for (pe, pi) in rec["deps"]:
                    key, val = sched.ops[pe][pi]["ev"]
                    if known.get(key, 0) >= val:
                        continue
                    if need.get(key, 0) < val:
                        need[key] = val
                for key, val in need.items():
                    eng.wait_ge(semof(key), val)
                    known[key] = val
                    sched.stats[e][1] += 1
                nm, a_, k_ = rec["call"]
                ins = getattr(eng, nm)(*a_, **k_)
                if rec["dma"]:
                    ins.then_inc(semof(rec["ev"][0]), 16)
                elif rec["signal"]:
                    ins.then_inc(esem[e], 1)
            return known

        @block.tensor
        def _(eng):
            run("pe", eng)

        @block.scalar
        def _(eng):
            run("act", eng)

        @block.vector
        def _(eng):
            run("dve", eng)

        @block.gpsimd
        def _(eng):
            run("pool", eng)

        @block.sync
        def _(eng):
            known = run("sp", eng)
            for (pe, pi) in sched.out_dmas:
                key, val = sched.ops[pe][pi]["ev"]
                if known.get(key, 0) < val:
                    eng.wait_ge(semof(key), val)
                    known[key] = val


DT_SIZE = {F32: 4, BF16: 2, U8: 1}


class Arena:
    def __init__(self, tens, size):
        self.t = tens
        self.size = size
        self.off = 0

    def reset(self):
        self.off = 0

    def alloc(self, shape, dt):
        n = 1
        for s in shape[1:]:
            n *= s
        nbytes = (n * DT_SIZE[dt] + 63) // 64 * 64
        assert self.off + nbytes <= self.size, ("arena overflow", self.off, nbytes, self.size)
        ap = self.t[0:shape[0], self.off:self.off + n * DT_SIZE[dt]].bitcast(dt)
        self.off += nbytes
        if len(shape) == 3:
            ap = ap.rearrange("p (a b) -> p a b", b=shape[2])
        elif len(shape) == 4:
            ap = ap.rearrange("p (a b c) -> p a b c", b=shape[2], c=shape[3])
        return ap


def tiles_of(r0, nr):
    return list(range(r0 // 128, (r0 + nr - 1) // 128 + 1))


class _Stop(Exception):
    pass


def build_program(dbg=None, lim=None):
    dbg = dbg or set()
    nc = bass.Bass("TRN2", target_bir_lowering=False)

    def din(name, shape, dt=F32):
        return nc.dram_tensor(name, list(shape), dt, kind="ExternalInput").ap()

    def dscr(name, shape, dt=F32):
        kind = "ExternalOutput" if name in dbg else "Internal"
        return nc.dram_tensor(name, list(shape), dt, kind=kind).ap()

    x_in = din("x", [NB, T, D])
    ctx_in = din("ctx", [NB, CT, D])
    cvec = din("cvec", [3, D])
    mod_w = din("mod_w", [2, D, 6 * D])
    mod_b = din("mod_b", [2, 6 * D])
    norm_g = din("norm_g", [2, 4, D])
    w_in0 = din("w_in0", [D, 21 * 128])
    attn_sink = din("attn_sink", [8])
    pool_w = din("pool_w", [4, 128, 128])
    pool_scale = din("pool_scale", [512])
    attn_out_w = din("attn_out_w", [D, D])
    rec_in_w = din("rec_in_w", [D, 3088])
    rec_gate_b = din("rec_gate_b", [16])
    rec_conv_w = din("rec_conv_w", [3, D])
    rec_conv_b = din("rec_conv_b", [D])
    rec_q_w = din("rec_q_w", [4, 256, 256])
    rec_k_w = din("rec_k_w", [4, 256, 256])
    rec_norm_g = din("rec_norm_g", [D])
    rec_skip = din("rec_skip", [D])
    rec_out_w = din("rec_out_w", [D, D])
    ffn_up_w = din("ffn_up_w", [2, D, 2 * DFF])
    ffn_conv_w = din("ffn_conv_w", [2, 3, 2 * DFF])
    ffn_conv_b = din("ffn_conv_b", [2, 2 * DFF])
    ffn_down_w = din("ffn_down_w", [2, DFF, D])
    ident_in = din("ident", [128, 128])
    rope_c = din("rope_c", [128, T])
    rope_s = din("rope_s", [128, T])
    amask_in = din("amask", [2, 128, 512])
    pool_edge = din("pool_edge", [128, 64])
    lmask_in = din("lmask", [2, 128, 128])

    out = nc.dram_tensor("out", [NB, T, D], F32, kind="ExternalOutput").ap()

    GV = dscr("GV", [2, 2, 3, D])
    QT0 = dscr("QT0", [NB, 8 * 128, TS], BF16)
    V0 = dscr("V0", [NB, TS, 130], BF16)
    U0 = dscr("U0", [NB, 512, TS])
    CATP = dscr("CATP", [NB, 512, TS], BF16)
    XA = dscr("XA", [NB, T, D])
    CA = dscr("CA", [NB, CT, D])
    XB = dscr("XB", [NB, T, D])
    CB = dscr("CB", [NB, CT, D])
    GT = dscr("GT", [NB, DFF, TS], BF16)

    st = ExitStack()
    with st:
        st.enter_context(nc.allow_non_contiguous_dma(reason="small strided parameter loads"))
        S = Sched(nc)
        parts = {}

        def wpart(base):
            lst = parts.setdefault(base, [])
            name = (base, len(lst))
            lst.append(name)
            return [name]

        def rparts(base):
            return list(parts.get(base, []))

        def sb(name, shape, dt):
            return st.enter_context(nc.sbuf_tensor("sb_" + name, list(shape), dt))

        ARENA_BYTES = 192 * 1024
        arena = Arena(sb("arena", [128, ARENA_BYTES], U8), ARENA_BYTES)
        PS2 = [st.enter_context(nc.psum_tensor("ps%d" % i, [128, 1024], F32)) for i in range(4)]

        def bank(i):
            return PS2[i // 2][:, (i % 2) * 512:(i % 2) * 512 + 512]

        def bank_bf(i):
            return PS2[i // 2][:, (i % 2) * 512:(i % 2) * 512 + 512].bitcast(BF16)

        ident = sb("ident", [128, 128], F32)
        identb = sb("identb", [128, 128], BF16)
        modT = [sb("modT%d" % l, [128, 48, 3], F32) for l in range(2)]
        A1 = [sb("A1_%d" % l, [128, 3, 8], F32) for l in range(2)]
        A2 = [sb("A2_%d" % l, [128, 3, 8], F32) for l in range(2)]
        SH1 = [sb("SH1_%d" % l, [128, 3, 8], F32) for l in range(2)]
        SH2 = [sb("SH2_%d" % l, [128, 3, 8], F32) for l in range(2)]
        epsb = sb("epsb", [128, 1], F32)

        def V(fn, reads, writes):
            return S.add("dve", fn, reads, writes)

        def A(fn, reads, writes):
            return S.add("act", fn, reads, writes)

        def G(fn, reads, writes):
            return S.add("pool", fn, reads, writes)

        def PE(fn, reads, writes):
            return S.add("pe", fn, reads, writes)

        def DMA(q, out_ap, in_ap, reads, writes, is_output=False):
            return S.add(q, lambda e: e.dma_start(out=out_ap, in_=in_ap), reads, writes, dma=True,
                         is_output=is_output)

        def dma3(q, dst3, src3, nmid, reads, writes):
            for j in range(nmid):
                DMA(q, dst3(j), src3(j), reads, writes)

        def load_wb(dst3, src2, K, blocks, res):
            for (c0, cw, key) in blocks:
                for k in range(K):
                    DMA("pool", dst3[:, k, c0:c0 + cw], src2[k * 128:(k + 1) * 128, c0:c0 + cw], [], [(res, key)])

        def load_w(dst3, src2, K, N, res):
            for k in range(K):
                c0 = 0
                while c0 < N:
                    cw = min(2048, N - c0)
                    DMA("pool", dst3[:, k, c0:c0 + cw], src2[k * 128:(k + 1) * 128, c0:c0 + cw], [], [res])
                    c0 += cw

        def chk(k):
            if lim is not None and k > lim:
                raise _Stop()

        try:
            DMA("sp", ident[:], ident_in, [], ["ident"])
            V(lambda e: e.tensor_copy(out=identb[:], in_=ident[:]), ["ident"], ["identb"])
            V(lambda e: e.memset(epsb[:], EPS), [], ["epsb"])

            arena.reset()
            cT = arena.alloc([128, 8, 3], F32)
            sT = arena.alloc([128, 8, 3], BF16)
            mb = arena.alloc([128, 48], F32)
            ng = arena.alloc([128, 4, 8], F32)
            g1t = arena.alloc([128, 8, 3], F32)
            g2t = arena.alloc([128, 8, 3], F32)
            mwt = [arena.alloc([128, 8, 512], BF16) for _ in range(3)]
            for r in range(3):
                DMA("sp", cT[:, :, r], cvec[r].rearrange("(k p) -> p k", p=128), [], ["cT"])
            A(lambda e: e.activation(out=sT, in_=cT, func=AF.Silu), ["cT"], ["sT"])
            for l in range(2):
                DMA("sp", mb, mod_b[l].rearrange("(c p) -> p c", p=128), [], ["mb"])
                for j in range(4):
                    DMA("sp", ng[:, j, :], norm_g[l, j].rearrange("(k p) -> p k", p=128), [], ["ng"])
                psm = bank(0)
                for nch in range(12):
                    wt = mwt[nch % 3]
                    wres = "mwt%d" % (nch % 3)
                    load_w(wt, mod_w[l][:, nch * 512:(nch + 1) * 512], 8, 512, wres)
                    for fc in range(4):
                        col = (nch * 4 + fc) * 3
                        for k in range(8):
                            PE(lambda e, wt=wt, k=k, fc=fc, col=col: e.matmul(
                                psm[:, col:col + 3], lhsT=wt[:, k, fc * 128:(fc + 1) * 128], rhs=sT[:, k, :],
                                start=(k == 0), stop=(k == 7)), [wres, "sT"], ["ps0"])
                mT = modT[l]
                V(lambda e, mT=mT: e.tensor_tensor(
                    out=mT[:], in0=psm[:, 0:144].rearrange("p (c r) -> p c r", r=3),
                    in1=mb.unsqueeze(2).to_broadcast([128, 48, 3]), op=ALU.add), ["ps0", "mb"], ["modT%d" % l])

                def ngb(j):
                    return ng[:, j, :].unsqueeze(2).to_broadcast([128, 8, 3])

                mres = ["modT%d" % l, "ng"]
                V(lambda e, mT=mT, l=l: e.scalar_tensor_tensor(
                    out=A1[l][:].rearrange("p r k -> p k r"), in0=mT[:, 8:16, :], scalar=1.0, in1=ngb(0),
                    op0=ALU.add, op1=ALU.mult), mres, ["A1_%d" % l])
                V(lambda e, mT=mT, l=l: e.scalar_tensor_tensor(
                    out=A2[l][:].rearrange("p r k -> p k r"), in0=mT[:, 32:40, :], scalar=1.0, in1=ngb(2),
                    op0=ALU.add, op1=ALU.mult), mres, ["A2_%d" % l])
                V(lambda e, mT=mT, l=l: e.tensor_copy(
                    out=SH1[l][:].rearrange("p r k -> p k r"), in_=mT[:, 0:8, :]), mres, ["SH1_%d" % l])
                V(lambda e, mT=mT, l=l: e.tensor_copy(
                    out=SH2[l][:].rearrange("p r k -> p k r"), in_=mT[:, 24:32, :]), mres, ["SH2_%d" % l])
                V(lambda e, mT=mT: e.tensor_tensor(out=g1t, in0=mT[:, 16:24, :], in1=ngb(1), op=ALU.mult),
                  mres, ["g1t"])
                V(lambda e, mT=mT: e.tensor_tensor(out=g2t, in0=mT[:, 40:48, :], in1=ngb(3), op=ALU.mult),
                  mres, ["g2t"])
                for r in range(3):
                    DMA("sp", GV[l, 0, r].rearrange("(k p) -> p k", p=128), g1t[:, :, r], ["g1t"], wpart(("GV", l, 0)))
                    DMA("sp", GV[l, 1, r].rearrange("(k p) -> p k", p=128), g2t[:, :, r], ["g2t"], wpart(("GV", l, 1)))

            ctxb = {}
            rr = {"n": 0, "r": 0}

            def alloc_norm_bufs():
                ctxb["xt"] = [arena.alloc([128, D], F32) for _ in range(3)]
                ctxb["junk"] = arena.alloc([128, D], BF16)
                ctxb["xn"] = [arena.alloc([128, D], BF16) for _ in range(3)]
                ctxb["st"] = [arena.alloc([128, 4], F32) for _ in range(3)]
                ctxb["tmpT"] = [arena.alloc([128, 8, 128], F32) for _ in range(2)]

            def rstd_chain(src_ap, srcres, stt, stres, nr):
                junk = ctxb["junk"]
                A(lambda e: e.activation(out=junk[0:nr, :], in_=src_ap, func=AF.Square, accum_out=stt[0:nr, 0:1]),
                  srcres, ["junk", stres])
                A(lambda e: e.activation(out=stt[0:nr, 1:2], in_=stt[0:nr, 0:1], func=AF.Sqrt, scale=1.0 / D,
                                         bias=epsb[0:nr, :]), [stres, "epsb"], [stres])
                V(lambda e: e.reciprocal(out=stt[0:nr, 1:2], in_=stt[0:nr, 1:2]), [stres], [stres])

            def make_hT(src, srcres_fn, r0, nr, Aap, SHap, ares, hT, hres, c0):
                i = rr["n"]
                rr["n"] += 1
                s3, s2 = i % 3, i % 2
                xt, xn, stt, tmpT = ctxb["xt"][s3], ctxb["xn"][s3], ctxb["st"][s3], ctxb["tmpT"][s2]
                xres, nres, sres, tres = "xt%d" % s3, "xn%d" % s3, "st%d" % s3, "tmpT%d" % s2
                pb = 6 + s2
                pres = "ps%d" % pb
                DMA("sp", xt[0:nr, :], src[r0:r0 + nr, :], srcres_fn(r0, nr), [xres])
                rstd_chain(xt[0:nr, :], [xres], stt, sres, nr)
                V(lambda e: e.tensor_scalar(out=xn[0:nr, :], in0=xt[0:nr, :], scalar1=stt[0:nr, 1:2], scalar2=None,
                                            op0=ALU.mult), [xres, sres], [nres])
                pt = bank_bf(pb).rearrange("p (k t) -> p k t", t=128)
                for k in range(8):
                    PE(lambda e, k=k: e.transpose(out=pt[:, k, 0:nr], in_=xn[0:nr, k * 128:(k + 1) * 128],
                                                  identity=identb[0:nr, 0:nr]), [nres, "identb"], [pres])
                V(lambda e: e.tensor_tensor(out=tmpT[:, :, 0:nr], in0=pt[:, :, 0:nr],
                                            in1=Aap.unsqueeze(2).to_broadcast([128, 8, nr]), op=ALU.mult),
                  [pres, ares], [tres])
                V(lambda e: e.tensor_tensor(out=hT[:, :, c0:c0 + nr], in0=tmpT[:, :, 0:nr],
                                            in1=SHap.unsqueeze(2).to_broadcast([128, 8, nr]), op=ALU.add),
                  [tres, ares], [hres])

            def residual_update(mix_ap, mixres, src, srcres, dst, dstres, r0, nr, gb, gbres, is_output=False):
                i = rr["r"]
                rr["r"] += 1
                s3, s2 = i % 3, i % 2
                xt, stt = ctxb["xt"][s3], ctxb["st"][s3]
                tmp = ctxb["mtmp"][s2]
                xres, sres, tres = "xt%d" % s3, "st%d" % s3, "mtmp%d" % s2
                DMA("sp", xt[0:nr, :], src[r0:r0 + nr, :], srcres, [xres])
                rstd_chain(mix_ap, mixres, stt, sres, nr)
                V(lambda e: e.scalar_tensor_tensor(out=tmp[0:nr, :], in0=mix_ap, scalar=stt[0:nr, 1:2],
                                                   in1=gb[0:nr, :], op0=ALU.mult, op1=ALU.mult),
                  mixres + [sres, gbres], [tres])
                V(lambda e: e.tensor_tensor(out=xt[0:nr, :], in0=xt[0:nr, :], in1=tmp[0:nr, :], op=ALU.add),
                  [xres, tres], [xres])
                DMA("sp", dst[r0:r0 + nr, :], xt[0:nr, :], [xres], dstres, is_output=is_output)

            def load_gb(l, j, r, tile, res):
                DMA("sp", tile, GV[l, j, r].partition_broadcast(128), rparts(("GV", l, j)), [res])

            def xres_fn(name, b):
                return lambda r0, nr: [(name, b, t) for t in tiles_of(r0, nr)]

            chk(1)
            S.barrier()
            arena.reset()
            alloc_norm_bufs()
            win = arena.alloc([128, 8, 21 * 128], BF16)
            rc = arena.alloc([128, T], F32)
            rs = arena.alloc([128, T], F32)
            hTw = [arena.alloc([128, 8, 512], BF16) for _ in range(2)]
            rt1 = [arena.alloc([128, 512], F32) for _ in range(2)]
            rt2 = [arena.alloc([128, 512], F32) for _ in range(2)]
            qst = [arena.alloc([128, 512], BF16) for _ in range(4)]
            ust = [arena.alloc([128, 512], F32) for _ in range(2)]
            vst = [arena.alloc([128, 130], BF16) for _ in range(2)]
            load_w(win, w_in0, 8, 21 * 128, "win")
            DMA("sp", rc, rope_c, [], ["rc"])
            DMA("sp", rs, rope_s, [], ["rs"])
            for s in range(2):
                V(lambda e, s=s: e.memset(vst[s][:, 64:65], 1.0), [], ["vst%d" % s])
                V(lambda e, s=s: e.memset(vst[s][:, 129:130], 1.0), [], ["vst%d" % s])
            cnt = {"w": 0, "q": 0, "u": 0, "v": 0, "pp": 0}
            for b in range(NB):
                for w in range(5):
                    isx = w < 4
                    nt = 512 if isx else 256
                    tok0 = w * 512
                    row = b if isx else 2
                    src = x_in[b] if isx else ctx_in[b]
                    hs = cnt["w"] % 2
                    cnt["w"] += 1
                    hT, hres = hTw[hs], "hTw%d" % hs
                    for i in range(nt // 128):
                        make_hT(src, lambda r0, nr: [], (tok0 if isx else 0) + i * 128 if isx else i * 128, 128,
                                A1[0][:, row, :], SH1[0][:, row, :], "A1_0", hT, hres, i * 128)

                    def proj(j, pb):
                        for k in range(8):
                            PE(lambda e, k=k: e.matmul(bank(pb)[:, 0:nt], lhsT=win[:, k, j * 128:(j + 1) * 128],
                                                       rhs=hT[:, k, 0:nt], start=(k == 0), stop=(k == 7)),
                               ["win", hres], ["ps%d" % pb])

                    for j in range(8):
                        pa = (cnt["pp"] % 2) * 2
                        cnt["pp"] += 1
                        qs = cnt["q"] % 4
                        cnt["q"] += 1
                        qt_, qres = qst[qs], "qst%d" % qs
                        proj(j, pa)
                        if isx:
                            proj(j + 8, pa + 1)
                            ts_ = cnt["q"] % 2
                            t1, t2 = rt1[ts_], rt2[ts_]
                            V(lambda e, t1=t1, pa=pa: e.tensor_tensor(out=t1[:, 0:nt], in0=bank(pa)[:, 0:nt],
                                                                      in1=rc[:, tok0:tok0 + nt], op=ALU.mult),
                              ["ps%d" % pa, "rc"], ["rt1_%d" % ts_])
                            V(lambda e, t2=t2, pa=pa: e.tensor_tensor(out=t2[:, 0:nt], in0=bank(pa + 1)[:, 0:nt],
                                                                      in1=rs[:, tok0:tok0 + nt], op=ALU.mult),
                              ["ps%d" % (pa + 1), "rs"], ["rt2_%d" % ts_])
                            G(lambda e, t1=t1, t2=t2, qt_=qt_: e.tensor_tensor(out=qt_[:, 0:nt], in0=t1[:, 0:nt],
                                                                               in1=t2[:, 0:nt], op=ALU.add),
                              ["rt1_%d" % ts_, "rt2_%d" % ts_], [qres])
                        else:
                            A(lambda e, qt_=qt_, pa=pa: e.activation(out=qt_[:, 0:nt], in_=bank(pa)[:, 0:nt],
                                                                     func=AF.Copy), ["ps%d" % pa], [qres])
                        DMA("sp", QT0[b, j * 128:(j + 1) * 128, tok0:tok0 + nt], qt_[:, 0:nt], [qres],
                            wpart(("QT0", b)))
                    for j in range(17, 21):
                        pa = (cnt["pp"] % 2) * 2
                        cnt["pp"] += 1
                        us = cnt["u"] % 2
                        cnt["u"] += 1
                        proj(j, pa)
                        A(lambda e, us=us, pa=pa: e.activation(out=ust[us][:, 0:nt], in_=bank(pa)[:, 0:nt],
                                                               func=AF.Copy), ["ps%d" % pa], ["ust%d" % us])
                        DMA("sp", U0[b, (j - 17) * 128:(j - 16) * 128, tok0:tok0 + nt], ust[us][:, 0:nt],
                            ["ust%d" % us], wpart(("U0", b, j - 17)))
                    for i in range(nt // 128):
                        vs = cnt["v"] % 2
                        cnt["v"] += 1
                        for k in range(8):
                            PE(lambda e, k=k, i=i: e.matmul(bank(4)[:, 0:128], lhsT=hT[:, k, i * 128:(i + 1) * 128],
                                                           rhs=win[:, k, 16 * 128:17 * 128], start=(k == 0),
                                                           stop=(k == 7)), ["win", hres], ["ps4"])
                        A(lambda e, vs=vs: e.activation(
                            out=vst[vs][:].rearrange("p (h d) -> p h d", d=65)[:, :, 0:64],
                            in_=bank(4)[:, 0:128].rearrange("p (h d) -> p h d", d=64), func=AF.Copy),
                          ["ps4"], ["vst%d" % vs])
                        DMA("sp", V0[b, tok0 + i * 128:tok0 + (i + 1) * 128, :], vst[vs][:], ["vst%d" % vs],
                            wpart(("V0", b)))

            chk(2)
            S.barrier()
            arena.reset()
            PADL = 16
            ub = [arena.alloc([128, TS + 48], F32) for _ in range(2)]
            pa_ = [arena.alloc([128, TS + 48], F32) for _ in range(2)]
            pb_ = [arena.alloc([128, TS + 48], F32) for _ in range(2)]
            dTt = [arena.alloc([128, TS], BF16) for _ in range(2)]
            cst = [arena.alloc([128, 512], BF16) for _ in range(2)]
            pwt = arena.alloc([128, 4, 128], BF16)
            psc = arena.alloc([128, 4], F32)
            pedge = arena.alloc([128, 64], F32)
            etmp = arena.alloc([128, 8], F32)
            for g_ in range(4):
                DMA("pool", pwt[:, g_, :], pool_w[g_], [], ["pwt"])
            DMA("sp", psc, pool_scale.rearrange("(g p) -> p g", p=128), [], ["psc"])
            DMA("sp", pedge, pool_edge, [], ["pedge"])
            segs = [(PADL, T, 0), (PADL + T + 16, CT, T)]
            pc = 0
            for b in range(NB):
                for g in range(4):
                    sl = pc % 2
                    pc += 1
                    u, p1, p2, dT = ub[sl], pa_[sl], pb_[sl], dTt[sl]
                    ur, p1r, p2r, dr = "ub%d" % sl, "pa%d" % sl, "pb%d" % sl, "dT%d" % sl
                    V(lambda e, u=u: e.memset(u[:, 0:PADL], 0.0), [], [ur])
                    V(lambda e, u=u: e.memset(u[:, PADL + T:PADL + T + 16], 0.0), [], [ur])
                    V(lambda e, u=u: e.memset(u[:, PADL + T + 16 + CT:TS + 48], 0.0), [], [ur])
                    DMA("sp", u[:, PADL:PADL + T], U0[b, g * 128:(g + 1) * 128, 0:T], rparts(("U0", b, g)), [ur])
                    DMA("sp", u[:, PADL + T + 16:PADL + T + 16 + CT], U0[b, g * 128:(g + 1) * 128, T:TS],
                        rparts(("U0", b, g)), [ur])
                    wlen = 2 ** (g + 1)
                    half = wlen // 2
                    NW = TS + 48
                    cur, curres, ln = u, ur, 1
                    bufs = [(p1, p1r), (p2, p2r)]
                    bi = 0
                    while ln < wlen:
                        nxt, nres = bufs[bi % 2]
                        bi += 1
                        n_el = NW - 2 * ln
                        V(lambda e, cur=cur, nxt=nxt, ln=ln, n_el=n_el: e.tensor_tensor(
                            out=nxt[:, 0:n_el], in0=cur[:, 0:n_el], in1=cur[:, ln:ln + n_el], op=ALU.add),
                          [curres], [nres])
                        cur, curres = nxt, nres
                        ln *= 2
                    for (off, L, toff) in segs:
                        V(lambda e, cur=cur, off=off, L=L, toff=toff: e.scalar_tensor_tensor(
                            out=dT[:, toff:toff + L], in0=cur[:, off - half:off - half + L], scalar=1.0 / wlen,
                            in1=u[:, off:off + L], op0=ALU.mult, op1=ALU.subtract), [curres, ur], [dr])
                        for side in range(2):
                            ne = half if side == 0 else half - 1
                            if ne == 0:
                                continue
                            t0 = 0 if side == 0 else L - half + 1
                            ec = (g * 2 + side) * 8
                            V(lambda e, cur=cur, off=off, t0=t0, ne=ne, ec=ec: e.tensor_tensor(
                                out=etmp[:, 0:ne], in0=cur[:, off - half + t0:off - half + t0 + ne],
                                in1=pedge[:, ec:ec + ne], op=ALU.mult), [curres, "pedge"], ["etmp"])
                            V(lambda e, off=off, t0=t0, ne=ne, toff=toff: e.tensor_tensor(
                                out=dT[:, toff + t0:toff + t0 + ne], in0=etmp[:, 0:ne],
                                in1=u[:, off + t0:off + t0 + ne], op=ALU.subtract), ["etmp", ur], [dr])
                    for w in range(5):
                        nt = 512 if w < 4 else 256
                        tok0 = w * 512
                        pbk = w % 2
                        cs = (pc + w) % 2
                        PE(lambda e, dT=dT, nt=nt, tok0=tok0, pbk=pbk, g=g: e.matmul(
                            bank(pbk)[:, 0:nt], lhsT=pwt[:, g, :], rhs=dT[:, tok0:tok0 + nt], start=True, stop=True),
                           ["pwt", dr], ["ps%d" % pbk])
                        A(lambda e, nt=nt, pbk=pbk, cs=cs, g=g: e.activation(
                            out=cst[cs][:, 0:nt], in_=bank(pbk)[:, 0:nt], func=AF.Copy, scale=psc[:, g:g + 1]),
                          ["ps%d" % pbk, "psc"], ["cst%d" % cs])
                        DMA("sp", CATP[b, g * 128:(g + 1) * 128, tok0:tok0 + nt], cst[cs][:, 0:nt], ["cst%d" % cs],
                            wpart(("CATP", b)))

            chk(2.05)
            S.barrier()
            arena.reset()
            alloc_norm_bufs()
            ctxb["mtmp"] = [arena.alloc([128, D], F32) for _ in range(2)]
            wout = arena.alloc([128, 8, D], BF16)
            qt = arena.alloc([128, 8, TS], BF16)
            vx = arena.alloc([128, 18, 130], BF16)
            am = arena.alloc([128, 2, 512], BF16)
            esb = arena.alloc([128, 8], F32)
            PT = [arena.alloc([128, 512], BF16) for _ in range(10)]
            atok = [arena.alloc([128, 512], BF16) for _ in range(2)]
            catT = [arena.alloc([128, 8, 128], BF16) for _ in range(2)]
            den = [arena.alloc([128, 8], F32) for _ in range(2)]
            gbx = arena.alloc([128, D], F32)
            gbc = arena.alloc([128, D], F32)
            load_w(wout, attn_out_w, 8, D, "wout")
            for m_ in range(2):
                DMA("pool", am[:, m_, :], amask_in[m_], [], ["am"])
            DMA("sp", esb, attn_sink.partition_broadcast(128), [], ["esb"])
            A(lambda e: e.activation(out=esb, in_=esb, func=AF.Exp), ["esb"], ["esb"])
            import os
            if os.environ.get("SWAP"):
                load_gb(0, 0, 0, gbx, "gbx")
            load_gb(0, 0, 2, gbc, "gbc")
            chk(2.1)
            ac = {"pt": 0, "blk": 0}
            for b in range(NB):
                import os
                SK = os.environ.get("SKIP", "")
                if "q" not in SK:
                    dma3("sp", lambda j: qt[:, j, :], lambda j: QT0[b, j * 128:(j + 1) * 128, :], 8, rparts(("QT0", b)), ["qt"])
                if "v" not in SK:
                    dma3("sp", lambda j: vx[:, j, :], lambda j: V0[b, j * 128:(j + 1) * 128, :], 18, rparts(("V0", b)), ["vx"])
                if "g" not in SK:
                    load_gb(0, 0, b, gbx, "gbx")
                chk(2.2)
                for n in range(18):
                    isx = n < 16
                    bs = ac["blk"] % 2
                    ac["blk"] += 1
                    at_, atres = atok[bs], "atok%d" % bs
                    cT_, cres = catT[bs], "catT%d" % bs
                    dn, dres = den[bs], "den%d" % bs
                    if isx:
                        chunks = []
                        if n > 0:
                            chunks.append((n - 1, 0))
                        chunks.append((n, None))
                        if n < 15:
                            chunks.append((n + 1, 1))
                        chunks += [(16, None), (17, None)]
                    else:
                        chunks = [(16, None), (17, None)]
                    if "c" in SK:
                        V(lambda e: e.memset(cT_[:, 4:8, :], 0.0), [], [cres])
                    else:
                        dma3("sp", lambda j: cT_[:, 4 + j, :], lambda j: CATP[b, j * 128:(j + 1) * 128, n * 128:(n + 1) * 128], 4,
                             rparts(("CATP", b)), [cres])
                    for h in range(2):
                        pts = []
                        for (kc, mk) in chunks:
                            pi = ac["pt"] % 10
                            ac["pt"] += 1
                            sbk = pi % 2
                            pts.append((pi, kc))
                            for g in range(4):
                                s_ = g % 2
                                c_ = 2 * h + g // 2
                                PE(lambda e, kc=kc, g=g, s_=s_, c_=c_, sbk=sbk: e.matmul(
                                    bank(sbk)[:, g * 128:(g + 1) * 128],
                                    lhsT=qt[:, 4 + 2 * h + s_, kc * 128:(kc + 1) * 128],
                                    rhs=qt[:, c_, n * 128:(n + 1) * 128],
                                    start=True, stop=True), ["qt"], ["ps%d" % sbk])
                            A(lambda e, pi=pi, sbk=sbk: e.activation(out=PT[pi][:], in_=bank(sbk), func=AF.Exp,
                                                                   scale=0.125), ["ps%d" % sbk], ["PT%d" % pi])
                            if mk is not None:
                                V(lambda e, pi=pi, mk=mk: e.tensor_tensor(out=PT[pi][:], in0=PT[pi][:],
                                                                           in1=am[:, mk, :], op=ALU.mult),
                                  ["PT%d" % pi, "am"], ["PT%d" % pi])
                        chk(2.3)
                        ob = 2 + h
                        ov = bank(ob)[:, 0:260].rearrange("p (g d) -> p g d", d=65)
                        for g in range(4):
                            for ci, (pi, kc) in enumerate(pts):
                                PE(lambda e, g=g, pi=pi, kc=kc, ci=ci, ob=ob: e.matmul(
                                    bank(ob)[:, g * 65:(g + 1) * 65], lhsT=PT[pi][:, g * 128:(g + 1) * 128],
                                    rhs=vx[:, kc, h * 65:(h + 1) * 65], start=(ci == 0), stop=(ci == len(pts) - 1)),
                                   ["PT%d" % pi, "vx"], ["ps%d" % ob])
                        chk(2.5)
                        V(lambda e, ov=ov, dn=dn, h=h: e.tensor_tensor(out=dn[:, h * 4:(h + 1) * 4], in0=ov[:, :, 64],
                                                                       in1=esb[:, h * 4:(h + 1) * 4], op=ALU.add),
                          ["ps%d" % ob, "esb"], [dres])
                        V(lambda e, dn=dn, h=h: e.reciprocal(out=dn[:, h * 4:(h + 1) * 4], in_=dn[:, h * 4:(h + 1) * 4]),
                          [dres], [dres])
                        V(lambda e, ov=ov, dn=dn, h=h, at_=at_: e.tensor_tensor(
                            out=at_[:, h * 256:(h + 1) * 256].rearrange("p (g d) -> p g d", d=64), in0=ov[:, :, 0:64],
                            in1=dn[:, h * 4:(h + 1) * 4].unsqueeze(2).to_broadcast([128, 4, 64]), op=ALU.mult),
                          ["ps%d" % ob, dres], [atres])
                    chk(2.6)
                    ptv = bank_bf(4).rearrange("p (k t) -> p k t", t=128)
                    for c_ in range(4):
                        PE(lambda e, c_=c_, at_=at_: e.transpose(out=ptv[:, c_, :], in_=at_[:, c_ * 128:(c_ + 1) * 128],
                                                                identity=identb[:]), [atres, "identb"], ["ps4"])
                    A(lambda e, cT_=cT_: e.activation(out=cT_[:, 0:4, :], in_=ptv[:, 0:4, :], func=AF.Copy),
                      ["ps4"], [cres])
                    chk(2.7)
                    mix = PS2[3]
                    for hf in range(2):
                        for k in range(8):
                            PE(lambda e, k=k, hf=hf, cT_=cT_: e.matmul(mix[:, hf * 512:(hf + 1) * 512], lhsT=cT_[:, k, :],
                                                                      rhs=wout[:, k, hf * 512:(hf + 1) * 512],
                                                                      start=(k == 0), stop=(k == 7)),
                               [cres, "wout"], ["ps%d" % (6 + hf)])
                    chk(2.8)
                    if isx:
                        residual_update(mix[:, :], ["ps6", "ps7"], x_in[b], [], XA[b], [("XA", b, n)], n * 128, 128,
                                        gbx, "gbx")
                    else:
                        residual_update(mix[:, :], ["ps6", "ps7"], ctx_in[b], [], CA[b], [("CA", b, n - 16)],
                                        (n - 16) * 128, 128, gbc, "gbc")

            def ffn_layer(l, segs_fn, final):
                chk(4 + 5 * l)
                S.barrier()
                arena.reset()
                alloc_norm_bufs()
                wup = arena.alloc([128, 8, 2 * DFF], BF16)
                cw = arena.alloc([128, 3, 44], F32)
                cb = arena.alloc([128, 44], F32)
                hTf = [arena.alloc([128, 8, 512], BF16) for _ in range(2)]
                ta = [arena.alloc([128, 512], F32) for _ in range(3)]
                tb = [arena.alloc([128, 512], F32) for _ in range(3)]
                sg = [arena.alloc([128, 512], F32) for _ in range(3)]
                tv = [arena.alloc([128, 512], F32) for _ in range(3)]
                tw = [arena.alloc([128, 512], F32) for _ in range(3)]
                gst = [arena.alloc([128, 512], BF16) for _ in range(3)]
                load_wb(wup, ffn_up_w[l], 8, [(0, 1408, 0), (2816, 1408, 2), (1408, 1408, 1), (4224, 1408, 3)], "wup")
                for j in range(3):
                    DMA("sp", cw[:, j, :], ffn_conv_w[l, j].rearrange("(c p) -> p c", p=128), [], ["cw"])
                DMA("sp", cb, ffn_conv_b[l].rearrange("(c p) -> p c", p=128), [], ["cb"])
                fc = {"w": 0, "c": 0, "g": 0}
                for b in range(NB):
                    for (src, sname, L, toff, row) in segs_fn(b):
                        wins = []
                        o = 0
                        while o < L:
                            r0 = max(o - 1, 0)
                            c0 = 1 if o == 0 else 0
                            nr = min(512 - c0, L - r0)
                            rz = (r0 + nr == L)
                            ncols = c0 + nr + (1 if rz else 0)
                            if ncols > 512:
                                nr -= 1
                                rz = False
                                ncols = 512
                            nout = ncols - 2
                            wins.append((r0, nr, c0, rz, ncols, o, nout))
                            o += nout
                        for (r0, nr, c0, rz, ncols, o, nout) in wins:
                            hs = fc["w"] % 2
                            fc["w"] += 1
                            hT, hres = hTf[hs], "hTf%d" % hs
                            if c0 == 1:
                                V(lambda e, hT=hT: e.memset(hT[:, :, 0:1], 0.0), [], [hres])
                            if rz:
                                V(lambda e, hT=hT, cc=c0 + nr: e.memset(hT[:, :, cc:cc + 1], 0.0), [], [hres])
                            rr_ = r0
                            while rr_ < r0 + nr:
                                n_ = min(128, r0 + nr - rr_)
                                make_hT(src, xres_fn(sname, b), rr_, n_, A2[l][:, row, :], SH2[l][:, row, :],
                                        "A2_%d" % l, hT, hres, c0 + rr_ - r0)
                                rr_ += n_
                            for c in range(22):
                                s2 = fc["c"] % 3
                                fc["c"] += 1
                                pg, pv = (s2 * 2), (s2 * 2 + 1)
                                for (jc, pbk) in ((c, pg), (22 + c, pv)):
                                    for k in range(8):
                                        PE(lambda e, k=k, jc=jc, pbk=pbk, hT=hT, ncols=ncols: e.matmul(
                                            bank(pbk)[:, 0:ncols], lhsT=wup[:, k, jc * 128:(jc + 1) * 128],
                                            rhs=hT[:, k, 0:ncols], start=(k == 0), stop=(k == 7)),
                                           [("wup", jc // 11), hres], ["ps%d" % pbk])
                                n = nout
                                A(lambda e, s2=s2, pg=pg, c=c, n=n: e.activation(
                                    out=ta[s2][:, 0:n], in_=bank(pg)[:, 1:1 + n], func=AF.Identity,
                                    scale=cw[:, 1, c:c + 1], bias=cb[:, c:c + 1]), ["ps%d" % pg, "cw", "cb"], ["ta%d" % s2])
                                V(lambda e, s2=s2, pg=pg, c=c, n=n: e.scalar_tensor_tensor(
                                    out=tb[s2][:, 0:n], in0=bank(pg)[:, 0:n], scalar=cw[:, 0, c:c + 1],
                                    in1=ta[s2][:, 0:n], op0=ALU.mult, op1=ALU.add), ["ps%d" % pg, "cw", "ta%d" % s2],
                                  ["tb%d" % s2])
                                V(lambda e, s2=s2, pg=pg, c=c, n=n: e.scalar_tensor_tensor(
                                    out=ta[s2][:, 0:n], in0=bank(pg)[:, 2:2 + n], scalar=cw[:, 2, c:c + 1],
                                    in1=tb[s2][:, 0:n], op0=ALU.mult, op1=ALU.add), ["ps%d" % pg, "cw", "tb%d" % s2],
                                  ["ta%d" % s2])
                                A(lambda e, s2=s2, n=n: e.activation(out=sg[s2][:, 0:n], in_=ta[s2][:, 0:n], func=AF.Silu),
                                  ["ta%d" % s2], ["sg%d" % s2])
                                cv = 22 + c
                                A(lambda e, s2=s2, pv=pv, cv=cv, n=n: e.activation(
                                    out=tv[s2][:, 0:n], in_=bank(pv)[:, 1:1 + n], func=AF.Identity,
                                    scale=cw[:, 1, cv:cv + 1], bias=cb[:, cv:cv + 1]), ["ps%d" % pv, "cw", "cb"],
                                  ["tv%d" % s2])
                                V(lambda e, s2=s2, pv=pv, cv=cv, n=n: e.scalar_tensor_tensor(
                                    out=tw[s2][:, 0:n], in0=bank(pv)[:, 0:n], scalar=cw[:, 0, cv:cv + 1],
                                    in1=tv[s2][:, 0:n], op0=ALU.mult, op1=ALU.add), ["ps%d" % pv, "cw", "tv%d" % s2],
                                  ["tw%d" % s2])
                                V(lambda e, s2=s2, pv=pv, cv=cv, n=n: e.scalar_tensor_tensor(
                                    out=tv[s2][:, 0:n], in0=bank(pv)[:, 2:2 + n], scalar=cw[:, 2, cv:cv + 1],
                                    in1=tw[s2][:, 0:n], op0=ALU.mult, op1=ALU.add), ["ps%d" % pv, "cw", "tw%d" % s2],
                                  ["tv%d" % s2])
                                gs = fc["g"] % 3
                                fc["g"] += 1
                                G(lambda e, s2=s2, gs=gs, n=n: e.tensor_tensor(out=gst[gs][:, 0:n], in0=sg[s2][:, 0:n],
                                                                               in1=tv[s2][:, 0:n], op=ALU.mult),
                                  ["sg%d" % s2, "tv%d" % s2], ["gst%d" % gs])
                                DMA("sp", GT[b, c * 128:(c + 1) * 128, toff + o:toff + o + n], gst[gs][:, 0:n],
                                    ["gst%d" % gs], wpart(("GT", b, toff)))
                chk(5 + 5 * l)
                S.barrier()
                arena.reset()
                alloc_norm_bufs()
                ctxb["mtmp"] = [arena.alloc([128, D], F32) for _ in range(2)]
                wdn = arena.alloc([128, 22, D], BF16)
                gTw = [arena.alloc([128, 22, 512], BF16) for _ in range(3)]
                gb = [arena.alloc([128, D], F32) for _ in range(2)]
                for c_ in range(22):
                    DMA("pool", wdn[:, c_, :], ffn_down_w[l][c_ * 128:(c_ + 1) * 128, :], [], [("wdn", c_)])
                dc = {"w": 0, "m": 0}
                load_gb(l, 1, 2, gb[1], "gbf1")
                for b in range(NB):
                    load_gb(l, 1, b, gb[0], "gbf0")
                    for (src, sname, L, toff, row, dst, dname, is_out) in final(b):
                        o = 0
                        while o < L:
                            nt = min(512, L - o)
                            ws = dc["w"] % 3
                            dc["w"] += 1
                            dma3("sp", lambda j: gTw[ws][:, j, 0:nt],
                                 lambda j: GT[b, j * 128:(j + 1) * 128, toff + o:toff + o + nt], 22,
                                 rparts(("GT", b, toff)), ["gTw%d" % ws])
                            for i in range(nt // 128):
                                ms = dc["m"] % 3
                                dc["m"] += 1
                                mix = PS2[1 + ms]
                                for hf in range(2):
                                    for c in range(22):
                                        PE(lambda e, c=c, hf=hf, ws=ws, i=i, mix=mix: e.matmul(
                                            mix[:, hf * 512:(hf + 1) * 512], lhsT=gTw[ws][:, c, i * 128:(i + 1) * 128],
                                            rhs=wdn[:, c, hf * 512:(hf + 1) * 512], start=(c == 0), stop=(c == 21)),
                                           ["gTw%d" % ws, ("wdn", c)], ["ps%d" % (2 + 2 * ms + hf)])
                                r0 = o + i * 128
                                g_ = gb[0] if row < 2 else gb[1]
                                residual_update(mix[:, :], ["ps%d" % (2 + 2 * ms), "ps%d" % (3 + 2 * ms)], src,
                                                [(sname, b, r0 // 128)], dst, [(dname, b, r0 // 128)], r0, 128, g_,
                                                "gbf0" if row < 2 else "gbf1", is_output=is_out)
                            o += nt

            ffn_layer(0,
                      lambda b: [(XA[b], "XA", T, 0, b), (CA[b], "CA", CT, T, 2)],
                      lambda b: [(XA[b], "XA", T, 0, b, XB[b], "XB", False), (CA[b], "CA", CT, T, 2, CB[b], "CB", False)])

            UC1 = dscr("UC1", [NB, D, T], BF16)
            OG1 = dscr("OG1", [NB, D, T], BF16)
            QT1 = dscr("QT1", [NB, D, T], BF16)
            KT1 = dscr("KT1", [NB, D, TS], BF16)
            KK1 = dscr("KK1", [NB, TS, D], BF16)
            VV1 = dscr("VV1", [NB, TS, D], BF16)
            GG1 = dscr("GG1", [NB, TS, 16])
            SC1 = dscr("SC1", [NB, TS, 24])
            DC1 = dscr("DC1", [NB, 2 * 4 * 18])

            def make_windows(L):
                wins = []
                o = 0
                while o < L:
                    r0 = max(o - 1, 0)
                    c0 = 1 if o == 0 else 0
                    nr = min(512 - c0, L - r0)
                    rz = (r0 + nr == L)
                    ncols = c0 + nr + (1 if rz else 0)
                    if ncols > 512:
                        nr -= 1
                        rz = False
                        ncols = 512
                    nout = ncols - 2
                    wins.append((r0, nr, c0, rz, ncols, o, nout))
                    o += nout
                return wins

            chk(6)
            S.barrier()
            arena.reset()
            alloc_norm_bufs()
            win1 = arena.alloc([128, 8, 3088], BF16)
            qw = arena.alloc([128, 4, 2, 256], BF16)
            kw = arena.alloc([128, 4, 2, 256], BF16)
            cwr = arena.alloc([128, 3, 8], F32)
            cbr = arena.alloc([128, 8], F32)
            gbias = arena.alloc([128, 16], F32)
            hT1 = [arena.alloc([128, 8, 512], BF16) for _ in range(2)]
            ucw = [arena.alloc([128, 8, 512], BF16) for _ in range(2)]
            ta1 = [arena.alloc([128, 512], F32) for _ in range(2)]
            tb1 = [arena.alloc([128, 512], F32) for _ in range(2)]
            fst = [arena.alloc([128, 512], BF16) for _ in range(4)]
            tst = [arena.alloc([128, D], BF16) for _ in range(2)]
            gst1 = [arena.alloc([128, 16], F32) for _ in range(2)]
            load_wb(win1, rec_in_w, 8, [(0, 1024, "u"), (1024, 1024, "v"), (3072, 16, "g"), (2048, 1024, "o")], "win1")
            for h in range(4):
                for dc in range(2):
                    DMA("pool", qw[:, h, dc, :], rec_q_w[h, dc * 128:(dc + 1) * 128, :], [], ["qw"])
                    DMA("pool", kw[:, h, dc, :], rec_k_w[h, dc * 128:(dc + 1) * 128, :], [], ["kw"])
            for j in range(3):
                DMA("sp", cwr[:, j, :], rec_conv_w[j].rearrange("(c p) -> p c", p=128), [], ["cwr"])
            DMA("sp", cbr, rec_conv_b.rearrange("(c p) -> p c", p=128), [], ["cbr"])
            DMA("sp", gbias, rec_gate_b.partition_broadcast(128), [], ["gbias"])
            c1 = {"w": 0, "p": 0, "f": 0, "t": 0, "g": 0}

            def nbank():
                c1["p"] += 1
                return c1["p"] % 4

            for b in range(NB):
                for (src, sname, L, toff, row, isx) in ((CB[b], "CB", CT, 0, 2, False), (XB[b], "XB", T, CT, b, True)):
                    for (r0, nr, c0, rz, ncols, o, n) in make_windows(L):
                        ws = c1["w"] % 2
                        c1["w"] += 1
                        hT, hres = hT1[ws], "hT1_%d" % ws
                        uc_, ures = ucw[ws], "ucw%d" % ws
                        if c0 == 1:
                            V(lambda e: e.memset(hT[:, :, 0:1], 0.0), [], [hres])
                        if rz:
                            V(lambda e: e.memset(hT[:, :, c0 + nr:c0 + nr + 1], 0.0), [], [hres])
                        rr_ = r0
                        while rr_ < r0 + nr:
                            n_ = min(128, r0 + nr - rr_)
                            make_hT(src, xres_fn(sname, b), rr_, n_, A1[1][:, row, :], SH1[1][:, row, :], "A1_1",
                                    hT, hres, c0 + rr_ - r0)
                            rr_ += n_

                        def fm_proj(col0, pbk):
                            for k in range(8):
                                PE(lambda e: e.matmul(bank(pbk)[:, 0:ncols], lhsT=win1[:, k, col0:col0 + 128],
                                                      rhs=hT[:, k, 0:ncols], start=(k == 0), stop=(k == 7)),
                                   [("win1", "u" if col0 < 1024 else "o"), hres], ["ps%d" % pbk])

                        for j in range(8):
                            pbk = nbank()
                            s2 = j % 2
                            fm_proj(j * 128, pbk)
                            A(lambda e: e.activation(out=ta1[s2][:, 0:n], in_=bank(pbk)[:, 1:1 + n], func=AF.Identity,
                                                     scale=cwr[:, 1, j:j + 1], bias=cbr[:, j:j + 1]),
                              ["ps%d" % pbk, "cwr", "cbr"], ["ta1_%d" % s2])
                            V(lambda e: e.scalar_tensor_tensor(out=tb1[s2][:, 0:n], in0=bank(pbk)[:, 0:n],
                                                               scalar=cwr[:, 0, j:j + 1], in1=ta1[s2][:, 0:n],
                                                               op0=ALU.mult, op1=ALU.add),
                              ["ps%d" % pbk, "cwr", "ta1_%d" % s2], ["tb1_%d" % s2])
                            V(lambda e: e.scalar_tensor_tensor(out=ta1[s2][:, 0:n], in0=bank(pbk)[:, 2:2 + n],
                                                               scalar=cwr[:, 2, j:j + 1], in1=tb1[s2][:, 0:n],
                                                               op0=ALU.mult, op1=ALU.add),
                              ["ps%d" % pbk, "cwr", "tb1_%d" % s2], ["ta1_%d" % s2])
                            A(lambda e: e.activation(out=uc_[:, j, 0:n], in_=ta1[s2][:, 0:n], func=AF.Silu),
                              ["ta1_%d" % s2], [ures])
                            if isx:
                                DMA("sp", UC1[b, j * 128:(j + 1) * 128, o:o + n], uc_[:, j, 0:n], [ures],
                                    wpart(("UC1", b)))
                        if isx:
                            for j in range(8):
                                pbk = nbank()
                                fs = c1["f"] % 4
                                c1["f"] += 1
                                fm_proj(2048 + j * 128, pbk)
                                A(lambda e: e.activation(out=fst[fs][:, 0:n], in_=bank(pbk)[:, 1:1 + n],
                                                         func=AF.Sigmoid), ["ps%d" % pbk], ["fst%d" % fs])
                                DMA("sp", OG1[b, j * 128:(j + 1) * 128, o:o + n], fst[fs][:, 0:n], ["fst%d" % fs],
                                    wpart(("OG1", b)))
                        for h in range(4):
                            for ec in range(2):
                                for (wt_, wres_, dst, scl, need) in ((kw, "kw", KT1, 1.0 / 16, True),
                                                                     (qw, "qw", QT1, 1.0, isx)):
                                    if not need:
                                        continue
                                    pbk = nbank()
                                    fs = c1["f"] % 4
                                    c1["f"] += 1
                                    for dc in range(2):
                                        PE(lambda e: e.matmul(bank(pbk)[:, 0:n],
                                                              lhsT=wt_[:, h, dc, ec * 128:(ec + 1) * 128],
                                                              rhs=uc_[:, 2 * h + dc, 0:n], start=(dc == 0),
                                                              stop=(dc == 1)), [wres_, ures], ["ps%d" % pbk])
                                    A(lambda e: e.activation(out=fst[fs][:, 0:n], in_=bank(pbk)[:, 0:n], func=AF.Copy,
                                                             scale=scl), ["ps%d" % pbk], ["fst%d" % fs])
                                    tcol = (toff + o) if dst is KT1 else o
                                    DMA("sp", dst[b, h * 256 + ec * 128:h * 256 + (ec + 1) * 128, tcol:tcol + n],
                                        fst[fs][:, 0:n], ["fst%d" % fs],
                                        wpart(("KT1", b)) if dst is KT1 else wpart(("QT1", b)))
                        i0 = 0
                        while i0 < n:
                            m = min(128, n - i0)
                            trow = toff + o + i0
                            ts_ = c1["t"] % 2
                            c1["t"] += 1
                            mix = PS2[2]
                            for h in range(4):
                                for dc in range(2):
                                    PE(lambda e: e.matmul(mix[0:m, h * 256:(h + 1) * 256],
                                                          lhsT=uc_[:, 2 * h + dc, i0:i0 + m], rhs=kw[:, h, dc, :],
                                                          start=(dc == 0), stop=(dc == 1)),
                                       [ures, "kw"], ["ps4", "ps5"])
                            A(lambda e: e.activation(out=tst[ts_][0:m, :], in_=mix[0:m, :], func=AF.Copy,
                                                     scale=1.0 / 16), ["ps4", "ps5"], ["tst%d" % ts_])
                            DMA("sp", KK1[b, trow:trow + m, :], tst[ts_][0:m, :], ["tst%d" % ts_], wpart(("KK1", b)))
                            ts_ = c1["t"] % 2
                            c1["t"] += 1
                            mix = PS2[3]
                            for hf in range(2):
                                for k in range(8):
                                    PE(lambda e: e.matmul(mix[0:m, hf * 512:(hf + 1) * 512],
                                                          lhsT=hT[:, k, 1 + i0:1 + i0 + m],
                                                          rhs=win1[:, k, 1024 + hf * 512:1024 + (hf + 1) * 512],
                                                          start=(k == 0), stop=(k == 7)),
                                       [hres, ("win1", "v")], ["ps%d" % (6 + hf)])
                            A(lambda e: e.activation(out=tst[ts_][0:m, :], in_=mix[0:m, :], func=AF.Copy),
                              ["ps6", "ps7"], ["tst%d" % ts_])
                            DMA("sp", VV1[b, trow:trow + m, :], tst[ts_][0:m, :], ["tst%d" % ts_], wpart(("VV1", b)))
                            gs = c1["g"] % 2
                            c1["g"] += 1
                            pbk = nbank()
                            for k in range(8):
                                PE(lambda e: e.matmul(bank(pbk)[0:m, 0:16], lhsT=hT[:, k, 1 + i0:1 + i0 + m],
                                                      rhs=win1[:, k, 3072:3088], start=(k == 0), stop=(k == 7)),
                                   [hres, ("win1", "g")], ["ps%d" % pbk])
                            V(lambda e: e.tensor_tensor(out=gst1[gs][0:m, :], in0=bank(pbk)[0:m, 0:16],
                                                        in1=gbias[0:m, :], op=ALU.add),
                              ["ps%d" % pbk, "gbias"], ["gst1_%d" % gs])
                            DMA("sp", GG1[b, trow:trow + m, :], gst1[gs][0:m, :], ["gst1_%d" % gs], wpart(("GG1", b)))
                            i0 += m

            chk(7)
            S.barrier()
            arena.reset()
            gg = arena.alloc([128, 18, 16], F32)
            ones4 = arena.alloc([4, TS], F32)
            IG = [arena.alloc([4, TS], F32) for _ in range(2)]
            FG = [arena.alloc([4, TS], F32) for _ in range(2)]
            t_a = arena.alloc([4, TS], F32)
            t_b = arena.alloc([4, TS], F32)
            Bc = arena.alloc([4, TS], F32)
            Mc = arena.alloc([4, TS], F32)
            QY = [[arena.alloc([4, TS], F32) for _ in range(3)] for _ in range(2)]
            Mpv = arena.alloc([4, 18], F32)
            dcy = arena.alloc([4, 18], F32)
            scs = arena.alloc([128, 18, 24], F32)
            V(lambda e: e.memset(ones4, 1.0), [], ["ones4"])

            def lb_tile(q):
                return q + 2 if q < 16 else q - 16

            for b in range(NB):
                dma3("sp", lambda j: gg[:, j, :], lambda j: GG1[b, j * 128:(j + 1) * 128, :], 18, rparts(("GG1", b)), ["gg"])
                for dr in range(2):
                    for ti, dstt, dres in ((0, IG[dr], "IG%d" % dr), (1, FG[dr], "FG%d" % dr)):
                        ty = dr * 2 + ti
                        for q in range(18):
                            tl = q if dr == 0 else lb_tile(q)
                            pq = PS2[q // 8]
                            PE(lambda e: e.matmul(pq[0:4, (q % 8) * 128:(q % 8 + 1) * 128],
                                                  lhsT=gg[:, tl, ty * 4:(ty + 1) * 4], rhs=ident[:, :], start=True,
                                                  stop=True), ["gg", "ident"], ["ps%d" % (2 * (q // 8) + (q % 8) // 4)])
                        for pi in range(3):
                            ncol = 1024 if pi < 2 else 256
                            A(lambda e: e.activation(out=dstt[:, pi * 1024:pi * 1024 + ncol], in_=PS2[pi][0:4, 0:ncol],
                                                     func=AF.Copy), ["ps%d" % (2 * pi), "ps%d" % (2 * pi + 1)], [dres])
                for dr in range(2):
                    ig, fg = IG[dr], FG[dr]
                    igr, fgr = "IG%d" % dr, "FG%d" % dr

                    def dview(ap):
                        return ap if dr == 0 else ap[:, ::-1]

                    A(lambda e: e.activation(out=t_a, in_=fg, func=AF.Abs), [fgr], ["t_a"])
                    A(lambda e: e.activation(out=t_a, in_=t_a, func=AF.Exp, scale=-1.0), ["t_a"], ["t_a"])
                    A(lambda e: e.activation(out=t_a, in_=t_a, func=AF.Ln, bias=1.0), ["t_a"], ["t_a"])
                    V(lambda e: e.tensor_scalar(out=t_b, in0=fg, scalar1=0.0, scalar2=None, op0=ALU.min), [fgr], ["t_b"])
                    V(lambda e: e.tensor_tensor(out=t_b, in0=t_b, in1=t_a, op=ALU.subtract), ["t_a", "t_b"], ["t_b"])
                    V(lambda e: e.tensor_tensor_scan(out=dview(Bc), data0=dview(ones4), data1=dview(t_b), initial=0.0,
                                                     op0=ALU.mult, op1=ALU.add), ["ones4", "t_b"], ["Bc"])
                    V(lambda e: e.tensor_tensor(out=t_a, in0=ig, in1=Bc, op=ALU.subtract), [igr, "Bc"], ["t_a"])
                    V(lambda e: e.tensor_tensor_scan(out=dview(Mc), data0=dview(ones4), data1=dview(t_a), initial=0.0,
                                                     op0=ALU.mult, op1=ALU.max), ["ones4", "t_a"], ["Mc"])
                    M3 = Mc.rearrange("p (q t) -> p q t", t=128)
                    a3 = t_a.rearrange("p (q t) -> p q t", t=128)
                    B3 = Bc.rearrange("p (q t) -> p q t", t=128)
                    V(lambda e: e.memset(Mpv, 0.0), [], ["Mpv"])
                    if dr == 0:
                        V(lambda e: e.tensor_copy(out=Mpv[:, 1:18], in_=M3[:, 0:17, 127]), ["Mc"], ["Mpv"])
                        Mend = M3[:, :, 127]
                    else:
                        V(lambda e: e.tensor_copy(out=Mpv[:, 0:17], in_=M3[:, 1:18, 0]), ["Mc"], ["Mpv"])
                        Mend = M3[:, :, 0]
                    Mpb = Mpv.unsqueeze(2).to_broadcast([4, 18, 128])
                    q0, q1, q2 = QY[dr]
                    qr = ["QY%d_%d" % (dr, i) for i in range(3)]
                    V(lambda e: e.tensor_tensor(out=q0.rearrange("p (q t) -> p q t", t=128), in0=a3, in1=Mpb,
                                                op=ALU.subtract), ["t_a", "Mpv"], [qr[0]])
                    A(lambda e: e.activation(out=q0, in_=q0, func=AF.Exp), [qr[0]], [qr[0]])
                    V(lambda e: e.tensor_tensor(out=q1.rearrange("p (q t) -> p q t", t=128), in0=a3,
                                                in1=Mend.unsqueeze(2).to_broadcast([4, 18, 128]), op=ALU.subtract),
                      ["t_a", "Mc"], [qr[1]])
                    A(lambda e: e.activation(out=q1, in_=q1, func=AF.Exp), [qr[1]], [qr[1]])
                    V(lambda e: e.scalar_tensor_tensor(out=q2.rearrange("p (q t) -> p q t", t=128), in0=B3, scalar=-1.0,
                                                       in1=Mpb, op0=ALU.mult, op1=ALU.subtract), ["Bc", "Mpv"], [qr[2]])
                    A(lambda e: e.activation(out=q2, in_=q2, func=AF.Exp), [qr[2]], [qr[2]])
                    V(lambda e: e.tensor_tensor(out=dcy, in0=Mpv, in1=Mend, op=ALU.subtract), ["Mpv", "Mc"], ["dcy"])
                    A(lambda e: e.activation(out=dcy, in_=dcy, func=AF.Exp), ["dcy"], ["dcy"])
                    dcv = DC1[b].rearrange("(d h q) -> d h q", d=2, h=4)[dr]
                    if dr == 0:
                        DMA("sp", dcv, dcy, ["dcy"], wpart(("DC1", b)))
                    else:
                        DMA("sp", dcv[:, 2:18], dcy[:, 0:16], ["dcy"], wpart(("DC1", b)))
                        DMA("sp", dcv[:, 0:2], dcy[:, 16:18], ["dcy"], wpart(("DC1", b)))
                    pst = bank(6)
                    for q in range(18):
                        tl = q if dr == 0 else lb_tile(q)
                        for qi in range(3):
                            col = tl * 24 + (dr * 3 + qi) * 4
                            PE(lambda e: e.matmul(pst[:, col:col + 4], lhsT=QY[dr][qi][:, q * 128:(q + 1) * 128],
                                                  rhs=ident[0:4, 0:4], start=True, stop=True), [qr[qi], "ident"], ["ps6"])
                A(lambda e: e.activation(out=scs, in_=bank(6)[:, 0:432].rearrange("p (n c) -> p n c", c=24), func=AF.Copy),
                  ["ps6"], ["scs"])
                dma3("sp", lambda j: SC1[b, j * 128:(j + 1) * 128, :], lambda j: scs[:, j, :], 18, ["scs"], wpart(("SC1", b)))

            chk(8)
            S.barrier()
            arena.reset()
            alloc_norm_bufs()
            ctxb["mtmp"] = [arena.alloc([128, D], F32) for _ in range(2)]
            wo1 = arena.alloc([128, 8, D], BF16)
            QTh = arena.alloc([128, 2, T], BF16)
            KTh = arena.alloc([128, 2, TS], BF16)
            KKh = arena.alloc([128, 18, 256], BF16)
            VVh = arena.alloc([128, 18, 257], BF16)
            ogh = arena.alloc([128, 2, T], BF16)
            uch = arena.alloc([128, 2, T], BF16)
            hnT = arena.alloc([128, 2, T], BF16)
            tyy = arena.alloc([128, T], F32)
            scb = arena.alloc([128, 18, 24], F32)
            dcb = arena.alloc([128, 144], F32)
            lmk = arena.alloc([128, 2, 128], F32)
            Sst = [arena.alloc([128, 2, 257], F32) for _ in range(2)]
            Sbf = [arena.alloc([128, 2, 257], BF16) for _ in range(2)]
            hs = arena.alloc([128, 16, 256], F32)
            ATb = [arena.alloc([128, 128], BF16) for _ in range(4)]
            Ktl = [arena.alloc([128, 256], BF16) for _ in range(4)]
            dnn = [arena.alloc([128, 2], F32) for _ in range(4)]
            hnb = [arena.alloc([128, 256], BF16) for _ in range(2)]
            yT = arena.alloc([128, 8, T], BF16)
            rng = arena.alloc([128, 8], F32)
            rsk = arena.alloc([128, 8], F32)
            gb1 = arena.alloc([128, D], F32)
            load_w(wo1, rec_out_w, 8, D, "wo1")
            for m_ in range(2):
                DMA("sp", lmk[:, m_, :], lmask_in[m_], [], ["lmk"])
            DMA("sp", rng, rec_norm_g.rearrange("(c p) -> p c", p=128), [], ["rng"])
            DMA("sp", rsk, rec_skip.rearrange("(c p) -> p c", p=128), [], ["rsk"])
            V(lambda e: e.memset(VVh[:, :, 256:257], 1.0), [], ["VVh"])
            c3 = {"i": 0}
            for b in range(NB):
                dma3("sp", lambda j: scb[:, j, :], lambda j: SC1[b, j * 128:(j + 1) * 128, :], 18, rparts(("SC1", b)), ["scb"])
                DMA("sp", dcb, DC1[b].partition_broadcast(128), rparts(("DC1", b)), ["dcb"])
                load_gb(1, 0, b, gb1, "gb1")
                for h in range(4):
                    dma3("sp", lambda j: QTh[:, j, :], lambda j: QT1[b, h * 256 + j * 128:h * 256 + (j + 1) * 128, :], 2,
                         rparts(("QT1", b)), ["QTh"])
                    dma3("sp", lambda j: KTh[:, j, :], lambda j: KT1[b, h * 256 + j * 128:h * 256 + (j + 1) * 128, :], 2,
                         rparts(("KT1", b)), ["KTh"])
                    dma3("sp", lambda j: KKh[:, j, :], lambda j: KK1[b, j * 128:(j + 1) * 128, h * 256:(h + 1) * 256], 18,
                         rparts(("KK1", b)), ["KKh"])
                    dma3("sp", lambda j: VVh[:, j, 0:256], lambda j: VV1[b, j * 128:(j + 1) * 128, h * 256:(h + 1) * 256], 18,
                         rparts(("VV1", b)), ["VVh"])
                    dma3("sp", lambda j: ogh[:, j, :], lambda j: OG1[b, h * 256 + j * 128:h * 256 + (j + 1) * 128, :], 2,
                         rparts(("OG1", b)), ["ogh"])
                    dma3("sp", lambda j: uch[:, j, :], lambda j: UC1[b, h * 256 + j * 128:h * 256 + (j + 1) * 128, :], 2,
                         rparts(("UC1", b)), ["uch"])
                    for dr in range(2):
                        V(lambda e: e.memset(Sst[dr], 0.0), [], ["Sst%d" % dr])
                        V(lambda e: e.memset(Sbf[dr], 0.0), [], ["Sbf%d" % dr])
                    order = [list(range(18)), [1, 0] + list(range(17, 1, -1))]
                    first_dir_done = set()
                    for step in range(18):
                        st_ = []
                        for dr in range(2):
                            tl = order[dr][step]
                            st_.append(dict(dr=dr, tl=tl, isx=tl >= 2, xt=tl - 2, pb0=dr * 4, bi=dr * 2 + step % 2,
                                            pslot=dr * 4 + step % 2, sres="Sst%d" % dr, bres="Sbf%d" % dr,
                                            colA=(dr * 3 + 0) * 4 + h, colB=(dr * 3 + 1) * 4 + h,
                                            colF=(dr * 3 + 2) * 4 + h, dcol=(dr * 4 + h) * 18 + tl))
                        for q_ in st_:
                            dr, tl, xt_, bi, pslot = q_["dr"], q_["tl"], q_["xt"], q_["bi"], q_["pslot"]
                            if q_["isx"]:
                                pA = bank(pslot)[:, 0:128]
                                for dc in range(2):
                                    PE(lambda e: e.matmul(pA[:, 0:128], lhsT=KTh[:, dc, tl * 128:(tl + 1) * 128],
                                                          rhs=QTh[:, dc, xt_ * 128:(xt_ + 1) * 128], start=(dc == 0),
                                                          stop=(dc == 1)), ["KTh", "QTh"], ["ps%d" % pslot])
                                V(lambda e: e.scalar_tensor_tensor(out=ATb[bi], in0=pA[:, 0:128],
                                                                   scalar=scb[:, tl, q_["colA"]:q_["colA"] + 1],
                                                                   in1=lmk[:, dr, :], op0=ALU.mult, op1=ALU.mult),
                                  ["ps%d" % pslot, "scb", "lmk"], ["ATb%d" % bi])
                            if step < 17:
                                A(lambda e: e.activation(out=Ktl[bi], in_=KKh[:, tl, :], func=AF.Copy,
                                                         scale=scb[:, tl, q_["colB"]:q_["colB"] + 1]),
                                  ["KKh", "scb"], ["Ktl%d" % bi])
                        if step < 17:
                            for q_ in st_:
                                tl, bi, pb0 = q_["tl"], q_["bi"], q_["pb0"]
                                for dc in range(2):
                                    pS = bank(pb0 + 2 + dc)
                                    PE(lambda e: e.matmul(pS[:, 0:257], lhsT=Ktl[bi][:, dc * 128:(dc + 1) * 128],
                                                          rhs=VVh[:, tl, :], start=True, stop=True),
                                       ["Ktl%d" % bi, "VVh"], ["ps%d" % (pb0 + 2 + dc)])
                        for q_ in st_:
                            dr, tl, xt_, bi, pslot = q_["dr"], q_["tl"], q_["xt"], q_["bi"], q_["pslot"]
                            if q_["isx"]:
                                pO = bank(pslot)[:, 128:512]
                                PE(lambda e: e.matmul(pO[:, 0:257], lhsT=ATb[bi], rhs=VVh[:, tl, :], start=True, stop=False),
                                   ["ATb%d" % bi, "VVh"], ["ps%d" % pslot])
                                for dc in range(2):
                                    PE(lambda e: e.matmul(pO[:, 0:257], lhsT=QTh[:, dc, xt_ * 128:(xt_ + 1) * 128],
                                                          rhs=Sbf[dr][:, dc, :], start=False, stop=(dc == 1)),
                                       ["QTh", q_["bres"]], ["ps%d" % pslot])
                        if step < 17:
                            for q_ in st_:
                                dr, pb0 = q_["dr"], q_["pb0"]
                                for dc in range(2):
                                    pS = bank(pb0 + 2 + dc)
                                    V(lambda e: e.scalar_tensor_tensor(out=Sst[dr][:, dc, :], in0=Sst[dr][:, dc, :],
                                                                       scalar=dcb[:, q_["dcol"]:q_["dcol"] + 1],
                                                                       in1=pS[:, 0:257], op0=ALU.mult, op1=ALU.add),
                                      [q_["sres"], "dcb", "ps%d" % (pb0 + 2 + dc)], [q_["sres"]])
                        for q_ in st_:
                            dr, tl, xt_, bi, pslot = q_["dr"], q_["tl"], q_["xt"], q_["bi"], q_["pslot"]
                            if q_["isx"]:
                                pO = bank(pslot)[:, 128:512]
                                dn = dnn[bi]
                                A(lambda e: e.activation(out=dn[:, 0:1], in_=pO[:, 256:257], func=AF.Abs),
                                  ["ps%d" % pslot], ["dnn%d" % bi])
                                V(lambda e: e.tensor_tensor(out=dn[:, 0:1], in0=dn[:, 0:1],
                                                            in1=scb[:, tl, q_["colF"]:q_["colF"] + 1], op=ALU.max),
                                  ["dnn%d" % bi, "scb"], ["dnn%d" % bi])
                                V(lambda e: e.reciprocal(out=dn[:, 1:2], in_=dn[:, 0:1]), ["dnn%d" % bi], ["dnn%d" % bi])
                                hres_ = ("hs", xt_)
                                if xt_ not in first_dir_done:
                                    first_dir_done.add(xt_)
                                    V(lambda e: e.tensor_scalar(out=hs[:, xt_, :], in0=pO[:, 0:256], scalar1=dn[:, 1:2],
                                                                scalar2=None, op0=ALU.mult),
                                      ["ps%d" % pslot, "dnn%d" % bi], [hres_])
                                else:
                                    V(lambda e: e.scalar_tensor_tensor(out=hs[:, xt_, :], in0=pO[:, 0:256],
                                                                       scalar=dn[:, 1:2], in1=hs[:, xt_, :],
                                                                       op0=ALU.mult, op1=ALU.add),
                                      ["ps%d" % pslot, "dnn%d" % bi, hres_], [hres_])
                        if step < 17:
                            for q_ in st_:
                                dr = q_["dr"]
                                A(lambda e: e.activation(out=Sbf[dr], in_=Sst[dr], func=AF.Copy), [q_["sres"]], [q_["bres"]])
                    for xt_ in range(16):
                        ci = xt_ % 2
                        stt = ctxb["st"][xt_ % 3]
                        sres_ = "st%d" % (xt_ % 3)
                        junk = ctxb["junk"]
                        A(lambda e: e.activation(out=junk[:, 0:256], in_=hs[:, xt_, :], func=AF.Square,
                                                 accum_out=stt[:, 0:1]), [("hs", xt_)], ["junk", sres_])
                        A(lambda e: e.activation(out=stt[:, 1:2], in_=stt[:, 0:1], func=AF.Sqrt, scale=1.0 / 256,
                                                 bias=epsb[:, :]), [sres_, "epsb"], [sres_])
                        V(lambda e: e.reciprocal(out=stt[:, 1:2], in_=stt[:, 1:2]), [sres_], [sres_])
                        V(lambda e: e.tensor_scalar(out=hnb[ci], in0=hs[:, xt_, :], scalar1=stt[:, 1:2], scalar2=None,
                                                    op0=ALU.mult), [("hs", xt_), sres_], ["hnb%d" % ci])
                        ptv = bank_bf(6 + ci).rearrange("p (k t) -> p k t", t=128)
                        for dc in range(2):
                            PE(lambda e: e.transpose(out=ptv[:, dc, :], in_=hnb[ci][:, dc * 128:(dc + 1) * 128],
                                                     identity=identb[:]), ["hnb%d" % ci, "identb"], ["ps%d" % (6 + ci)])
                        A(lambda e: e.activation(out=hnT[:, :, xt_ * 128:(xt_ + 1) * 128], in_=ptv[:, 0:2, :],
                                                 func=AF.Copy), ["ps%d" % (6 + ci)], ["hnT"])
                    for dc in range(2):
                        fcx = 2 * h + dc
                        V(lambda e: e.tensor_scalar(out=tyy, in0=hnT[:, dc, :], scalar1=rng[:, fcx:fcx + 1], scalar2=None,
                                                    op0=ALU.mult), ["hnT", "rng"], ["tyy"])
                        V(lambda e: e.scalar_tensor_tensor(out=tyy, in0=uch[:, dc, :], scalar=rsk[:, fcx:fcx + 1], in1=tyy,
                                                           op0=ALU.mult, op1=ALU.add), ["uch", "rsk", "tyy"], ["tyy"])
                        V(lambda e: e.tensor_tensor(out=yT[:, fcx, :], in0=tyy, in1=ogh[:, dc, :], op=ALU.mult),
                          ["tyy", "ogh"], [("yT", fcx)])
                for n in range(16):
                    mix = PS2[n % 2]
                    for hf in range(2):
                        for k in range(8):
                            PE(lambda e: e.matmul(mix[:, hf * 512:(hf + 1) * 512], lhsT=yT[:, k, n * 128:(n + 1) * 128],
                                                  rhs=wo1[:, k, hf * 512:(hf + 1) * 512], start=(k == 0), stop=(k == 7)),
                               [("yT", k), "wo1"], ["ps%d" % (2 * (n % 2) + hf)])
                    residual_update(mix[:, :], ["ps%d" % (2 * (n % 2)), "ps%d" % (2 * (n % 2) + 1)], XB[b],
                                    [("XB", b, n)], XA[b], [("XA", b, n)], n * 128, 128, gb1, "gb1")

            ffn_layer(1,
                      lambda b: [(XA[b], "XA", T, 0, b)],
                      lambda b: [(XA[b], "XA", T, 0, b, out[b], "out", True)])

        except _Stop:
            S.barrier()
        S.emit(st)
        build_program.stats = S.stats
    return nc


def _consts():
    ident = np.eye(128, dtype=np.float32)
    inv = (10000.0 ** (-np.arange(16, dtype=np.float32) / 16)).astype(np.float32)
    t = np.arange(T)
    row = (t // 64).astype(np.float32)
    col = (t % 64).astype(np.float32)
    rc = np.zeros((128, T), np.float32)
    rs = np.zeros((128, T), np.float32)
    for p in range(128):
        d = p % 64
        axis, half, f = d // 32, (d % 32) // 16, d % 16
        pos = row if axis == 0 else col
        ang = (pos * inv[f]).astype(np.float32)
        rc[p] = np.cos(ang)
        rs[p] = np.sin(ang) * (-1.0 if half == 0 else 1.0)
    j = np.arange(128)[:, None]
    i = np.arange(128)[None, :]
    prev = (j >= i).astype(np.float32)
    nxt = (j <= i).astype(np.float32)
    amask = np.stack([np.tile(prev, (1, 4)), np.tile(nxt, (1, 4))]).astype(np.float32)
    pe = np.zeros((4, 2, 8), np.float32)
    for g in range(4):
        w = 2 ** (g + 1)
        half = w // 2
        for k in range(half):
            pe[g, 0, k] = 1.0 / (k + half)
        for k in range(half - 1):
            pe[g, 1, k] = 1.0 / (2 * half - 1 - k)
    pool_edge = np.tile(pe.reshape(1, 64), (128, 1)).astype(np.float32)
    lmask = np.stack([(j <= i).astype(np.float32), (j >= i).astype(np.float32)])
    return dict(ident=ident, rope_c=rc, rope_s=rs, amask=amask, pool_edge=pool_edge, lmask=lmask)


def _perm_head():
    p = np.zeros(64, np.int64)
    for d in range(64):
        axis, half, f = d // 32, (d % 32) // 16, d % 16
        p[d] = axis * 32 + (1 - half) * 16 + f
    return p


def _prep_shared(inp):
    w = np.asarray(inp["attn_in_w"][0], np.float32)
    ph = _perm_head()
    wz = np.concatenate([w, np.zeros((w.shape[0], 1), np.float32)], axis=1)
    Z = np.full(64, w.shape[1], np.int64)
    q = [np.arange(c * 128, (c + 1) * 128) for c in range(4)]
    k0 = 512 + np.arange(64)
    k1 = 576 + np.arange(64)
    kz = [np.concatenate([k0, Z]), np.concatenate([Z, k0]), np.concatenate([k1, Z]), np.concatenate([Z, k1])]
    base = q + kz

    def partner(idx):
        o = idx.copy()
        for h0 in range(0, 128, 64):
            blk = idx[h0:h0 + 64]
            o[h0:h0 + 64] = blk[ph]
        return o

    cols = base + [partner(c) for c in base] + [np.arange(640, 768)] + [np.arange(768 + g * 128, 896 + g * 128) for g in range(4)]
    cols = np.concatenate(cols)
    w = wz
    sh = dict(
        mod_w=np.ascontiguousarray(inp["mod_w"], np.float32),
        mod_b=np.ascontiguousarray(inp["mod_b"], np.float32),
        norm_g=np.ascontiguousarray(inp["norm_g"], np.float32),
        w_in0=np.ascontiguousarray(w[:, cols]),
        attn_sink=np.ascontiguousarray(inp["attn_sink"][0], np.float32),
        pool_w=np.ascontiguousarray(inp["pool_w"][0], np.float32),
        pool_scale=np.ascontiguousarray(inp["pool_scale"][0], np.float32),
        attn_out_w=np.ascontiguousarray(inp["attn_out_w"][0], np.float32),
        rec_in_w=np.ascontiguousarray(inp["rec_in_w"][0], np.float32),
        rec_gate_b=np.ascontiguousarray(inp["rec_gate_b"][0].reshape(16), np.float32),
        rec_conv_w=np.ascontiguousarray(inp["rec_conv_w"][0], np.float32),
        rec_conv_b=np.ascontiguousarray(inp["rec_conv_b"][0], np.float32),
        rec_q_w=np.ascontiguousarray(inp["rec_q_w"][0], np.float32),
        rec_k_w=np.ascontiguousarray(inp["rec_k_w"][0], np.float32),
        rec_norm_g=np.ascontiguousarray(inp["rec_norm_g"][0], np.float32),
        rec_skip=np.ascontiguousarray(inp["rec_skip"][0], np.float32),
        rec_out_w=np.ascontiguousarray(inp["rec_out_w"][0], np.float32),
        ffn_up_w=np.ascontiguousarray(inp["ffn_up_w"], np.float32),
        ffn_conv_w=np.ascontiguousarray(inp["ffn_conv_w"], np.float32),
        ffn_conv_b=np.ascontiguousarray(inp["ffn_conv_b"], np.float32),
        ffn_down_w=np.ascontiguousarray(inp["ffn_down_w"], np.float32),
    )
    sh.update(_consts())
    return sh


def make_in_maps(inp, cores):
    sh = _prep_shared(inp)
    x = np.asarray(inp["x"], np.float32)
    c = np.asarray(inp["c"], np.float32)
    ctx = np.asarray(inp["ctx"], np.float32)
    c_ctx = np.asarray(inp["c_ctx"], np.float32)
    maps = []
    for i in cores:
        m = dict(sh)
        m["x"] = np.ascontiguousarray(x[NB * i:NB * (i + 1)])
        m["ctx"] = np.ascontiguousarray(ctx[NB * i:NB * (i + 1)])
        m["cvec"] = np.ascontiguousarray(np.concatenate([c[NB * i:NB * (i + 1)], c_ctx[None, :]], axis=0))
        maps.append(m)
    return maps


def kernel(**inputs):
    nc = build_program()
    maps = make_in_maps(inputs, list(range(8)))
    res = run_bass_kernel_spmd(nc, maps, core_ids=list(range(8)))
    return np.concatenate([np.asarray(r["out"], np.float32) for r in res.results], axis=0)
```

```python
import math
from contextlib import ExitStack

import numpy as np
import concourse.bass as bass
import concourse.mybir as mybir
from concourse.bass_utils import run_bass_kernel_spmd

F32 = mybir.dt.float32
BF16 = mybir.dt.bfloat16
U8 = mybir.dt.uint8
AF = mybir.ActivationFunctionType
ALU = mybir.AluOpType

ENGS = ["pe", "act", "dve", "pool", "sp"]
import os as _os
N_DMA_SEMS = int(_os.environ.get("NDS", "24"))

D = 1024
T = 2048
CT = 256
NB = 2
TS = T + CT
DFF = 2816
EPS = 1e-6


class _Proxy:
    def __getattr__(self, name):
        def f(*a, **k):
            self.call = (name, a, k)
            return self
        return f


class Sched:
    def __init__(self, nc):
        self.nc = nc
        self.ops = {e: [] for e in ENGS}
        self.lastw = {}
        self.readers = {}
        self.dma_rr = {e: 0 for e in ENGS}
        self.dma_last = {}
        self.out_dmas = []
        self.pending_dmas = []

    def add(self, eng, fn, reads=(), writes=(), dma=False, is_output=False, extra_deps=()):
        ops = self.ops[eng]
        me = (eng, len(ops))
        deps = {}

        def dep(p, kind):
            if p is None or p == me:
                return
            if deps.get(p) == "raw":
                return
            deps[p] = kind

        for r in reads:
            dep(self.lastw.get(r), "raw")
        for r in writes:
            dep(self.lastw.get(r), "order")
            for rd in self.readers.get(r, ()):
                dep(rd, "order")
        for p in extra_deps:
            dep(p, "raw")
        prox = _Proxy()
        fn(prox)
        rec = dict(call=prox.call, dma=dma, signal=False, slot=None)
        if dma:
            slot = self.dma_rr[eng] % N_DMA_SEMS
            self.dma_rr[eng] += 1
            rec["slot"] = slot
            prev = self.dma_last.get((eng, slot))
            if prev is not None:
                dep(prev, "raw")
            self.dma_last[(eng, slot)] = me
            self.pending_dmas.append(me)
            if is_output:
                self.out_dmas.append(me)
        final = {}
        for p, kind in deps.items():
            pe, pi = p
            prod = self.ops[pe][pi]
            if pe == eng and not prod["dma"] and not dma:
                if kind == "order" or eng in ("pe", "sp"):
                    continue
            final[p] = kind
        rec["deps"] = final
        ops.append(rec)
        for r in reads:
            lst = self.readers.setdefault(r, [])
            if not dma:
                lst[:] = [q for q in lst if not (q[0] == eng and not self.ops[q[0]][q[1]]["dma"])]
            lst.append(me)
        for r in writes:
            self.lastw[r] = me
            self.readers[r] = []
        return me

    def barrier(self):
        lasts = []
        for e in ENGS:
            for i in range(len(self.ops[e]) - 1, -1, -1):
                if not self.ops[e][i]["dma"]:
                    if not self.ops[e][i].get("nop"):
                        lasts.append((e, i))
                    break
        pend = list(self.pending_dmas)
        self.pending_dmas = []
        for e in ENGS:
            me = self.add(e, lambda eng: eng.nop(), extra_deps=[p for p in lasts if p[0] != e] + pend)
            self.ops[me[0]][me[1]]["nop"] = True

    def emit(self, stack):
        nc = self.nc
        for e in ENGS:
            for rec in self.ops[e]:
                for (pe, pi) in rec["deps"]:
                    self.ops[pe][pi]["signal"] = True
        esem = {e: stack.enter_context(nc.semaphore("s_" + e)) for e in ENGS}
        dsem = {e: [None] * N_DMA_SEMS for e in ENGS}
        for e in ENGS:
            for s in range(min(N_DMA_SEMS, self.dma_rr[e])):
                dsem[e][s] = stack.enter_context(nc.semaphore("d_%s_%d" % (e, s)))
        cnt = {e: 0 for e in ENGS}
        dcnt = {}
        for e in ENGS:
            for rec in self.ops[e]:
                if rec["dma"]:
                    k = (e, rec["slot"])
                    dcnt[k] = dcnt.get(k, 0) + 16
                    rec["ev"] = (k, dcnt[k])
                elif rec["signal"]:
                    cnt[e] += 1
                    rec["ev"] = (e, cnt[e])
                else:
                    rec["ev"] = None
        self.stats = {e: [len(self.ops[e]), 0, cnt[e]] for e in ENGS}
        block = stack.enter_context(nc.Block())
        sched = self

        def semof(key):
            if isinstance(key, tuple):
                return dsem[key[0]][key[1]]
            return esem[key]

        def run(e, eng):
            known = {}
            for rec in sched.ops[e]:
                need = {}
                for (pe, pi) in rec["deps"]:
                    key, val = sched.ops[pe][pi]["ev"]
                    if known.get(key, 0) >= val:
                        continue
                    if need.get(key, 0) < val:
                        need[key] = val
                for key, val in need.items():
                    eng.wait_ge(semof(key), val)
                    known[key] = val
                    sched.stats[e][1] += 1
                nm, a_, k_ = rec["call"]
                ins = getattr(eng, nm)(*a_, **k_)
                if rec["dma"]:
                    ins.then_inc(semof(rec["ev"][0]), 16)
                elif rec["signal"]:
                    ins.then_inc(esem[e], 1)
            return known

        @block.tensor
        def _(eng):
            run("pe", eng)

        @block.scalar
        def _(eng):
            run("act", eng)

        @block.vector
        def _(eng):
            run("dve", eng)

        @block.gpsimd
        def _(eng):
            run("pool", eng)

        @block.sync
        def _(eng):
            known = run("sp", eng)
            for (pe, pi) in sched.out_dmas:
                key, val = sched.ops[pe][pi]["ev"]
                if known.get(key, 0) < val:
                    eng.wait_ge(semof(key), val)
                    known[key] = val


DT_SIZE = {F32: 4, BF16: 2, U8: 1}


class Arena:
    def __init__(self, tens, size):
        self.t = tens
        self.size = size
        self.off = 0

    def reset(self):
        self.off = 0

    def alloc(self, shape, dt):
        n = 1
        for s in shape[1:]:
            n *= s
        nbytes = (n * DT_SIZE[dt] + 63) // 64 * 64
        assert self.off + nbytes <= self.size, ("arena overflow", self.off, nbytes, self.size)
        ap = self.t[0:shape[0], self.off:self.off + n * DT_SIZE[dt]].bitcast(dt)
        self.off += nbytes
        if len(shape) == 3:
            ap = ap.rearrange("p (a b) -> p a b", b=shape[2])
        elif len(shape) == 4:
            ap = ap.rearrange("p (a b c) -> p a b c", b=shape[2], c=shape[3])
        return ap


def tiles_of(r0, nr):
    return list(range(r0 // 128, (r0 + nr - 1) // 128 + 1))


class _Stop(Exception):
    pass


def build_program(dbg=None, lim=None):
    dbg = dbg or set()
    nc = bass.Bass("TRN2", target_bir_lowering=False)

    def din(name, shape, dt=F32):
        return nc.dram_tensor(name, list(shape), dt, kind="ExternalInput").ap()

    def dscr(name, shape, dt=F32):
        kind = "ExternalOutput" if name in dbg else "Internal"
        return nc.dram_tensor(name, list(shape), dt, kind=kind).ap()

    x_in = din("x", [NB, T, D])
    ctx_in = din("ctx", [NB, CT, D])
    cvec = din("cvec", [3, D])
    mod_w = din("mod_w", [2, D, 6 * D])
    mod_b = din("mod_b", [2, 6 * D])
    norm_g = din("norm_g", [2, 4, D])
    w_in0 = din("w_in0", [D, 21 * 128])
    attn_sink = din("attn_sink", [8])
    pool_w = din("pool_w", [4, 128, 128])
    pool_scale = din("pool_scale", [512])
    attn_out_w = din("attn_out_w", [D, D])
    rec_in_w = din("rec_in_w", [D, 3088])
    rec_gate_b = din("rec_gate_b", [16])
    rec_conv_w = din("rec_conv_w", [3, D])
    rec_conv_b = din("rec_conv_b", [D])
    rec_q_w = din("rec_q_w", [4, 256, 256])
    rec_k_w = din("rec_k_w", [4, 256, 256])
    rec_norm_g = din("rec_norm_g", [D])
    rec_skip = din("rec_skip", [D])
    rec_out_w = din("rec_out_w", [D, D])
    ffn_up_w = din("ffn_up_w", [2, D, 2 * DFF])
    ffn_conv_w = din("ffn_conv_w", [2, 3, 2 * DFF])
    ffn_conv_b = din("ffn_conv_b", [2, 2 * DFF])
    ffn_down_w = din("ffn_down_w", [2, DFF, D])
    ident_in = din("ident", [128, 128])
    rope_c = din("rope_c", [128, T])
    rope_s = din("rope_s", [128, T])
    amask_in = din("amask", [2, 128, 512])
    pool_edge = din("pool_edge", [128, 64])
    lmask_in = din("lmask", [2, 128, 128])

    out = nc.dram_tensor("out", [NB, T, D], F32, kind="ExternalOutput").ap()

    GV = dscr("GV", [2, 2, 3, D])
    QT0 = dscr("QT0", [NB, 8 * 128, TS], BF16)
    V0 = dscr("V0", [NB, TS, 130], BF16)
    U0 = dscr("U0", [NB, 512, TS])
    CATP = dscr("CATP", [NB, 512, TS], BF16)
    XA = dscr("XA", [NB, T, D])
    CA = dscr("CA", [NB, CT, D])
    XB = dscr("XB", [NB, T, D])
    CB = dscr("CB", [NB, CT, D])
    GT = dscr("GT", [NB, DFF, TS], BF16)

    st = ExitStack()
    with st:
        st.enter_context(nc.allow_non_contiguous_dma(reason="small strided parameter loads"))
        S = Sched(nc)
        import os
        STQ = os.environ.get("STQ", "pool")
        parts = {}

        def wpart(base):
            lst = parts.setdefault(base, [])
            name = (base, len(lst))
            lst.append(name)
            return [name]

        def rparts(base):
            return list(parts.get(base, []))

        def sb(name, shape, dt):
            return st.enter_context(nc.sbuf_tensor("sb_" + name, list(shape), dt))

        ARENA_BYTES = 192 * 1024
        arena = Arena(sb("arena", [128, ARENA_BYTES], U8), ARENA_BYTES)
        PS2 = [st.enter_context(nc.psum_tensor("ps%d" % i, [128, 1024], F32)) for i in range(4)]

        def bank(i):
            return PS2[i // 2][:, (i % 2) * 512:(i % 2) * 512 + 512]

        def bank_bf(i):
            return PS2[i // 2][:, (i % 2) * 512:(i % 2) * 512 + 512].bitcast(BF16)

        ident = sb("ident", [128, 128], F32)
        identb = sb("identb", [128, 128], BF16)
        modT = [sb("modT%d" % l, [128, 48, 3], F32) for l in range(2)]
        A1 = [sb("A1_%d" % l, [128, 3, 8], F32) for l in range(2)]
        A2 = [sb("A2_%d" % l, [128, 3, 8], F32) for l in range(2)]
        SH1 = [sb("SH1_%d" % l, [128, 3, 8], F32) for l in range(2)]
        SH2 = [sb("SH2_%d" % l, [128, 3, 8], F32) for l in range(2)]
        epsb = sb("epsb", [128, 1], F32)

        def V(fn, reads, writes):
            return S.add("dve", fn, reads, writes)

        def A(fn, reads, writes):
            return S.add("act", fn, reads, writes)

        def G(fn, reads, writes):
            return S.add("pool", fn, reads, writes)

        def PE(fn, reads, writes):
            return S.add("pe", fn, reads, writes)

        def DMA(q, out_ap, in_ap, reads, writes, is_output=False):
            return S.add(q, lambda e: e.dma_start(out=out_ap, in_=in_ap), reads, writes, dma=True,
                         is_output=is_output)

        def dma3(q, dst3, src3, nmid, reads, writes):
            for j in range(nmid):
                DMA(q, dst3(j), src3(j), reads, writes)

        def load_wb(dst3, src2, K, blocks, res):
            for (c0, cw, key) in blocks:
                for k in range(K):
                    DMA("pool", dst3[:, k, c0:c0 + cw], src2[k * 128:(k + 1) * 128, c0:c0 + cw], [], [(res, key)])

        def load_w(dst3, src2, K, N, res):
            for k in range(K):
                c0 = 0
                while c0 < N:
                    cw = min(2048, N - c0)
                    DMA("pool", dst3[:, k, c0:c0 + cw], src2[k * 128:(k + 1) * 128, c0:c0 + cw], [], [res])
                    c0 += cw

        def chk(k):
            if lim is not None and k > lim:
                raise _Stop()

        try:
            DMA("sp", ident[:], ident_in, [], ["ident"])
            V(lambda e: e.tensor_copy(out=identb[:], in_=ident[:]), ["ident"], ["identb"])
            V(lambda e: e.memset(epsb[:], EPS), [], ["epsb"])

            arena.reset()
            cT = arena.alloc([128, 8, 3], F32)
            sT = arena.alloc([128, 8, 3], BF16)
            sTf = arena.alloc([128, 8, 3], F32)
            mb = arena.alloc([128, 48], F32)
            ng = arena.alloc([128, 4, 8], F32)
            g1t = arena.alloc([128, 8, 3], F32)
            g2t = arena.alloc([128, 8, 3], F32)
            mwt = [arena.alloc([128, 8, 512], BF16) for _ in range(3)]
            mwf = [arena.alloc([128, 8, 512], F32) for _ in range(3)]
            for r in range(3):
                DMA("sp", cT[:, :, r], cvec[r].rearrange("(k p) -> p k", p=128), [], ["cT"])
            A(lambda e: e.activation(out=sTf, in_=cT, func=AF.Silu), ["cT"], ["sTf"])
            V(lambda e: e.tensor_copy(out=sT, in_=sTf), ["sTf"], ["sT"])
            for nch in range(12):
                wt = mwt[nch % 3]
                wres = "mwt%d" % (nch % 3)
                load_w(wt, mod_w[0][:, nch * 512:(nch + 1) * 512], 8, 512, wres)
                wf = mwf[nch % 3]
                fres = "mwf%d" % (nch % 3)
                for k in range(8):
                    DMA("sp", wf[:, k, :], mod_w[1][k * 128:(k + 1) * 128, nch * 512:(nch + 1) * 512], [], [fres])
                for fc in range(4):
                    col = (nch * 4 + fc) * 3
                    for k in range(8):
                        PE(lambda e: e.matmul(bank(0)[:, col:col + 3], lhsT=wt[:, k, fc * 128:(fc + 1) * 128],
                                              rhs=sT[:, k, :], start=(k == 0), stop=(k == 7)), [wres, "sT"], ["ps0"])
                for fc in range(4):
                    col = (nch * 4 + fc) * 3
                    for k in range(8):
                        PE(lambda e: e.matmul(bank(1)[:, col:col + 3], lhsT=wf[:, k, fc * 128:(fc + 1) * 128],
                                              rhs=sTf[:, k, :], start=(k == 0), stop=(k == 7)), [fres, "sTf"], ["ps1"])
            for l in range(2):
                DMA("sp", mb, mod_b[l].rearrange("(c p) -> p c", p=128), [], ["mb"])
                for j in range(4):
                    DMA("sp", ng[:, j, :], norm_g[l, j].rearrange("(k p) -> p k", p=128), [], ["ng"])
                psm = bank(l)
                mT = modT[l]
                V(lambda e, mT=mT: e.tensor_tensor(
                    out=mT[:], in0=psm[:, 0:144].rearrange("p (c r) -> p c r", r=3),
                    in1=mb.unsqueeze(2).to_broadcast([128, 48, 3]), op=ALU.add), ["ps%d" % l, "mb"], ["modT%d" % l])

                def ngb(j):
                    return ng[:, j, :].unsqueeze(2).to_broadcast([128, 8, 3])

                mres = ["modT%d" % l, "ng"]
                V(lambda e, mT=mT, l=l: e.scalar_tensor_tensor(
                    out=A1[l][:].rearrange("p r k -> p k r"), in0=mT[:, 8:16, :], scalar=1.0, in1=ngb(0),
                    op0=ALU.add, op1=ALU.mult), mres, ["A1_%d" % l])
                V(lambda e, mT=mT, l=l: e.scalar_tensor_tensor(
                    out=A2[l][:].rearrange("p r k -> p k r"), in0=mT[:, 32:40, :], scalar=1.0, in1=ngb(2),
                    op0=ALU.add, op1=ALU.mult), mres, ["A2_%d" % l])
                V(lambda e, mT=mT, l=l: e.tensor_copy(
                    out=SH1[l][:].rearrange("p r k -> p k r"), in_=mT[:, 0:8, :]), mres, ["SH1_%d" % l])
                V(lambda e, mT=mT, l=l: e.tensor_copy(
                    out=SH2[l][:].rearrange("p r k -> p k r"), in_=mT[:, 24:32, :]), mres, ["SH2_%d" % l])
                V(lambda e, mT=mT: e.tensor_tensor(out=g1t, in0=mT[:, 16:24, :], in1=ngb(1), op=ALU.mult),
                  mres, ["g1t"])
                V(lambda e, mT=mT: e.tensor_tensor(out=g2t, in0=mT[:, 40:48, :], in1=ngb(3), op=ALU.mult),
                  mres, ["g2t"])
                for r in range(3):
                    DMA("sp", GV[l, 0, r].rearrange("(k p) -> p k", p=128), g1t[:, :, r], ["g1t"], wpart(("GV", l, 0)))
                    DMA("sp", GV[l, 1, r].rearrange("(k p) -> p k", p=128), g2t[:, :, r], ["g2t"], wpart(("GV", l, 1)))

            ctxb = {}
            rr = {"n": 0, "r": 0}

            def alloc_norm_bufs():
                ctxb["xt"] = [arena.alloc([128, D], F32) for _ in range(3)]
                ctxb["junk"] = arena.alloc([128, D], BF16)
                ctxb["xn"] = [arena.alloc([128, D], BF16) for _ in range(3)]
                ctxb["st"] = [arena.alloc([128, 4], F32) for _ in range(3)]
                ctxb["tmpT"] = [arena.alloc([128, 8, 128], F32) for _ in range(2)]

            def rstd_chain(src_ap, srcres, stt, stres, nr):
                junk = ctxb["junk"]
                A(lambda e: e.activation(out=junk[0:nr, :], in_=src_ap, func=AF.Square, accum_out=stt[0:nr, 0:1]),
                  srcres, ["junk", stres])
                A(lambda e: e.activation(out=stt[0:nr, 1:2], in_=stt[0:nr, 0:1], func=AF.Sqrt, scale=1.0 / D,
                                         bias=epsb[0:nr, :]), [stres, "epsb"], [stres])
                V(lambda e: e.reciprocal(out=stt[0:nr, 1:2], in_=stt[0:nr, 1:2]), [stres], [stres])

            def make_hT(src, srcres_fn, r0, nr, Aap, SHap, ares, hT, hres, c0):
                i = rr["n"]
                rr["n"] += 1
                s3, s2 = i % 3, i % 2
                xt, xn, stt, tmpT = ctxb["xt"][s3], ctxb["xn"][s3], ctxb["st"][s3], ctxb["tmpT"][s2]
                xres, nres, sres, tres = "xt%d" % s3, "xn%d" % s3, "st%d" % s3, "tmpT%d" % s2
                pb = 6 + s2
                pres = "ps%d" % pb
                DMA("sp", xt[0:nr, :], src[r0:r0 + nr, :], srcres_fn(r0, nr), [xres])
                rstd_chain(xt[0:nr, :], [xres], stt, sres, nr)
                V(lambda e: e.tensor_scalar(out=xn[0:nr, :], in0=xt[0:nr, :], scalar1=stt[0:nr, 1:2], scalar2=None,
                                            op0=ALU.mult), [xres, sres], [nres])
                pt = bank_bf(pb).rearrange("p (k t) -> p k t", t=128)
                for k in range(8):
                    PE(lambda e, k=k: e.transpose(out=pt[:, k, 0:nr], in_=xn[0:nr, k * 128:(k + 1) * 128],
                                                  identity=identb[0:nr, 0:nr]), [nres, "identb"], [pres])
                V(lambda e: e.tensor_tensor(out=tmpT[:, :, 0:nr], in0=pt[:, :, 0:nr],
                                            in1=Aap.unsqueeze(2).to_broadcast([128, 8, nr]), op=ALU.mult),
                  [pres, ares], [tres])
                V(lambda e: e.tensor_tensor(out=hT[:, :, c0:c0 + nr], in0=tmpT[:, :, 0:nr],
                                            in1=SHap.unsqueeze(2).to_broadcast([128, 8, nr]), op=ALU.add),
                  [tres, ares], [hres])

            def residual_update(mix_ap, mixres, src, srcres, dst, dstres, r0, nr, gb, gbres, is_output=False):
                i = rr["r"]
                rr["r"] += 1
                s3, s2 = i % 3, i % 2
                xt, stt = ctxb["xt"][s3], ctxb["st"][s3]
                tmp = ctxb["mtmp"][s2]
                xres, sres, tres = "xt%d" % s3, "st%d" % s3, "mtmp%d" % s2
                DMA("sp", xt[0:nr, :], src[r0:r0 + nr, :], srcres, [xres])
                rstd_chain(mix_ap, mixres, stt, sres, nr)
                V(lambda e: e.scalar_tensor_tensor(out=tmp[0:nr, :], in0=mix_ap, scalar=stt[0:nr, 1:2],
                                                   in1=gb[0:nr, :], op0=ALU.mult, op1=ALU.mult),
                  mixres + [sres, gbres], [tres])
                V(lambda e: e.tensor_tensor(out=xt[0:nr, :], in0=xt[0:nr, :], in1=tmp[0:nr, :], op=ALU.add),
                  [xres, tres], [xres])
                DMA(STQ, dst[r0:r0 + nr, :], xt[0:nr, :], [xres], dstres, is_output=is_output)

            def load_gb(l, j, r, tile, res):
                DMA("sp", tile, GV[l, j, r].partition_broadcast(128), rparts(("GV", l, j)), [res])

            def xres_fn(name, b):
                return lambda r0, nr: [(name, b, t) for t in tiles_of(r0, nr)]

            chk(1)
            S.barrier()
            arena.reset()
            alloc_norm_bufs()
            win = arena.alloc([128, 8, 21 * 128], BF16)
            rc = arena.alloc([128, T], F32)
            rs = arena.alloc([128, T], F32)
            hTw = [arena.alloc([128, 8, 512], BF16) for _ in range(2)]
            rt1 = [arena.alloc([128, 512], F32) for _ in range(2)]
            rt2 = [arena.alloc([128, 512], F32) for _ in range(2)]
            qst = [arena.alloc([128, 512], BF16) for _ in range(4)]
            ust = [arena.alloc([128, 512], F32) for _ in range(2)]
            vst = [arena.alloc([128, 130], BF16) for _ in range(2)]
            load_w(win, w_in0, 8, 21 * 128, "win")
            DMA("sp", rc, rope_c, [], ["rc"])
            DMA("sp", rs, rope_s, [], ["rs"])
            for s in range(2):
                V(lambda e, s=s: e.memset(vst[s][:, 64:65], 1.0), [], ["vst%d" % s])
                V(lambda e, s=s: e.memset(vst[s][:, 129:130], 1.0), [], ["vst%d" % s])
            cnt = {"w": 0, "q": 0, "u": 0, "v": 0, "pp": 0}
            for b in range(NB):
                for w in range(5):
                    isx = w < 4
                    nt = 512 if isx else 256
                    tok0 = w * 512
                    row = b if isx else 2
                    src = x_in[b] if isx else ctx_in[b]
                    hs = cnt["w"] % 2
                    cnt["w"] += 1
                    hT, hres = hTw[hs], "hTw%d" % hs
                    for i in range(nt // 128):
                        make_hT(src, lambda r0, nr: [], (tok0 if isx else 0) + i * 128 if isx else i * 128, 128,
                                A1[0][:, row, :], SH1[0][:, row, :], "A1_0", hT, hres, i * 128)

                    def proj(j, pb):
                        for k in range(8):
                            PE(lambda e, k=k: e.matmul(bank(pb)[:, 0:nt], lhsT=win[:, k, j * 128:(j + 1) * 128],
                                                       rhs=hT[:, k, 0:nt], start=(k == 0), stop=(k == 7)),
                               ["win", hres], ["ps%d" % pb])

                    for j in range(8):
                        pa = (cnt["pp"] % 2) * 2
                        cnt["pp"] += 1
                        qs = cnt["q"] % 4
                        cnt["q"] += 1
                        qt_, qres = qst[qs], "qst%d" % qs
                        proj(j, pa)
                        if isx:
                            proj(j + 8, pa + 1)
                            ts_ = cnt["q"] % 2
                            t1, t2 = rt1[ts_], rt2[ts_]
                            V(lambda e, t1=t1, pa=pa: e.tensor_tensor(out=t1[:, 0:nt], in0=bank(pa)[:, 0:nt],
                                                                      in1=rc[:, tok0:tok0 + nt], op=ALU.mult),
                              ["ps%d" % pa, "rc"], ["rt1_%d" % ts_])
                            V(lambda e, t2=t2, pa=pa: e.tensor_tensor(out=t2[:, 0:nt], in0=bank(pa + 1)[:, 0:nt],
                                                                      in1=rs[:, tok0:tok0 + nt], op=ALU.mult),
                              ["ps%d" % (pa + 1), "rs"], ["rt2_%d" % ts_])
                            G(lambda e, t1=t1, t2=t2, qt_=qt_: e.tensor_tensor(out=qt_[:, 0:nt], in0=t1[:, 0:nt],
                                                                               in1=t2[:, 0:nt], op=ALU.add),
                              ["rt1_%d" % ts_, "rt2_%d" % ts_], [qres])
                        else:
                            A(lambda e, qt_=qt_, pa=pa: e.activation(out=qt_[:, 0:nt], in_=bank(pa)[:, 0:nt],
                                                                     func=AF.Copy), ["ps%d" % pa], [qres])
                        DMA(STQ, QT0[b, j * 128:(j + 1) * 128, tok0:tok0 + nt], qt_[:, 0:nt], [qres],
                            wpart(("QT0", b)))
                    for j in range(17, 21):
                        pa = (cnt["pp"] % 2) * 2
                        cnt["pp"] += 1
                        us = cnt["u"] % 2
                        cnt["u"] += 1
                        proj(j, pa)
                        A(lambda e, us=us, pa=pa: e.activation(out=ust[us][:, 0:nt], in_=bank(pa)[:, 0:nt],
                                                               func=AF.Copy), ["ps%d" % pa], ["ust%d" % us])
                        DMA(STQ, U0[b, (j - 17) * 128:(j - 16) * 128, tok0:tok0 + nt], ust[us][:, 0:nt],
                            ["ust%d" % us], wpart(("U0", b, j - 17)))
                    for i in range(nt // 128):
                        vs = cnt["v"] % 2
                        cnt["v"] += 1
                        for k in range(8):
                            PE(lambda e, k=k, i=i: e.matmul(bank(4)[:, 0:128], lhsT=hT[:, k, i * 128:(i + 1) * 128],
                                                           rhs=win[:, k, 16 * 128:17 * 128], start=(k == 0),
                                                           stop=(k == 7)), ["win", hres], ["ps4"])
                        A(lambda e, vs=vs: e.activation(
                            out=vst[vs][:].rearrange("p (h d) -> p h d", d=65)[:, :, 0:64],
                            in_=bank(4)[:, 0:128].rearrange("p (h d) -> p h d", d=64), func=AF.Copy),
                          ["ps4"], ["vst%d" % vs])
                        DMA(STQ, V0[b, tok0 + i * 128:tok0 + (i + 1) * 128, :], vst[vs][:], ["vst%d" % vs],
                            wpart(("V0", b)))

            chk(2)
            S.barrier()
            arena.reset()
            PADL = 16
            ub = [arena.alloc([128, TS + 48], F32) for _ in range(2)]
            pa_ = [arena.alloc([128, TS + 48], F32) for _ in range(2)]
            pb_ = [arena.alloc([128, TS + 48], F32) for _ in range(2)]
            dTt = [arena.alloc([128, TS], BF16) for _ in range(2)]
            cst = [arena.alloc([128, 512], BF16) for _ in range(2)]
            pwt = arena.alloc([128, 4, 128], BF16)
            psc = arena.alloc([128, 4], F32)
            pedge = arena.alloc([128, 64], F32)
            etmp = arena.alloc([128, 8], F32)
            for g_ in range(4):
                DMA("pool", pwt[:, g_, :], pool_w[g_], [], ["pwt"])
            DMA("sp", psc, pool_scale.rearrange("(g p) -> p g", p=128), [], ["psc"])
            DMA("sp", pedge, pool_edge, [], ["pedge"])
            segs = [(PADL, T, 0), (PADL + T + 16, CT, T)]
            pc = 0
            for b in range(NB):
                for g in range(4):
                    sl = pc % 2
                    pc += 1
                    u, p1, p2, dT = ub[sl], pa_[sl], pb_[sl], dTt[sl]
                    ur, p1r, p2r, dr = "ub%d" % sl, "pa%d" % sl, "pb%d" % sl, "dT%d" % sl
                    V(lambda e, u=u: e.memset(u[:, 0:PADL], 0.0), [], [ur])
                    V(lambda e, u=u: e.memset(u[:, PADL + T:PADL + T + 16], 0.0), [], [ur])
                    V(lambda e, u=u: e.memset(u[:, PADL + T + 16 + CT:TS + 48], 0.0), [], [ur])
                    DMA("sp", u[:, PADL:PADL + T], U0[b, g * 128:(g + 1) * 128, 0:T], rparts(("U0", b, g)), [ur])
                    DMA("sp", u[:, PADL + T + 16:PADL + T + 16 + CT], U0[b, g * 128:(g + 1) * 128, T:TS],
                        rparts(("U0", b, g)), [ur])
                    wlen = 2 ** (g + 1)
                    half = wlen // 2
                    NW = TS + 48
                    cur, curres, ln = u, ur, 1
                    bufs = [(p1, p1r), (p2, p2r)]
                    bi = 0
                    while ln < wlen:
                        nxt, nres = bufs[bi % 2]
                        bi += 1
                        n_el = NW - 2 * ln
                        V(lambda e, cur=cur, nxt=nxt, ln=ln, n_el=n_el: e.tensor_tensor(
                            out=nxt[:, 0:n_el], in0=cur[:, 0:n_el], in1=cur[:, ln:ln + n_el], op=ALU.add),
                          [curres], [nres])
                        cur, curres = nxt, nres
                        ln *= 2
                    for (off, L, toff) in segs:
                        V(lambda e, cur=cur, off=off, L=L, toff=toff: e.scalar_tensor_tensor(
                            out=dT[:, toff:toff + L], in0=cur[:, off - half:off - half + L], scalar=1.0 / wlen,
                            in1=u[:, off:off + L], op0=ALU.mult, op1=ALU.subtract), [curres, ur], [dr])
                        for side in range(2):
                            ne = half if side == 0 else half - 1
                            if ne == 0:
                                continue
                            t0 = 0 if side == 0 else L - half + 1
                            ec = (g * 2 + side) * 8
                            V(lambda e, cur=cur, off=off, t0=t0, ne=ne, ec=ec: e.tensor_tensor(
                                out=etmp[:, 0:ne], in0=cur[:, off - half + t0:off - half + t0 + ne],
                                in1=pedge[:, ec:ec + ne], op=ALU.mult), [curres, "pedge"], ["etmp"])
                            V(lambda e, off=off, t0=t0, ne=ne, toff=toff: e.tensor_tensor(
                                out=dT[:, toff + t0:toff + t0 + ne], in0=etmp[:, 0:ne],
                                in1=u[:, off + t0:off + t0 + ne], op=ALU.subtract), ["etmp", ur], [dr])
                    for w in range(5):
                        nt = 512 if w < 4 else 256
                        tok0 = w * 512
                        pbk = w % 2
                        cs = (pc + w) % 2
                        PE(lambda e, dT=dT, nt=nt, tok0=tok0, pbk=pbk, g=g: e.matmul(
                            bank(pbk)[:, 0:nt], lhsT=pwt[:, g, :], rhs=dT[:, tok0:tok0 + nt], start=True, stop=True),
                           ["pwt", dr], ["ps%d" % pbk])
                        A(lambda e, nt=nt, pbk=pbk, cs=cs, g=g: e.activation(
                            out=cst[cs][:, 0:nt], in_=bank(pbk)[:, 0:nt], func=AF.Copy, scale=psc[:, g:g + 1]),
                          ["ps%d" % pbk, "psc"], ["cst%d" % cs])
                        DMA(STQ, CATP[b, g * 128:(g + 1) * 128, tok0:tok0 + nt], cst[cs][:, 0:nt], ["cst%d" % cs],
                            wpart(("CATP", b)))

            chk(2.05)
            S.barrier()
            arena.reset()
            alloc_norm_bufs()
            ctxb["mtmp"] = [arena.alloc([128, D], F32) for _ in range(2)]
            wout = arena.alloc([128, 8, D], BF16)
            qt = arena.alloc([128, 8, TS], BF16)
            vx = arena.alloc([128, 18, 130], BF16)
            am = arena.alloc([128, 2, 512], BF16)
            esb = arena.alloc([128, 8], F32)
            PT = [arena.alloc([128, 512], BF16) for _ in range(10)]
            atok = [arena.alloc([128, 512], BF16) for _ in range(2)]
            catT = [arena.alloc([128, 8, 128], BF16) for _ in range(2)]
            den = [arena.alloc([128, 8], F32) for _ in range(2)]
            gbx = arena.alloc([128, D], F32)
            gbc = arena.alloc([128, D], F32)
            load_w(wout, attn_out_w, 8, D, "wout")
            for m_ in range(2):
                DMA("pool", am[:, m_, :], amask_in[m_], [], ["am"])
            DMA("sp", esb, attn_sink.partition_broadcast(128), [], ["esb"])
            A(lambda e: e.activation(out=esb, in_=esb, func=AF.Exp), ["esb"], ["esb"])
            import os
            if os.environ.get("SWAP"):
                load_gb(0, 0, 0, gbx, "gbx")
            load_gb(0, 0, 2, gbc, "gbc")
            chk(2.1)
            ac = {"pt": 0, "blk": 0}
            for b in range(NB):
                import os
                SK = os.environ.get("SKIP", "")
                if "q" not in SK:
                    dma3("sp", lambda j: qt[:, j, :], lambda j: QT0[b, j * 128:(j + 1) * 128, :], 8, rparts(("QT0", b)), ["qt"])
                if "v" not in SK:
                    dma3("sp", lambda j: vx[:, j, :], lambda j: V0[b, j * 128:(j + 1) * 128, :], 18, rparts(("V0", b)), ["vx"])
                if "g" not in SK:
                    load_gb(0, 0, b, gbx, "gbx")
                chk(2.2)
                for n in range(18):
                    isx = n < 16
                    bs = ac["blk"] % 2
                    ac["blk"] += 1
                    at_, atres = atok[bs], "atok%d" % bs
                    cT_, cres = catT[bs], "catT%d" % bs
                    dn, dres = den[bs], "den%d" % bs
                    if isx:
                        chunks = []
                        if n > 0:
                            chunks.append((n - 1, 0))
                        chunks.append((n, None))
                        if n < 15:
                            chunks.append((n + 1, 1))
                        chunks += [(16, None), (17, None)]
                    else:
                        chunks = [(16, None), (17, None)]
                    if "c" in SK:
                        V(lambda e: e.memset(cT_[:, 4:8, :], 0.0), [], [cres])
                    else:
                        dma3("sp", lambda j: cT_[:, 4 + j, :], lambda j: CATP[b, j * 128:(j + 1) * 128, n * 128:(n + 1) * 128], 4,
                             rparts(("CATP", b)), [cres])
                    for h in range(2):
                        pts = []
                        for (kc, mk) in chunks:
                            pi = ac["pt"] % 10
                            ac["pt"] += 1
                            sbk = pi % 2
                            pts.append((pi, kc))
                            for g in range(4):
                                s_ = g % 2
                                c_ = 2 * h + g // 2
                                PE(lambda e, kc=kc, g=g, s_=s_, c_=c_, sbk=sbk: e.matmul(
                                    bank(sbk)[:, g * 128:(g + 1) * 128],
                                    lhsT=qt[:, 4 + 2 * h + s_, kc * 128:(kc + 1) * 128],
                                    rhs=qt[:, c_, n * 128:(n + 1) * 128],
                                    start=True, stop=True), ["qt"], ["ps%d" % sbk])
                            A(lambda e, pi=pi, sbk=sbk: e.activation(out=PT[pi][:], in_=bank(sbk), func=AF.Exp,
                                                                   scale=0.125), ["ps%d" % sbk], ["PT%d" % pi])
                            if mk is not None:
                                V(lambda e, pi=pi, mk=mk: e.tensor_tensor(out=PT[pi][:], in0=PT[pi][:],
                                                                           in1=am[:, mk, :], op=ALU.mult),
                                  ["PT%d" % pi, "am"], ["PT%d" % pi])
                        chk(2.3)
                        ob = 2 + h
                        ov = bank(ob)[:, 0:260].rearrange("p (g d) -> p g d", d=65)
                        for g in range(4):
                            for ci, (pi, kc) in enumerate(pts):
                                PE(lambda e, g=g, pi=pi, kc=kc, ci=ci, ob=ob: e.matmul(
                                    bank(ob)[:, g * 65:(g + 1) * 65], lhsT=PT[pi][:, g * 128:(g + 1) * 128],
                                    rhs=vx[:, kc, h * 65:(h + 1) * 65], start=(ci == 0), stop=(ci == len(pts) - 1)),
                                   ["PT%d" % pi, "vx"], ["ps%d" % ob])
                        chk(2.5)
                        V(lambda e, ov=ov, dn=dn, h=h: e.tensor_tensor(out=dn[:, h * 4:(h + 1) * 4], in0=ov[:, :, 64],
                                                                       in1=esb[:, h * 4:(h + 1) * 4], op=ALU.add),
                          ["ps%d" % ob, "esb"], [dres])
                        V(lambda e, dn=dn, h=h: e.reciprocal(out=dn[:, h * 4:(h + 1) * 4], in_=dn[:, h * 4:(h + 1) * 4]),
                          [dres], [dres])
                        V(lambda e, ov=ov, dn=dn, h=h, at_=at_: e.tensor_tensor(
                            out=at_[:, h * 256:(h + 1) * 256].rearrange("p (g d) -> p g d", d=64), in0=ov[:, :, 0:64],
                            in1=dn[:, h * 4:(h + 1) * 4].unsqueeze(2).to_broadcast([128, 4, 64]), op=ALU.mult),
                          ["ps%d" % ob, dres], [atres])
                    chk(2.6)
                    ptv = bank_bf(4).rearrange("p (k t) -> p k t", t=128)
                    for c_ in range(4):
                        PE(lambda e, c_=c_, at_=at_: e.transpose(out=ptv[:, c_, :], in_=at_[:, c_ * 128:(c_ + 1) * 128],
                                                                identity=identb[:]), [atres, "identb"], ["ps4"])
                    A(lambda e, cT_=cT_: e.activation(out=cT_[:, 0:4, :], in_=ptv[:, 0:4, :], func=AF.Copy),
                      ["ps4"], [cres])
                    chk(2.7)
                    mix = PS2[3]
                    for hf in range(2):
                        for k in range(8):
                            PE(lambda e, k=k, hf=hf, cT_=cT_: e.matmul(mix[:, hf * 512:(hf + 1) * 512], lhsT=cT_[:, k, :],
                                                                      rhs=wout[:, k, hf * 512:(hf + 1) * 512],
                                                                      start=(k == 0), stop=(k == 7)),
                               [cres, "wout"], ["ps%d" % (6 + hf)])
                    chk(2.8)
                    if isx:
                        residual_update(mix[:, :], ["ps6", "ps7"], x_in[b], [], XA[b], [("XA", b, n)], n * 128, 128,
                                        gbx, "gbx")
                    else:
                        residual_update(mix[:, :], ["ps6", "ps7"], ctx_in[b], [], CA[b], [("CA", b, n - 16)],
                                        (n - 16) * 128, 128, gbc, "gbc")

            def ffn_layer(l, segs_fn, final):
                chk(4 + 5 * l)
                S.barrier()
                arena.reset()
                alloc_norm_bufs()
                wup = arena.alloc([128, 8, 2 * DFF], BF16)
                cw = arena.alloc([128, 3, 44], F32)
                cb = arena.alloc([128, 44], F32)
                hTf = [arena.alloc([128, 8, 512], BF16) for _ in range(2)]
                ta = [arena.alloc([128, 512], F32) for _ in range(3)]
                tb = [arena.alloc([128, 512], F32) for _ in range(3)]
                sg = [arena.alloc([128, 512], F32) for _ in range(3)]
                tv = [arena.alloc([128, 512], F32) for _ in range(3)]
                tw = [arena.alloc([128, 512], F32) for _ in range(3)]
                gst = [arena.alloc([128, 512], BF16) for _ in range(3)]
                load_wb(wup, ffn_up_w[l], 8, [(0, 1408, 0), (2816, 1408, 2), (1408, 1408, 1), (4224, 1408, 3)], "wup")
                for j in range(3):
                    DMA("sp", cw[:, j, :], ffn_conv_w[l, j].rearrange("(c p) -> p c", p=128), [], ["cw"])
                DMA("sp", cb, ffn_conv_b[l].rearrange("(c p) -> p c", p=128), [], ["cb"])
                fc = {"w": 0, "c": 0, "g": 0}
                for b in range(NB):
                    for (src, sname, L, toff, row) in segs_fn(b):
                        wins = []
                        o = 0
                        while o < L:
                            r0 = max(o - 1, 0)
                            c0 = 1 if o == 0 else 0
                            nr = min(512 - c0, L - r0)
                            rz = (r0 + nr == L)
                            ncols = c0 + nr + (1 if rz else 0)
                            if ncols > 512:
                                nr -= 1
                                rz = False
                                ncols = 512
                            nout = ncols - 2
                            wins.append((r0, nr, c0, rz, ncols, o, nout))
                            o += nout
                        for (r0, nr, c0, rz, ncols, o, nout) in wins:
                            hs = fc["w"] % 2
                            fc["w"] += 1
                            hT, hres = hTf[hs], "hTf%d" % hs
                            if c0 == 1:
                                V(lambda e, hT=hT: e.memset(hT[:, :, 0:1], 0.0), [], [hres])
                            if rz:
                                V(lambda e, hT=hT, cc=c0 + nr: e.memset(hT[:, :, cc:cc + 1], 0.0), [], [hres])
                            rr_ = r0
                            while rr_ < r0 + nr:
                                n_ = min(128, r0 + nr - rr_)
                                make_hT(src, xres_fn(sname, b), rr_, n_, A2[l][:, row, :], SH2[l][:, row, :],
                                        "A2_%d" % l, hT, hres, c0 + rr_ - r0)
                                rr_ += n_
                            for c in range(22):
                                s2 = fc["c"] % 3
                                fc["c"] += 1
                                pg, pv = (s2 * 2), (s2 * 2 + 1)
                                for (jc, pbk) in ((c, pg), (22 + c, pv)):
                                    for k in range(8):
                                        PE(lambda e, k=k, jc=jc, pbk=pbk, hT=hT, ncols=ncols: e.matmul(
                                            bank(pbk)[:, 0:ncols], lhsT=wup[:, k, jc * 128:(jc + 1) * 128],
                                            rhs=hT[:, k, 0:ncols], start=(k == 0), stop=(k == 7)),
                                           [("wup", jc // 11), hres], ["ps%d" % pbk])
                                n = nout
                                A(lambda e, s2=s2, pg=pg, c=c, n=n: e.activation(
                                    out=ta[s2][:, 0:n], in_=bank(pg)[:, 1:1 + n], func=AF.Identity,
                                    scale=cw[:, 1, c:c + 1], bias=cb[:, c:c + 1]), ["ps%d" % pg, "cw", "cb"], ["ta%d" % s2])
                                V(lambda e, s2=s2, pg=pg, c=c, n=n: e.scalar_tensor_tensor(
                                    out=tb[s2][:, 0:n], in0=bank(pg)[:, 0:n], scalar=cw[:, 0, c:c + 1],
                                    in1=ta[s2][:, 0:n], op0=ALU.mult, op1=ALU.add), ["ps%d" % pg, "cw", "ta%d" % s2],
                                  ["tb%d" % s2])
                                V(lambda e, s2=s2, pg=pg, c=c, n=n: e.scalar_tensor_tensor(
                                    out=ta[s2][:, 0:n], in0=bank(pg)[:, 2:2 + n], scalar=cw[:, 2, c:c + 1],
                                    in1=tb[s2][:, 0:n], op0=ALU.mult, op1=ALU.add), ["ps%d" % pg, "cw", "tb%d" % s2],
                                  ["ta%d" % s2])
                                A(lambda e, s2=s2, n=n: e.activation(out=sg[s2][:, 0:n], in_=ta[s2][:, 0:n], func=AF.Silu),
                                  ["ta%d" % s2], ["sg%d" % s2])
                                cv = 22 + c
                                A(lambda e, s2=s2, pv=pv, cv=cv, n=n: e.activation(
                                    out=tv[s2][:, 0:n], in_=bank(pv)[:, 1:1 + n], func=AF.Identity,
                                    scale=cw[:, 1, cv:cv + 1], bias=cb[:, cv:cv + 1]), ["ps%d" % pv, "cw", "cb"],
                                  ["tv%d" % s2])
                                V(lambda e, s2=s2, pv=pv, cv=cv, n=n: e.scalar_tensor_tensor(
                                    out=tw[s2][:, 0:n], in0=bank(pv)[:, 0:n], scalar=cw[:, 0, cv:cv + 1],
                                    in1=tv[s2][:, 0:n], op0=ALU.mult, op1=ALU.add), ["ps%d" % pv, "cw", "tv%d" % s2],
                                  ["tw%d" % s2])
                                V(lambda e, s2=s2, pv=pv, cv=cv, n=n: e.scalar_tensor_tensor(
                                    out=tv[s2][:, 0:n], in0=bank(pv)[:, 2:2 + n], scalar=cw[:, 2, cv:cv + 1],
                                    in1=tw[s2][:, 0:n], op0=ALU.mult, op1=ALU.add), ["ps%d" % pv, "cw", "tw%d" % s2],
                                  ["tv%d" % s2])
                                gs = fc["g"] % 3
                                fc["g"] += 1
                                G(lambda e, s2=s2, gs=gs, n=n: e.tensor_tensor(out=gst[gs][:, 0:n], in0=sg[s2][:, 0:n],
                                                                               in1=tv[s2][:, 0:n], op=ALU.mult),
                                  ["sg%d" % s2, "tv%d" % s2], ["gst%d" % gs])
                                DMA(STQ, GT[b, c * 128:(c + 1) * 128, toff + o:toff + o + n], gst[gs][:, 0:n],
                                    ["gst%d" % gs], wpart(("GT", b, toff)))
                chk(5 + 5 * l)
                S.barrier()
                arena.reset()
                alloc_norm_bufs()
                ctxb["mtmp"] = [arena.alloc([128, D], F32) for _ in range(2)]
                wdn = arena.alloc([128, 22, D], BF16)
                gTw = [arena.alloc([128, 22, 512], BF16) for _ in range(3)]
                gb = [arena.alloc([128, D], F32) for _ in range(2)]
                for c_ in range(22):
                    DMA("pool", wdn[:, c_, :], ffn_down_w[l][c_ * 128:(c_ + 1) * 128, :], [], [("wdn", c_)])
                dc = {"w": 0, "m": 0}
                load_gb(l, 1, 2, gb[1], "gbf1")
                for b in range(NB):
                    load_gb(l, 1, b, gb[0], "gbf0")
                    for (src, sname, L, toff, row, dst, dname, is_out) in final(b):
                        o = 0
                        while o < L:
                            nt = min(512, L - o)
                            ws = dc["w"] % 3
                            dc["w"] += 1
                            dma3("sp", lambda j: gTw[ws][:, j, 0:nt],
                                 lambda j: GT[b, j * 128:(j + 1) * 128, toff + o:toff + o + nt], 22,
                                 rparts(("GT", b, toff)), ["gTw%d" % ws])
                            for i in range(nt // 128):
                                ms = dc["m"] % 3
                                dc["m"] += 1
                                mix = PS2[1 + ms]
                                for hf in range(2):
                                    for c in range(22):
                                        PE(lambda e, c=c, hf=hf, ws=ws, i=i, mix=mix: e.matmul(
                                            mix[:, hf * 512:(hf + 1) * 512], lhsT=gTw[ws][:, c, i * 128:(i + 1) * 128],
                                            rhs=wdn[:, c, hf * 512:(hf + 1) * 512], start=(c == 0), stop=(c == 21)),
                                           ["gTw%d" % ws, ("wdn", c)], ["ps%d" % (2 + 2 * ms + hf)])
                                r0 = o + i * 128
                                g_ = gb[0] if row < 2 else gb[1]
                                residual_update(mix[:, :], ["ps%d" % (2 + 2 * ms), "ps%d" % (3 + 2 * ms)], src,
                                                [(sname, b, r0 // 128)], dst, [(dname, b, r0 // 128)], r0, 128, g_,
                                                "gbf0" if row < 2 else "gbf1", is_output=is_out)
                            o += nt

            ffn_layer(0,
                      lambda b: [(XA[b], "XA", T, 0, b), (CA[b], "CA", CT, T, 2)],
                      lambda b: [(XA[b], "XA", T, 0, b, XB[b], "XB", False), (CA[b], "CA", CT, T, 2, CB[b], "CB", False)])

            UC1 = dscr("UC1", [NB, D, T], BF16)
            OG1 = dscr("OG1", [NB, D, T], BF16)
            QT1 = dscr("QT1", [NB, D, T], BF16)
            KT1 = dscr("KT1", [NB, D, TS], BF16)
            KK1 = dscr("KK1", [NB, TS, D], BF16)
            VV1 = dscr("VV1", [NB, TS, D], BF16)
            GG1 = dscr("GG1", [NB, TS, 16])
            SC1 = dscr("SC1", [NB, TS, 24])
            DC1 = dscr("DC1", [NB, 2 * 4 * 18])

            def make_windows(L):
                wins = []
                o = 0
                while o < L:
                    r0 = max(o - 1, 0)
                    c0 = 1 if o == 0 else 0
                    nr = min(512 - c0, L - r0)
                    rz = (r0 + nr == L)
                    ncols = c0 + nr + (1 if rz else 0)
                    if ncols > 512:
                        nr -= 1
                        rz = False
                        ncols = 512
                    nout = ncols - 2
                    wins.append((r0, nr, c0, rz, ncols, o, nout))
                    o += nout
                return wins

            chk(6)
            S.barrier()
            arena.reset()
            alloc_norm_bufs()
            win1 = arena.alloc([128, 8, 3088], BF16)
            qw = arena.alloc([128, 4, 2, 256], BF16)
            kw = arena.alloc([128, 4, 2, 256], BF16)
            cwr = arena.alloc([128, 3, 8], F32)
            cbr = arena.alloc([128, 8], F32)
            gbias = arena.alloc([128, 16], F32)
            hT1 = [arena.alloc([128, 8, 512], BF16) for _ in range(2)]
            ucw = [arena.alloc([128, 8, 512], BF16) for _ in range(2)]
            ta1 = [arena.alloc([128, 512], F32) for _ in range(2)]
            tb1 = [arena.alloc([128, 512], F32) for _ in range(2)]
            fst = [arena.alloc([128, 512], BF16) for _ in range(4)]
            tst = [arena.alloc([128, D], BF16) for _ in range(2)]
            gst1 = [arena.alloc([128, 16], F32) for _ in range(2)]
            load_wb(win1, rec_in_w, 8, [(0, 1024, "u"), (1024, 1024, "v"), (3072, 16, "g"), (2048, 1024, "o")], "win1")
            for h in range(4):
                for dc in range(2):
                    DMA("pool", qw[:, h, dc, :], rec_q_w[h, dc * 128:(dc + 1) * 128, :], [], ["qw"])
                    DMA("pool", kw[:, h, dc, :], rec_k_w[h, dc * 128:(dc + 1) * 128, :], [], ["kw"])
            for j in range(3):
                DMA("sp", cwr[:, j, :], rec_conv_w[j].rearrange("(c p) -> p c", p=128), [], ["cwr"])
            DMA("sp", cbr, rec_conv_b.rearrange("(c p) -> p c", p=128), [], ["cbr"])
            DMA("sp", gbias, rec_gate_b.partition_broadcast(128), [], ["gbias"])
            c1 = {"w": 0, "p": 0, "f": 0, "t": 0, "g": 0}

            def nbank():
                c1["p"] += 1
                return c1["p"] % 4

            for b in range(NB):
                for (src, sname, L, toff, row, isx) in ((CB[b], "CB", CT, 0, 2, False), (XB[b], "XB", T, CT, b, True)):
                    for (r0, nr, c0, rz, ncols, o, n) in make_windows(L):
                        ws = c1["w"] % 2
                        c1["w"] += 1
                        hT, hres = hT1[ws], "hT1_%d" % ws
                        uc_, ures = ucw[ws], "ucw%d" % ws
                        if c0 == 1:
                            V(lambda e: e.memset(hT[:, :, 0:1], 0.0), [], [hres])
                        if rz:
                            V(lambda e: e.memset(hT[:, :, c0 + nr:c0 + nr + 1], 0.0), [], [hres])
                        rr_ = r0
                        while rr_ < r0 + nr:
                            n_ = min(128, r0 + nr - rr_)
                            make_hT(src, xres_fn(sname, b), rr_, n_, A1[1][:, row, :], SH1[1][:, row, :], "A1_1",
                                    hT, hres, c0 + rr_ - r0)
                            rr_ += n_

                        def fm_proj(col0, pbk):
                            for k in range(8):
                                PE(lambda e: e.matmul(bank(pbk)[:, 0:ncols], lhsT=win1[:, k, col0:col0 + 128],
                                                      rhs=hT[:, k, 0:ncols], start=(k == 0), stop=(k == 7)),
                                   [("win1", "u" if col0 < 1024 else "o"), hres], ["ps%d" % pbk])

                        for j in range(8):
                            pbk = nbank()
                            s2 = j % 2
                            fm_proj(j * 128, pbk)
                            A(lambda e: e.activation(out=ta1[s2][:, 0:n], in_=bank(pbk)[:, 1:1 + n], func=AF.Identity,
                                                     scale=cwr[:, 1, j:j + 1], bias=cbr[:, j:j + 1]),
                              ["ps%d" % pbk, "cwr", "cbr"], ["ta1_%d" % s2])
                            V(lambda e: e.scalar_tensor_tensor(out=tb1[s2][:, 0:n], in0=bank(pbk)[:, 0:n],
                                                               scalar=cwr[:, 0, j:j + 1], in1=ta1[s2][:, 0:n],
                                                               op0=ALU.mult, op1=ALU.add),
                              ["ps%d" % pbk, "cwr", "ta1_%d" % s2], ["tb1_%d" % s2])
                            V(lambda e: e.scalar_tensor_tensor(out=ta1[s2][:, 0:n], in0=bank(pbk)[:, 2:2 + n],
                                                               scalar=cwr[:, 2, j:j + 1], in1=tb1[s2][:, 0:n],
                                                               op0=ALU.mult, op1=ALU.add),
                              ["ps%d" % pbk, "cwr", "tb1_%d" % s2], ["ta1_%d" % s2])
                            A(lambda e: e.activation(out=uc_[:, j, 0:n], in_=ta1[s2][:, 0:n], func=AF.Silu),
                              ["ta1_%d" % s2], [ures])
                            if isx:
                                DMA(STQ, UC1[b, j * 128:(j + 1) * 128, o:o + n], uc_[:, j, 0:n], [ures],
                                    wpart(("UC1", b)))
                        if isx:
                            for j in range(8):
                                pbk = nbank()
                                fs = c1["f"] % 4
                                c1["f"] += 1
                                fm_proj(2048 + j * 128, pbk)
                                A(lambda e: e.activation(out=fst[fs][:, 0:n], in_=bank(pbk)[:, 1:1 + n],
                                                         func=AF.Sigmoid), ["ps%d" % pbk], ["fst%d" % fs])
                                DMA(STQ, OG1[b, j * 128:(j + 1) * 128, o:o + n], fst[fs][:, 0:n], ["fst%d" % fs],
                                    wpart(("OG1", b)))
                        for h in range(4):
                            for ec in range(2):
                                for (wt_, wres_, dst, scl, need) in ((kw, "kw", KT1, 1.0 / 16, True),
                                                                     (qw, "qw", QT1, 1.0, isx)):
                                    if not need:
                                        continue
                                    pbk = nbank()
                                    fs = c1["f"] % 4
                                    c1["f"] += 1
                                    for dc in range(2):
                                        PE(lambda e: e.matmul(bank(pbk)[:, 0:n],
                                                              lhsT=wt_[:, h, dc, ec * 128:(ec + 1) * 128],
                                                              rhs=uc_[:, 2 * h + dc, 0:n], start=(dc == 0),
                                                              stop=(dc == 1)), [wres_, ures], ["ps%d" % pbk])
                                    A(lambda e: e.activation(out=fst[fs][:, 0:n], in_=bank(pbk)[:, 0:n], func=AF.Copy,
                                                             scale=scl), ["ps%d" % pbk], ["fst%d" % fs])
                                    tcol = (toff + o) if dst is KT1 else o
                                    DMA(STQ, dst[b, h * 256 + ec * 128:h * 256 + (ec + 1) * 128, tcol:tcol + n],
                                        fst[fs][:, 0:n], ["fst%d" % fs],
                                        wpart(("KT1", b)) if dst is KT1 else wpart(("QT1", b)))
                        i0 = 0
                        while i0 < n:
                            m = min(128, n - i0)
                            trow = toff + o + i0
                            ts_ = c1["t"] % 2
                            c1["t"] += 1
                            mix = PS2[2]
                            for h in range(4):
                                for dc in range(2):
                                    PE(lambda e: e.matmul(mix[0:m, h * 256:(h + 1) * 256],
                                                          lhsT=uc_[:, 2 * h + dc, i0:i0 + m], rhs=kw[:, h, dc, :],
                                                          start=(dc == 0), stop=(dc == 1)),
                                       [ures, "kw"], ["ps4", "ps5"])
                            A(lambda e: e.activation(out=tst[ts_][0:m, :], in_=mix[0:m, :], func=AF.Copy,
                                                     scale=1.0 / 16), ["ps4", "ps5"], ["tst%d" % ts_])
                            DMA(STQ, KK1[b, trow:trow + m, :], tst[ts_][0:m, :], ["tst%d" % ts_], wpart(("KK1", b)))
                            ts_ = c1["t"] % 2
                            c1["t"] += 1
                            mix = PS2[3]
                            for hf in range(2):
                                for k in range(8):
                                    PE(lambda e: e.matmul(mix[0:m, hf * 512:(hf + 1) * 512],
                                                          lhsT=hT[:, k, 1 + i0:1 + i0 + m],
                                                          rhs=win1[:, k, 1024 + hf * 512:1024 + (hf + 1) * 512],
                                                          start=(k == 0), stop=(k == 7)),
                                       [hres, ("win1", "v")], ["ps%d" % (6 + hf)])
                            A(lambda e: e.activation(out=tst[ts_][0:m, :], in_=mix[0:m, :], func=AF.Copy),
                              ["ps6", "ps7"], ["tst%d" % ts_])
                            DMA(STQ, VV1[b, trow:trow + m, :], tst[ts_][0:m, :], ["tst%d" % ts_], wpart(("VV1", b)))
                            gs = c1["g"] % 2
                            c1["g"] += 1
                            pbk = nbank()
                            for k in range(8):
                                PE(lambda e: e.matmul(bank(pbk)[0:m, 0:16], lhsT=hT[:, k, 1 + i0:1 + i0 + m],
                                                      rhs=win1[:, k, 3072:3088], start=(k == 0), stop=(k == 7)),
                                   [hres, ("win1", "g")], ["ps%d" % pbk])
                            V(lambda e: e.tensor_tensor(out=gst1[gs][0:m, :], in0=bank(pbk)[0:m, 0:16],
                                                        in1=gbias[0:m, :], op=ALU.add),
                              ["ps%d" % pbk, "gbias"], ["gst1_%d" % gs])
                            DMA(STQ, GG1[b, trow:trow + m, :], gst1[gs][0:m, :], ["gst1_%d" % gs], wpart(("GG1", b)))
                            i0 += m

            chk(7)
            S.barrier()
            arena.reset()
            gg = arena.alloc([128, 18, 16], F32)
            ones4 = arena.alloc([4, TS], F32)
            IG = [arena.alloc([4, TS], F32) for _ in range(2)]
            FG = [arena.alloc([4, TS], F32) for _ in range(2)]
            t_a = arena.alloc([4, TS], F32)
            t_b = arena.alloc([4, TS], F32)
            Bc = arena.alloc([4, TS], F32)
            Mc = arena.alloc([4, TS], F32)
            QY = [[arena.alloc([4, TS], F32) for _ in range(3)] for _ in range(2)]
            Mpv = arena.alloc([4, 18], F32)
            dcy = arena.alloc([4, 18], F32)
            scs = arena.alloc([128, 18, 24], F32)
            V(lambda e: e.memset(ones4, 1.0), [], ["ones4"])

            def lb_tile(q):
                return q + 2 if q < 16 else q - 16

            for b in range(NB):
                dma3("sp", lambda j: gg[:, j, :], lambda j: GG1[b, j * 128:(j + 1) * 128, :], 18, rparts(("GG1", b)), ["gg"])
                for dr in range(2):
                    for ti, dstt, dres in ((0, IG[dr], "IG%d" % dr), (1, FG[dr], "FG%d" % dr)):
                        ty = dr * 2 + ti
                        for q in range(18):
                            tl = q if dr == 0 else lb_tile(q)
                            pq = PS2[q // 8]
                            PE(lambda e: e.matmul(pq[0:4, (q % 8) * 128:(q % 8 + 1) * 128],
                                                  lhsT=gg[:, tl, ty * 4:(ty + 1) * 4], rhs=ident[:, :], start=True,
                                                  stop=True), ["gg", "ident"], ["ps%d" % (2 * (q // 8) + (q % 8) // 4)])
                        for pi in range(3):
                            ncol = 1024 if pi < 2 else 256
                            A(lambda e: e.activation(out=dstt[:, pi * 1024:pi * 1024 + ncol], in_=PS2[pi][0:4, 0:ncol],
                                                     func=AF.Copy), ["ps%d" % (2 * pi), "ps%d" % (2 * pi + 1)], [dres])
                for dr in range(2):
                    ig, fg = IG[dr], FG[dr]
                    igr, fgr = "IG%d" % dr, "FG%d" % dr

                    def dview(ap):
                        return ap if dr == 0 else ap[:, ::-1]

                    A(lambda e: e.activation(out=t_a, in_=fg, func=AF.Abs), [fgr], ["t_a"])
                    A(lambda e: e.activation(out=t_a, in_=t_a, func=AF.Exp, scale=-1.0), ["t_a"], ["t_a"])
                    A(lambda e: e.activation(out=t_a, in_=t_a, func=AF.Ln, bias=1.0), ["t_a"], ["t_a"])
                    V(lambda e: e.tensor_scalar(out=t_b, in0=fg, scalar1=0.0, scalar2=None, op0=ALU.min), [fgr], ["t_b"])
                    V(lambda e: e.tensor_tensor(out=t_b, in0=t_b, in1=t_a, op=ALU.subtract), ["t_a", "t_b"], ["t_b"])
                    V(lambda e: e.tensor_tensor_scan(out=dview(Bc), data0=dview(ones4), data1=dview(t_b), initial=0.0,
                                                     op0=ALU.mult, op1=ALU.add), ["ones4", "t_b"], ["Bc"])
                    V(lambda e: e.tensor_tensor(out=t_a, in0=ig, in1=Bc, op=ALU.subtract), [igr, "Bc"], ["t_a"])
                    V(lambda e: e.tensor_tensor_scan(out=dview(Mc), data0=dview(ones4), data1=dview(t_a), initial=0.0,
                                                     op0=ALU.mult, op1=ALU.max), ["ones4", "t_a"], ["Mc"])
                    M3 = Mc.rearrange("p (q t) -> p q t", t=128)
                    a3 = t_a.rearrange("p (q t) -> p q t", t=128)
                    B3 = Bc.rearrange("p (q t) -> p q t", t=128)
                    V(lambda e: e.memset(Mpv, 0.0), [], ["Mpv"])
                    if dr == 0:
                        V(lambda e: e.tensor_copy(out=Mpv[:, 1:18], in_=M3[:, 0:17, 127]), ["Mc"], ["Mpv"])
                        Mend = M3[:, :, 127]
                    else:
                        V(lambda e: e.tensor_copy(out=Mpv[:, 0:17], in_=M3[:, 1:18, 0]), ["Mc"], ["Mpv"])
                        Mend = M3[:, :, 0]
                    Mpb = Mpv.unsqueeze(2).to_broadcast([4, 18, 128])
                    q0, q1, q2 = QY[dr]
                    qr = ["QY%d_%d" % (dr, i) for i in range(3)]
                    V(lambda e: e.tensor_tensor(out=q0.rearrange("p (q t) -> p q t", t=128), in0=a3, in1=Mpb,
                                                op=ALU.subtract), ["t_a", "Mpv"], [qr[0]])
                    A(lambda e: e.activation(out=q0, in_=q0, func=AF.Exp), [qr[0]], [qr[0]])
                    V(lambda e: e.tensor_tensor(out=q1.rearrange("p (q t) -> p q t", t=128), in0=a3,
                                                in1=Mend.unsqueeze(2).to_broadcast([4, 18, 128]), op=ALU.subtract),
                      ["t_a", "Mc"], [qr[1]])
                    A(lambda e: e.activation(out=q1, in_=q1, func=AF.Exp), [qr[1]], [qr[1]])
                    V(lambda e: e.scalar_tensor_tensor(out=q2.rearrange("p (q t) -> p q t", t=128), in0=B3, scalar=-1.0,
                                                       in1=Mpb, op0=ALU.mult, op1=ALU.subtract), ["Bc", "Mpv"], [qr[2]])
                    A(lambda e: e.activation(out=q2, in_=q2, func=AF.Exp), [qr[2]], [qr[2]])
                    V(lambda e: e.tensor_tensor(out=dcy, in0=Mpv, in1=Mend, op=ALU.subtract), ["Mpv", "Mc"], ["dcy"])
                    A(lambda e: e.activation(out=dcy, in_=dcy, func=AF.Exp), ["dcy"], ["dcy"])
                    dcv = DC1[b].rearrange("(d h q) -> d h q", d=2, h=4)[dr]
                    if dr == 0:
                        DMA(STQ, dcv, dcy, ["dcy"], wpart(("DC1", b)))
                    else:
                        DMA(STQ, dcv[:, 2:18], dcy[:, 0:16], ["dcy"], wpart(("DC1", b)))
                        DMA(STQ, dcv[:, 0:2], dcy[:, 16:18], ["dcy"], wpart(("DC1", b)))
                    pst = bank(6)
                    for q in range(18):
                        tl = q if dr == 0 else lb_tile(q)
                        for qi in range(3):
                            col = tl * 24 + (dr * 3 + qi) * 4
                            PE(lambda e: e.matmul(pst[:, col:col + 4], lhsT=QY[dr][qi][:, q * 128:(q + 1) * 128],
                                                  rhs=ident[0:4, 0:4], start=True, stop=True), [qr[qi], "ident"], ["ps6"])
                A(lambda e: e.activation(out=scs, in_=bank(6)[:, 0:432].rearrange("p (n c) -> p n c", c=24), func=AF.Copy),
                  ["ps6"], ["scs"])
                dma3(STQ, lambda j: SC1[b, j * 128:(j + 1) * 128, :], lambda j: scs[:, j, :], 18, ["scs"], wpart(("SC1", b)))

            chk(8)
            S.barrier()
            arena.reset()
            alloc_norm_bufs()
            ctxb["mtmp"] = [arena.alloc([128, D], F32) for _ in range(2)]
            wo1 = arena.alloc([128, 8, D], BF16)
            QTh = arena.alloc([128, 2, T], BF16)
            KTh = arena.alloc([128, 2, TS], BF16)
            KKh = arena.alloc([128, 18, 256], BF16)
            VVh = arena.alloc([128, 18, 257], BF16)
            ogh = arena.alloc([128, 2, T], BF16)
            uch = arena.alloc([128, 2, T], BF16)
            hnT = arena.alloc([128, 2, T], BF16)
            tyy = arena.alloc([128, T], F32)
            scb = arena.alloc([128, 18, 24], F32)
            dcb = arena.alloc([128, 144], F32)
            lmk = arena.alloc([128, 2, 128], F32)
            Sst = [arena.alloc([128, 2, 257], F32) for _ in range(2)]
            Sbf = [arena.alloc([128, 2, 257], BF16) for _ in range(2)]
            hs = arena.alloc([128, 16, 256], F32)
            ATb = [arena.alloc([128, 128], BF16) for _ in range(4)]
            Ktl = [arena.alloc([128, 256], BF16) for _ in range(4)]
            dnn = [arena.alloc([128, 2], F32) for _ in range(4)]
            hnb = [arena.alloc([128, 256], BF16) for _ in range(2)]
            yT = arena.alloc([128, 8, T], BF16)
            rng = arena.alloc([128, 8], F32)
            rsk = arena.alloc([128, 8], F32)
            gb1 = arena.alloc([128, D], F32)
            load_w(wo1, rec_out_w, 8, D, "wo1")
            for m_ in range(2):
                DMA("sp", lmk[:, m_, :], lmask_in[m_], [], ["lmk"])
            DMA("sp", rng, rec_norm_g.rearrange("(c p) -> p c", p=128), [], ["rng"])
            DMA("sp", rsk, rec_skip.rearrange("(c p) -> p c", p=128), [], ["rsk"])
            V(lambda e: e.memset(VVh[:, :, 256:257], 1.0), [], ["VVh"])
            c3 = {"i": 0}
            for b in range(NB):
                dma3("sp", lambda j: scb[:, j, :], lambda j: SC1[b, j * 128:(j + 1) * 128, :], 18, rparts(("SC1", b)), ["scb"])
                DMA("sp", dcb, DC1[b].partition_broadcast(128), rparts(("DC1", b)), ["dcb"])
                load_gb(1, 0, b, gb1, "gb1")
                for h in range(4):
                    dma3("sp", lambda j: QTh[:, j, :], lambda j: QT1[b, h * 256 + j * 128:h * 256 + (j + 1) * 128, :], 2,
                         rparts(("QT1", b)), ["QTh"])
                    dma3("sp", lambda j: KTh[:, j, :], lambda j: KT1[b, h * 256 + j * 128:h * 256 + (j + 1) * 128, :], 2,
                         rparts(("KT1", b)), ["KTh"])
                    dma3("sp", lambda j: KKh[:, j, :], lambda j: KK1[b, j * 128:(j + 1) * 128, h * 256:(h + 1) * 256], 18,
                         rparts(("KK1", b)), ["KKh"])
                    dma3("sp", lambda j: VVh[:, j, 0:256], lambda j: VV1[b, j * 128:(j + 1) * 128, h * 256:(h + 1) * 256], 18,
                         rparts(("VV1", b)), ["VVh"])
                    dma3("sp", lambda j: ogh[:, j, :], lambda j: OG1[b, h * 256 + j * 128:h * 256 + (j + 1) * 128, :], 2,
                         rparts(("OG1", b)), ["ogh"])
                    dma3("sp", lambda j: uch[:, j, :], lambda j: UC1[b, h * 256 + j * 128:h * 256 + (j + 1) * 128, :], 2,
                         rparts(("UC1", b)), ["uch"])
                    for dr in range(2):
                        V(lambda e: e.memset(Sst[dr], 0.0), [], ["Sst%d" % dr])
                        V(lambda e: e.memset(Sbf[dr], 0.0), [], ["Sbf%d" % dr])
                    order = [list(range(18)), [1, 0] + list(range(17, 1, -1))]
                    first_dir_done = set()
                    for step in range(18):
                        st_ = []
                        for dr in range(2):
                            tl = order[dr][step]
                            st_.append(dict(dr=dr, tl=tl, isx=tl >= 2, xt=tl - 2, pb0=dr * 4, bi=dr * 2 + step % 2,
                                            pslot=dr * 4 + step % 2, sres="Sst%d" % dr, bres="Sbf%d" % dr,
                                            colA=(dr * 3 + 0) * 4 + h, colB=(dr * 3 + 1) * 4 + h,
                                            colF=(dr * 3 + 2) * 4 + h, dcol=(dr * 4 + h) * 18 + tl))
                        for q_ in st_:
                            dr, tl, xt_, bi, pslot = q_["dr"], q_["tl"], q_["xt"], q_["bi"], q_["pslot"]
                            if q_["isx"]:
                                pA = bank(pslot)[:, 0:128]
                                for dc in range(2):
                                    PE(lambda e: e.matmul(pA[:, 0:128], lhsT=KTh[:, dc, tl * 128:(tl + 1) * 128],
                                                          rhs=QTh[:, dc, xt_ * 128:(xt_ + 1) * 128], start=(dc == 0),
                                                          stop=(dc == 1)), ["KTh", "QTh"], ["ps%d" % pslot])
                                V(lambda e: e.scalar_tensor_tensor(out=ATb[bi], in0=pA[:, 0:128],
                                                                   scalar=scb[:, tl, q_["colA"]:q_["colA"] + 1],
                                                                   in1=lmk[:, dr, :], op0=ALU.mult, op1=ALU.mult),
                                  ["ps%d" % pslot, "scb", "lmk"], ["ATb%d" % bi])
                            if step < 17:
                                A(lambda e: e.activation(out=Ktl[bi], in_=KKh[:, tl, :], func=AF.Copy,
                                                         scale=scb[:, tl, q_["colB"]:q_["colB"] + 1]),
                                  ["KKh", "scb"], ["Ktl%d" % bi])
                        if step < 17:
                            for q_ in st_:
                                tl, bi, pb0 = q_["tl"], q_["bi"], q_["pb0"]
                                for dc in range(2):
                                    pS = bank(pb0 + 2 + dc)
                                    PE(lambda e: e.matmul(pS[:, 0:257], lhsT=Ktl[bi][:, dc * 128:(dc + 1) * 128],
                                                          rhs=VVh[:, tl, :], start=True, stop=True),
                                       ["Ktl%d" % bi, "VVh"], ["ps%d" % (pb0 + 2 + dc)])
                        for q_ in st_:
                            dr, tl, xt_, bi, pslot = q_["dr"], q_["tl"], q_["xt"], q_["bi"], q_["pslot"]
                            if q_["isx"]:
                                pO = bank(pslot)[:, 128:512]
                                PE(lambda e: e.matmul(pO[:, 0:257], lhsT=ATb[bi], rhs=VVh[:, tl, :], start=True, stop=False),
                                   ["ATb%d" % bi, "VVh"], ["ps%d" % pslot])
                                for dc in range(2):
                                    PE(lambda e: e.matmul(pO[:, 0:257], lhsT=QTh[:, dc, xt_ * 128:(xt_ + 1) * 128],
                                                          rhs=Sbf[dr][:, dc, :], start=False, stop=(dc == 1)),
                                       ["QTh", q_["bres"]], ["ps%d" % pslot])
                        if step < 17:
                            for q_ in st_:
                                dr, pb0 = q_["dr"], q_["pb0"]
                                for dc in range(2):
                                    pS = bank(pb0 + 2 + dc)
                                    V(lambda e: e.scalar_tensor_tensor(out=Sst[dr][:, dc, :], in0=Sst[dr][:, dc, :],
                                                                       scalar=dcb[:, q_["dcol"]:q_["dcol"] + 1],
                                                                       in1=pS[:, 0:257], op0=ALU.mult, op1=ALU.add),
                                      [q_["sres"], "dcb", "ps%d" % (pb0 + 2 + dc)], [q_["sres"]])
                        for q_ in st_:
                            dr, tl, xt_, bi, pslot = q_["dr"], q_["tl"], q_["xt"], q_["bi"], q_["pslot"]
                            if q_["isx"]:
                                pO = bank(pslot)[:, 128:512]
                                dn = dnn[bi]
                                A(lambda e: e.activation(out=dn[:, 0:1], in_=pO[:, 256:257], func=AF.Abs),
                                  ["ps%d" % pslot], ["dnn%d" % bi])
                                V(lambda e: e.tensor_tensor(out=dn[:, 0:1], in0=dn[:, 0:1],
                                                            in1=scb[:, tl, q_["colF"]:q_["colF"] + 1], op=ALU.max),
                                  ["dnn%d" % bi, "scb"], ["dnn%d" % bi])
                                V(lambda e: e.reciprocal(out=dn[:, 1:2], in_=dn[:, 0:1]), ["dnn%d" % bi], ["dnn%d" % bi])
                                hres_ = ("hs", xt_)
                                if xt_ not in first_dir_done:
                                    first_dir_done.add(xt_)
                                    V(lambda e: e.tensor_scalar(out=hs[:, xt_, :], in0=pO[:, 0:256], scalar1=dn[:, 1:2],
                                                                scalar2=None, op0=ALU.mult),
                                      ["ps%d" % pslot, "dnn%d" % bi], [hres_])
                                else:
                                    V(lambda e: e.scalar_tensor_tensor(out=hs[:, xt_, :], in0=pO[:, 0:256],
                                                                       scalar=dn[:, 1:2], in1=hs[:, xt_, :],
                                                                       op0=ALU.mult, op1=ALU.add),
                                      ["ps%d" % pslot, "dnn%d" % bi, hres_], [hres_])
                        if step < 17:
                            for q_ in st_:
                                dr = q_["dr"]
                                A(lambda e: e.activation(out=Sbf[dr], in_=Sst[dr], func=AF.Copy), [q_["sres"]], [q_["bres"]])
                    for xt_ in range(16):
                        ci = xt_ % 2
                        stt = ctxb["st"][xt_ % 3]
                        sres_ = "st%d" % (xt_ % 3)
                        junk = ctxb["junk"]
                        A(lambda e: e.activation(out=junk[:, 0:256], in_=hs[:, xt_, :], func=AF.Square,
                                                 accum_out=stt[:, 0:1]), [("hs", xt_)], ["junk", sres_])
                        A(lambda e: e.activation(out=stt[:, 1:2], in_=stt[:, 0:1], func=AF.Sqrt, scale=1.0 / 256,
                                                 bias=epsb[:, :]), [sres_, "epsb"], [sres_])
                        V(lambda e: e.reciprocal(out=stt[:, 1:2], in_=stt[:, 1:2]), [sres_], [sres_])
                        V(lambda e: e.tensor_scalar(out=hnb[ci], in0=hs[:, xt_, :], scalar1=stt[:, 1:2], scalar2=None,
                                                    op0=ALU.mult), [("hs", xt_), sres_], ["hnb%d" % ci])
                        ptv = bank_bf(6 + ci).rearrange("p (k t) -> p k t", t=128)
                        for dc in range(2):
                            PE(lambda e: e.transpose(out=ptv[:, dc, :], in_=hnb[ci][:, dc * 128:(dc + 1) * 128],
                                                     identity=identb[:]), ["hnb%d" % ci, "identb"], ["ps%d" % (6 + ci)])
                        A(lambda e: e.activation(out=hnT[:, :, xt_ * 128:(xt_ + 1) * 128], in_=ptv[:, 0:2, :],
                                                 func=AF.Copy), ["ps%d" % (6 + ci)], ["hnT"])
                    for dc in range(2):
                        fcx = 2 * h + dc
                        V(lambda e: e.tensor_scalar(out=tyy, in0=hnT[:, dc, :], scalar1=rng[:, fcx:fcx + 1], scalar2=None,
                                                    op0=ALU.mult), ["hnT", "rng"], ["tyy"])
                        V(lambda e: e.scalar_tensor_tensor(out=tyy, in0=uch[:, dc, :], scalar=rsk[:, fcx:fcx + 1], in1=tyy,
                                                           op0=ALU.mult, op1=ALU.add), ["uch", "rsk", "tyy"], ["tyy"])
                        V(lambda e: e.tensor_tensor(out=yT[:, fcx, :], in0=tyy, in1=ogh[:, dc, :], op=ALU.mult),
                          ["tyy", "ogh"], [("yT", fcx)])
                for n in range(16):
                    mix = PS2[n % 2]
                    for hf in range(2):
                        for k in range(8):
                            PE(lambda e: e.matmul(mix[:, hf * 512:(hf + 1) * 512], lhsT=yT[:, k, n * 128:(n + 1) * 128],
                                                  rhs=wo1[:, k, hf * 512:(hf + 1) * 512], start=(k == 0), stop=(k == 7)),
                               [("yT", k), "wo1"], ["ps%d" % (2 * (n % 2) + hf)])
                    residual_update(mix[:, :], ["ps%d" % (2 * (n % 2)), "ps%d" % (2 * (n % 2) + 1)], XB[b],
                                    [("XB", b, n)], XA[b], [("XA", b, n)], n * 128, 128, gb1, "gb1")

            ffn_layer(1,
                      lambda b: [(XA[b], "XA", T, 0, b)],
                      lambda b: [(XA[b], "XA", T, 0, b, out[b], "out", True)])

        except _Stop:
            S.barrier()
        S.emit(st)
        build_program.stats = S.stats
    return nc


def _consts():
    ident = np.eye(128, dtype=np.float32)
    inv = (10000.0 ** (-np.arange(16, dtype=np.float32) / 16)).astype(np.float32)
    t = np.arange(T)
    row = (t // 64).astype(np.float32)
    col = (t % 64).astype(np.float32)
    rc = np.zeros((128, T), np.float32)
    rs = np.zeros((128, T), np.float32)
    for p in range(128):
        d = p % 64
        axis, half, f = d // 32, (d % 32) // 16, d % 16
        pos = row if axis == 0 else col
        ang = (pos * inv[f]).astype(np.float32)
        rc[p] = np.cos(ang)
        rs[p] = np.sin(ang) * (-1.0 if half == 0 else 1.0)
    j = np.arange(128)[:, None]
    i = np.arange(128)[None, :]
    prev = (j >= i).astype(np.float32)
    nxt = (j <= i).astype(np.float32)
    amask = np.stack([np.tile(prev, (1, 4)), np.tile(nxt, (1, 4))]).astype(np.float32)
    pe = np.zeros((4, 2, 8), np.float32)
    for g in range(4):
        w = 2 ** (g + 1)
        half = w // 2
        for k in range(half):
            pe[g, 0, k] = 1.0 / (k + half)
        for k in range(half - 1):
            pe[g, 1, k] = 1.0 / (2 * half - 1 - k)
    pool_edge = np.tile(pe.reshape(1, 64), (128, 1)).astype(np.float32)
    lmask = np.stack([(j <= i).astype(np.float32), (j >= i).astype(np.float32)])
    return dict(ident=ident, rope_c=rc, rope_s=rs, amask=amask, pool_edge=pool_edge, lmask=lmask)


def _perm_head():
    p = np.zeros(64, np.int64)
    for d in range(64):
        axis, half, f = d // 32, (d % 32) // 16, d % 16
        p[d] = axis * 32 + (1 - half) * 16 + f
    return p


def _prep_shared(inp):
    w = np.asarray(inp["attn_in_w"][0], np.float32)
    ph = _perm_head()
    wz = np.concatenate([w, np.zeros((w.shape[0], 1), np.float32)], axis=1)
    Z = np.full(64, w.shape[1], np.int64)
    q = [np.arange(c * 128, (c + 1) * 128) for c in range(4)]
    k0 = 512 + np.arange(64)
    k1 = 576 + np.arange(64)
    kz = [np.concatenate([k0, Z]), np.concatenate([Z, k0]), np.concatenate([k1, Z]), np.concatenate([Z, k1])]
    base = q + kz

    def partner(idx):
        o = idx.copy()
        for h0 in range(0, 128, 64):
            blk = idx[h0:h0 + 64]
            o[h0:h0 + 64] = blk[ph]
        return o

    cols = base + [partner(c) for c in base] + [np.arange(640, 768)] + [np.arange(768 + g * 128, 896 + g * 128) for g in range(4)]
    cols = np.concatenate(cols)
    w = wz
    sh = dict(
        mod_w=np.ascontiguousarray(inp["mod_w"], np.float32),
        mod_b=np.ascontiguousarray(inp["mod_b"], np.float32),
        norm_g=np.ascontiguousarray(inp["norm_g"], np.float32),
        w_in0=np.ascontiguousarray(w[:, cols]),
        attn_sink=np.ascontiguousarray(inp["attn_sink"][0], np.float32),
        pool_w=np.ascontiguousarray(inp["pool_w"][0], np.float32),
        pool_scale=np.ascontiguousarray(inp["pool_scale"][0], np.float32),
        attn_out_w=np.ascontiguousarray(inp["attn_out_w"][0], np.float32),
        rec_in_w=np.ascontiguousarray(inp["rec_in_w"][0], np.float32),
        rec_gate_b=np.ascontiguousarray(inp["rec_gate_b"][0].reshape(16), np.float32),
        rec_conv_w=np.ascontiguousarray(inp["rec_conv_w"][0], np.float32),
        rec_conv_b=np.ascontiguousarray(inp["rec_conv_b"][0], np.float32),
        rec_q_w=np.ascontiguousarray(inp["rec_q_w"][0], np.float32),
        rec_k_w=np.ascontiguousarray(inp["rec_k_w"][0], np.float32),
        rec_norm_g=np.ascontiguousarray(inp["rec_norm_g"][0], np.float32),
        rec_skip=np.ascontiguousarray(inp["rec_skip"][0], np.float32),
        rec_out_w=np.ascontiguousarray(inp["rec_out_w"][0], np.float32),
        ffn_up_w=np.ascontiguousarray(inp["ffn_up_w"], np.float32),
        ffn_conv_w=np.ascontiguousarray(inp["ffn_conv_w"], np.float32),
        ffn_conv_b=np.ascontiguousarray(inp["ffn_conv_b"], np.float32),
        ffn_down_w=np.ascontiguousarray(inp["ffn_down_w"], np.float32),
    )
    sh.update(_consts())
    return sh


def make_in_maps(inp, cores):
    sh = _prep_shared(inp)
    x = np.asarray(inp["x"], np.float32)
    c = np.asarray(inp["c"], np.float32)
    ctx = np.asarray(inp["ctx"], np.float32)
    c_ctx = np.asarray(inp["c_ctx"], np.float32)
    maps = []
    for i in cores:
        m = dict(sh)
        m["x"] = np.ascontiguousarray(x[NB * i:NB * (i + 1)])
        m["ctx"] = np.ascontiguousarray(ctx[NB * i:NB * (i + 1)])
        m["cvec"] = np.ascontiguousarray(np.concatenate([c[NB * i:NB * (i + 1)], c_ctx[None, :]], axis=0))
        maps.append(m)
    return maps


def kernel(**inputs):
    nc = build_program()
    maps = make_in_maps(inputs, list(range(8)))
    res = run_bass_kernel_spmd(nc, maps, core_ids=list(range(8)))
    return np.concatenate([np.asarray(r["out"], np.float32) for r in res.results], axis=0)
```

```python
import math
from contextlib import ExitStack

import numpy as np
import concourse.bass as bass
import concourse.mybir as mybir
from concourse.bass_utils import run_bass_kernel_spmd

F32 = mybir.dt.float32
BF16 = mybir.dt.bfloat16
U8 = mybir.dt.uint8
AF = mybir.ActivationFunctionType
ALU = mybir.AluOpType

ENGS = ["pe", "act", "dve", "pool", "sp"]
import os as _os
N_DMA_SEMS = int(_os.environ.get("NDS", "24"))

D = 1024
T = 2048
CT = 256
NB = 2
TS = T + CT
DFF = 2816
EPS = 1e-6


class _Proxy:
    def __getattr__(self, name):
        def f(*a, **k):
            self.call = (name, a, k)
            return self
        return f


class Sched:
    def __init__(self, nc):
        self.nc = nc
        self.ops = {e: [] for e in ENGS}
        self.lastw = {}
        self.readers = {}
        self.dma_rr = {e: 0 for e in ENGS}
        self.dma_last = {}
        self.out_dmas = []
        self.pending_dmas = []

    def add(self, eng, fn, reads=(), writes=(), dma=False, is_output=False, extra_deps=()):
        ops = self.ops[eng]
        me = (eng, len(ops))
        deps = {}

        def dep(p, kind):
            if p is None or p == me:
                return
            if deps.get(p) == "raw":
                return
            deps[p] = kind

        for r in reads:
            dep(self.lastw.get(r), "raw")
        for r in writes:
            dep(self.lastw.get(r), "order")
            for rd in self.readers.get(r, ()):
                dep(rd, "order")
        for p in extra_deps:
            dep(p, "raw")
        prox = _Proxy()
        fn(prox)
        rec = dict(call=prox.call, dma=dma, signal=False, slot=None)
        if dma:
            slot = self.dma_rr[eng] % N_DMA_SEMS
            self.dma_rr[eng] += 1
            rec["slot"] = slot
            prev = self.dma_last.get((eng, slot))
            if prev is not None:
                dep(prev, "raw")
            self.dma_last[(eng, slot)] = me
            self.pending_dmas.append(me)
            if is_output:
                self.out_dmas.append(me)
        final = {}
        for p, kind in deps.items():
            pe, pi = p
            prod = self.ops[pe][pi]
            if pe == eng and not prod["dma"] and not dma:
                if kind == "order" or eng in ("pe", "sp"):
                    continue
            final[p] = kind
        rec["deps"] = final
        ops.append(rec)
        for r in reads:
            lst = self.readers.setdefault(r, [])
            if not dma:
                lst[:] = [q for q in lst if not (q[0] == eng and not self.ops[q[0]][q[1]]["dma"])]
            lst.append(me)
        for r in writes:
            self.lastw[r] = me
            self.readers[r] = []
        return me

    def barrier(self):
        lasts = []
        for e in ENGS:
            for i in range(len(self.ops[e]) - 1, -1, -1):
                if not self.ops[e][i]["dma"]:
                    if not self.ops[e][i].get("nop"):
                        lasts.append((e, i))
                    break
        pend = list(self.pending_dmas)
        self.pending_dmas = []
        for e in ENGS:
            me = self.add(e, lambda eng: eng.nop(), extra_deps=[p for p in lasts if p[0] != e] + pend)
            self.ops[me[0]][me[1]]["nop"] = True

    def emit(self, stack):
        nc = self.nc
        for e in ENGS:
            for rec in self.ops[e]:
                for (pe, pi) in rec["deps"]:
                    self.ops[pe][pi]["signal"] = True
        esem = {e: stack.enter_context(nc.semaphore("s_" + e)) for e in ENGS}
        dsem = {e: [None] * N_DMA_SEMS for e in ENGS}
        for e in ENGS:
            for s in range(min(N_DMA_SEMS, self.dma_rr[e])):
                dsem[e][s] = stack.enter_context(nc.semaphore("d_%s_%d" % (e, s)))
        cnt = {e: 0 for e in ENGS}
        dcnt = {}
        for e in ENGS:
            for rec in self.ops[e]:
                if rec["dma"]:
                    k = (e, rec["slot"])
                    dcnt[k] = dcnt.get(k, 0) + 16
                    rec["ev"] = (k, dcnt[k])
                elif rec["signal"]:
                    cnt[e] += 1
                    rec["ev"] = (e, cnt[e])
                else:
                    rec["ev"] = None
        self.stats = {e: [len(self.ops[e]), 0, cnt[e]] for e in ENGS}
        block = stack.enter_context(nc.Block())
        sched = self

        def semof(key):
            if isinstance(key, tuple):
                return dsem[key[0]][key[1]]
            return esem[key]

        def run(e, eng):
            known = {}
            for rec in sched.ops[e]:
                need = {}
                for (pe, pi) in rec["deps"]:
                    key, val = sched.ops[pe][pi]["ev"]
                    if known.get(key, 0) >= val:
                        continue
                    if need.get(key, 0) < val:
                        need[key] = val
                for key, val in need.items():
                    eng.wait_ge(semof(key), val)
                    known[key] = val
                    sched.stats[e][1] += 1
                nm, a_, k_ = rec["call"]
                ins = getattr(eng, nm)(*a_, **k_)
                if rec["dma"]:
                    ins.then_inc(semof(rec["ev"][0]), 16)
                elif rec["signal"]:
                    ins.then_inc(esem[e], 1)
            return known

        @block.tensor
        def _(eng):
            run("pe", eng)

        @block.scalar
        def _(eng):
            run("act", eng)

        @block.vector
        def _(eng):
            run("dve", eng)

        @block.gpsimd
        def _(eng):
            run("pool", eng)

        @block.sync
        def _(eng):
            known = run("sp", eng)
            for (pe, pi) in sched.out_dmas:
                key, val = sched.ops[pe][pi]["ev"]
                if known.get(key, 0) < val:
                    eng.wait_ge(semof(key), val)
                    known[key] = val


DT_SIZE = {F32: 4, BF16: 2, U8: 1}


class Arena:
    def __init__(self, tens, size):
        self.t = tens
        self.size = size
        self.off = 0

    def reset(self):
        self.off = 0

    def alloc(self, shape, dt):
        n = 1
        for s in shape[1:]:
            n *= s
        nbytes = (n * DT_SIZE[dt] + 63) // 64 * 64
        assert self.off + nbytes <= self.size, ("arena overflow", self.off, nbytes, self.size)
        ap = self.t[0:shape[0], self.off:self.off + n * DT_SIZE[dt]].bitcast(dt)
        self.off += nbytes
        if len(shape) == 3:
            ap = ap.rearrange("p (a b) -> p a b", b=shape[2])
        elif len(shape) == 4:
            ap = ap.rearrange("p (a b c) -> p a b c", b=shape[2], c=shape[3])
        return ap


def tiles_of(r0, nr):
    return list(range(r0 // 128, (r0 + nr - 1) // 128 + 1))


class _Stop(Exception):
    pass


def build_program(dbg=None, lim=None):
    dbg = dbg or set()
    nc = bass.Bass("TRN2", target_bir_lowering=False)

    def din(name, shape, dt=F32):
        return nc.dram_tensor(name, list(shape), dt, kind="ExternalInput").ap()

    def dscr(name, shape, dt=F32):
        kind = "ExternalOutput" if name in dbg else "Internal"
        return nc.dram_tensor(name, list(shape), dt, kind=kind).ap()

    x_in = din("x", [NB, T, D])
    ctx_in = din("ctx", [NB, CT, D])
    cvec = din("cvec", [3, D])
    mod_w = din("mod_w", [2, D, 6 * D])
    mod_b = din("mod_b", [2, 6 * D])
    norm_g = din("norm_g", [2, 4, D])
    w_in0 = din("w_in0", [D, 21 * 128])
    attn_sink = din("attn_sink", [8])
    pool_w = din("pool_w", [4, 128, 128])
    pool_scale = din("pool_scale", [512])
    attn_out_w = din("attn_out_w", [D, D])
    rec_in_w = din("rec_in_w", [D, 3088])
    rec_gate_b = din("rec_gate_b", [16])
    rec_conv_w = din("rec_conv_w", [3, D])
    rec_conv_b = din("rec_conv_b", [D])
    rec_q_w = din("rec_q_w", [4, 256, 256])
    rec_k_w = din("rec_k_w", [4, 256, 256])
    rec_norm_g = din("rec_norm_g", [D])
    rec_skip = din("rec_skip", [D])
    rec_out_w = din("rec_out_w", [D, D])
    ffn_up_w = din("ffn_up_w", [2, D, 2 * DFF])
    ffn_conv_w = din("ffn_conv_w", [2, 3, 2 * DFF])
    ffn_conv_b = din("ffn_conv_b", [2, 2 * DFF])
    ffn_down_w = din("ffn_down_w", [2, DFF, D])
    ident_in = din("ident", [128, 128])
    rope_c = din("rope_c", [128, T])
    rope_s = din("rope_s", [128, T])
    amask_in = din("amask", [2, 128, 512])
    pool_edge = din("pool_edge", [128, 64])
    lmask_in = din("lmask", [2, 128, 128])

    out = nc.dram_tensor("out", [NB, T, D], F32, kind="ExternalOutput").ap()

    GV = dscr("GV", [2, 2, 3, D])
    QT0 = dscr("QT0", [NB, 8 * 128, TS], BF16)
    V0 = dscr("V0", [NB, TS, 130], BF16)
    U0 = dscr("U0", [NB, 512, TS])
    CATP = dscr("CATP", [NB, 512, TS], BF16)
    XA = dscr("XA", [NB, T, D])
    CA = dscr("CA", [NB, CT, D])
    XB = dscr("XB", [NB, T, D])
    CB = dscr("CB", [NB, CT, D])
    GT = dscr("GT", [NB, DFF, TS], BF16)

    st = ExitStack()
    with st:
        st.enter_context(nc.allow_non_contiguous_dma(reason="small strided parameter loads"))
        S = Sched(nc)
        import os
        STQ = os.environ.get("STQ", "pool")
        parts = {}

        def wpart(base):
            lst = parts.setdefault(base, [])
            name = (base, len(lst))
            lst.append(name)
            return [name]

        def rparts(base):
            return list(parts.get(base, []))

        def sb(name, shape, dt):
            return st.enter_context(nc.sbuf_tensor("sb_" + name, list(shape), dt))

        ARENA_BYTES = 192 * 1024
        arena = Arena(sb("arena", [128, ARENA_BYTES], U8), ARENA_BYTES)
        PS2 = [st.enter_context(nc.psum_tensor("ps%d" % i, [128, 1024], F32)) for i in range(4)]

        def bank(i):
            return PS2[i // 2][:, (i % 2) * 512:(i % 2) * 512 + 512]

        def bank_bf(i):
            return PS2[i // 2][:, (i % 2) * 512:(i % 2) * 512 + 512].bitcast(BF16)

        ident = sb("ident", [128, 128], F32)
        identb = sb("identb", [128, 128], BF16)
        modT = [sb("modT%d" % l, [128, 48, 3], F32) for l in range(2)]
        A1 = [sb("A1_%d" % l, [128, 3, 8], F32) for l in range(2)]
        A2 = [sb("A2_%d" % l, [128, 3, 8], F32) for l in range(2)]
        SH1 = [sb("SH1_%d" % l, [128, 3, 8], F32) for l in range(2)]
        SH2 = [sb("SH2_%d" % l, [128, 3, 8], F32) for l in range(2)]
        epsb = sb("epsb", [128, 1], F32)

        def V(fn, reads, writes):
            return S.add("dve", fn, reads, writes)

        def A(fn, reads, writes):
            return S.add("act", fn, reads, writes)

        def G(fn, reads, writes):
            return S.add("pool", fn, reads, writes)

        def PE(fn, reads, writes):
            return S.add("pe", fn, reads, writes)

        def DMA(q, out_ap, in_ap, reads, writes, is_output=False):
            return S.add(q, lambda e: e.dma_start(out=out_ap, in_=in_ap), reads, writes, dma=True,
                         is_output=is_output)

        def dma3(q, dst3, src3, nmid, reads, writes):
            for j in range(nmid):
                DMA(q, dst3(j), src3(j), reads, writes)

        def load_wb(dst3, src2, K, blocks, res):
            for (c0, cw, key) in blocks:
                for k in range(K):
                    DMA("pool", dst3[:, k, c0:c0 + cw], src2[k * 128:(k + 1) * 128, c0:c0 + cw], [], [(res, key)])

        def load_w(dst3, src2, K, N, res):
            for k in range(K):
                c0 = 0
                while c0 < N:
                    cw = min(2048, N - c0)
                    DMA("pool", dst3[:, k, c0:c0 + cw], src2[k * 128:(k + 1) * 128, c0:c0 + cw], [], [res])
                    c0 += cw

        def chk(k):
            if lim is not None and k > lim:
                raise _Stop()

        try:
            DMA("sp", ident[:], ident_in, [], ["ident"])
            V(lambda e: e.tensor_copy(out=identb[:], in_=ident[:]), ["ident"], ["identb"])
            V(lambda e: e.memset(epsb[:], EPS), [], ["epsb"])

            arena.reset()
            cT = arena.alloc([128, 8, 3], F32)
            sT = arena.alloc([128, 8, 3], BF16)
            sTf = arena.alloc([128, 8, 3], F32)
            mb = arena.alloc([128, 48], F32)
            ng = arena.alloc([128, 4, 8], F32)
            g1t = arena.alloc([128, 8, 3], F32)
            g2t = arena.alloc([128, 8, 3], F32)
            mwt = [arena.alloc([128, 8, 512], BF16) for _ in range(3)]
            mwf = [arena.alloc([128, 8, 512], F32) for _ in range(3)]
            for r in range(3):
                DMA("sp", cT[:, :, r], cvec[r].rearrange("(k p) -> p k", p=128), [], ["cT"])
            A(lambda e: e.activation(out=sTf, in_=cT, func=AF.Silu), ["cT"], ["sTf"])
            V(lambda e: e.tensor_copy(out=sT, in_=sTf), ["sTf"], ["sT"])
            for nch in range(12):
                wt = mwt[nch % 3]
                wres = "mwt%d" % (nch % 3)
                load_w(wt, mod_w[0][:, nch * 512:(nch + 1) * 512], 8, 512, wres)
                wf = mwf[nch % 3]
                fres = "mwf%d" % (nch % 3)
                for k in range(8):
                    DMA("sp", wf[:, k, :], mod_w[1][k * 128:(k + 1) * 128, nch * 512:(nch + 1) * 512], [], [fres])
                for fc in range(4):
                    col = (nch * 4 + fc) * 3
                    for k in range(8):
                        PE(lambda e: e.matmul(bank(0)[:, col:col + 3], lhsT=wt[:, k, fc * 128:(fc + 1) * 128],
                                              rhs=sT[:, k, :], start=(k == 0), stop=(k == 7)), [wres, "sT"], ["ps0"])
                for fc in range(4):
                    col = (nch * 4 + fc) * 3
                    for k in range(8):
                        PE(lambda e: e.matmul(bank(1)[:, col:col + 3], lhsT=wf[:, k, fc * 128:(fc + 1) * 128],
                                              rhs=sTf[:, k, :], start=(k == 0), stop=(k == 7)), [fres, "sTf"], ["ps1"])
            for l in range(2):
                DMA("sp", mb, mod_b[l].rearrange("(c p) -> p c", p=128), [], ["mb"])
                for j in range(4):
                    DMA("sp", ng[:, j, :], norm_g[l, j].rearrange("(k p) -> p k", p=128), [], ["ng"])
                psm = bank(l)
                mT = modT[l]
                V(lambda e, mT=mT: e.tensor_tensor(
                    out=mT[:], in0=psm[:, 0:144].rearrange("p (c r) -> p c r", r=3),
                    in1=mb.unsqueeze(2).to_broadcast([128, 48, 3]), op=ALU.add), ["ps%d" % l, "mb"], ["modT%d" % l])

                def ngb(j):
                    return ng[:, j, :].unsqueeze(2).to_broadcast([128, 8, 3])

                mres = ["modT%d" % l, "ng"]
                V(lambda e, mT=mT, l=l: e.scalar_tensor_tensor(
                    out=A1[l][:].rearrange("p r k -> p k r"), in0=mT[:, 8:16, :], scalar=1.0, in1=ngb(0),
                    op0=ALU.add, op1=ALU.mult), mres, ["A1_%d" % l])
                V(lambda e, mT=mT, l=l: e.scalar_tensor_tensor(
                    out=A2[l][:].rearrange("p r k -> p k r"), in0=mT[:, 32:40, :], scalar=1.0, in1=ngb(2),
                    op0=ALU.add, op1=ALU.mult), mres, ["A2_%d" % l])
                V(lambda e, mT=mT, l=l: e.tensor_copy(
                    out=SH1[l][:].rearrange("p r k -> p k r"), in_=mT[:, 0:8, :]), mres, ["SH1_%d" % l])
                V(lambda e, mT=mT, l=l: e.tensor_copy(
                    out=SH2[l][:].rearrange("p r k -> p k r"), in_=mT[:, 24:32, :]), mres, ["SH2_%d" % l])
                V(lambda e, mT=mT: e.tensor_tensor(out=g1t, in0=mT[:, 16:24, :], in1=ngb(1), op=ALU.mult),
                  mres, ["g1t"])
                V(lambda e, mT=mT: e.tensor_tensor(out=g2t, in0=mT[:, 40:48, :], in1=ngb(3), op=ALU.mult),
                  mres, ["g2t"])
                for r in range(3):
                    DMA("sp", GV[l, 0, r].rearrange("(k p) -> p k", p=128), g1t[:, :, r], ["g1t"], wpart(("GV", l, 0)))
                    DMA("sp", GV[l, 1, r].rearrange("(k p) -> p k", p=128), g2t[:, :, r], ["g2t"], wpart(("GV", l, 1)))

            ctxb = {}
            rr = {"n": 0, "r": 0}

            def alloc_norm_bufs():
                ctxb["xt"] = [arena.alloc([128, D], F32) for _ in range(3)]
                ctxb["junk"] = arena.alloc([128, D], BF16)
                ctxb["xn"] = [arena.alloc([128, D], BF16) for _ in range(3)]
                ctxb["st"] = [arena.alloc([128, 4], F32) for _ in range(3)]
                ctxb["tmpT"] = [arena.alloc([128, 8, 128], F32) for _ in range(2)]

            def rstd_chain(src_ap, srcres, stt, stres, nr):
                junk = ctxb["junk"]
                A(lambda e: e.activation(out=junk[0:nr, :], in_=src_ap, func=AF.Square, accum_out=stt[0:nr, 0:1]),
                  srcres, ["junk", stres])
                A(lambda e: e.activation(out=stt[0:nr, 1:2], in_=stt[0:nr, 0:1], func=AF.Sqrt, scale=1.0 / D,
                                         bias=epsb[0:nr, :]), [stres, "epsb"], [stres])
                V(lambda e: e.reciprocal(out=stt[0:nr, 1:2], in_=stt[0:nr, 1:2]), [stres], [stres])

            def make_hT(src, srcres_fn, r0, nr, Aap, SHap, ares, hT, hres, c0):
                i = rr["n"]
                rr["n"] += 1
                s3, s2 = i % 3, i % 2
                xt, xn, stt, tmpT = ctxb["xt"][s3], ctxb["xn"][s3], ctxb["st"][s3], ctxb["tmpT"][s2]
                xres, nres, sres, tres = "xt%d" % s3, "xn%d" % s3, "st%d" % s3, "tmpT%d" % s2
                pb = 6 + s2
                pres = "ps%d" % pb
                DMA("sp", xt[0:nr, :], src[r0:r0 + nr, :], srcres_fn(r0, nr), [xres])
                rstd_chain(xt[0:nr, :], [xres], stt, sres, nr)
                V(lambda e: e.tensor_scalar(out=xn[0:nr, :], in0=xt[0:nr, :], scalar1=stt[0:nr, 1:2], scalar2=None,
                                            op0=ALU.mult), [xres, sres], [nres])
                pt = bank_bf(pb).rearrange("p (k t) -> p k t", t=128)
                for k in range(8):
                    PE(lambda e, k=k: e.transpose(out=pt[:, k, 0:nr], in_=xn[0:nr, k * 128:(k + 1) * 128],
                                                  identity=identb[0:nr, 0:nr]), [nres, "identb"], [pres])
                V(lambda e: e.tensor_tensor(out=tmpT[:, :, 0:nr], in0=pt[:, :, 0:nr],
                                            in1=Aap.unsqueeze(2).to_broadcast([128, 8, nr]), op=ALU.mult),
                  [pres, ares], [tres])
                V(lambda e: e.tensor_tensor(out=hT[:, :, c0:c0 + nr], in0=tmpT[:, :, 0:nr],
                                            in1=SHap.unsqueeze(2).to_broadcast([128, 8, nr]), op=ALU.add),
                  [tres, ares], [hres])

            def residual_update(mix_ap, mixres, src, srcres, dst, dstres, r0, nr, gb, gbres, is_output=False):
                i = rr["r"]
                rr["r"] += 1
                s3, s2 = i % 3, i % 2
                xt, stt = ctxb["xt"][s3], ctxb["st"][s3]
                tmp = ctxb["mtmp"][s2]
                xres, sres, tres = "xt%d" % s3, "st%d" % s3, "mtmp%d" % s2
                DMA("sp", xt[0:nr, :], src[r0:r0 + nr, :], srcres, [xres])
                rstd_chain(mix_ap, mixres, stt, sres, nr)
                V(lambda e: e.scalar_tensor_tensor(out=tmp[0:nr, :], in0=mix_ap, scalar=stt[0:nr, 1:2],
                                                   in1=gb[0:nr, :], op0=ALU.mult, op1=ALU.mult),
                  mixres + [sres, gbres], [tres])
                V(lambda e: e.tensor_tensor(out=xt[0:nr, :], in0=xt[0:nr, :], in1=tmp[0:nr, :], op=ALU.add),
                  [xres, tres], [xres])
                DMA(STQ, dst[r0:r0 + nr, :], xt[0:nr, :], [xres], dstres, is_output=is_output)

            def load_gb(l, j, r, tile, res):
                DMA("sp", tile, GV[l, j, r].partition_broadcast(128), rparts(("GV", l, j)), [res])

            def xres_fn(name, b):
                return lambda r0, nr: [(name, b, t) for t in tiles_of(r0, nr)]

            chk(1)
            S.barrier()
            arena.reset()
            alloc_norm_bufs()
            win = arena.alloc([128, 8, 21 * 128], BF16)
            rc = arena.alloc([128, T], F32)
            rs = arena.alloc([128, T], F32)
            hTw = [arena.alloc([128, 8, 512], BF16) for _ in range(2)]
            rt1 = [arena.alloc([128, 512], F32) for _ in range(2)]
            rt2 = [arena.alloc([128, 512], F32) for _ in range(2)]
            qst = [arena.alloc([128, 512], BF16) for _ in range(4)]
            ust = [arena.alloc([128, 512], F32) for _ in range(2)]
            vst = [arena.alloc([128, 130], BF16) for _ in range(2)]
            load_w(win, w_in0, 8, 21 * 128, "win")
            DMA("sp", rc, rope_c, [], ["rc"])
            DMA("sp", rs, rope_s, [], ["rs"])
            for s in range(2):
                V(lambda e, s=s: e.memset(vst[s][:, 64:65], 1.0), [], ["vst%d" % s])
                V(lambda e, s=s: e.memset(vst[s][:, 129:130], 1.0), [], ["vst%d" % s])
            cnt = {"w": 0, "q": 0, "u": 0, "v": 0, "pp": 0}
            for b in range(NB):
                for w in range(5):
                    isx = w < 4
                    nt = 512 if isx else 256
                    tok0 = w * 512
                    row = b if isx else 2
                    src = x_in[b] if isx else ctx_in[b]
                    hs = cnt["w"] % 2
                    cnt["w"] += 1
                    hT, hres = hTw[hs], "hTw%d" % hs
                    for i in range(nt // 128):
                        make_hT(src, lambda r0, nr: [], (tok0 if isx else 0) + i * 128 if isx else i * 128, 128,
                                A1[0][:, row, :], SH1[0][:, row, :], "A1_0", hT, hres, i * 128)

                    def proj(j, pb):
                        for k in range(8):
                            PE(lambda e, k=k: e.matmul(bank(pb)[:, 0:nt], lhsT=win[:, k, j * 128:(j + 1) * 128],
                                                       rhs=hT[:, k, 0:nt], start=(k == 0), stop=(k == 7)),
                               ["win", hres], ["ps%d" % pb])

                    for j in range(8):
                        pa = (cnt["pp"] % 2) * 2
                        cnt["pp"] += 1
                        qs = cnt["q"] % 4
                        cnt["q"] += 1
                        qt_, qres = qst[qs], "qst%d" % qs
                        proj(j, pa)
                        if isx:
                            proj(j + 8, pa + 1)
                            ts_ = cnt["q"] % 2
                            t1, t2 = rt1[ts_], rt2[ts_]
                            V(lambda e, t1=t1, pa=pa: e.tensor_tensor(out=t1[:, 0:nt], in0=bank(pa)[:, 0:nt],
                                                                      in1=rc[:, tok0:tok0 + nt], op=ALU.mult),
                              ["ps%d" % pa, "rc"], ["rt1_%d" % ts_])
                            V(lambda e, t2=t2, pa=pa: e.tensor_tensor(out=t2[:, 0:nt], in0=bank(pa + 1)[:, 0:nt],
                                                                      in1=rs[:, tok0:tok0 + nt], op=ALU.mult),
                              ["ps%d" % (pa + 1), "rs"], ["rt2_%d" % ts_])
                            G(lambda e, t1=t1, t2=t2, qt_=qt_: e.tensor_tensor(out=qt_[:, 0:nt], in0=t1[:, 0:nt],
                                                                               in1=t2[:, 0:nt], op=ALU.add),
                              ["rt1_%d" % ts_, "rt2_%d" % ts_], [qres])
                        else:
                            A(lambda e, qt_=qt_, pa=pa: e.activation(out=qt_[:, 0:nt], in_=bank(pa)[:, 0:nt],
                                                                     func=AF.Copy), ["ps%d" % pa], [qres])
                        DMA(STQ, QT0[b, j * 128:(j + 1) * 128, tok0:tok0 + nt], qt_[:, 0:nt], [qres],
                            wpart(("QT0", b)))
                    for j in range(17, 21):
                        pa = (cnt["pp"] % 2) * 2
                        cnt["pp"] += 1
                        us = cnt["u"] % 2
                        cnt["u"] += 1
                        proj(j, pa)
                        A(lambda e, us=us, pa=pa: e.activation(out=ust[us][:, 0:nt], in_=bank(pa)[:, 0:nt],
                                                               func=AF.Copy), ["ps%d" % pa], ["ust%d" % us])
                        DMA(STQ, U0[b, (j - 17) * 128:(j - 16) * 128, tok0:tok0 + nt], ust[us][:, 0:nt],
                            ["ust%d" % us], wpart(("U0", b, j - 17)))
                    for i in range(nt // 128):
                        vs = cnt["v"] % 2
                        cnt["v"] += 1
                        for k in range(8):
                            PE(lambda e, k=k, i=i: e.matmul(bank(4)[:, 0:128], lhsT=hT[:, k, i * 128:(i + 1) * 128],
                                                           rhs=win[:, k, 16 * 128:17 * 128], start=(k == 0),
                                                           stop=(k == 7)), ["win", hres], ["ps4"])
                        A(lambda e, vs=vs: e.activation(
                            out=vst[vs][:].rearrange("p (h d) -> p h d", d=65)[:, :, 0:64],
                            in_=bank(4)[:, 0:128].rearrange("p (h d) -> p h d", d=64), func=AF.Copy),
                          ["ps4"], ["vst%d" % vs])
                        DMA(STQ, V0[b, tok0 + i * 128:tok0 + (i + 1) * 128, :], vst[vs][:], ["vst%d" % vs],
                            wpart(("V0", b)))

            chk(2)
            S.barrier()
            arena.reset()
            PADL = 16
            ub = [arena.alloc([128, TS + 48], F32) for _ in range(2)]
            pa_ = [arena.alloc([128, TS + 48], F32) for _ in range(2)]
            pb_ = [arena.alloc([128, TS + 48], F32) for _ in range(2)]
            dTt = [arena.alloc([128, TS], BF16) for _ in range(2)]
            cst = [arena.alloc([128, 512], BF16) for _ in range(2)]
            pwt = arena.alloc([128, 4, 128], BF16)
            psc = arena.alloc([128, 4], F32)
            pedge = arena.alloc([128, 64], F32)
            etmp = arena.alloc([128, 8], F32)
            for g_ in range(4):
                DMA("pool", pwt[:, g_, :], pool_w[g_], [], ["pwt"])
            DMA("sp", psc, pool_scale.rearrange("(g p) -> p g", p=128), [], ["psc"])
            DMA("sp", pedge, pool_edge, [], ["pedge"])
            segs = [(PADL, T, 0), (PADL + T + 16, CT, T)]
            pc = 0
            for b in range(NB):
                for g in range(4):
                    sl = pc % 2
                    pc += 1
                    u, p1, p2, dT = ub[sl], pa_[sl], pb_[sl], dTt[sl]
                    ur, p1r, p2r, dr = "ub%d" % sl, "pa%d" % sl, "pb%d" % sl, "dT%d" % sl
                    V(lambda e, u=u: e.memset(u[:, 0:PADL], 0.0), [], [ur])
                    V(lambda e, u=u: e.memset(u[:, PADL + T:PADL + T + 16], 0.0), [], [ur])
                    V(lambda e, u=u: e.memset(u[:, PADL + T + 16 + CT:TS + 48], 0.0), [], [ur])
                    DMA("sp", u[:, PADL:PADL + T], U0[b, g * 128:(g + 1) * 128, 0:T], rparts(("U0", b, g)), [ur])
                    DMA("sp", u[:, PADL + T + 16:PADL + T + 16 + CT], U0[b, g * 128:(g + 1) * 128, T:TS],
                        rparts(("U0", b, g)), [ur])
                    wlen = 2 ** (g + 1)
                    half = wlen // 2
                    NW = TS + 48
                    cur, curres, ln = u, ur, 1
                    bufs = [(p1, p1r), (p2, p2r)]
                    bi = 0
                    while ln < wlen:
                        nxt, nres = bufs[bi % 2]
                        bi += 1
                        n_el = NW - 2 * ln
                        V(lambda e, cur=cur, nxt=nxt, ln=ln, n_el=n_el: e.tensor_tensor(
                            out=nxt[:, 0:n_el], in0=cur[:, 0:n_el], in1=cur[:, ln:ln + n_el], op=ALU.add),
                          [curres], [nres])
                        cur, curres = nxt, nres
                        ln *= 2
                    for (off, L, toff) in segs:
                        V(lambda e, cur=cur, off=off, L=L, toff=toff: e.scalar_tensor_tensor(
                            out=dT[:, toff:toff + L], in0=cur[:, off - half:off - half + L], scalar=1.0 / wlen,
                            in1=u[:, off:off + L], op0=ALU.mult, op1=ALU.subtract), [curres, ur], [dr])
                        for side in range(2):
                            ne = half if side == 0 else half - 1
                            if ne == 0:
                                continue
                            t0 = 0 if side == 0 else L - half + 1
                            ec = (g * 2 + side) * 8
                            V(lambda e, cur=cur, off=off, t0=t0, ne=ne, ec=ec: e.tensor_tensor(
                                out=etmp[:, 0:ne], in0=cur[:, off - half + t0:off - half + t0 + ne],
                                in1=pedge[:, ec:ec + ne], op=ALU.mult), [curres, "pedge"], ["etmp"])
                            V(lambda e, off=off, t0=t0, ne=ne, toff=toff: e.tensor_tensor(
                                out=dT[:, toff + t0:toff + t0 + ne], in0=etmp[:, 0:ne],
                                in1=u[:, off + t0:off + t0 + ne], op=ALU.subtract), ["etmp", ur], [dr])
                    for w in range(5):
                        nt = 512 if w < 4 else 256
                        tok0 = w * 512
                        pbk = w % 2
                        cs = (pc + w) % 2
                        PE(lambda e, dT=dT, nt=nt, tok0=tok0, pbk=pbk, g=g: e.matmul(
                            bank(pbk)[:, 0:nt], lhsT=pwt[:, g, :], rhs=dT[:, tok0:tok0 + nt], start=True, stop=True),
                           ["pwt", dr], ["ps%d" % pbk])
                        A(lambda e, nt=nt, pbk=pbk, cs=cs, g=g: e.activation(
                            out=cst[cs][:, 0:nt], in_=bank(pbk)[:, 0:nt], func=AF.Copy, scale=psc[:, g:g + 1]),
                          ["ps%d" % pbk, "psc"], ["cst%d" % cs])
                        DMA(STQ, CATP[b, g * 128:(g + 1) * 128, tok0:tok0 + nt], cst[cs][:, 0:nt], ["cst%d" % cs],
                            wpart(("CATP", b)))

            chk(2.05)
            S.barrier()
            arena.reset()
            alloc_norm_bufs()
            ctxb["mtmp"] = [arena.alloc([128, D], F32) for _ in range(2)]
            wout = arena.alloc([128, 8, D], BF16)
            qt = arena.alloc([128, 8, TS], BF16)
            vx = arena.alloc([128, 18, 130], BF16)
            am = arena.alloc([128, 2, 512], BF16)
            esb = arena.alloc([128, 8], F32)
            PT = [arena.alloc([128, 512], BF16) for _ in range(10)]
            atok = [arena.alloc([128, 512], BF16) for _ in range(2)]
            catT = [arena.alloc([128, 8, 128], BF16) for _ in range(2)]
            den = [arena.alloc([128, 8], F32) for _ in range(2)]
            gbx = arena.alloc([128, D], F32)
            gbc = arena.alloc([128, D], F32)
            load_w(wout, attn_out_w, 8, D, "wout")
            for m_ in range(2):
                DMA("pool", am[:, m_, :], amask_in[m_], [], ["am"])
            DMA("sp", esb, attn_sink.partition_broadcast(128), [], ["esb"])
            A(lambda e: e.activation(out=esb, in_=esb, func=AF.Exp), ["esb"], ["esb"])
            import os
            if os.environ.get("SWAP"):
                load_gb(0, 0, 0, gbx, "gbx")
            load_gb(0, 0, 2, gbc, "gbc")
            chk(2.1)
            ac = {"pt": 0, "blk": 0}
            for b in range(NB):
                import os
                SK = os.environ.get("SKIP", "")
                if "q" not in SK:
                    dma3("sp", lambda j: qt[:, j, :], lambda j: QT0[b, j * 128:(j + 1) * 128, :], 8, rparts(("QT0", b)), ["qt"])
                if "v" not in SK:
                    dma3("sp", lambda j: vx[:, j, :], lambda j: V0[b, j * 128:(j + 1) * 128, :], 18, rparts(("V0", b)), ["vx"])
                if "g" not in SK:
                    load_gb(0, 0, b, gbx, "gbx")
                chk(2.2)
                for n in range(18):
                    isx = n < 16
                    bs = ac["blk"] % 2
                    ac["blk"] += 1
                    at_, atres = atok[bs], "atok%d" % bs
                    cT_, cres = catT[bs], "catT%d" % bs
                    dn, dres = den[bs], "den%d" % bs
                    if isx:
                        chunks = []
                        if n > 0:
                            chunks.append((n - 1, 0))
                        chunks.append((n, None))
                        if n < 15:
                            chunks.append((n + 1, 1))
                        chunks += [(16, None), (17, None)]
                    else:
                        chunks = [(16, None), (17, None)]
                    if "c" in SK:
                        V(lambda e: e.memset(cT_[:, 4:8, :], 0.0), [], [cres])
                    else:
                        dma3("sp", lambda j: cT_[:, 4 + j, :], lambda j: CATP[b, j * 128:(j + 1) * 128, n * 128:(n + 1) * 128], 4,
                             rparts(("CATP", b)), [cres])
                    for h in range(2):
                        pts = []
                        for (kc, mk) in chunks:
                            pi = ac["pt"] % 10
                            ac["pt"] += 1
                            sbk = pi % 2
                            pts.append((pi, kc))
                            for g in range(4):
                                s_ = g % 2
                                c_ = 2 * h + g // 2
                                PE(lambda e, kc=kc, g=g, s_=s_, c_=c_, sbk=sbk: e.matmul(
                                    bank(sbk)[:, g * 128:(g + 1) * 128],
                                    lhsT=qt[:, 4 + 2 * h + s_, kc * 128:(kc + 1) * 128],
                                    rhs=qt[:, c_, n * 128:(n + 1) * 128],
                                    start=True, stop=True), ["qt"], ["ps%d" % sbk])
                            A(lambda e, pi=pi, sbk=sbk: e.activation(out=PT[pi][:], in_=bank(sbk), func=AF.Exp,
                                                                   scale=0.125), ["ps%d" % sbk], ["PT%d" % pi])
                            if mk is not None:
                                V(lambda e, pi=pi, mk=mk: e.tensor_tensor(out=PT[pi][:], in0=PT[pi][:],
                                                                           in1=am[:, mk, :], op=ALU.mult),
                                  ["PT%d" % pi, "am"], ["PT%d" % pi])
                        chk(2.3)
                        ob = 2 + h
                        ov = bank(ob)[:, 0:260].rearrange("p (g d) -> p g d", d=65)
                        for g in range(4):
                            for ci, (pi, kc) in enumerate(pts):
                                PE(lambda e, g=g, pi=pi, kc=kc, ci=ci, ob=ob: e.matmul(
                                    bank(ob)[:, g * 65:(g + 1) * 65], lhsT=PT[pi][:, g * 128:(g + 1) * 128],
                                    rhs=vx[:, kc, h * 65:(h + 1) * 65], start=(ci == 0), stop=(ci == len(pts) - 1)),
                                   ["PT%d" % pi, "vx"], ["ps%d" % ob])
                        chk(2.5)
                        V(lambda e, ov=ov, dn=dn, h=h: e.tensor_tensor(out=dn[:, h * 4:(h + 1) * 4], in0=ov[:, :, 64],
                                                                       in1=esb[:, h * 4:(h + 1) * 4], op=ALU.add),
                          ["ps%d" % ob, "esb"], [dres])
                        V(lambda e, dn=dn, h=h: e.reciprocal(out=dn[:, h * 4:(h + 1) * 4], in_=dn[:, h * 4:(h + 1) * 4]),
                          [dres], [dres])
                        V(lambda e, ov=ov, dn=dn, h=h, at_=at_: e.tensor_tensor(
                            out=at_[:, h * 256:(h + 1) * 256].rearrange("p (g d) -> p g d", d=64), in0=ov[:, :, 0:64],
                            in1=dn[:, h * 4:(h + 1) * 4].unsqueeze(2).to_broadcast([128, 4, 64]), op=ALU.mult),
                          ["ps%d" % ob, dres], [atres])
                    chk(2.6)
                    ptv = bank_bf(4).rearrange("p (k t) -> p k t", t=128)
                    for c_ in range(4):
                        PE(lambda e, c_=c_, at_=at_: e.transpose(out=ptv[:, c_, :], in_=at_[:, c_ * 128:(c_ + 1) * 128],
                                                                identity=identb[:]), [atres, "identb"], ["ps4"])
                    A(lambda e, cT_=cT_: e.activation(out=cT_[:, 0:4, :], in_=ptv[:, 0:4, :], func=AF.Copy),
                      ["ps4"], [cres])
                    chk(2.7)
                    mix = PS2[3]
                    for hf in range(2):
                        for k in range(8):
                            PE(lambda e, k=k, hf=hf, cT_=cT_: e.matmul(mix[:, hf * 512:(hf + 1) * 512], lhsT=cT_[:, k, :],
                                                                      rhs=wout[:, k, hf * 512:(hf + 1) * 512],
                                                                      start=(k == 0), stop=(k == 7)),
                               [cres, "wout"], ["ps%d" % (6 + hf)])
                    chk(2.8)
                    if isx:
                        residual_update(mix[:, :], ["ps6", "ps7"], x_in[b], [], XA[b], [("XA", b, n)], n * 128, 128,
                                        gbx, "gbx")
                    else:
                        residual_update(mix[:, :], ["ps6", "ps7"], ctx_in[b], [], CA[b], [("CA", b, n - 16)],
                                        (n - 16) * 128, 128, gbc, "gbc")

            def make_windows(L):
                wins = []
                o = 0
                while o < L:
                    r0 = max(o - 1, 0)
                    c0 = 1 if o == 0 else 0
                    nr = min(512 - c0, L - r0)
                    rz = (r0 + nr == L)
                    ncols = c0 + nr + (1 if rz else 0)
                    if ncols > 512:
                        nr -= 1
                        rz = False
                        ncols = 512
                    nout = ncols - 2
                    wins.append((r0, nr, c0, rz, ncols, o, nout))
                    o += nout
                return wins

            def ffn_layer(l, segs_fn, final):
                chk(4 + 5 * l)
                S.barrier()
                arena.reset()
                alloc_norm_bufs()
                wup = arena.alloc([128, 8, 2 * DFF], BF16)
                cw = arena.alloc([128, 3, 44], F32)
                cb = arena.alloc([128, 44], F32)
                hTf = [arena.alloc([128, 8, 512], BF16) for _ in range(2)]
                ta = [arena.alloc([128, 512], F32) for _ in range(3)]
                tb = [arena.alloc([128, 512], F32) for _ in range(3)]
                sg = [arena.alloc([128, 512], F32) for _ in range(3)]
                tv = [arena.alloc([128, 512], F32) for _ in range(3)]
                tw = [arena.alloc([128, 512], F32) for _ in range(3)]
                gst = [arena.alloc([128, 512], BF16) for _ in range(3)]
                load_wb(wup, ffn_up_w[l], 8, [(0, 1408, 0), (2816, 1408, 2), (1408, 1408, 1), (4224, 1408, 3)], "wup")
                for j in range(3):
                    DMA("sp", cw[:, j, :], ffn_conv_w[l, j].rearrange("(c p) -> p c", p=128), [], ["cw"])
                DMA("sp", cb, ffn_conv_b[l].rearrange("(c p) -> p c", p=128), [], ["cb"])
                fc = {"w": 0, "c": 0, "g": 0}
                allw = []
                for b in range(NB):
                    for (src, sname, L, toff, row) in segs_fn(b):
                        for w_ in make_windows(L):
                            allw.append((b, src, sname, L, toff, row) + w_)

                def prep_u(i):
                    (b, src, sname, L, toff, row, r0, nr, c0, rz, ncols, o, nout) = allw[i]
                    hT, hres = hTf[i % 2], "hTf%d" % (i % 2)
                    if c0 == 1:
                        V(lambda e: e.memset(hT[:, :, 0:1], 0.0), [], [hres])
                    if rz:
                        V(lambda e: e.memset(hT[:, :, c0 + nr:c0 + nr + 1], 0.0), [], [hres])
                    rr_ = r0
                    while rr_ < r0 + nr:
                        n_ = min(128, r0 + nr - rr_)
                        make_hT(src, xres_fn(sname, b), rr_, n_, A2[l][:, row, :], SH2[l][:, row, :],
                                "A2_%d" % l, hT, hres, c0 + rr_ - r0)
                        rr_ += n_

                def comp_u(i):
                    (b, src, sname, L, toff, row, r0, nr, c0, rz, ncols, o, nout) = allw[i]
                    hT, hres = hTf[i % 2], "hTf%d" % (i % 2)
                    for c in range(22):
                        s2 = fc["c"] % 3
                        fc["c"] += 1
                        pg, pv = (s2 * 2), (s2 * 2 + 1)
                        for (jc, pbk) in ((c, pg), (22 + c, pv)):
                            for k in range(8):
                                PE(lambda e, k=k, jc=jc, pbk=pbk, hT=hT, ncols=ncols: e.matmul(
                                    bank(pbk)[:, 0:ncols], lhsT=wup[:, k, jc * 128:(jc + 1) * 128],
                                    rhs=hT[:, k, 0:ncols], start=(k == 0), stop=(k == 7)),
                                   [("wup", jc // 11), hres], ["ps%d" % pbk])
                        n = nout
                        A(lambda e, s2=s2, pg=pg, c=c, n=n: e.activation(
                            out=ta[s2][:, 0:n], in_=bank(pg)[:, 1:1 + n], func=AF.Identity,
                            scale=cw[:, 1, c:c + 1], bias=cb[:, c:c + 1]), ["ps%d" % pg, "cw", "cb"], ["ta%d" % s2])
                        V(lambda e, s2=s2, pg=pg, c=c, n=n: e.scalar_tensor_tensor(
                            out=tb[s2][:, 0:n], in0=bank(pg)[:, 0:n], scalar=cw[:, 0, c:c + 1],
                            in1=ta[s2][:, 0:n], op0=ALU.mult, op1=ALU.add), ["ps%d" % pg, "cw", "ta%d" % s2],
                          ["tb%d" % s2])
                        V(lambda e, s2=s2, pg=pg, c=c, n=n: e.scalar_tensor_tensor(
                            out=ta[s2][:, 0:n], in0=bank(pg)[:, 2:2 + n], scalar=cw[:, 2, c:c + 1],
                            in1=tb[s2][:, 0:n], op0=ALU.mult, op1=ALU.add), ["ps%d" % pg, "cw", "tb%d" % s2],
                          ["ta%d" % s2])
                        A(lambda e, s2=s2, n=n: e.activation(out=sg[s2][:, 0:n], in_=ta[s2][:, 0:n], func=AF.Silu),
                          ["ta%d" % s2], ["sg%d" % s2])
                        cv = 22 + c
                        A(lambda e, s2=s2, pv=pv, cv=cv, n=n: e.activation(
                            out=tv[s2][:, 0:n], in_=bank(pv)[:, 1:1 + n], func=AF.Identity,
                            scale=cw[:, 1, cv:cv + 1], bias=cb[:, cv:cv + 1]), ["ps%d" % pv, "cw", "cb"],
                          ["tv%d" % s2])
                        V(lambda e, s2=s2, pv=pv, cv=cv, n=n: e.scalar_tensor_tensor(
                            out=tw[s2][:, 0:n], in0=bank(pv)[:, 0:n], scalar=cw[:, 0, cv:cv + 1],
                            in1=tv[s2][:, 0:n], op0=ALU.mult, op1=ALU.add), ["ps%d" % pv, "cw", "tv%d" % s2],
                          ["tw%d" % s2])
                        V(lambda e, s2=s2, pv=pv, cv=cv, n=n: e.scalar_tensor_tensor(
                            out=tv[s2][:, 0:n], in0=bank(pv)[:, 2:2 + n], scalar=cw[:, 2, cv:cv + 1],
                            in1=tw[s2][:, 0:n], op0=ALU.mult, op1=ALU.add), ["ps%d" % pv, "cw", "tw%d" % s2],
                          ["tv%d" % s2])
                        gs = fc["g"] % 3
                        fc["g"] += 1
                        G(lambda e, s2=s2, gs=gs, n=n: e.tensor_tensor(out=gst[gs][:, 0:n], in0=sg[s2][:, 0:n],
                                                                       in1=tv[s2][:, 0:n], op=ALU.mult),
                          ["sg%d" % s2, "tv%d" % s2], ["gst%d" % gs])
                        DMA(STQ, GT[b, c * 128:(c + 1) * 128, toff + o:toff + o + n], gst[gs][:, 0:n],
                            ["gst%d" % gs], wpart(("GT", b, toff)))

                prep_u(0)
                for i_ in range(len(allw)):
                    if i_ + 1 < len(allw):
                        prep_u(i_ + 1)
                    comp_u(i_)
                chk(5 + 5 * l)
                S.barrier()
                arena.reset()
                alloc_norm_bufs()
                ctxb["mtmp"] = [arena.alloc([128, D], F32) for _ in range(2)]
                wdn = arena.alloc([128, 22, D], BF16)
                gTw = [arena.alloc([128, 22, 512], BF16) for _ in range(3)]
                gb = [arena.alloc([128, D], F32) for _ in range(2)]
                for c_ in range(22):
                    DMA("pool", wdn[:, c_, :], ffn_down_w[l][c_ * 128:(c_ + 1) * 128, :], [], [("wdn", c_)])
                dc = {"w": 0, "m": 0}
                load_gb(l, 1, 2, gb[1], "gbf1")
                dw = []
                for b in range(NB):
                    first = True
                    for (src, sname, L, toff, row, dst, dname, is_out) in final(b):
                        o = 0
                        while o < L:
                            nt = min(512, L - o)
                            dw.append((b, src, sname, toff, row, dst, dname, is_out, o, nt, first))
                            first = False
                            o += nt

                def load_d(i):
                    (b, src, sname, toff, row, dst, dname, is_out, o, nt, first) = dw[i]
                    ws = i % 3
                    dma3("sp", lambda j: gTw[ws][:, j, 0:nt],
                         lambda j: GT[b, j * 128:(j + 1) * 128, toff + o:toff + o + nt], 22,
                         rparts(("GT", b, toff)), ["gTw%d" % ws])

                def comp_d(i):
                    (b, src, sname, toff, row, dst, dname, is_out, o, nt, first) = dw[i]
                    ws = i % 3
                    if first:
                        load_gb(l, 1, b, gb[0], "gbf0")
                    for i2 in range(nt // 128):
                        ms = dc["m"] % 3
                        dc["m"] += 1
                        mix = PS2[1 + ms]
                        for hf in range(2):
                            for c in range(22):
                                PE(lambda e: e.matmul(
                                    mix[:, hf * 512:(hf + 1) * 512], lhsT=gTw[ws][:, c, i2 * 128:(i2 + 1) * 128],
                                    rhs=wdn[:, c, hf * 512:(hf + 1) * 512], start=(c == 0), stop=(c == 21)),
                                   ["gTw%d" % ws, ("wdn", c)], ["ps%d" % (2 + 2 * ms + hf)])
                        r0 = o + i2 * 128
                        g_ = gb[0] if row < 2 else gb[1]
                        residual_update(mix[:, :], ["ps%d" % (2 + 2 * ms), "ps%d" % (3 + 2 * ms)], src,
                                        [(sname, b, r0 // 128)], dst, [(dname, b, r0 // 128)], r0, 128, g_,
                                        "gbf0" if row < 2 else "gbf1", is_output=is_out)

                load_d(0)
                if len(dw) > 1:
                    load_d(1)
                for i_ in range(len(dw)):
                    if i_ + 2 < len(dw):
                        load_d(i_ + 2)
                    comp_d(i_)

            ffn_layer(0,
                      lambda b: [(XA[b], "XA", T, 0, b), (CA[b], "CA", CT, T, 2)],
                      lambda b: [(XA[b], "XA", T, 0, b, XB[b], "XB", False), (CA[b], "CA", CT, T, 2, CB[b], "CB", False)])

            UC1 = dscr("UC1", [NB, D, T], BF16)
            OG1 = dscr("OG1", [NB, D, T], BF16)
            QT1 = dscr("QT1", [NB, D, T], BF16)
            KT1 = dscr("KT1", [NB, D, TS], BF16)
            KK1 = dscr("KK1", [NB, TS, D], BF16)
            VV1 = dscr("VV1", [NB, TS, D], BF16)
            GG1 = dscr("GG1", [NB, TS, 16])
            SC1 = dscr("SC1", [NB, TS, 24])
            DC1 = dscr("DC1", [NB, 2 * 4 * 18])

            chk(6)
            S.barrier()
            arena.reset()
            alloc_norm_bufs()
            win1 = arena.alloc([128, 8, 3088], BF16)
            qw = arena.alloc([128, 4, 2, 256], BF16)
            kw = arena.alloc([128, 4, 2, 256], BF16)
            cwr = arena.alloc([128, 3, 8], F32)
            cbr = arena.alloc([128, 8], F32)
            gbias = arena.alloc([128, 16], F32)
            hT1 = [arena.alloc([128, 8, 512], BF16) for _ in range(2)]
            ucw = [arena.alloc([128, 8, 512], BF16) for _ in range(2)]
            ta1 = [arena.alloc([128, 512], F32) for _ in range(2)]
            tb1 = [arena.alloc([128, 512], F32) for _ in range(2)]
            fst = [arena.alloc([128, 512], BF16) for _ in range(4)]
            tst = [arena.alloc([128, D], BF16) for _ in range(2)]
            gst1 = [arena.alloc([128, 16], F32) for _ in range(2)]
            load_wb(win1, rec_in_w, 8, [(0, 1024, "u"), (1024, 1024, "v"), (3072, 16, "g"), (2048, 1024, "o")], "win1")
            for h in range(4):
                for dc in range(2):
                    DMA("pool", qw[:, h, dc, :], rec_q_w[h, dc * 128:(dc + 1) * 128, :], [], ["qw"])
                    DMA("pool", kw[:, h, dc, :], rec_k_w[h, dc * 128:(dc + 1) * 128, :], [], ["kw"])
            for j in range(3):
                DMA("sp", cwr[:, j, :], rec_conv_w[j].rearrange("(c p) -> p c", p=128), [], ["cwr"])
            DMA("sp", cbr, rec_conv_b.rearrange("(c p) -> p c", p=128), [], ["cbr"])
            DMA("sp", gbias, rec_gate_b.partition_broadcast(128), [], ["gbias"])
            c1 = {"w": 0, "p": 0, "f": 0, "t": 0, "g": 0}

            def nbank():
                c1["p"] += 1
                return c1["p"] % 4

            allw1 = []
            for b in range(NB):
                for (src, sname, L, toff, row, isx) in ((CB[b], "CB", CT, 0, 2, False), (XB[b], "XB", T, CT, b, True)):
                    for w_ in make_windows(L):
                        allw1.append((b, src, sname, L, toff, row, isx) + w_)

            def prep_1(i):
                (b, src, sname, L, toff, row, isx, r0, nr, c0, rz, ncols, o, n) = allw1[i]
                hT, hres = hT1[i % 2], "hT1_%d" % (i % 2)
                if c0 == 1:
                    V(lambda e: e.memset(hT[:, :, 0:1], 0.0), [], [hres])
                if rz:
                    V(lambda e: e.memset(hT[:, :, c0 + nr:c0 + nr + 1], 0.0), [], [hres])
                rr_ = r0
                while rr_ < r0 + nr:
                    n_ = min(128, r0 + nr - rr_)
                    make_hT(src, xres_fn(sname, b), rr_, n_, A1[1][:, row, :], SH1[1][:, row, :], "A1_1",
                            hT, hres, c0 + rr_ - r0)
                    rr_ += n_

            def comp_1(i):
                (b, src, sname, L, toff, row, isx, r0, nr, c0, rz, ncols, o, n) = allw1[i]
                hT, hres = hT1[i % 2], "hT1_%d" % (i % 2)
                uc_, ures = ucw[i % 2], "ucw%d" % (i % 2)

                def fm_proj(col0, pbk):
                    for k in range(8):
                        PE(lambda e: e.matmul(bank(pbk)[:, 0:ncols], lhsT=win1[:, k, col0:col0 + 128],
                                              rhs=hT[:, k, 0:ncols], start=(k == 0), stop=(k == 7)),
                           [("win1", "u" if col0 < 1024 else "o"), hres], ["ps%d" % pbk])

                for j in range(8):
                    pbk = nbank()
                    s2 = j % 2
                    fm_proj(j * 128, pbk)
                    A(lambda e: e.activation(out=ta1[s2][:, 0:n], in_=bank(pbk)[:, 1:1 + n], func=AF.Identity,
                                             scale=cwr[:, 1, j:j + 1], bias=cbr[:, j:j + 1]),
                      ["ps%d" % pbk, "cwr", "cbr"], ["ta1_%d" % s2])
                    V(lambda e: e.scalar_tensor_tensor(out=tb1[s2][:, 0:n], in0=bank(pbk)[:, 0:n],
                                                       scalar=cwr[:, 0, j:j + 1], in1=ta1[s2][:, 0:n],
                                                       op0=ALU.mult, op1=ALU.add),
                      ["ps%d" % pbk, "cwr", "ta1_%d" % s2], ["tb1_%d" % s2])
                    V(lambda e: e.scalar_tensor_tensor(out=ta1[s2][:, 0:n], in0=bank(pbk)[:, 2:2 + n],
                                                       scalar=cwr[:, 2, j:j + 1], in1=tb1[s2][:, 0:n],
                                                       op0=ALU.mult, op1=ALU.add),
                      ["ps%d" % pbk, "cwr", "tb1_%d" % s2], ["ta1_%d" % s2])
                    A(lambda e: e.activation(out=uc_[:, j, 0:n], in_=ta1[s2][:, 0:n], func=AF.Silu),
                      ["ta1_%d" % s2], [ures])
                    if isx:
                        DMA(STQ, UC1[b, j * 128:(j + 1) * 128, o:o + n], uc_[:, j, 0:n], [ures],
                            wpart(("UC1", b)))
                if isx:
                    for j in range(8):
                        pbk = nbank()
                        fs = c1["f"] % 4
                        c1["f"] += 1
                        fm_proj(2048 + j * 128, pbk)
                        A(lambda e: e.activation(out=fst[fs][:, 0:n], in_=bank(pbk)[:, 1:1 + n],
                                                 func=AF.Sigmoid), ["ps%d" % pbk], ["fst%d" % fs])
                        DMA(STQ, OG1[b, j * 128:(j + 1) * 128, o:o + n], fst[fs][:, 0:n], ["fst%d" % fs],
                            wpart(("OG1", b)))
                for h in range(4):
                    for ec in range(2):
                        for (wt_, wres_, dst, scl, need) in ((kw, "kw", KT1, 1.0 / 16, True),
                                                             (qw, "qw", QT1, 1.0, isx)):
                            if not need:
                                continue
                            pbk = nbank()
                            fs = c1["f"] % 4
                            c1["f"] += 1
                            for dc in range(2):
                                PE(lambda e: e.matmul(bank(pbk)[:, 0:n],
                                                      lhsT=wt_[:, h, dc, ec * 128:(ec + 1) * 128],
                                                      rhs=uc_[:, 2 * h + dc, 0:n], start=(dc == 0),
                                                      stop=(dc == 1)), [wres_, ures], ["ps%d" % pbk])
                            A(lambda e: e.activation(out=fst[fs][:, 0:n], in_=bank(pbk)[:, 0:n], func=AF.Copy,
                                                     scale=scl), ["ps%d" % pbk], ["fst%d" % fs])
                            tcol = (toff + o) if dst is KT1 else o
                            DMA(STQ, dst[b, h * 256 + ec * 128:h * 256 + (ec + 1) * 128, tcol:tcol + n],
                                fst[fs][:, 0:n], ["fst%d" % fs],
                                wpart(("KT1", b)) if dst is KT1 else wpart(("QT1", b)))
                i0 = 0
                while i0 < n:
                    m = min(128, n - i0)
                    trow = toff + o + i0
                    ts_ = c1["t"] % 2
                    c1["t"] += 1
                    mix = PS2[2]
                    for h in range(4):
                        for dc in range(2):
                            PE(lambda e: e.matmul(mix[0:m, h * 256:(h + 1) * 256],
                                                  lhsT=uc_[:, 2 * h + dc, i0:i0 + m], rhs=kw[:, h, dc, :],
                                                  start=(dc == 0), stop=(dc == 1)),
                               [ures, "kw"], ["ps4", "ps5"])
                    A(lambda e: e.activation(out=tst[ts_][0:m, :], in_=mix[0:m, :], func=AF.Copy,
                                             scale=1.0 / 16), ["ps4", "ps5"], ["tst%d" % ts_])
                    DMA(STQ, KK1[b, trow:trow + m, :], tst[ts_][0:m, :], ["tst%d" % ts_], wpart(("KK1", b)))
                    ts_ = c1["t"] % 2
                    c1["t"] += 1
                    mix = PS2[3]
                    for hf in range(2):
                        for k in range(8):
                            PE(lambda e: e.matmul(mix[0:m, hf * 512:(hf + 1) * 512],
                                                  lhsT=hT[:, k, 1 + i0:1 + i0 + m],
                                                  rhs=win1[:, k, 1024 + hf * 512:1024 + (hf + 1) * 512],
                                                  start=(k == 0), stop=(k == 7)),
                               [hres, ("win1", "v")], ["ps%d" % (6 + hf)])
                    A(lambda e: e.activation(out=tst[ts_][0:m, :], in_=mix[0:m, :], func=AF.Copy),
                      ["ps6", "ps7"], ["tst%d" % ts_])
                    DMA(STQ, VV1[b, trow:trow + m, :], tst[ts_][0:m, :], ["tst%d" % ts_], wpart(("VV1", b)))
                    gs = c1["g"] % 2
                    c1["g"] += 1
                    pbk = nbank()
                    for k in range(8):
                        PE(lambda e: e.matmul(bank(pbk)[0:m, 0:16], lhsT=hT[:, k, 1 + i0:1 + i0 + m],
                                              rhs=win1[:, k, 3072:3088], start=(k == 0), stop=(k == 7)),
                           [hres, ("win1", "g")], ["ps%d" % pbk])
                    V(lambda e: e.tensor_tensor(out=gst1[gs][0:m, :], in0=bank(pbk)[0:m, 0:16],
                                                in1=gbias[0:m, :], op=ALU.add),
                      ["ps%d" % pbk, "gbias"], ["gst1_%d" % gs])
                    DMA(STQ, GG1[b, trow:trow + m, :], gst1[gs][0:m, :], ["gst1_%d" % gs], wpart(("GG1", b)))
                    i0 += m


            prep_1(0)
            for i_ in range(len(allw1)):
                if i_ + 1 < len(allw1):
                    prep_1(i_ + 1)
                comp_1(i_)

            chk(7)
            S.barrier()
            arena.reset()
            gg = arena.alloc([128, 18, 16], F32)
            ones4 = arena.alloc([4, TS], F32)
            IG = [arena.alloc([4, TS], F32) for _ in range(2)]
            FG = [arena.alloc([4, TS], F32) for _ in range(2)]
            t_a = arena.alloc([4, TS], F32)
            t_b = arena.alloc([4, TS], F32)
            Bc = arena.alloc([4, TS], F32)
            Mc = arena.alloc([4, TS], F32)
            QY = [[arena.alloc([4, TS], F32) for _ in range(3)] for _ in range(2)]
            Mpv = arena.alloc([4, 18], F32)
            dcy = arena.alloc([4, 18], F32)
            scs = arena.alloc([128, 18, 24], F32)
            V(lambda e: e.memset(ones4, 1.0), [], ["ones4"])

            def lb_tile(q):
                return q + 2 if q < 16 else q - 16

            for b in range(NB):
                dma3("sp", lambda j: gg[:, j, :], lambda j: GG1[b, j * 128:(j + 1) * 128, :], 18, rparts(("GG1", b)), ["gg"])
                for dr in range(2):
                    for ti, dstt, dres in ((0, IG[dr], "IG%d" % dr), (1, FG[dr], "FG%d" % dr)):
                        ty = dr * 2 + ti
                        for q in range(18):
                            tl = q if dr == 0 else lb_tile(q)
                            pq = PS2[q // 8]
                            PE(lambda e: e.matmul(pq[0:4, (q % 8) * 128:(q % 8 + 1) * 128],
                                                  lhsT=gg[:, tl, ty * 4:(ty + 1) * 4], rhs=ident[:, :], start=True,
                                                  stop=True), ["gg", "ident"], ["ps%d" % (2 * (q // 8) + (q % 8) // 4)])
                        for pi in range(3):
                            ncol = 1024 if pi < 2 else 256
                            A(lambda e: e.activation(out=dstt[:, pi * 1024:pi * 1024 + ncol], in_=PS2[pi][0:4, 0:ncol],
                                                     func=AF.Copy), ["ps%d" % (2 * pi), "ps%d" % (2 * pi + 1)], [dres])
                for dr in range(2):
                    ig, fg = IG[dr], FG[dr]
                    igr, fgr = "IG%d" % dr, "FG%d" % dr

                    def dview(ap):
                        return ap if dr == 0 else ap[:, ::-1]

                    A(lambda e: e.activation(out=t_a, in_=fg, func=AF.Abs), [fgr], ["t_a"])
                    A(lambda e: e.activation(out=t_a, in_=t_a, func=AF.Exp, scale=-1.0), ["t_a"], ["t_a"])
                    A(lambda e: e.activation(out=t_a, in_=t_a, func=AF.Ln, bias=1.0), ["t_a"], ["t_a"])
                    V(lambda e: e.tensor_scalar(out=t_b, in0=fg, scalar1=0.0, scalar2=None, op0=ALU.min), [fgr], ["t_b"])
                    V(lambda e: e.tensor_tensor(out=t_b, in0=t_b, in1=t_a, op=ALU.subtract), ["t_a", "t_b"], ["t_b"])
                    V(lambda e: e.tensor_tensor_scan(out=dview(Bc), data0=dview(ones4), data1=dview(t_b), initial=0.0,
                                                     op0=ALU.mult, op1=ALU.add), ["ones4", "t_b"], ["Bc"])
                    V(lambda e: e.tensor_tensor(out=t_a, in0=ig, in1=Bc, op=ALU.subtract), [igr, "Bc"], ["t_a"])
                    V(lambda e: e.tensor_tensor_scan(out=dview(Mc), data0=dview(ones4), data1=dview(t_a), initial=0.0,
                                                     op0=ALU.mult, op1=ALU.max), ["ones4", "t_a"], ["Mc"])
                    M3 = Mc.rearrange("p (q t) -> p q t", t=128)
                    a3 = t_a.rearrange("p (q t) -> p q t", t=128)
                    B3 = Bc.rearrange("p (q t) -> p q t", t=128)
                    V(lambda e: e.memset(Mpv, 0.0), [], ["Mpv"])
                    if dr == 0:
                        V(lambda e: e.tensor_copy(out=Mpv[:, 1:18], in_=M3[:, 0:17, 127]), ["Mc"], ["Mpv"])
                        Mend = M3[:, :, 127]
                    else:
                        V(lambda e: e.tensor_copy(out=Mpv[:, 0:17], in_=M3[:, 1:18, 0]), ["Mc"], ["Mpv"])
                        Mend = M3[:, :, 0]
                    Mpb = Mpv.unsqueeze(2).to_broadcast([4, 18, 128])
                    q0, q1, q2 = QY[dr]
                    qr = ["QY%d_%d" % (dr, i) for i in range(3)]
                    V(lambda e: e.tensor_tensor(out=q0.rearrange("p (q t) -> p q t", t=128), in0=a3, in1=Mpb,
                                                op=ALU.subtract), ["t_a", "Mpv"], [qr[0]])
                    A(lambda e: e.activation(out=q0, in_=q0, func=AF.Exp), [qr[0]], [qr[0]])
                    V(lambda e: e.tensor_tensor(out=q1.rearrange("p (q t) -> p q t", t=128), in0=a3,
                                                in1=Mend.unsqueeze(2).to_broadcast([4, 18, 128]), op=ALU.subtract),
                      ["t_a", "Mc"], [qr[1]])
                    A(lambda e: e.activation(out=q1, in_=q1, func=AF.Exp), [qr[1]], [qr[1]])
                    V(lambda e: e.scalar_tensor_tensor(out=q2.rearrange("p (q t) -> p q t", t=128), in0=B3, scalar=-1.0,
                                                       in1=Mpb, op0=ALU.mult, op1=ALU.subtract), ["Bc", "Mpv"], [qr[2]])
                    A(lambda e: e.activation(out=q2, in_=q2, func=AF.Exp), [qr[2]], [qr[2]])
                    V(lambda e: e.tensor_tensor(out=dcy, in0=Mpv, in1=Mend, op=ALU.subtract), ["Mpv", "Mc"], ["dcy"])
                    A(lambda e: e.activation(out=dcy, in_=dcy, func=AF.Exp), ["dcy"], ["dcy"])
                    dcv = DC1[b].rearrange("(d h q) -> d h q", d=2, h=4)[dr]
                    if dr == 0:
                        DMA(STQ, dcv, dcy, ["dcy"], wpart(("DC1", b)))
                    else:
                        DMA(STQ, dcv[:, 2:18], dcy[:, 0:16], ["dcy"], wpart(("DC1", b)))
                        DMA(STQ, dcv[:, 0:2], dcy[:, 16:18], ["dcy"], wpart(("DC1", b)))
                    pst = bank(6)
                    for q in range(18):
                        tl = q if dr == 0 else lb_tile(q)
                        for qi in range(3):
                            col = tl * 24 + (dr * 3 + qi) * 4
                            PE(lambda e: e.matmul(pst[:, col:col + 4], lhsT=QY[dr][qi][:, q * 128:(q + 1) * 128],
                                                  rhs=ident[0:4, 0:4], start=True, stop=True), [qr[qi], "ident"], ["ps6"])
                A(lambda e: e.activation(out=scs, in_=bank(6)[:, 0:432].rearrange("p (n c) -> p n c", c=24), func=AF.Copy),
                  ["ps6"], ["scs"])
                dma3(STQ, lambda j: SC1[b, j * 128:(j + 1) * 128, :], lambda j: scs[:, j, :], 18, ["scs"], wpart(("SC1", b)))

            chk(8)
            S.barrier()
            arena.reset()
            alloc_norm_bufs()
            ctxb["mtmp"] = [arena.alloc([128, D], F32) for _ in range(2)]
            wo1 = arena.alloc([128, 8, D], BF16)
            QTh = arena.alloc([128, 2, T], BF16)
            KTh = arena.alloc([128, 2, TS], BF16)
            KKh = arena.alloc([128, 18, 256], BF16)
            VVh = arena.alloc([128, 18, 257], BF16)
            ogh = arena.alloc([128, 2, T], BF16)
            uch = arena.alloc([128, 2, T], BF16)
            hnT = arena.alloc([128, 2, T], BF16)
            tyy = arena.alloc([128, T], F32)
            scb = arena.alloc([128, 18, 24], F32)
            dcb = arena.alloc([128, 144], F32)
            lmk = arena.alloc([128, 2, 128], F32)
            Sst = [arena.alloc([128, 2, 257], F32) for _ in range(2)]
            Sbf = [arena.alloc([128, 2, 257], BF16) for _ in range(2)]
            hs = arena.alloc([128, 16, 256], F32)
            ATb = [arena.alloc([128, 128], BF16) for _ in range(4)]
            Ktl = [arena.alloc([128, 256], BF16) for _ in range(4)]
            dnn = [arena.alloc([128, 2], F32) for _ in range(4)]
            hnb = [arena.alloc([128, 256], BF16) for _ in range(2)]
            yT = arena.alloc([128, 8, T], BF16)
            rng = arena.alloc([128, 8], F32)
            rsk = arena.alloc([128, 8], F32)
            gb1 = arena.alloc([128, D], F32)
            load_w(wo1, rec_out_w, 8, D, "wo1")
            for m_ in range(2):
                DMA("sp", lmk[:, m_, :], lmask_in[m_], [], ["lmk"])
            DMA("sp", rng, rec_norm_g.rearrange("(c p) -> p c", p=128), [], ["rng"])
            DMA("sp", rsk, rec_skip.rearrange("(c p) -> p c", p=128), [], ["rsk"])
            V(lambda e: e.memset(VVh[:, :, 256:257], 1.0), [], ["VVh"])
            c3 = {"i": 0}
            for b in range(NB):
                dma3("sp", lambda j: scb[:, j, :], lambda j: SC1[b, j * 128:(j + 1) * 128, :], 18, rparts(("SC1", b)), ["scb"])
                DMA("sp", dcb, DC1[b].partition_broadcast(128), rparts(("DC1", b)), ["dcb"])
                load_gb(1, 0, b, gb1, "gb1")
                for h in range(4):
                    dma3("sp", lambda j: QTh[:, j, :], lambda j: QT1[b, h * 256 + j * 128:h * 256 + (j + 1) * 128, :], 2,
                         rparts(("QT1", b)), ["QTh"])
                    dma3("sp", lambda j: KTh[:, j, :], lambda j: KT1[b, h * 256 + j * 128:h * 256 + (j + 1) * 128, :], 2,
                         rparts(("KT1", b)), ["KTh"])
                    dma3("sp", lambda j: KKh[:, j, :], lambda j: KK1[b, j * 128:(j + 1) * 128, h * 256:(h + 1) * 256], 18,
                         rparts(("KK1", b)), ["KKh"])
                    dma3("sp", lambda j: VVh[:, j, 0:256], lambda j: VV1[b, j * 128:(j + 1) * 128, h * 256:(h + 1) * 256], 18,
                         rparts(("VV1", b)), ["VVh"])
                    dma3("sp", lambda j: ogh[:, j, :], lambda j: OG1[b, h * 256 + j * 128:h * 256 + (j + 1) * 128, :], 2,
                         rparts(("OG1", b)), ["ogh"])
                    dma3("sp", lambda j: uch[:, j, :], lambda j: UC1[b, h * 256 + j * 128:h * 256 + (j + 1) * 128, :], 2,
                         rparts(("UC1", b)), ["uch"])
                    for dr in range(2):
                        V(lambda e: e.memset(Sst[dr], 0.0), [], ["Sst%d" % dr])
                        V(lambda e: e.memset(Sbf[dr], 0.0), [], ["Sbf%d" % dr])
                    order = [list(range(18)), [1, 0] + list(range(17, 1, -1))]
                    first_dir_done = set()
                    for step in range(18):
                        st_ = []
                        for dr in range(2):
                            tl = order[dr][step]
                            st_.append(dict(dr=dr, tl=tl, isx=tl >= 2, xt=tl - 2, pb0=dr * 4, bi=dr * 2 + step % 2,
                                            pslot=dr * 4 + step % 2, sres="Sst%d" % dr, bres="Sbf%d" % dr,
                                            colA=(dr * 3 + 0) * 4 + h, colB=(dr * 3 + 1) * 4 + h,
                                            colF=(dr * 3 + 2) * 4 + h, dcol=(dr * 4 + h) * 18 + tl))
                        for q_ in st_:
                            dr, tl, xt_, bi, pslot = q_["dr"], q_["tl"], q_["xt"], q_["bi"], q_["pslot"]
                            if q_["isx"]:
                                pA = bank(pslot)[:, 0:128]
                                for dc in range(2):
                                    PE(lambda e: e.matmul(pA[:, 0:128], lhsT=KTh[:, dc, tl * 128:(tl + 1) * 128],
                                                          rhs=QTh[:, dc, xt_ * 128:(xt_ + 1) * 128], start=(dc == 0),
                                                          stop=(dc == 1)), ["KTh", "QTh"], ["ps%d" % pslot])
                                V(lambda e: e.scalar_tensor_tensor(out=ATb[bi], in0=pA[:, 0:128],
                                                                   scalar=scb[:, tl, q_["colA"]:q_["colA"] + 1],
                                                                   in1=lmk[:, dr, :], op0=ALU.mult, op1=ALU.mult),
                                  ["ps%d" % pslot, "scb", "lmk"], ["ATb%d" % bi])
                            if step < 17:
                                A(lambda e: e.activation(out=Ktl[bi], in_=KKh[:, tl, :], func=AF.Copy,
                                                         scale=scb[:, tl, q_["colB"]:q_["colB"] + 1]),
                                  ["KKh", "scb"], ["Ktl%d" % bi])
                        if step < 17:
                            for q_ in st_:
                                tl, bi, pb0 = q_["tl"], q_["bi"], q_["pb0"]
                                for dc in range(2):
                                    pS = bank(pb0 + 2 + dc)
                                    PE(lambda e: e.matmul(pS[:, 0:257], lhsT=Ktl[bi][:, dc * 128:(dc + 1) * 128],
                                                          rhs=VVh[:, tl, :], start=True, stop=True),
                                       ["Ktl%d" % bi, "VVh"], ["ps%d" % (pb0 + 2 + dc)])
                        for q_ in st_:
                            dr, tl, xt_, bi, pslot = q_["dr"], q_["tl"], q_["xt"], q_["bi"], q_["pslot"]
                            if q_["isx"]:
                                pO = bank(pslot)[:, 128:512]
                                PE(lambda e: e.matmul(pO[:, 0:257], lhsT=ATb[bi], rhs=VVh[:, tl, :], start=True, stop=False),
                                   ["ATb%d" % bi, "VVh"], ["ps%d" % pslot])
                                for dc in range(2):
                                    PE(lambda e: e.matmul(pO[:, 0:257], lhsT=QTh[:, dc, xt_ * 128:(xt_ + 1) * 128],
                                                          rhs=Sbf[dr][:, dc, :], start=False, stop=(dc == 1)),
                                       ["QTh", q_["bres"]], ["ps%d" % pslot])
                        if step < 17:
                            for q_ in st_:
                                dr, pb0 = q_["dr"], q_["pb0"]
                                for dc in range(2):
                                    pS = bank(pb0 + 2 + dc)
                                    V(lambda e: e.scalar_tensor_tensor(out=Sst[dr][:, dc, :], in0=Sst[dr][:, dc, :],
                                                                       scalar=dcb[:, q_["dcol"]:q_["dcol"] + 1],
                                                                       in1=pS[:, 0:257], op0=ALU.mult, op1=ALU.add),
                                      [q_["sres"], "dcb", "ps%d" % (pb0 + 2 + dc)], [q_["sres"]])
                        for q_ in st_:
                            dr, tl, xt_, bi, pslot = q_["dr"], q_["tl"], q_["xt"], q_["bi"], q_["pslot"]
                            if q_["isx"]:
                                pO = bank(pslot)[:, 128:512]
                                dn = dnn[bi]
                                A(lambda e: e.activation(out=dn[:, 0:1], in_=pO[:, 256:257], func=AF.Abs),
                                  ["ps%d" % pslot], ["dnn%d" % bi])
                                V(lambda e: e.tensor_tensor(out=dn[:, 0:1], in0=dn[:, 0:1],
                                                            in1=scb[:, tl, q_["colF"]:q_["colF"] + 1], op=ALU.max),
                                  ["dnn%d" % bi, "scb"], ["dnn%d" % bi])
                                V(lambda e: e.reciprocal(out=dn[:, 1:2], in_=dn[:, 0:1]), ["dnn%d" % bi], ["dnn%d" % bi])
                                hres_ = ("hs", xt_)
                                if xt_ not in first_dir_done:
                                    first_dir_done.add(xt_)
                                    V(lambda e: e.tensor_scalar(out=hs[:, xt_, :], in0=pO[:, 0:256], scalar1=dn[:, 1:2],
                                                                scalar2=None, op0=ALU.mult),
                                      ["ps%d" % pslot, "dnn%d" % bi], [hres_])
                                else:
                                    V(lambda e: e.scalar_tensor_tensor(out=hs[:, xt_, :], in0=pO[:, 0:256],
                                                                       scalar=dn[:, 1:2], in1=hs[:, xt_, :],
                                                                       op0=ALU.mult, op1=ALU.add),
                                      ["ps%d" % pslot, "dnn%d" % bi, hres_], [hres_])
                        if step < 17:
                            for q_ in st_:
                                dr = q_["dr"]
                                A(lambda e: e.activation(out=Sbf[dr], in_=Sst[dr], func=AF.Copy), [q_["sres"]], [q_["bres"]])
                    for xt_ in range(16):
                        ci = xt_ % 2
                        stt = ctxb["st"][xt_ % 3]
                        sres_ = "st%d" % (xt_ % 3)
                        junk = ctxb["junk"]
                        A(lambda e: e.activation(out=junk[:, 0:256], in_=hs[:, xt_, :], func=AF.Square,
                                                 accum_out=stt[:, 0:1]), [("hs", xt_)], ["junk", sres_])
                        A(lambda e: e.activation(out=stt[:, 1:2], in_=stt[:, 0:1], func=AF.Sqrt, scale=1.0 / 256,
                                                 bias=epsb[:, :]), [sres_, "epsb"], [sres_])
                        V(lambda e: e.reciprocal(out=stt[:, 1:2], in_=stt[:, 1:2]), [sres_], [sres_])
                        V(lambda e: e.tensor_scalar(out=hnb[ci], in0=hs[:, xt_, :], scalar1=stt[:, 1:2], scalar2=None,
                                                    op0=ALU.mult), [("hs", xt_), sres_], ["hnb%d" % ci])
                        ptv = bank_bf(6 + ci).rearrange("p (k t) -> p k t", t=128)
                        for dc in range(2):
                            PE(lambda e: e.transpose(out=ptv[:, dc, :], in_=hnb[ci][:, dc * 128:(dc + 1) * 128],
                                                     identity=identb[:]), ["hnb%d" % ci, "identb"], ["ps%d" % (6 + ci)])
                        A(lambda e: e.activation(out=hnT[:, :, xt_ * 128:(xt_ + 1) * 128], in_=ptv[:, 0:2, :],
                                                 func=AF.Copy), ["ps%d" % (6 + ci)], ["hnT"])
                    for dc in range(2):
                        fcx = 2 * h + dc
                        V(lambda e: e.tensor_scalar(out=tyy, in0=hnT[:, dc, :], scalar1=rng[:, fcx:fcx + 1], scalar2=None,
                                                    op0=ALU.mult), ["hnT", "rng"], ["tyy"])
                        V(lambda e: e.scalar_tensor_tensor(out=tyy, in0=uch[:, dc, :], scalar=rsk[:, fcx:fcx + 1], in1=tyy,
                                                           op0=ALU.mult, op1=ALU.add), ["uch", "rsk", "tyy"], ["tyy"])
                        V(lambda e: e.tensor_tensor(out=yT[:, fcx, :], in0=tyy, in1=ogh[:, dc, :], op=ALU.mult),
                          ["tyy", "ogh"], [("yT", fcx)])
                for n in range(16):
                    mix = PS2[n % 2]
                    for hf in range(2):
                        for k in range(8):
                            PE(lambda e: e.matmul(mix[:, hf * 512:(hf + 1) * 512], lhsT=yT[:, k, n * 128:(n + 1) * 128],
                                                  rhs=wo1[:, k, hf * 512:(hf + 1) * 512], start=(k == 0), stop=(k == 7)),
                               [("yT", k), "wo1"], ["ps%d" % (2 * (n % 2) + hf)])
                    residual_update(mix[:, :], ["ps%d" % (2 * (n % 2)), "ps%d" % (2 * (n % 2) + 1)], XB[b],
                                    [("XB", b, n)], XA[b], [("XA", b, n)], n * 128, 128, gb1, "gb1")

            ffn_layer(1,
                      lambda b: [(XA[b], "XA", T, 0, b)],
                      lambda b: [(XA[b], "XA", T, 0, b, out[b], "out", True)])

        except _Stop:
            S.barrier()
        S.emit(st)
        build_program.stats = S.stats
    return nc


def _consts():
    ident = np.eye(128, dtype=np.float32)
    inv = (10000.0 ** (-np.arange(16, dtype=np.float32) / 16)).astype(np.float32)
    t = np.arange(T)
    row = (t // 64).astype(np.float32)
    col = (t % 64).astype(np.float32)
    rc = np.zeros((128, T), np.float32)
    rs = np.zeros((128, T), np.float32)
    for p in range(128):
        d = p % 64
        axis, half, f = d // 32, (d % 32) // 16, d % 16
        pos = row if axis == 0 else col
        ang = (pos * inv[f]).astype(np.float32)
        rc[p] = np.cos(ang)
        rs[p] = np.sin(ang) * (-1.0 if half == 0 else 1.0)
    j = np.arange(128)[:, None]
    i = np.arange(128)[None, :]
    prev = (j >= i).astype(np.float32)
    nxt = (j <= i).astype(np.float32)
    amask = np.stack([np.tile(prev, (1, 4)), np.tile(nxt, (1, 4))]).astype(np.float32)
    pe = np.zeros((4, 2, 8), np.float32)
    for g in range(4):
        w = 2 ** (g + 1)
        half = w // 2
        for k in range(half):
            pe[g, 0, k] = 1.0 / (k + half)
        for k in range(half - 1):
            pe[g, 1, k] = 1.0 / (2 * half - 1 - k)
    pool_edge = np.tile(pe.reshape(1, 64), (128, 1)).astype(np.float32)
    lmask = np.stack([(j <= i).astype(np.float32), (j >= i).astype(np.float32)])
    return dict(ident=ident, rope_c=rc, rope_s=rs, amask=amask, pool_edge=pool_edge, lmask=lmask)


def _perm_head():
    p = np.zeros(64, np.int64)
    for d in range(64):
        axis, half, f = d // 32, (d % 32) // 16, d % 16
        p[d] = axis * 32 + (1 - half) * 16 + f
    return p


def _prep_shared(inp):
    w = np.asarray(inp["attn_in_w"][0], np.float32)
    ph = _perm_head()
    wz = np.concatenate([w, np.zeros((w.shape[0], 1), np.float32)], axis=1)
    Z = np.full(64, w.shape[1], np.int64)
    q = [np.arange(c * 128, (c + 1) * 128) for c in range(4)]
    k0 = 512 + np.arange(64)
    k1 = 576 + np.arange(64)
    kz = [np.concatenate([k0, Z]), np.concatenate([Z, k0]), np.concatenate([k1, Z]), np.concatenate([Z, k1])]
    base = q + kz

    def partner(idx):
        o = idx.copy()
        for h0 in range(0, 128, 64):
            blk = idx[h0:h0 + 64]
            o[h0:h0 + 64] = blk[ph]
        return o

    cols = base + [partner(c) for c in base] + [np.arange(640, 768)] + [np.arange(768 + g * 128, 896 + g * 128) for g in range(4)]
    cols = np.concatenate(cols)
    w = wz
    sh = dict(
        mod_w=np.ascontiguousarray(inp["mod_w"], np.float32),
        mod_b=np.ascontiguousarray(inp["mod_b"], np.float32),
        norm_g=np.ascontiguousarray(inp["norm_g"], np.float32),
        w_in0=np.ascontiguousarray(w[:, cols]),
        attn_sink=np.ascontiguousarray(inp["attn_sink"][0], np.float32),
        pool_w=np.ascontiguousarray(inp["pool_w"][0], np.float32),
        pool_scale=np.ascontiguousarray(inp["pool_scale"][0], np.float32),
        attn_out_w=np.ascontiguousarray(inp["attn_out_w"][0], np.float32),
        rec_in_w=np.ascontiguousarray(inp["rec_in_w"][0], np.float32),
        rec_gate_b=np.ascontiguousarray(inp["rec_gate_b"][0].reshape(16), np.float32),
        rec_conv_w=np.ascontiguousarray(inp["rec_conv_w"][0], np.float32),
        rec_conv_b=np.ascontiguousarray(inp["rec_conv_b"][0], np.float32),
        rec_q_w=np.ascontiguousarray(inp["rec_q_w"][0], np.float32),
        rec_k_w=np.ascontiguousarray(inp["rec_k_w"][0], np.float32),
        rec_norm_g=np.ascontiguousarray(inp["rec_norm_g"][0], np.float32),
        rec_skip=np.ascontiguousarray(inp["rec_skip"][0], np.float32),
        rec_out_w=np.ascontiguousarray(inp["rec_out_w"][0], np.float32),
        ffn_up_w=np.ascontiguousarray(inp["ffn_up_w"], np.float32),
        ffn_conv_w=np.ascontiguousarray(inp["ffn_conv_w"], np.float32),
        ffn_conv_b=np.ascontiguousarray(inp["ffn_conv_b"], np.float32),
        ffn_down_w=np.ascontiguousarray(inp["ffn_down_w"], np.float32),
    )
    sh.update(_consts())
    return sh


def make_in_maps(inp, cores):
    sh = _prep_shared(inp)
    x = np.asarray(inp["x"], np.float32)
    c = np.asarray(inp["c"], np.float32)
    ctx = np.asarray(inp["ctx"], np.float32)
    c_ctx = np.asarray(inp["c_ctx"], np.float32)
    maps = []
    for i in cores:
        m = dict(sh)
        m["x"] = np.ascontiguousarray(x[NB * i:NB * (i + 1)])
        m["ctx"] = np.ascontiguousarray(ctx[NB * i:NB * (i + 1)])
        m["cvec"] = np.ascontiguousarray(np.concatenate([c[NB * i:NB * (i + 1)], c_ctx[None, :]], axis=0))
        maps.append(m)
    return maps


def kernel(**inputs):
    nc = build_program()
    maps = make_in_maps(inputs, list(range(8)))
    res = run_bass_kernel_spmd(nc, maps, core_ids=list(range(8)))
    return np.concatenate([np.asarray(r["out"], np.float32) for r in res.results], axis=0)
```

```python
import math
from contextlib import ExitStack

import numpy as np
import concourse.bass as bass
import concourse.mybir as mybir
from concourse.bass_utils import run_bass_kernel_spmd

F32 = mybir.dt.float32
BF16 = mybir.dt.bfloat16
U8 = mybir.dt.uint8
AF = mybir.ActivationFunctionType
ALU = mybir.AluOpType

ENGS = ["pe", "act", "dve", "pool", "sp"]
import os as _os
N_DMA_SEMS = int(_os.environ.get("NDS", "24"))

D = 1024
T = 2048
CT = 256
NB = 2
TS = T + CT
DFF = 2816
EPS = 1e-6


class _Proxy:
    def __getattr__(self, name):
        def f(*a, **k):
            self.call = (name, a, k)
            return self
        return f


class Sched:
    def __init__(self, nc):
        self.nc = nc
        self.ops = {e: [] for e in ENGS}
        self.lastw = {}
        self.readers = {}
        self.dma_rr = {e: 0 for e in ENGS}
        self.dma_last = {}
        self.out_dmas = []
        self.pending_dmas = []

    def add(self, eng, fn, reads=(), writes=(), dma=False, is_output=False, extra_deps=()):
        ops = self.ops[eng]
        me = (eng, len(ops))
        deps = {}

        def dep(p, kind):
            if p is None or p == me:
                return
            if deps.get(p) == "raw":
                return
            deps[p] = kind

        for r in reads:
            dep(self.lastw.get(r), "raw")
        for r in writes:
            dep(self.lastw.get(r), "order")
            for rd in self.readers.get(r, ()):
                dep(rd, "order")
        for p in extra_deps:
            dep(p, "raw")
        prox = _Proxy()
        fn(prox)
        rec = dict(call=prox.call, dma=dma, signal=False, slot=None)
        if dma:
            slot = self.dma_rr[eng] % N_DMA_SEMS
            self.dma_rr[eng] += 1
            rec["slot"] = slot
            prev = self.dma_last.get((eng, slot))
            if prev is not None:
                dep(prev, "raw")
            self.dma_last[(eng, slot)] = me
            self.pending_dmas.append(me)
            if is_output:
                self.out_dmas.append(me)
        final = {}
        for p, kind in deps.items():
            pe, pi = p
            prod = self.ops[pe][pi]
            if pe == eng and not prod["dma"] and not dma:
                if kind == "order" or eng in ("pe", "sp"):
                    continue
            final[p] = kind
        rec["deps"] = final
        ops.append(rec)
        for r in reads:
            lst = self.readers.setdefault(r, [])
            if not dma:
                lst[:] = [q for q in lst if not (q[0] == eng and not self.ops[q[0]][q[1]]["dma"])]
            lst.append(me)
        for r in writes:
            self.lastw[r] = me
            self.readers[r] = []
        return me

    def barrier(self):
        lasts = []
        for e in ENGS:
            for i in range(len(self.ops[e]) - 1, -1, -1):
                if not self.ops[e][i]["dma"]:
                    if not self.ops[e][i].get("nop"):
                        lasts.append((e, i))
                    break
        pend = list(self.pending_dmas)
        self.pending_dmas = []
        for e in ENGS:
            me = self.add(e, lambda eng: eng.nop(), extra_deps=[p for p in lasts if p[0] != e] + pend)
            self.ops[me[0]][me[1]]["nop"] = True

    def emit(self, stack):
        nc = self.nc
        for e in ENGS:
            for rec in self.ops[e]:
                for (pe, pi) in rec["deps"]:
                    self.ops[pe][pi]["signal"] = True
        esem = {e: stack.enter_context(nc.semaphore("s_" + e)) for e in ENGS}
        dsem = {e: [None] * N_DMA_SEMS for e in ENGS}
        for e in ENGS:
            for s in range(min(N_DMA_SEMS, self.dma_rr[e])):
                dsem[e][s] = stack.enter_context(nc.semaphore("d_%s_%d" % (e, s)))
        cnt = {e: 0 for e in ENGS}
        dcnt = {}
        for e in ENGS:
            for rec in self.ops[e]:
                if rec["dma"]:
                    k = (e, rec["slot"])
                    dcnt[k] = dcnt.get(k, 0) + 16
                    rec["ev"] = (k, dcnt[k])
                elif rec["signal"]:
                    cnt[e] += 1
                    rec["ev"] = (e, cnt[e])
                else:
                    rec["ev"] = None
        self.stats = {e: [len(self.ops[e]), 0, cnt[e]] for e in ENGS}
        block = stack.enter_context(nc.Block())
        sched = self

        def semof(key):
            if isinstance(key, tuple):
                return dsem[key[0]][key[1]]
            return esem[key]

        def run(e, eng):
            known = {}
            for rec in sched.ops[e]:
                need = {}
                for (pe, pi) in rec["deps"]:
                    key, val = sched.ops[pe][pi]["ev"]
                    if known.get(key, 0) >= val:
                        continue
                    if need.get(key, 0) < val:
                        need[key] = val
                for key, val in need.items():
                    eng.wait_ge(semof(key), val)
                    known[key] = val
                    sched.stats[e][1] += 1
                nm, a_, k_ = rec["call"]
                ins = getattr(eng, nm)(*a_, **k_)
                if rec["dma"]:
                    ins.then_inc(semof(rec["ev"][0]), 16)
                elif rec["signal"]:
                    ins.then_inc(esem[e], 1)
            return known

        @block.tensor
        def _(eng):
            run("pe", eng)

        @block.scalar
        def _(eng):
            run("act", eng)

        @block.vector
        def _(eng):
            run("dve", eng)

        @block.gpsimd
        def _(eng):
            run("pool", eng)

        @block.sync
        def _(eng):
            known = run("sp", eng)
            for (pe, pi) in sched.out_dmas:
                key, val = sched.ops[pe][pi]["ev"]
                if known.get(key, 0) < val:
                    eng.wait_ge(semof(key), val)
                    known[key] = val


DT_SIZE = {F32: 4, BF16: 2, U8: 1}


class Arena:
    def __init__(self, tens, size):
        self.t = tens
        self.size = size
        self.off = 0

    def reset(self):
        self.off = 0

    def alloc(self, shape, dt):
        n = 1
        for s in shape[1:]:
            n *= s
        nbytes = (n * DT_SIZE[dt] + 63) // 64 * 64
        assert self.off + nbytes <= self.size, ("arena overflow", self.off, nbytes, self.size)
        ap = self.t[0:shape[0], self.off:self.off + n * DT_SIZE[dt]].bitcast(dt)
        self.off += nbytes
        if len(shape) == 3:
            ap = ap.rearrange("p (a b) -> p a b", b=shape[2])
        elif len(shape) == 4:
            ap = ap.rearrange("p (a b c) -> p a b c", b=shape[2], c=shape[3])
        return ap


def tiles_of(r0, nr):
    return list(range(r0 // 128, (r0 + nr - 1) // 128 + 1))


class _Stop(Exception):
    pass


def build_program(dbg=None, lim=None):
    dbg = dbg or set()
    nc = bass.Bass("TRN2", target_bir_lowering=False)

    def din(name, shape, dt=F32):
        return nc.dram_tensor(name, list(shape), dt, kind="ExternalInput").ap()

    def dscr(name, shape, dt=F32):
        kind = "ExternalOutput" if name in dbg else "Internal"
        return nc.dram_tensor(name, list(shape), dt, kind=kind).ap()

    x_in = din("x", [NB, T, D])
    ctx_in = din("ctx", [NB, CT, D])
    cvec = din("cvec", [3, D])
    mod_w = din("mod_w", [2, D, 6 * D])
    mod_b = din("mod_b", [2, 6 * D])
    norm_g = din("norm_g", [2, 4, D])
    w_in0 = din("w_in0", [D, 21 * 128])
    attn_sink = din("attn_sink", [8])
    pool_w = din("pool_w", [4, 128, 128])
    pool_scale = din("pool_scale", [512])
    attn_out_w = din("attn_out_w", [D, D])
    rec_in_w = din("rec_in_w", [D, 3088])
    rec_gate_b = din("rec_gate_b", [16])
    rec_conv_w = din("rec_conv_w", [3, D])
    rec_conv_b = din("rec_conv_b", [D])
    rec_q_w = din("rec_q_w", [4, 256, 256])
    rec_k_w = din("rec_k_w", [4, 256, 256])
    rec_norm_g = din("rec_norm_g", [D])
    rec_skip = din("rec_skip", [D])
    rec_out_w = din("rec_out_w", [D, D])
    ffn_up_w = din("ffn_up_w", [2, D, 2 * DFF])
    ffn_conv_w = din("ffn_conv_w", [2, 3, 2 * DFF])
    ffn_conv_b = din("ffn_conv_b", [2, 2 * DFF])
    ffn_down_w = din("ffn_down_w", [2, DFF, D])
    ident_in = din("ident", [128, 128])
    rope_c = din("rope_c", [128, T])
    rope_s = din("rope_s", [128, T])
    amask_in = din("amask", [2, 128, 512])
    pool_edge = din("pool_edge", [128, 64])
    lmask_in = din("lmask", [2, 128, 128])

    out = nc.dram_tensor("out", [NB, T, D], F32, kind="ExternalOutput").ap()

    GV = dscr("GV", [2, 2, 3, D])
    QT0 = dscr("QT0", [NB, 8 * 128, TS], BF16)
    V0 = dscr("V0", [NB, TS, 130], BF16)
    U0 = dscr("U0", [NB, 512, TS])
    CATP = dscr("CATP", [NB, 512, TS], BF16)
    XA = dscr("XA", [NB, T, D])
    CA = dscr("CA", [NB, CT, D])
    XB = dscr("XB", [NB, T, D])
    CB = dscr("CB", [NB, CT, D])
    GT = dscr("GT", [NB, DFF, TS], BF16)

    st = ExitStack()
    with st:
        st.enter_context(nc.allow_non_contiguous_dma(reason="small strided parameter loads"))
        S = Sched(nc)
        import os
        STQ = os.environ.get("STQ", "pool")
        parts = {}

        def wpart(base):
            lst = parts.setdefault(base, [])
            name = (base, len(lst))
            lst.append(name)
            return [name]

        def rparts(base):
            return list(parts.get(base, []))

        def sb(name, shape, dt):
            return st.enter_context(nc.sbuf_tensor("sb_" + name, list(shape), dt))

        ARENA_BYTES = 192 * 1024
        arena = Arena(sb("arena", [128, ARENA_BYTES], U8), ARENA_BYTES)
        PS2 = [st.enter_context(nc.psum_tensor("ps%d" % i, [128, 1024], F32)) for i in range(4)]

        def bank(i):
            return PS2[i // 2][:, (i % 2) * 512:(i % 2) * 512 + 512]

        def bank_bf(i):
            return PS2[i // 2][:, (i % 2) * 512:(i % 2) * 512 + 512].bitcast(BF16)

        ident = sb("ident", [128, 128], F32)
        identb = sb("identb", [128, 128], BF16)
        modT = [sb("modT%d" % l, [128, 48, 3], F32) for l in range(2)]
        A1 = [sb("A1_%d" % l, [128, 3, 8], F32) for l in range(2)]
        A2 = [sb("A2_%d" % l, [128, 3, 8], F32) for l in range(2)]
        SH1 = [sb("SH1_%d" % l, [128, 3, 8], F32) for l in range(2)]
        SH2 = [sb("SH2_%d" % l, [128, 3, 8], F32) for l in range(2)]
        epsb = sb("epsb", [128, 1], F32)

        def V(fn, reads, writes):
            return S.add("dve", fn, reads, writes)

        def A(fn, reads, writes):
            return S.add("act", fn, reads, writes)

        def G(fn, reads, writes):
            return S.add("pool", fn, reads, writes)

        def PE(fn, reads, writes):
            return S.add("pe", fn, reads, writes)

        def DMA(q, out_ap, in_ap, reads, writes, is_output=False):
            return S.add(q, lambda e: e.dma_start(out=out_ap, in_=in_ap), reads, writes, dma=True,
                         is_output=is_output)

        def dma3(q, dst3, src3, nmid, reads, writes):
            for j in range(nmid):
                DMA(q, dst3(j), src3(j), reads, writes)

        def load_wb(dst3, src2, K, blocks, res):
            for (c0, cw, key) in blocks:
                for k in range(K):
                    DMA("pool", dst3[:, k, c0:c0 + cw], src2[k * 128:(k + 1) * 128, c0:c0 + cw], [], [(res, key)])

        def load_w(dst3, src2, K, N, res):
            for k in range(K):
                c0 = 0
                while c0 < N:
                    cw = min(2048, N - c0)
                    DMA("pool", dst3[:, k, c0:c0 + cw], src2[k * 128:(k + 1) * 128, c0:c0 + cw], [], [res])
                    c0 += cw

        def chk(k):
            if lim is not None and k > lim:
                raise _Stop()

        try:
            DMA("sp", ident[:], ident_in, [], ["ident"])
            V(lambda e: e.tensor_copy(out=identb[:], in_=ident[:]), ["ident"], ["identb"])
            V(lambda e: e.memset(epsb[:], EPS), [], ["epsb"])

            arena.reset()
            cT = arena.alloc([128, 8, 3], F32)
            sT = arena.alloc([128, 8, 3], BF16)
            sTf = arena.alloc([128, 8, 3], F32)
            mb = arena.alloc([128, 48], F32)
            ng = arena.alloc([128, 4, 8], F32)
            g1t = arena.alloc([128, 8, 3], F32)
            g2t = arena.alloc([128, 8, 3], F32)
            mwt = [arena.alloc([128, 8, 512], BF16) for _ in range(3)]
            mwf = [arena.alloc([128, 8, 512], F32) for _ in range(3)]
            for r in range(3):
                DMA("sp", cT[:, :, r], cvec[r].rearrange("(k p) -> p k", p=128), [], ["cT"])
            A(lambda e: e.activation(out=sTf, in_=cT, func=AF.Silu), ["cT"], ["sTf"])
            V(lambda e: e.tensor_copy(out=sT, in_=sTf), ["sTf"], ["sT"])
            for nch in range(12):
                wt = mwt[nch % 3]
                wres = "mwt%d" % (nch % 3)
                load_w(wt, mod_w[0][:, nch * 512:(nch + 1) * 512], 8, 512, wres)
                wf = mwf[nch % 3]
                fres = "mwf%d" % (nch % 3)
                for k in range(8):
                    DMA("sp", wf[:, k, :], mod_w[1][k * 128:(k + 1) * 128, nch * 512:(nch + 1) * 512], [], [fres])
                for fc in range(4):
                    col = (nch * 4 + fc) * 3
                    for k in range(8):
                        PE(lambda e: e.matmul(bank(0)[:, col:col + 3], lhsT=wt[:, k, fc * 128:(fc + 1) * 128],
                                              rhs=sT[:, k, :], start=(k == 0), stop=(k == 7)), [wres, "sT"], ["ps0"])
                for fc in range(4):
                    col = (nch * 4 + fc) * 3
                    for k in range(8):
                        PE(lambda e: e.matmul(bank(1)[:, col:col + 3], lhsT=wf[:, k, fc * 128:(fc + 1) * 128],
                                              rhs=sTf[:, k, :], start=(k == 0), stop=(k == 7)), [fres, "sTf"], ["ps1"])
            for l in range(2):
                DMA("sp", mb, mod_b[l].rearrange("(c p) -> p c", p=128), [], ["mb"])
                for j in range(4):
                    DMA("sp", ng[:, j, :], norm_g[l, j].rearrange("(k p) -> p k", p=128), [], ["ng"])
                psm = bank(l)
                mT = modT[l]
                V(lambda e, mT=mT: e.tensor_tensor(
                    out=mT[:], in0=psm[:, 0:144].rearrange("p (c r) -> p c r", r=3),
                    in1=mb.unsqueeze(2).to_broadcast([128, 48, 3]), op=ALU.add), ["ps%d" % l, "mb"], ["modT%d" % l])

                def ngb(j):
                    return ng[:, j, :].unsqueeze(2).to_broadcast([128, 8, 3])

                mres = ["modT%d" % l, "ng"]
                V(lambda e, mT=mT, l=l: e.scalar_tensor_tensor(
                    out=A1[l][:].rearrange("p r k -> p k r"), in0=mT[:, 8:16, :], scalar=1.0, in1=ngb(0),
                    op0=ALU.add, op1=ALU.mult), mres, ["A1_%d" % l])
                V(lambda e, mT=mT, l=l: e.scalar_tensor_tensor(
                    out=A2[l][:].rearrange("p r k -> p k r"), in0=mT[:, 32:40, :], scalar=1.0, in1=ngb(2),
                    op0=ALU.add, op1=ALU.mult), mres, ["A2_%d" % l])
                V(lambda e, mT=mT, l=l: e.tensor_copy(
                    out=SH1[l][:].rearrange("p r k -> p k r"), in_=mT[:, 0:8, :]), mres, ["SH1_%d" % l])
                V(lambda e, mT=mT, l=l: e.tensor_copy(
                    out=SH2[l][:].rearrange("p r k -> p k r"), in_=mT[:, 24:32, :]), mres, ["SH2_%d" % l])
                V(lambda e, mT=mT: e.tensor_tensor(out=g1t, in0=mT[:, 16:24, :], in1=ngb(1), op=ALU.mult),
                  mres, ["g1t"])
                V(lambda e, mT=mT: e.tensor_tensor(out=g2t, in0=mT[:, 40:48, :], in1=ngb(3), op=ALU.mult),
                  mres, ["g2t"])
                for r in range(3):
                    DMA("sp", GV[l, 0, r].rearrange("(k p) -> p k", p=128), g1t[:, :, r], ["g1t"], wpart(("GV", l, 0)))
                    DMA("sp", GV[l, 1, r].rearrange("(k p) -> p k", p=128), g2t[:, :, r], ["g2t"], wpart(("GV", l, 1)))

            ctxb = {}
            rr = {"n": 0, "r": 0}

            def alloc_norm_bufs():
                ctxb["xt"] = [arena.alloc([128, D], F32) for _ in range(3)]
                ctxb["junk"] = arena.alloc([128, D], BF16)
                ctxb["xn"] = [arena.alloc([128, D], BF16) for _ in range(3)]
                ctxb["st"] = [arena.alloc([128, 4], F32) for _ in range(3)]
                ctxb["tmpT"] = [arena.alloc([128, 8, 128], F32) for _ in range(2)]

            def rstd_chain(src_ap, srcres, stt, stres, nr):
                junk = ctxb["junk"]
                A(lambda e: e.activation(out=junk[0:nr, :], in_=src_ap, func=AF.Square, accum_out=stt[0:nr, 0:1]),
                  srcres, ["junk", stres])
                A(lambda e: e.activation(out=stt[0:nr, 1:2], in_=stt[0:nr, 0:1], func=AF.Sqrt, scale=1.0 / D,
                                         bias=epsb[0:nr, :]), [stres, "epsb"], [stres])
                V(lambda e: e.reciprocal(out=stt[0:nr, 1:2], in_=stt[0:nr, 1:2]), [stres], [stres])

            def make_hT(src, srcres_fn, r0, nr, Aap, SHap, ares, hT, hres, c0):
                i = rr["n"]
                rr["n"] += 1
                s3, s2 = i % 3, i % 2
                xt, xn, stt, tmpT = ctxb["xt"][s3], ctxb["xn"][s3], ctxb["st"][s3], ctxb["tmpT"][s2]
                xres, nres, sres, tres = "xt%d" % s3, "xn%d" % s3, "st%d" % s3, "tmpT%d" % s2
                pb = 6 + s2
                pres = "ps%d" % pb
                DMA("sp", xt[0:nr, :], src[r0:r0 + nr, :], srcres_fn(r0, nr), [xres])
                rstd_chain(xt[0:nr, :], [xres], stt, sres, nr)
                V(lambda e: e.tensor_scalar(out=xn[0:nr, :], in0=xt[0:nr, :], scalar1=stt[0:nr, 1:2], scalar2=None,
                                            op0=ALU.mult), [xres, sres], [nres])
                pt = bank_bf(pb).rearrange("p (k t) -> p k t", t=128)
                for k in range(8):
                    PE(lambda e, k=k: e.transpose(out=pt[:, k, 0:nr], in_=xn[0:nr, k * 128:(k + 1) * 128],
                                                  identity=identb[0:nr, 0:nr]), [nres, "identb"], [pres])
                V(lambda e: e.tensor_tensor(out=tmpT[:, :, 0:nr], in0=pt[:, :, 0:nr],
                                            in1=Aap.unsqueeze(2).to_broadcast([128, 8, nr]), op=ALU.mult),
                  [pres, ares], [tres])
                V(lambda e: e.tensor_tensor(out=hT[:, :, c0:c0 + nr], in0=tmpT[:, :, 0:nr],
                                            in1=SHap.unsqueeze(2).to_broadcast([128, 8, nr]), op=ALU.add),
                  [tres, ares], [hres])

            def residual_update(mix_ap, mixres, src, srcres, dst, dstres, r0, nr, gb, gbres, is_output=False):
                i = rr["r"]
                rr["r"] += 1
                s3, s2 = i % 3, i % 2
                xt, stt = ctxb["xt"][s3], ctxb["st"][s3]
                tmp = ctxb["mtmp"][s2]
                xres, sres, tres = "xt%d" % s3, "st%d" % s3, "mtmp%d" % s2
                DMA("sp", xt[0:nr, :], src[r0:r0 + nr, :], srcres, [xres])
                rstd_chain(mix_ap, mixres, stt, sres, nr)
                V(lambda e: e.scalar_tensor_tensor(out=tmp[0:nr, :], in0=mix_ap, scalar=stt[0:nr, 1:2],
                                                   in1=gb[0:nr, :], op0=ALU.mult, op1=ALU.mult),
                  mixres + [sres, gbres], [tres])
                V(lambda e: e.tensor_tensor(out=xt[0:nr, :], in0=xt[0:nr, :], in1=tmp[0:nr, :], op=ALU.add),
                  [xres, tres], [xres])
                DMA(STQ, dst[r0:r0 + nr, :], xt[0:nr, :], [xres], dstres, is_output=is_output)

            def load_gb(l, j, r, tile, res):
                DMA("sp", tile, GV[l, j, r].partition_broadcast(128), rparts(("GV", l, j)), [res])

            def xres_fn(name, b):
                return lambda r0, nr: [(name, b, t) for t in tiles_of(r0, nr)]

            chk(1)
            S.barrier()
            arena.reset()
            alloc_norm_bufs()
            win = arena.alloc([128, 8, 21 * 128], BF16)
            rc = arena.alloc([128, T], F32)
            rs = arena.alloc([128, T], F32)
            hTw = [arena.alloc([128, 8, 512], BF16) for _ in range(2)]
            rt1 = [arena.alloc([128, 512], F32) for _ in range(2)]
            rt2 = [arena.alloc([128, 512], F32) for _ in range(2)]
            qst = [arena.alloc([128, 512], BF16) for _ in range(4)]
            ust = [arena.alloc([128, 512], F32) for _ in range(2)]
            vst = [arena.alloc([128, 130], BF16) for _ in range(2)]
            load_w(win, w_in0, 8, 21 * 128, "win")
            DMA("sp", rc, rope_c, [], ["rc"])
            DMA("sp", rs, rope_s, [], ["rs"])
            for s in range(2):
                V(lambda e, s=s: e.memset(vst[s][:, 64:65], 1.0), [], ["vst%d" % s])
                V(lambda e, s=s: e.memset(vst[s][:, 129:130], 1.0), [], ["vst%d" % s])
            cnt = {"w": 0, "q": 0, "u": 0, "v": 0, "pp": 0}
            for b in range(NB):
                for w in range(5):
                    isx = w < 4
                    nt = 512 if isx else 256
                    tok0 = w * 512
                    row = b if isx else 2
                    src = x_in[b] if isx else ctx_in[b]
                    hs = cnt["w"] % 2
                    cnt["w"] += 1
                    hT, hres = hTw[hs], "hTw%d" % hs
                    for i in range(nt // 128):
                        make_hT(src, lambda r0, nr: [], (tok0 if isx else 0) + i * 128 if isx else i * 128, 128,
                                A1[0][:, row, :], SH1[0][:, row, :], "A1_0", hT, hres, i * 128)

                    def proj(j, pb):
                        for k in range(8):
                            PE(lambda e, k=k: e.matmul(bank(pb)[:, 0:nt], lhsT=win[:, k, j * 128:(j + 1) * 128],
                                                       rhs=hT[:, k, 0:nt], start=(k == 0), stop=(k == 7)),
                               ["win", hres], ["ps%d" % pb])

                    for j in range(8):
                        pa = (cnt["pp"] % 2) * 2
                        cnt["pp"] += 1
                        qs = cnt["q"] % 4
                        cnt["q"] += 1
                        qt_, qres = qst[qs], "qst%d" % qs
                        proj(j, pa)
                        if isx:
                            proj(j + 8, pa + 1)
                            ts_ = cnt["q"] % 2
                            t1, t2 = rt1[ts_], rt2[ts_]
                            V(lambda e, t1=t1, pa=pa: e.tensor_tensor(out=t1[:, 0:nt], in0=bank(pa)[:, 0:nt],
                                                                      in1=rc[:, tok0:tok0 + nt], op=ALU.mult),
                              ["ps%d" % pa, "rc"], ["rt1_%d" % ts_])
                            V(lambda e, t2=t2, pa=pa: e.tensor_tensor(out=t2[:, 0:nt], in0=bank(pa + 1)[:, 0:nt],
                                                                      in1=rs[:, tok0:tok0 + nt], op=ALU.mult),
                              ["ps%d" % (pa + 1), "rs"], ["rt2_%d" % ts_])
                            G(lambda e, t1=t1, t2=t2, qt_=qt_: e.tensor_tensor(out=qt_[:, 0:nt], in0=t1[:, 0:nt],
                                                                               in1=t2[:, 0:nt], op=ALU.add),
                              ["rt1_%d" % ts_, "rt2_%d" % ts_], [qres])
                        else:
                            A(lambda e, qt_=qt_, pa=pa: e.activation(out=qt_[:, 0:nt], in_=bank(pa)[:, 0:nt],
                                                                     func=AF.Copy), ["ps%d" % pa], [qres])
                        DMA(STQ, QT0[b, j * 128:(j + 1) * 128, tok0:tok0 + nt], qt_[:, 0:nt], [qres],
                            wpart(("QT0", b)))
                    for j in range(17, 21):
                        pa = (cnt["pp"] % 2) * 2
                        cnt["pp"] += 1
                        us = cnt["u"] % 2
                        cnt["u"] += 1
                        proj(j, pa)
                        A(lambda e, us=us, pa=pa: e.activation(out=ust[us][:, 0:nt], in_=bank(pa)[:, 0:nt],
                                                               func=AF.Copy), ["ps%d" % pa], ["ust%d" % us])
                        DMA(STQ, U0[b, (j - 17) * 128:(j - 16) * 128, tok0:tok0 + nt], ust[us][:, 0:nt],
                            ["ust%d" % us], wpart(("U0", b, j - 17)))
                    for i in range(nt // 128):
                        vs = cnt["v"] % 2
                        cnt["v"] += 1
                        for k in range(8):
                            PE(lambda e, k=k, i=i: e.matmul(bank(4)[:, 0:128], lhsT=hT[:, k, i * 128:(i + 1) * 128],
                                                           rhs=win[:, k, 16 * 128:17 * 128], start=(k == 0),
                                                           stop=(k == 7)), ["win", hres], ["ps4"])
                        A(lambda e, vs=vs: e.activation(
                            out=vst[vs][:].rearrange("p (h d) -> p h d", d=65)[:, :, 0:64],
                            in_=bank(4)[:, 0:128].rearrange("p (h d) -> p h d", d=64), func=AF.Copy),
                          ["ps4"], ["vst%d" % vs])
                        DMA(STQ, V0[b, tok0 + i * 128:tok0 + (i + 1) * 128, :], vst[vs][:], ["vst%d" % vs],
                            wpart(("V0", b)))

            chk(2)
            S.barrier()
            arena.reset()
            PADL = 16
            ub = [arena.alloc([128, TS + 48], F32) for _ in range(2)]
            pa_ = [arena.alloc([128, TS + 48], F32) for _ in range(2)]
            pb_ = [arena.alloc([128, TS + 48], F32) for _ in range(2)]
            dTt = [arena.alloc([128, TS], BF16) for _ in range(2)]
            cst = [arena.alloc([128, 512], BF16) for _ in range(2)]
            pwt = arena.alloc([128, 4, 128], BF16)
            psc = arena.alloc([128, 4], F32)
            pedge = arena.alloc([128, 64], F32)
            etmp = arena.alloc([128, 8], F32)
            for g_ in range(4):
                DMA("pool", pwt[:, g_, :], pool_w[g_], [], ["pwt"])
            DMA("sp", psc, pool_scale.rearrange("(g p) -> p g", p=128), [], ["psc"])
            DMA("sp", pedge, pool_edge, [], ["pedge"])
            segs = [(PADL, T, 0), (PADL + T + 16, CT, T)]
            pc = 0
            for b in range(NB):
                for g in range(4):
                    sl = pc % 2
                    pc += 1
                    u, p1, p2, dT = ub[sl], pa_[sl], pb_[sl], dTt[sl]
                    ur, p1r, p2r, dr = "ub%d" % sl, "pa%d" % sl, "pb%d" % sl, "dT%d" % sl
                    V(lambda e, u=u: e.memset(u[:, 0:PADL], 0.0), [], [ur])
                    V(lambda e, u=u: e.memset(u[:, PADL + T:PADL + T + 16], 0.0), [], [ur])
                    V(lambda e, u=u: e.memset(u[:, PADL + T + 16 + CT:TS + 48], 0.0), [], [ur])
                    DMA("sp", u[:, PADL:PADL + T], U0[b, g * 128:(g + 1) * 128, 0:T], rparts(("U0", b, g)), [ur])
                    DMA("sp", u[:, PADL + T + 16:PADL + T + 16 + CT], U0[b, g * 128:(g + 1) * 128, T:TS],
                        rparts(("U0", b, g)), [ur])
                    wlen = 2 ** (g + 1)
                    half = wlen // 2
                    NW = TS + 48
                    cur, curres, ln = u, ur, 1
                    bufs = [(p1, p1r), (p2, p2r)]
                    bi = 0
                    while ln < wlen:
                        nxt, nres = bufs[bi % 2]
                        bi += 1
                        n_el = NW - 2 * ln
                        V(lambda e, cur=cur, nxt=nxt, ln=ln, n_el=n_el: e.tensor_tensor(
                            out=nxt[:, 0:n_el], in0=cur[:, 0:n_el], in1=cur[:, ln:ln + n_el], op=ALU.add),
                          [curres], [nres])
                        cur, curres = nxt, nres
                        ln *= 2
                    for (off, L, toff) in segs:
                        V(lambda e, cur=cur, off=off, L=L, toff=toff: e.scalar_tensor_tensor(
                            out=dT[:, toff:toff + L], in0=cur[:, off - half:off - half + L], scalar=1.0 / wlen,
                            in1=u[:, off:off + L], op0=ALU.mult, op1=ALU.subtract), [curres, ur], [dr])
                        for side in range(2):
                            ne = half if side == 0 else half - 1
                            if ne == 0:
                                continue
                            t0 = 0 if side == 0 else L - half + 1
                            ec = (g * 2 + side) * 8
                            V(lambda e, cur=cur, off=off, t0=t0, ne=ne, ec=ec: e.tensor_tensor(
                                out=etmp[:, 0:ne], in0=cur[:, off - half + t0:off - half + t0 + ne],
                                in1=pedge[:, ec:ec + ne], op=ALU.mult), [curres, "pedge"], ["etmp"])
                            V(lambda e, off=off, t0=t0, ne=ne, toff=toff: e.tensor_tensor(
                                out=dT[:, toff + t0:toff + t0 + ne], in0=etmp[:, 0:ne],
                                in1=u[:, off + t0:off + t0 + ne], op=ALU.subtract), ["etmp", ur], [dr])
                    for w in range(5):
                        nt = 512 if w < 4 else 256
                        tok0 = w * 512
                        pbk = w % 2
                        cs = (pc + w) % 2
                        PE(lambda e, dT=dT, nt=nt, tok0=tok0, pbk=pbk, g=g: e.matmul(
                            bank(pbk)[:, 0:nt], lhsT=pwt[:, g, :], rhs=dT[:, tok0:tok0 + nt], start=True, stop=True),
                           ["pwt", dr], ["ps%d" % pbk])
                        A(lambda e, nt=nt, pbk=pbk, cs=cs, g=g: e.activation(
                            out=cst[cs][:, 0:nt], in_=bank(pbk)[:, 0:nt], func=AF.Copy, scale=psc[:, g:g + 1]),
                          ["ps%d" % pbk, "psc"], ["cst%d" % cs])
                        DMA(STQ, CATP[b, g * 128:(g + 1) * 128, tok0:tok0 + nt], cst[cs][:, 0:nt], ["cst%d" % cs],
                            wpart(("CATP", b)))

            chk(2.05)
            S.barrier()
            arena.reset()
            alloc_norm_bufs()
            ctxb["mtmp"] = [arena.alloc([128, D], F32) for _ in range(2)]
            wout = arena.alloc([128, 8, D], BF16)
            qt = arena.alloc([128, 8, TS], BF16)
            vx = arena.alloc([128, 18, 130], BF16)
            am = arena.alloc([128, 2, 512], BF16)
            esb = arena.alloc([128, 8], F32)
            PT = [arena.alloc([128, 512], BF16) for _ in range(10)]
            atok = [arena.alloc([128, 512], BF16) for _ in range(2)]
            catT = [arena.alloc([128, 8, 128], BF16) for _ in range(2)]
            den = [arena.alloc([128, 8], F32) for _ in range(2)]
            gbx = arena.alloc([128, D], F32)
            gbc = arena.alloc([128, D], F32)
            load_w(wout, attn_out_w, 8, D, "wout")
            for m_ in range(2):
                DMA("pool", am[:, m_, :], amask_in[m_], [], ["am"])
            DMA("sp", esb, attn_sink.partition_broadcast(128), [], ["esb"])
            A(lambda e: e.activation(out=esb, in_=esb, func=AF.Exp), ["esb"], ["esb"])
            import os
            if os.environ.get("SWAP"):
                load_gb(0, 0, 0, gbx, "gbx")
            load_gb(0, 0, 2, gbc, "gbc")
            chk(2.1)
            ac = {"pt": 0, "blk": 0}
            for b in range(NB):
                import os
                SK = os.environ.get("SKIP", "")
                if "q" not in SK:
                    dma3("sp", lambda j: qt[:, j, :], lambda j: QT0[b, j * 128:(j + 1) * 128, :], 8, rparts(("QT0", b)), ["qt"])
                if "v" not in SK:
                    dma3("sp", lambda j: vx[:, j, :], lambda j: V0[b, j * 128:(j + 1) * 128, :], 18, rparts(("V0", b)), ["vx"])
                if "g" not in SK:
                    load_gb(0, 0, b, gbx, "gbx")
                chk(2.2)
                def a_setup(n):
                    bs = n % 2
                    cT_, cres = catT[bs], "catT%d" % bs
                    dma3("sp", lambda j: cT_[:, 4 + j, :], lambda j: CATP[b, j * 128:(j + 1) * 128, n * 128:(n + 1) * 128], 4,
                         rparts(("CATP", b)), [cres])

                def a_chunks(n):
                    if n < 16:
                        chunks = []
                        if n > 0:
                            chunks.append((n - 1, 0))
                        chunks.append((n, None))
                        if n < 15:
                            chunks.append((n + 1, 1))
                        return chunks + [(16, None), (17, None)]
                    return [(16, None), (17, None)]

                def a_scores(n, h):
                    pts = []
                    for (kc, mk) in a_chunks(n):
                        pi = ac["pt"] % 10
                        ac["pt"] += 1
                        sbk = pi % 2
                        pts.append((pi, kc))
                        for g in range(4):
                            s_ = g % 2
                            c_ = 2 * h + g // 2
                            PE(lambda e: e.matmul(bank(sbk)[:, g * 128:(g + 1) * 128],
                                                  lhsT=qt[:, 4 + 2 * h + s_, kc * 128:(kc + 1) * 128],
                                                  rhs=qt[:, c_, n * 128:(n + 1) * 128], start=True, stop=True),
                               ["qt"], ["ps%d" % sbk])
                        A(lambda e: e.activation(out=PT[pi][:], in_=bank(sbk), func=AF.Exp, scale=0.125),
                          ["ps%d" % sbk], ["PT%d" % pi])
                        if mk is not None:
                            V(lambda e: e.tensor_tensor(out=PT[pi][:], in0=PT[pi][:], in1=am[:, mk, :], op=ALU.mult),
                              ["PT%d" % pi, "am"], ["PT%d" % pi])
                    return pts

                def a_pv(n, h, pts):
                    bs = n % 2
                    at_, atres = atok[bs], "atok%d" % bs
                    dn, dres = den[bs], "den%d" % bs
                    ob = 2 + h
                    ov = bank(ob)[:, 0:260].rearrange("p (g d) -> p g d", d=65)
                    for g in range(4):
                        for ci, (pi, kc) in enumerate(pts):
                            PE(lambda e: e.matmul(bank(ob)[:, g * 65:(g + 1) * 65], lhsT=PT[pi][:, g * 128:(g + 1) * 128],
                                                  rhs=vx[:, kc, h * 65:(h + 1) * 65], start=(ci == 0),
                                                  stop=(ci == len(pts) - 1)), ["PT%d" % pi, "vx"], ["ps%d" % ob])
                    V(lambda e: e.tensor_tensor(out=dn[:, h * 4:(h + 1) * 4], in0=ov[:, :, 64],
                                                in1=esb[:, h * 4:(h + 1) * 4], op=ALU.add), ["ps%d" % ob, "esb"], [dres])
                    V(lambda e: e.reciprocal(out=dn[:, h * 4:(h + 1) * 4], in_=dn[:, h * 4:(h + 1) * 4]), [dres], [dres])
                    V(lambda e: e.tensor_tensor(
                        out=at_[:, h * 256:(h + 1) * 256].rearrange("p (g d) -> p g d", d=64), in0=ov[:, :, 0:64],
                        in1=dn[:, h * 4:(h + 1) * 4].unsqueeze(2).to_broadcast([128, 4, 64]), op=ALU.mult),
                      ["ps%d" % ob, dres], [atres])

                def a_tail(n):
                    bs = n % 2
                    at_, atres = atok[bs], "atok%d" % bs
                    cT_, cres = catT[bs], "catT%d" % bs
                    ptv = bank_bf(4).rearrange("p (k t) -> p k t", t=128)
                    for c_ in range(4):
                        PE(lambda e: e.transpose(out=ptv[:, c_, :], in_=at_[:, c_ * 128:(c_ + 1) * 128],
                                                 identity=identb[:]), [atres, "identb"], ["ps4"])
                    A(lambda e: e.activation(out=cT_[:, 0:4, :], in_=ptv[:, 0:4, :], func=AF.Copy), ["ps4"], [cres])
                    mix = PS2[3]
                    for hf in range(2):
                        for k in range(8):
                            PE(lambda e: e.matmul(mix[:, hf * 512:(hf + 1) * 512], lhsT=cT_[:, k, :],
                                                  rhs=wout[:, k, hf * 512:(hf + 1) * 512], start=(k == 0), stop=(k == 7)),
                               [cres, "wout"], ["ps%d" % (6 + hf)])
                    if n < 16:
                        residual_update(mix[:, :], ["ps6", "ps7"], x_in[b], [], XA[b], [("XA", b, n)], n * 128, 128,
                                        gbx, "gbx")
                    else:
                        residual_update(mix[:, :], ["ps6", "ps7"], ctx_in[b], [], CA[b], [("CA", b, n - 16)],
                                        (n - 16) * 128, 128, gbc, "gbc")

                pend = None
                for n in range(18):
                    a_setup(n)
                    pts0 = a_scores(n, 0)
                    pts1 = a_scores(n, 1)
                    if pend is not None:
                        a_tail(pend)
                    a_pv(n, 0, pts0)
                    a_pv(n, 1, pts1)
                    pend = n
                a_tail(pend)

            def make_windows(L):
                wins = []
                o = 0
                while o < L:
                    r0 = max(o - 1, 0)
                    c0 = 1 if o == 0 else 0
                    nr = min(512 - c0, L - r0)
                    rz = (r0 + nr == L)
                    ncols = c0 + nr + (1 if rz else 0)
                    if ncols > 512:
                        nr -= 1
                        rz = False
                        ncols = 512
                    nout = ncols - 2
                    wins.append((r0, nr, c0, rz, ncols, o, nout))
                    o += nout
                return wins

            def ffn_layer(l, segs_fn, final):
                chk(4 + 5 * l)
                S.barrier()
                arena.reset()
                alloc_norm_bufs()
                wup = arena.alloc([128, 8, 2 * DFF], BF16)
                cw = arena.alloc([128, 3, 44], F32)
                cb = arena.alloc([128, 44], F32)
                hTf = [arena.alloc([128, 8, 512], BF16) for _ in range(2)]
                ta = [arena.alloc([128, 512], F32) for _ in range(3)]
                tb = [arena.alloc([128, 512], F32) for _ in range(3)]
                sg = [arena.alloc([128, 512], F32) for _ in range(3)]
                tv = [arena.alloc([128, 512], F32) for _ in range(3)]
                tw = [arena.alloc([128, 512], F32) for _ in range(3)]
                gst = [arena.alloc([128, 512], BF16) for _ in range(3)]
                load_wb(wup, ffn_up_w[l], 8, [(0, 1408, 0), (2816, 1408, 2), (1408, 1408, 1), (4224, 1408, 3)], "wup")
                for j in range(3):
                    DMA("sp", cw[:, j, :], ffn_conv_w[l, j].rearrange("(c p) -> p c", p=128), [], ["cw"])
                DMA("sp", cb, ffn_conv_b[l].rearrange("(c p) -> p c", p=128), [], ["cb"])
                fc = {"w": 0, "c": 0, "g": 0}
                allw = []
                for b in range(NB):
                    for (src, sname, L, toff, row) in segs_fn(b):
                        for w_ in make_windows(L):
                            allw.append((b, src, sname, L, toff, row) + w_)

                def prep_u(i):
                    (b, src, sname, L, toff, row, r0, nr, c0, rz, ncols, o, nout) = allw[i]
                    hT, hres = hTf[i % 2], "hTf%d" % (i % 2)
                    if c0 == 1:
                        V(lambda e: e.memset(hT[:, :, 0:1], 0.0), [], [hres])
                    if rz:
                        V(lambda e: e.memset(hT[:, :, c0 + nr:c0 + nr + 1], 0.0), [], [hres])
                    rr_ = r0
                    while rr_ < r0 + nr:
                        n_ = min(128, r0 + nr - rr_)
                        make_hT(src, xres_fn(sname, b), rr_, n_, A2[l][:, row, :], SH2[l][:, row, :],
                                "A2_%d" % l, hT, hres, c0 + rr_ - r0)
                        rr_ += n_

                def comp_u(i):
                    (b, src, sname, L, toff, row, r0, nr, c0, rz, ncols, o, nout) = allw[i]
                    hT, hres = hTf[i % 2], "hTf%d" % (i % 2)
                    for c in range(22):
                        s2 = fc["c"] % 3
                        fc["c"] += 1
                        pg, pv = (s2 * 2), (s2 * 2 + 1)
                        for (jc, pbk) in ((c, pg), (22 + c, pv)):
                            for k in range(8):
                                PE(lambda e, k=k, jc=jc, pbk=pbk, hT=hT, ncols=ncols: e.matmul(
                                    bank(pbk)[:, 0:ncols], lhsT=wup[:, k, jc * 128:(jc + 1) * 128],
                                    rhs=hT[:, k, 0:ncols], start=(k == 0), stop=(k == 7)),
                                   [("wup", jc // 11), hres], ["ps%d" % pbk])
                        n = nout
                        A(lambda e, s2=s2, pg=pg, c=c, n=n: e.activation(
                            out=ta[s2][:, 0:n], in_=bank(pg)[:, 1:1 + n], func=AF.Identity,
                            scale=cw[:, 1, c:c + 1], bias=cb[:, c:c + 1]), ["ps%d" % pg, "cw", "cb"], ["ta%d" % s2])
                        V(lambda e, s2=s2, pg=pg, c=c, n=n: e.scalar_tensor_tensor(
                            out=tb[s2][:, 0:n], in0=bank(pg)[:, 0:n], scalar=cw[:, 0, c:c + 1],
                            in1=ta[s2][:, 0:n], op0=ALU.mult, op1=ALU.add), ["ps%d" % pg, "cw", "ta%d" % s2],
                          ["tb%d" % s2])
                        V(lambda e, s2=s2, pg=pg, c=c, n=n: e.scalar_tensor_tensor(
                            out=ta[s2][:, 0:n], in0=bank(pg)[:, 2:2 + n], scalar=cw[:, 2, c:c + 1],
                            in1=tb[s2][:, 0:n], op0=ALU.mult, op1=ALU.add), ["ps%d" % pg, "cw", "tb%d" % s2],
                          ["ta%d" % s2])
                        A(lambda e, s2=s2, n=n: e.activation(out=sg[s2][:, 0:n], in_=ta[s2][:, 0:n], func=AF.Silu),
                          ["ta%d" % s2], ["sg%d" % s2])
                        cv = 22 + c
                        A(lambda e, s2=s2, pv=pv, cv=cv, n=n: e.activation(
                            out=tv[s2][:, 0:n], in_=bank(pv)[:, 1:1 + n], func=AF.Identity,
                            scale=cw[:, 1, cv:cv + 1], bias=cb[:, cv:cv + 1]), ["ps%d" % pv, "cw", "cb"],
                          ["tv%d" % s2])
                        V(lambda e, s2=s2, pv=pv, cv=cv, n=n: e.scalar_tensor_tensor(
                            out=tw[s2][:, 0:n], in0=bank(pv)[:, 0:n], scalar=cw[:, 0, cv:cv + 1],
                            in1=tv[s2][:, 0:n], op0=ALU.mult, op1=ALU.add), ["ps%d" % pv, "cw", "tv%d" % s2],
                          ["tw%d" % s2])
                        V(lambda e, s2=s2, pv=pv, cv=cv, n=n: e.scalar_tensor_tensor(
                            out=tv[s2][:, 0:n], in0=bank(pv)[:, 2:2 + n], scalar=cw[:, 2, cv:cv + 1],
                            in1=tw[s2][:, 0:n], op0=ALU.mult, op1=ALU.add), ["ps%d" % pv, "cw", "tw%d" % s2],
                          ["tv%d" % s2])
                        gs = fc["g"] % 3
                        fc["g"] += 1
                        G(lambda e, s2=s2, gs=gs, n=n: e.tensor_tensor(out=gst[gs][:, 0:n], in0=sg[s2][:, 0:n],
                                                                       in1=tv[s2][:, 0:n], op=ALU.mult),
                          ["sg%d" % s2, "tv%d" % s2], ["gst%d" % gs])
                        DMA(STQ, GT[b, c * 128:(c + 1) * 128, toff + o:toff + o + n], gst[gs][:, 0:n],
                            ["gst%d" % gs], wpart(("GT", b, toff)))

                prep_u(0)
                for i_ in range(len(allw)):
                    if i_ + 1 < len(allw):
                        prep_u(i_ + 1)
                    comp_u(i_)
                chk(5 + 5 * l)
                S.barrier()
                arena.reset()
                alloc_norm_bufs()
                ctxb["mtmp"] = [arena.alloc([128, D], F32) for _ in range(2)]
                wdn = arena.alloc([128, 22, D], BF16)
                gTw = [arena.alloc([128, 22, 512], BF16) for _ in range(3)]
                gb = [arena.alloc([128, D], F32) for _ in range(2)]
                for c_ in range(22):
                    DMA("pool", wdn[:, c_, :], ffn_down_w[l][c_ * 128:(c_ + 1) * 128, :], [], [("wdn", c_)])
                dc = {"w": 0, "m": 0}
                load_gb(l, 1, 2, gb[1], "gbf1")
                dw = []
                for b in range(NB):
                    first = True
                    for (src, sname, L, toff, row, dst, dname, is_out) in final(b):
                        o = 0
                        while o < L:
                            nt = min(512, L - o)
                            dw.append((b, src, sname, toff, row, dst, dname, is_out, o, nt, first))
                            first = False
                            o += nt

                def load_d(i):
                    (b, src, sname, toff, row, dst, dname, is_out, o, nt, first) = dw[i]
                    ws = i % 3
                    dma3("sp", lambda j: gTw[ws][:, j, 0:nt],
                         lambda j: GT[b, j * 128:(j + 1) * 128, toff + o:toff + o + nt], 22,
                         rparts(("GT", b, toff)), ["gTw%d" % ws])

                def comp_d(i):
                    (b, src, sname, toff, row, dst, dname, is_out, o, nt, first) = dw[i]
                    ws = i % 3
                    if first:
                        load_gb(l, 1, b, gb[0], "gbf0")
                    for i2 in range(nt // 128):
                        ms = dc["m"] % 3
                        dc["m"] += 1
                        mix = PS2[1 + ms]
                        for hf in range(2):
                            for c in range(22):
                                PE(lambda e: e.matmul(
                                    mix[:, hf * 512:(hf + 1) * 512], lhsT=gTw[ws][:, c, i2 * 128:(i2 + 1) * 128],
                                    rhs=wdn[:, c, hf * 512:(hf + 1) * 512], start=(c == 0), stop=(c == 21)),
                                   ["gTw%d" % ws, ("wdn", c)], ["ps%d" % (2 + 2 * ms + hf)])
                        r0 = o + i2 * 128
                        g_ = gb[0] if row < 2 else gb[1]
                        residual_update(mix[:, :], ["ps%d" % (2 + 2 * ms), "ps%d" % (3 + 2 * ms)], src,
                                        [(sname, b, r0 // 128)], dst, [(dname, b, r0 // 128)], r0, 128, g_,
                                        "gbf0" if row < 2 else "gbf1", is_output=is_out)

                load_d(0)
                if len(dw) > 1:
                    load_d(1)
                for i_ in range(len(dw)):
                    if i_ + 2 < len(dw):
                        load_d(i_ + 2)
                    comp_d(i_)

            ffn_layer(0,
                      lambda b: [(XA[b], "XA", T, 0, b), (CA[b], "CA", CT, T, 2)],
                      lambda b: [(XA[b], "XA", T, 0, b, XB[b], "XB", False), (CA[b], "CA", CT, T, 2, CB[b], "CB", False)])

            UC1 = dscr("UC1", [NB, D, T], BF16)
            OG1 = dscr("OG1", [NB, D, T], BF16)
            QT1 = dscr("QT1", [NB, D, T], BF16)
            KT1 = dscr("KT1", [NB, D, TS], BF16)
            KK1 = dscr("KK1", [NB, TS, D], BF16)
            VV1 = dscr("VV1", [NB, TS, D], BF16)
            GG1 = dscr("GG1", [NB, TS, 16])
            SC1 = dscr("SC1", [NB, TS, 24])
            DC1 = dscr("DC1", [NB, 2 * 4 * 18])

            chk(6)
            S.barrier()
            arena.reset()
            alloc_norm_bufs()
            win1 = arena.alloc([128, 8, 3088], BF16)
            qw = arena.alloc([128, 4, 2, 256], BF16)
            kw = arena.alloc([128, 4, 2, 256], BF16)
            cwr = arena.alloc([128, 3, 8], F32)
            cbr = arena.alloc([128, 8], F32)
            gbias = arena.alloc([128, 16], F32)
            hT1 = [arena.alloc([128, 8, 512], BF16) for _ in range(2)]
            ucw = [arena.alloc([128, 8, 512], BF16) for _ in range(2)]
            ta1 = [arena.alloc([128, 512], F32) for _ in range(2)]
            tb1 = [arena.alloc([128, 512], F32) for _ in range(2)]
            fst = [arena.alloc([128, 512], BF16) for _ in range(4)]
            tst = [arena.alloc([128, D], BF16) for _ in range(2)]
            gst1 = [arena.alloc([128, 16], F32) for _ in range(2)]
            load_wb(win1, rec_in_w, 8, [(0, 1024, "u"), (1024, 1024, "v"), (3072, 16, "g"), (2048, 1024, "o")], "win1")
            for h in range(4):
                for dc in range(2):
                    DMA("pool", qw[:, h, dc, :], rec_q_w[h, dc * 128:(dc + 1) * 128, :], [], ["qw"])
                    DMA("pool", kw[:, h, dc, :], rec_k_w[h, dc * 128:(dc + 1) * 128, :], [], ["kw"])
            for j in range(3):
                DMA("sp", cwr[:, j, :], rec_conv_w[j].rearrange("(c p) -> p c", p=128), [], ["cwr"])
            DMA("sp", cbr, rec_conv_b.rearrange("(c p) -> p c", p=128), [], ["cbr"])
            DMA("sp", gbias, rec_gate_b.partition_broadcast(128), [], ["gbias"])
            c1 = {"w": 0, "p": 0, "f": 0, "t": 0, "g": 0}

            def nbank():
                c1["p"] += 1
                return c1["p"] % 4

            allw1 = []
            for b in range(NB):
                for (src, sname, L, toff, row, isx) in ((CB[b], "CB", CT, 0, 2, False), (XB[b], "XB", T, CT, b, True)):
                    for w_ in make_windows(L):
                        allw1.append((b, src, sname, L, toff, row, isx) + w_)

            def prep_1(i):
                (b, src, sname, L, toff, row, isx, r0, nr, c0, rz, ncols, o, n) = allw1[i]
                hT, hres = hT1[i % 2], "hT1_%d" % (i % 2)
                if c0 == 1:
                    V(lambda e: e.memset(hT[:, :, 0:1], 0.0), [], [hres])
                if rz:
                    V(lambda e: e.memset(hT[:, :, c0 + nr:c0 + nr + 1], 0.0), [], [hres])
                rr_ = r0
                while rr_ < r0 + nr:
                    n_ = min(128, r0 + nr - rr_)
                    make_hT(src, xres_fn(sname, b), rr_, n_, A1[1][:, row, :], SH1[1][:, row, :], "A1_1",
                            hT, hres, c0 + rr_ - r0)
                    rr_ += n_

            def comp_1(i):
                (b, src, sname, L, toff, row, isx, r0, nr, c0, rz, ncols, o, n) = allw1[i]
                hT, hres = hT1[i % 2], "hT1_%d" % (i % 2)
                uc_, ures = ucw[i % 2], "ucw%d" % (i % 2)

                def fm_proj(col0, pbk):
                    for k in range(8):
                        PE(lambda e: e.matmul(bank(pbk)[:, 0:ncols], lhsT=win1[:, k, col0:col0 + 128],
                                              rhs=hT[:, k, 0:ncols], start=(k == 0), stop=(k == 7)),
                           [("win1", "u" if col0 < 1024 else "o"), hres], ["ps%d" % pbk])

                for j in range(8):
                    pbk = nbank()
                    s2 = j % 2
                    fm_proj(j * 128, pbk)
                    A(lambda e: e.activation(out=ta1[s2][:, 0:n], in_=bank(pbk)[:, 1:1 + n], func=AF.Identity,
                                             scale=cwr[:, 1, j:j + 1], bias=cbr[:, j:j + 1]),
                      ["ps%d" % pbk, "cwr", "cbr"], ["ta1_%d" % s2])
                    V(lambda e: e.scalar_tensor_tensor(out=tb1[s2][:, 0:n], in0=bank(pbk)[:, 0:n],
                                                       scalar=cwr[:, 0, j:j + 1], in1=ta1[s2][:, 0:n],
                                                       op0=ALU.mult, op1=ALU.add),
                      ["ps%d" % pbk, "cwr", "ta1_%d" % s2], ["tb1_%d" % s2])
                    V(lambda e: e.scalar_tensor_tensor(out=ta1[s2][:, 0:n], in0=bank(pbk)[:, 2:2 + n],
                                                       scalar=cwr[:, 2, j:j + 1], in1=tb1[s2][:, 0:n],
                                                       op0=ALU.mult, op1=ALU.add),
                      ["ps%d" % pbk, "cwr", "tb1_%d" % s2], ["ta1_%d" % s2])
                    A(lambda e: e.activation(out=uc_[:, j, 0:n], in_=ta1[s2][:, 0:n], func=AF.Silu),
                      ["ta1_%d" % s2], [ures])
                    if isx:
                        DMA(STQ, UC1[b, j * 128:(j + 1) * 128, o:o + n], uc_[:, j, 0:n], [ures],
                            wpart(("UC1", b)))
                if isx:
                    for j in range(8):
                        pbk = nbank()
                        fs = c1["f"] % 4
                        c1["f"] += 1
                        fm_proj(2048 + j * 128, pbk)
                        A(lambda e: e.activation(out=fst[fs][:, 0:n], in_=bank(pbk)[:, 1:1 + n],
                                                 func=AF.Sigmoid), ["ps%d" % pbk], ["fst%d" % fs])
                        DMA(STQ, OG1[b, j * 128:(j + 1) * 128, o:o + n], fst[fs][:, 0:n], ["fst%d" % fs],
                            wpart(("OG1", b)))
                for h in range(4):
                    for ec in range(2):
                        for (wt_, wres_, dst, scl, need) in ((kw, "kw", KT1, 1.0 / 16, True),
                                                             (qw, "qw", QT1, 1.0, isx)):
                            if not need:
                                continue
                            pbk = nbank()
                            fs = c1["f"] % 4
                            c1["f"] += 1
                            for dc in range(2):
                                PE(lambda e: e.matmul(bank(pbk)[:, 0:n],
                                                      lhsT=wt_[:, h, dc, ec * 128:(ec + 1) * 128],
                                                      rhs=uc_[:, 2 * h + dc, 0:n], start=(dc == 0),
                                                      stop=(dc == 1)), [wres_, ures], ["ps%d" % pbk])
                            A(lambda e: e.activation(out=fst[fs][:, 0:n], in_=bank(pbk)[:, 0:n], func=AF.Copy,
                                                     scale=scl), ["ps%d" % pbk], ["fst%d" % fs])
                            tcol = (toff + o) if dst is KT1 else o
                            DMA(STQ, dst[b, h * 256 + ec * 128:h * 256 + (ec + 1) * 128, tcol:tcol + n],
                                fst[fs][:, 0:n], ["fst%d" % fs],
                                wpart(("KT1", b)) if dst is KT1 else wpart(("QT1", b)))
                i0 = 0
                while i0 < n:
                    m = min(128, n - i0)
                    trow = toff + o + i0
                    ts_ = c1["t"] % 2
                    c1["t"] += 1
                    mix = PS2[2]
                    for h in range(4):
                        for dc in range(2):
                            PE(lambda e: e.matmul(mix[0:m, h * 256:(h + 1) * 256],
                                                  lhsT=uc_[:, 2 * h + dc, i0:i0 + m], rhs=kw[:, h, dc, :],
                                                  start=(dc == 0), stop=(dc == 1)),
                               [ures, "kw"], ["ps4", "ps5"])
                    A(lambda e: e.activation(out=tst[ts_][0:m, :], in_=mix[0:m, :], func=AF.Copy,
                                             scale=1.0 / 16), ["ps4", "ps5"], ["tst%d" % ts_])
                    DMA(STQ, KK1[b, trow:trow + m, :], tst[ts_][0:m, :], ["tst%d" % ts_], wpart(("KK1", b)))
                    ts_ = c1["t"] % 2
                    c1["t"] += 1
                    mix = PS2[3]
                    for hf in range(2):
                        for k in range(8):
                            PE(lambda e: e.matmul(mix[0:m, hf * 512:(hf + 1) * 512],
                                                  lhsT=hT[:, k, 1 + i0:1 + i0 + m],
                                                  rhs=win1[:, k, 1024 + hf * 512:1024 + (hf + 1) * 512],
                                                  start=(k == 0), stop=(k == 7)),
                               [hres, ("win1", "v")], ["ps%d" % (6 + hf)])
                    A(lambda e: e.activation(out=tst[ts_][0:m, :], in_=mix[0:m, :], func=AF.Copy),
                      ["ps6", "ps7"], ["tst%d" % ts_])
                    DMA(STQ, VV1[b, trow:trow + m, :], tst[ts_][0:m, :], ["tst%d" % ts_], wpart(("VV1", b)))
                    gs = c1["g"] % 2
                    c1["g"] += 1
                    pbk = nbank()
                    for k in range(8):
                        PE(lambda e: e.matmul(bank(pbk)[0:m, 0:16], lhsT=hT[:, k, 1 + i0:1 + i0 + m],
                                              rhs=win1[:, k, 3072:3088], start=(k == 0), stop=(k == 7)),
                           [hres, ("win1", "g")], ["ps%d" % pbk])
                    V(lambda e: e.tensor_tensor(out=gst1[gs][0:m, :], in0=bank(pbk)[0:m, 0:16],
                                                in1=gbias[0:m, :], op=ALU.add),
                      ["ps%d" % pbk, "gbias"], ["gst1_%d" % gs])
                    DMA(STQ, GG1[b, trow:trow + m, :], gst1[gs][0:m, :], ["gst1_%d" % gs], wpart(("GG1", b)))
                    i0 += m


            prep_1(0)
            for i_ in range(len(allw1)):
                if i_ + 1 < len(allw1):
                    prep_1(i_ + 1)
                comp_1(i_)

            chk(7)
            S.barrier()
            arena.reset()
            gg = arena.alloc([128, 18, 16], F32)
            ones4 = arena.alloc([4, TS], F32)
            IG = [arena.alloc([4, TS], F32) for _ in range(2)]
            FG = [arena.alloc([4, TS], F32) for _ in range(2)]
            t_a = arena.alloc([4, TS], F32)
            t_b = arena.alloc([4, TS], F32)
            Bc = arena.alloc([4, TS], F32)
            Mc = arena.alloc([4, TS], F32)
            QY = [[arena.alloc([4, TS], F32) for _ in range(3)] for _ in range(2)]
            Mpv = arena.alloc([4, 18], F32)
            dcy = arena.alloc([4, 18], F32)
            scs = arena.alloc([128, 18, 24], F32)
            V(lambda e: e.memset(ones4, 1.0), [], ["ones4"])

            def lb_tile(q):
                return q + 2 if q < 16 else q - 16

            for b in range(NB):
                dma3("sp", lambda j: gg[:, j, :], lambda j: GG1[b, j * 128:(j + 1) * 128, :], 18, rparts(("GG1", b)), ["gg"])
                for dr in range(2):
                    for ti, dstt, dres in ((0, IG[dr], "IG%d" % dr), (1, FG[dr], "FG%d" % dr)):
                        ty = dr * 2 + ti
                        for q in range(18):
                            tl = q if dr == 0 else lb_tile(q)
                            pq = PS2[q // 8]
                            PE(lambda e: e.matmul(pq[0:4, (q % 8) * 128:(q % 8 + 1) * 128],
                                                  lhsT=gg[:, tl, ty * 4:(ty + 1) * 4], rhs=ident[:, :], start=True,
                                                  stop=True), ["gg", "ident"], ["ps%d" % (2 * (q // 8) + (q % 8) // 4)])
                        for pi in range(3):
                            ncol = 1024 if pi < 2 else 256
                            A(lambda e: e.activation(out=dstt[:, pi * 1024:pi * 1024 + ncol], in_=PS2[pi][0:4, 0:ncol],
                                                     func=AF.Copy), ["ps%d" % (2 * pi), "ps%d" % (2 * pi + 1)], [dres])
                for dr in range(2):
                    ig, fg = IG[dr], FG[dr]
                    igr, fgr = "IG%d" % dr, "FG%d" % dr

                    def dview(ap):
                        return ap if dr == 0 else ap[:, ::-1]

                    A(lambda e: e.activation(out=t_a, in_=fg, func=AF.Abs), [fgr], ["t_a"])
                    A(lambda e: e.activation(out=t_a, in_=t_a, func=AF.Exp, scale=-1.0), ["t_a"], ["t_a"])
                    A(lambda e: e.activation(out=t_a, in_=t_a, func=AF.Ln, bias=1.0), ["t_a"], ["t_a"])
                    V(lambda e: e.tensor_scalar(out=t_b, in0=fg, scalar1=0.0, scalar2=None, op0=ALU.min), [fgr], ["t_b"])
                    V(lambda e: e.tensor_tensor(out=t_b, in0=t_b, in1=t_a, op=ALU.subtract), ["t_a", "t_b"], ["t_b"])
                    V(lambda e: e.tensor_tensor_scan(out=dview(Bc), data0=dview(ones4), data1=dview(t_b), initial=0.0,
                                                     op0=ALU.mult, op1=ALU.add), ["ones4", "t_b"], ["Bc"])
                    V(lambda e: e.tensor_tensor(out=t_a, in0=ig, in1=Bc, op=ALU.subtract), [igr, "Bc"], ["t_a"])
                    V(lambda e: e.tensor_tensor_scan(out=dview(Mc), data0=dview(ones4), data1=dview(t_a), initial=0.0,
                                                     op0=ALU.mult, op1=ALU.max), ["ones4", "t_a"], ["Mc"])
                    M3 = Mc.rearrange("p (q t) -> p q t", t=128)
                    a3 = t_a.rearrange("p (q t) -> p q t", t=128)
                    B3 = Bc.rearrange("p (q t) -> p q t", t=128)
                    V(lambda e: e.memset(Mpv, 0.0), [], ["Mpv"])
                    if dr == 0:
                        V(lambda e: e.tensor_copy(out=Mpv[:, 1:18], in_=M3[:, 0:17, 127]), ["Mc"], ["Mpv"])
                        Mend = M3[:, :, 127]
                    else:
                        V(lambda e: e.tensor_copy(out=Mpv[:, 0:17], in_=M3[:, 1:18, 0]), ["Mc"], ["Mpv"])
                        Mend = M3[:, :, 0]
                    Mpb = Mpv.unsqueeze(2).to_broadcast([4, 18, 128])
                    q0, q1, q2 = QY[dr]
                    qr = ["QY%d_%d" % (dr, i) for i in range(3)]
                    V(lambda e: e.tensor_tensor(out=q0.rearrange("p (q t) -> p q t", t=128), in0=a3, in1=Mpb,
                                                op=ALU.subtract), ["t_a", "Mpv"], [qr[0]])
                    A(lambda e: e.activation(out=q0, in_=q0, func=AF.Exp), [qr[0]], [qr[0]])
                    V(lambda e: e.tensor_tensor(out=q1.rearrange("p (q t) -> p q t", t=128), in0=a3,
                                                in1=Mend.unsqueeze(2).to_broadcast([4, 18, 128]), op=ALU.subtract),
                      ["t_a", "Mc"], [qr[1]])
                    A(lambda e: e.activation(out=q1, in_=q1, func=AF.Exp), [qr[1]], [qr[1]])
                    V(lambda e: e.scalar_tensor_tensor(out=q2.rearrange("p (q t) -> p q t", t=128), in0=B3, scalar=-1.0,
                                                       in1=Mpb, op0=ALU.mult, op1=ALU.subtract), ["Bc", "Mpv"], [qr[2]])
                    A(lambda e: e.activation(out=q2, in_=q2, func=AF.Exp), [qr[2]], [qr[2]])
                    V(lambda e: e.tensor_tensor(out=dcy, in0=Mpv, in1=Mend, op=ALU.subtract), ["Mpv", "Mc"], ["dcy"])
                    A(lambda e: e.activation(out=dcy, in_=dcy, func=AF.Exp), ["dcy"], ["dcy"])
                    dcv = DC1[b].rearrange("(d h q) -> d h q", d=2, h=4)[dr]
                    if dr == 0:
                        DMA(STQ, dcv, dcy, ["dcy"], wpart(("DC1", b)))
                    else:
                        DMA(STQ, dcv[:, 2:18], dcy[:, 0:16], ["dcy"], wpart(("DC1", b)))
                        DMA(STQ, dcv[:, 0:2], dcy[:, 16:18], ["dcy"], wpart(("DC1", b)))
                    pst = bank(6)
                    for q in range(18):
                        tl = q if dr == 0 else lb_tile(q)
                        for qi in range(3):
                            col = tl * 24 + (dr * 3 + qi) * 4
                            PE(lambda e: e.matmul(pst[:, col:col + 4], lhsT=QY[dr][qi][:, q * 128:(q + 1) * 128],
                                                  rhs=ident[0:4, 0:4], start=True, stop=True), [qr[qi], "ident"], ["ps6"])
                A(lambda e: e.activation(out=scs, in_=bank(6)[:, 0:432].rearrange("p (n c) -> p n c", c=24), func=AF.Copy),
                  ["ps6"], ["scs"])
                dma3(STQ, lambda j: SC1[b, j * 128:(j + 1) * 128, :], lambda j: scs[:, j, :], 18, ["scs"], wpart(("SC1", b)))

            chk(8)
            S.barrier()
            arena.reset()
            alloc_norm_bufs()
            ctxb["mtmp"] = [arena.alloc([128, D], F32) for _ in range(2)]
            wo1 = arena.alloc([128, 8, D], BF16)
            QTh = arena.alloc([128, 2, T], BF16)
            KTh = arena.alloc([128, 2, TS], BF16)
            KKh = arena.alloc([128, 18, 256], BF16)
            VVh = arena.alloc([128, 18, 257], BF16)
            ogh = arena.alloc([128, 2, T], BF16)
            uch = arena.alloc([128, 2, T], BF16)
            hnT = arena.alloc([128, 2, T], BF16)
            tyy = arena.alloc([128, T], F32)
            scb = arena.alloc([128, 18, 24], F32)
            dcb = arena.alloc([128, 144], F32)
            lmk = arena.alloc([128, 2, 128], F32)
            Sst = [arena.alloc([128, 2, 257], F32) for _ in range(2)]
            Sbf = [arena.alloc([128, 2, 257], BF16) for _ in range(2)]
            hs = arena.alloc([128, 16, 256], F32)
            ATb = [arena.alloc([128, 128], BF16) for _ in range(4)]
            Ktl = [arena.alloc([128, 256], BF16) for _ in range(4)]
            dnn = [arena.alloc([128, 2], F32) for _ in range(4)]
            hnb = [arena.alloc([128, 256], BF16) for _ in range(2)]
            yT = arena.alloc([128, 8, T], BF16)
            rng = arena.alloc([128, 8], F32)
            rsk = arena.alloc([128, 8], F32)
            gb1 = arena.alloc([128, D], F32)
            load_w(wo1, rec_out_w, 8, D, "wo1")
            for m_ in range(2):
                DMA("sp", lmk[:, m_, :], lmask_in[m_], [], ["lmk"])
            DMA("sp", rng, rec_norm_g.rearrange("(c p) -> p c", p=128), [], ["rng"])
            DMA("sp", rsk, rec_skip.rearrange("(c p) -> p c", p=128), [], ["rsk"])
            V(lambda e: e.memset(VVh[:, :, 256:257], 1.0), [], ["VVh"])
            c3 = {"i": 0}
            for b in range(NB):
                dma3("sp", lambda j: scb[:, j, :], lambda j: SC1[b, j * 128:(j + 1) * 128, :], 18, rparts(("SC1", b)), ["scb"])
                DMA("sp", dcb, DC1[b].partition_broadcast(128), rparts(("DC1", b)), ["dcb"])
                load_gb(1, 0, b, gb1, "gb1")
                for h in range(4):
                    dma3("sp", lambda j: QTh[:, j, :], lambda j: QT1[b, h * 256 + j * 128:h * 256 + (j + 1) * 128, :], 2,
                         rparts(("QT1", b)), ["QTh"])
                    dma3("sp", lambda j: KTh[:, j, :], lambda j: KT1[b, h * 256 + j * 128:h * 256 + (j + 1) * 128, :], 2,
                         rparts(("KT1", b)), ["KTh"])
                    dma3("sp", lambda j: KKh[:, j, :], lambda j: KK1[b, j * 128:(j + 1) * 128, h * 256:(h + 1) * 256], 18,
                         rparts(("KK1", b)), ["KKh"])
                    dma3("sp", lambda j: VVh[:, j, 0:256], lambda j: VV1[b, j * 128:(j + 1) * 128, h * 256:(h + 1) * 256], 18,
                         rparts(("VV1", b)), ["VVh"])
                    dma3("sp", lambda j: ogh[:, j, :], lambda j: OG1[b, h * 256 + j * 128:h * 256 + (j + 1) * 128, :], 2,
                         rparts(("OG1", b)), ["ogh"])
                    dma3("sp", lambda j: uch[:, j, :], lambda j: UC1[b, h * 256 + j * 128:h * 256 + (j + 1) * 128, :], 2,
                         rparts(("UC1", b)), ["uch"])
                    for dr in range(2):
                        V(lambda e: e.memset(Sst[dr], 0.0), [], ["Sst%d" % dr])
                        V(lambda e: e.memset(Sbf[dr], 0.0), [], ["Sbf%d" % dr])
                    order = [list(range(18)), [1, 0] + list(range(17, 1, -1))]
                    first_dir_done = set()
                    for step in range(18):
                        st_ = []
                        for dr in range(2):
                            tl = order[dr][step]
                            st_.append(dict(dr=dr, tl=tl, isx=tl >= 2, xt=tl - 2, pb0=dr * 4, bi=dr * 2 + step % 2,
                                            pslot=dr * 4 + step % 2, sres="Sst%d" % dr, bres="Sbf%d" % dr,
                                            colA=(dr * 3 + 0) * 4 + h, colB=(dr * 3 + 1) * 4 + h,
                                            colF=(dr * 3 + 2) * 4 + h, dcol=(dr * 4 + h) * 18 + tl))
                        for q_ in st_:
                            dr, tl, xt_, bi, pslot = q_["dr"], q_["tl"], q_["xt"], q_["bi"], q_["pslot"]
                            if q_["isx"]:
                                pA = bank(pslot)[:, 0:128]
                                for dc in range(2):
                                    PE(lambda e: e.matmul(pA[:, 0:128], lhsT=KTh[:, dc, tl * 128:(tl + 1) * 128],
                                                          rhs=QTh[:, dc, xt_ * 128:(xt_ + 1) * 128], start=(dc == 0),
                                                          stop=(dc == 1)), ["KTh", "QTh"], ["ps%d" % pslot])
                                V(lambda e: e.scalar_tensor_tensor(out=ATb[bi], in0=pA[:, 0:128],
                                                                   scalar=scb[:, tl, q_["colA"]:q_["colA"] + 1],
                                                                   in1=lmk[:, dr, :], op0=ALU.mult, op1=ALU.mult),
                                  ["ps%d" % pslot, "scb", "lmk"], ["ATb%d" % bi])
                            if step < 17:
                                A(lambda e: e.activation(out=Ktl[bi], in_=KKh[:, tl, :], func=AF.Copy,
                                                         scale=scb[:, tl, q_["colB"]:q_["colB"] + 1]),
                                  ["KKh", "scb"], ["Ktl%d" % bi])
                        if step < 17:
                            for q_ in st_:
                                tl, bi, pb0 = q_["tl"], q_["bi"], q_["pb0"]
                                for dc in range(2):
                                    pS = bank(pb0 + 2 + dc)
                                    PE(lambda e: e.matmul(pS[:, 0:257], lhsT=Ktl[bi][:, dc * 128:(dc + 1) * 128],
                                                          rhs=VVh[:, tl, :], start=True, stop=True),
                                       ["Ktl%d" % bi, "VVh"], ["ps%d" % (pb0 + 2 + dc)])
                        for q_ in st_:
                            dr, tl, xt_, bi, pslot = q_["dr"], q_["tl"], q_["xt"], q_["bi"], q_["pslot"]
                            if q_["isx"]:
                                pO = bank(pslot)[:, 128:512]
                                PE(lambda e: e.matmul(pO[:, 0:257], lhsT=ATb[bi], rhs=VVh[:, tl, :], start=True, stop=False),
                                   ["ATb%d" % bi, "VVh"], ["ps%d" % pslot])
                                for dc in range(2):
                                    PE(lambda e: e.matmul(pO[:, 0:257], lhsT=QTh[:, dc, xt_ * 128:(xt_ + 1) * 128],
                                                          rhs=Sbf[dr][:, dc, :], start=False, stop=(dc == 1)),
                                       ["QTh", q_["bres"]], ["ps%d" % pslot])
                        if step < 17:
                            for q_ in st_:
                                dr, pb0 = q_["dr"], q_["pb0"]
                                for dc in range(2):
                                    pS = bank(pb0 + 2 + dc)
                                    V(lambda e: e.scalar_tensor_tensor(out=Sst[dr][:, dc, :], in0=Sst[dr][:, dc, :],
                                                                       scalar=dcb[:, q_["dcol"]:q_["dcol"] + 1],
                                                                       in1=pS[:, 0:257], op0=ALU.mult, op1=ALU.add),
                                      [q_["sres"], "dcb", "ps%d" % (pb0 + 2 + dc)], [q_["sres"]])
                        for q_ in st_:
                            dr, tl, xt_, bi, pslot = q_["dr"], q_["tl"], q_["xt"], q_["bi"], q_["pslot"]
                            if q_["isx"]:
                                pO = bank(pslot)[:, 128:512]
                                dn = dnn[bi]
                                A(lambda e: e.activation(out=dn[:, 0:1], in_=pO[:, 256:257], func=AF.Abs),
                                  ["ps%d" % pslot], ["dnn%d" % bi])
                                V(lambda e: e.tensor_tensor(out=dn[:, 0:1], in0=dn[:, 0:1],
                                                            in1=scb[:, tl, q_["colF"]:q_["colF"] + 1], op=ALU.max),
                                  ["dnn%d" % bi, "scb"], ["dnn%d" % bi])
                                V(lambda e: e.reciprocal(out=dn[:, 1:2], in_=dn[:, 0:1]), ["dnn%d" % bi], ["dnn%d" % bi])
                                hres_ = ("hs", xt_)
                                if xt_ not in first_dir_done:
                                    first_dir_done.add(xt_)
                                    V(lambda e: e.tensor_scalar(out=hs[:, xt_, :], in0=pO[:, 0:256], scalar1=dn[:, 1:2],
                                                                scalar2=None, op0=ALU.mult),
                                      ["ps%d" % pslot, "dnn%d" % bi], [hres_])
                                else:
                                    V(lambda e: e.scalar_tensor_tensor(out=hs[:, xt_, :], in0=pO[:, 0:256],
                                                                       scalar=dn[:, 1:2], in1=hs[:, xt_, :],
                                                                       op0=ALU.mult, op1=ALU.add),
                                      ["ps%d" % pslot, "dnn%d" % bi, hres_], [hres_])
                        if step < 17:
                            for q_ in st_:
                                dr = q_["dr"]
                                A(lambda e: e.activation(out=Sbf[dr], in_=Sst[dr], func=AF.Copy), [q_["sres"]], [q_["bres"]])
                    for xt_ in range(16):
                        ci = xt_ % 2
                        stt = ctxb["st"][xt_ % 3]
                        sres_ = "st%d" % (xt_ % 3)
                        junk = ctxb["junk"]
                        A(lambda e: e.activation(out=junk[:, 0:256], in_=hs[:, xt_, :], func=AF.Square,
                                                 accum_out=stt[:, 0:1]), [("hs", xt_)], ["junk", sres_])
                        A(lambda e: e.activation(out=stt[:, 1:2], in_=stt[:, 0:1], func=AF.Sqrt, scale=1.0 / 256,
                                                 bias=epsb[:, :]), [sres_, "epsb"], [sres_])
                        V(lambda e: e.reciprocal(out=stt[:, 1:2], in_=stt[:, 1:2]), [sres_], [sres_])
                        V(lambda e: e.tensor_scalar(out=hnb[ci], in0=hs[:, xt_, :], scalar1=stt[:, 1:2], scalar2=None,
                                                    op0=ALU.mult), [("hs", xt_), sres_], ["hnb%d" % ci])
                        ptv = bank_bf(6 + ci).rearrange("p (k t) -> p k t", t=128)
                        for dc in range(2):
                            PE(lambda e: e.transpose(out=ptv[:, dc, :], in_=hnb[ci][:, dc * 128:(dc + 1) * 128],
                                                     identity=identb[:]), ["hnb%d" % ci, "identb"], ["ps%d" % (6 + ci)])
                        A(lambda e: e.activation(out=hnT[:, :, xt_ * 128:(xt_ + 1) * 128], in_=ptv[:, 0:2, :],
                                                 func=AF.Copy), ["ps%d" % (6 + ci)], ["hnT"])
                    for dc in range(2):
                        fcx = 2 * h + dc
                        V(lambda e: e.tensor_scalar(out=tyy, in0=hnT[:, dc, :], scalar1=rng[:, fcx:fcx + 1], scalar2=None,
                                                    op0=ALU.mult), ["hnT", "rng"], ["tyy"])
                        V(lambda e: e.scalar_tensor_tensor(out=tyy, in0=uch[:, dc, :], scalar=rsk[:, fcx:fcx + 1], in1=tyy,
                                                           op0=ALU.mult, op1=ALU.add), ["uch", "rsk", "tyy"], ["tyy"])
                        V(lambda e: e.tensor_tensor(out=yT[:, fcx, :], in0=tyy, in1=ogh[:, dc, :], op=ALU.mult),
                          ["tyy", "ogh"], [("yT", fcx)])
                for n in range(16):
                    mix = PS2[n % 2]
                    for hf in range(2):
                        for k in range(8):
                            PE(lambda e: e.matmul(mix[:, hf * 512:(hf + 1) * 512], lhsT=yT[:, k, n * 128:(n + 1) * 128],
                                                  rhs=wo1[:, k, hf * 512:(hf + 1) * 512], start=(k == 0), stop=(k == 7)),
                               [("yT", k), "wo1"], ["ps%d" % (2 * (n % 2) + hf)])
                    residual_update(mix[:, :], ["ps%d" % (2 * (n % 2)), "ps%d" % (2 * (n % 2) + 1)], XB[b],
                                    [("XB", b, n)], XA[b], [("XA", b, n)], n * 128, 128, gb1, "gb1")

            ffn_layer(1,
                      lambda b: [(XA[b], "XA", T, 0, b)],
                      lambda b: [(XA[b], "XA", T, 0, b, out[b], "out", True)])

        except _Stop:
            S.barrier()
        S.emit(st)
        build_program.stats = S.stats
    return nc


def _consts():
    ident = np.eye(128, dtype=np.float32)
    inv = (10000.0 ** (-np.arange(16, dtype=np.float32) / 16)).astype(np.float32)
    t = np.arange(T)
    row = (t // 64).astype(np.float32)
    col = (t % 64).astype(np.float32)
    rc = np.zeros((128, T), np.float32)
    rs = np.zeros((128, T), np.float32)
    for p in range(128):
        d = p % 64
        axis, half, f = d // 32, (d % 32) // 16, d % 16
        pos = row if axis == 0 else col
        ang = (pos * inv[f]).astype(np.float32)
        rc[p] = np.cos(ang)
        rs[p] = np.sin(ang) * (-1.0 if half == 0 else 1.0)
    j = np.arange(128)[:, None]
    i = np.arange(128)[None, :]
    prev = (j >= i).astype(np.float32)
    nxt = (j <= i).astype(np.float32)
    amask = np.stack([np.tile(prev, (1, 4)), np.tile(nxt, (1, 4))]).astype(np.float32)
    pe = np.zeros((4, 2, 8), np.float32)
    for g in range(4):
        w = 2 ** (g + 1)
        half = w // 2
        for k in range(half):
            pe[g, 0, k] = 1.0 / (k + half)
        for k in range(half - 1):
            pe[g, 1, k] = 1.0 / (2 * half - 1 - k)
    pool_edge = np.tile(pe.reshape(1, 64), (128, 1)).astype(np.float32)
    lmask = np.stack([(j <= i).astype(np.float32), (j >= i).astype(np.float32)])
    return dict(ident=ident, rope_c=rc, rope_s=rs, amask=amask, pool_edge=pool_edge, lmask=lmask)


def _perm_head():
    p = np.zeros(64, np.int64)
    for d in range(64):
        axis, half, f = d // 32, (d % 32) // 16, d % 16
        p[d] = axis * 32 + (1 - half) * 16 + f
    return p


def _prep_shared(inp):
    w = np.asarray(inp["attn_in_w"][0], np.float32)
    ph = _perm_head()
    wz = np.concatenate([w, np.zeros((w.shape[0], 1), np.float32)], axis=1)
    Z = np.full(64, w.shape[1], np.int64)
    q = [np.arange(c * 128, (c + 1) * 128) for c in range(4)]
    k0 = 512 + np.arange(64)
    k1 = 576 + np.arange(64)
    kz = [np.concatenate([k0, Z]), np.concatenate([Z, k0]), np.concatenate([k1, Z]), np.concatenate([Z, k1])]
    base = q + kz

    def partner(idx):
        o = idx.copy()
        for h0 in range(0, 128, 64):
            blk = idx[h0:h0 + 64]
            o[h0:h0 + 64] = blk[ph]
        return o

    cols = base + [partner(c) for c in base] + [np.arange(640, 768)] + [np.arange(768 + g * 128, 896 + g * 128) for g in range(4)]
    cols = np.concatenate(cols)
    w = wz
    sh = dict(
        mod_w=np.ascontiguousarray(inp["mod_w"], np.float32),
        mod_b=np.ascontiguousarray(inp["mod_b"], np.float32),
        norm_g=np.ascontiguousarray(inp["norm_g"], np.float32),
        w_in0=np.ascontiguousarray(w[:, cols]),
        attn_sink=np.ascontiguousarray(inp["attn_sink"][0], np.float32),
        pool_w=np.ascontiguousarray(inp["pool_w"][0], np.float32),
        pool_scale=np.ascontiguousarray(inp["pool_scale"][0], np.float32),
        attn_out_w=np.ascontiguousarray(inp["attn_out_w"][0], np.float32),
        rec_in_w=np.ascontiguousarray(inp["rec_in_w"][0], np.float32),
        rec_gate_b=np.ascontiguousarray(inp["rec_gate_b"][0].reshape(16), np.float32),
        rec_conv_w=np.ascontiguousarray(inp["rec_conv_w"][0], np.float32),
        rec_conv_b=np.ascontiguousarray(inp["rec_conv_b"][0], np.float32),
        rec_q_w=np.ascontiguousarray(inp["rec_q_w"][0], np.float32),
        rec_k_w=np.ascontiguousarray(inp["rec_k_w"][0], np.float32),
        rec_norm_g=np.ascontiguousarray(inp["rec_norm_g"][0], np.float32),
        rec_skip=np.ascontiguousarray(inp["rec_skip"][0], np.float32),
        rec_out_w=np.ascontiguousarray(inp["rec_out_w"][0], np.float32),
        ffn_up_w=np.ascontiguousarray(inp["ffn_up_w"], np.float32),
        ffn_conv_w=np.ascontiguousarray(inp["ffn_conv_w"], np.float32),
        ffn_conv_b=np.ascontiguousarray(inp["ffn_conv_b"], np.float32),
        ffn_down_w=np.ascontiguousarray(inp["ffn_down_w"], np.float32),
    )
    sh.update(_consts())
    return sh


def make_in_maps(inp, cores):
    sh = _prep_shared(inp)
    x = np.asarray(inp["x"], np.float32)
    c = np.asarray(inp["c"], np.float32)
    ctx = np.asarray(inp["ctx"], np.float32)
    c_ctx = np.asarray(inp["c_ctx"], np.float32)
    maps = []
    for i in cores:
        m = dict(sh)
        m["x"] = np.ascontiguousarray(x[NB * i:NB * (i + 1)])
        m["ctx"] = np.ascontiguousarray(ctx[NB * i:NB * (i + 1)])
        m["cvec"] = np.ascontiguousarray(np.concatenate([c[NB * i:NB * (i + 1)], c_ctx[None, :]], axis=0))
        maps.append(m)
    return maps


def kernel(**inputs):
    nc = build_program()
    maps = make_in_maps(inputs, list(range(8)))
    res = run_bass_kernel_spmd(nc, maps, core_ids=list(range(8)))
    return np.concatenate([np.asarray(r["out"], np.float32) for r in res.results], axis=0)
```
